# Optimizing a Trainium2 kernel written in Bass

```python
import math
import jax, jax.numpy as jnp
from jax import lax
import numpy as np

D_MODEL = 1024
BATCH = 2
SEQ = 16384
DEPTH = 4

A_HEADS = 4
A_HEAD_DIM = 64
A_V_DIM = 2 * A_HEAD_DIM
B_HEADS = 4
B_HEAD_DIM = 128
CONV_WIDTH = 4
DELTA_CHUNK = 64
C_HEADS = 16
C_HEAD_DIM = 64
T5_BUCKETS = 32
T5_MAX_DISTANCE = 2048
Q_BLOCK = 128
N_EXPERTS = 32
TOP_K = 4
D_FF = D_MODEL
SWIGLU_LIMIT = 7.0
SWIGLU_ALPHA = 1.702
MOE_BLOCK = 256
DEEPNORM_ALPHA = (2 * DEPTH) ** 0.25
DEEPNORM_BETA = (8 * DEPTH) ** -0.25
LN_EPS = 1e-5
RMS_EPS = 1e-6
N_EVEN = (DEPTH + 1) // 2
N_ODD = DEPTH // 2

A_Q = A_HEADS * 2 * A_HEAD_DIM
A_K = A_HEADS * 2 * A_HEAD_DIM
A_V = A_HEADS * A_V_DIM
B_W = B_HEADS * B_HEAD_DIM
EVEN_SPLITS = [A_Q, A_Q + A_K, A_Q + A_K + A_V, A_Q + A_K + A_V + 3 * B_W,
               A_Q + A_K + A_V + 4 * B_W, A_Q + A_K + A_V + 4 * B_W + B_HEADS]
EVEN_IN = A_Q + A_K + A_V + 4 * B_W + 2 * B_HEADS
EVEN_OUT = A_V + B_W
C_W = C_HEADS * C_HEAD_DIM
ODD_SPLITS = [C_W, 2 * C_W, 3 * C_W, 4 * C_W]
ODD_IN = 4 * C_W + C_HEADS
ODD_OUT = C_W

kernel_name = "hybrid_diffattn_gdn_fox_moe_deepnorm"


def layer_norm(x, g, b):
    xf = x.astype(jnp.float32)
    mu = jnp.mean(xf, -1, keepdims=True)
    var = jnp.mean(jnp.square(xf - mu), -1, keepdims=True)
    return ((xf - mu) * lax.rsqrt(var + LN_EPS) * g.astype(jnp.float32) + b.astype(jnp.float32)).astype(x.dtype)


def rms_norm(x, w):
    xf = x.astype(jnp.float32)
    return (xf * lax.rsqrt(jnp.mean(xf * xf, -1, keepdims=True) + RMS_EPS) * w.astype(jnp.float32)).astype(x.dtype)


def l2_norm(x):
    xf = x.astype(jnp.float32)
    return (xf * lax.rsqrt(jnp.sum(xf * xf, -1, keepdims=True) + RMS_EPS)).astype(x.dtype)


def t5_bucket(rel):
    n = jnp.maximum(rel, 0)
    max_exact = T5_BUCKETS // 2
    nf = jnp.maximum(n, 1).astype(jnp.float32)
    large = max_exact + (jnp.log(nf / max_exact) / math.log(T5_MAX_DISTANCE / max_exact)
                         * (T5_BUCKETS - max_exact)).astype(jnp.int32)
    large = jnp.minimum(large, T5_BUCKETS - 1)
    return jnp.where(n < max_exact, n, large)


def diff_attention(q, k, v, lam, bias_table):
    Bsz, H, _, T, d = q.shape
    scale = d ** -0.5
    k_pos = jnp.arange(T)
    table = bias_table.astype(jnp.float32).T

    def block(i):
        start = i * Q_BLOCK
        qb = lax.dynamic_slice_in_dim(q, start, Q_BLOCK, axis=3)
        rel = (start + jnp.arange(Q_BLOCK))[:, None] - k_pos[None, :]
        bias = table[:, t5_bucket(rel)]
        s = jnp.einsum('bhmqd,bhmkd->bhmqk', qb, k).astype(jnp.float32) * scale + bias[None, :, None]
        p = jax.nn.softmax(jnp.where(rel >= 0, s, -jnp.inf), axis=-1)
        a = p[:, :, 0] - lam * p[:, :, 1]
        return jnp.einsum('bhqk,bhkd->bhqd', a.astype(v.dtype), v)

    out = lax.map(block, jnp.arange(T // Q_BLOCK))
    return out.transpose(1, 2, 0, 3, 4).reshape(Bsz, H, T, v.shape[-1])


def forgetting_attention(q, k, v, logf):
    Bsz, H, T, d = q.shape
    scale = d ** -0.5
    cum = jnp.cumsum(logf.astype(jnp.float32), axis=-1)
    k_pos = jnp.arange(T)

    def block(i):
        start = i * Q_BLOCK
        qb = lax.dynamic_slice_in_dim(q, start, Q_BLOCK, axis=2)
        cq = lax.dynamic_slice_in_dim(cum, start, Q_BLOCK, axis=2)
        causal = (start + jnp.arange(Q_BLOCK))[:, None] >= k_pos[None, :]
        s = jnp.einsum('bhqd,bhkd->bhqk', qb, k).astype(jnp.float32) * scale + cq[..., :, None] - cum[..., None, :]
        p = jax.nn.softmax(jnp.where(causal, s, -jnp.inf), axis=-1)
        return jnp.einsum('bhqk,bhkd->bhqd', p.astype(v.dtype), v)

    out = lax.map(block, jnp.arange(T // Q_BLOCK))
    return out.transpose(1, 2, 0, 3, 4).reshape(Bsz, H, T, d)


def gated_delta_rule(q, k, v, g, beta):
    f32 = jnp.float32
    Bsz, H, T, dk = q.shape
    dv = v.shape[-1]
    C = DELTA_CHUNK
    N = T // C
    q = (q.astype(f32) * dk ** -0.5).reshape(Bsz, H, N, C, dk)
    k = k.astype(f32).reshape(Bsz, H, N, C, dk)
    v = v.astype(f32).reshape(Bsz, H, N, C, dv)
    beta = beta.astype(f32).reshape(Bsz, H, N, C, 1)
    gc = jnp.cumsum(g.astype(f32).reshape(Bsz, H, N, C), axis=-1)
    idx = jnp.arange(C)
    incl = idx[:, None] >= idx[None, :]
    strict = (idx[:, None] > idx[None, :]).astype(f32)
    decay = jnp.exp(jnp.where(incl, gc[..., :, None] - gc[..., None, :], -jnp.inf))
    kb = k * beta
    lower = jnp.einsum('bhncd,bhnmd->bhncm', kb, k) * decay * strict
    rhs = jnp.concatenate([v * beta, kb * jnp.exp(gc)[..., None]], axis=-1)
    sol = lax.linalg.triangular_solve(jnp.eye(C, dtype=f32) + lower, rhs,
                                      left_side=True, lower=True, unit_diagonal=True)
    u, w = sol[..., :dv], sol[..., dv:]
    qk_intra = jnp.einsum('bhncd,bhnmd->bhncm', q, k) * decay
    q_dec = q * jnp.exp(gc)[..., None]
    k_dec = k * jnp.exp(gc[..., -1:] - gc)[..., None]
    g_last = jnp.exp(gc[..., -1])

    def step(S, xs):
        u_n, w_n, qk_n, qd_n, kd_n, gl_n = xs
        v_new = u_n - jnp.einsum('bhck,bhkv->bhcv', w_n, S)
        o_n = jnp.einsum('bhck,bhkv->bhcv', qd_n, S) + jnp.einsum('bhcm,bhmv->bhcv', qk_n, v_new)
        S = S * gl_n[..., None, None] + jnp.einsum('bhck,bhcv->bhkv', kd_n, v_new)
        return S, o_n

    xs = tuple(jnp.moveaxis(a, 2, 0) for a in (u, w, qk_intra, q_dec, k_dec, g_last))
    _, o = lax.scan(step, jnp.zeros((Bsz, H, dk, dv), f32), xs)
    return jnp.moveaxis(o, 0, 2).reshape(Bsz, H, T, dv)


def causal_conv_silu(x, w):
    K, C = w.shape
    y = lax.conv_general_dilated(x, w[:, None, :].astype(x.dtype), window_strides=(1,),
                                 padding=[(K - 1, 0)], dimension_numbers=('NWC', 'WIO', 'NWC'),
                                 feature_group_count=C)
    return jax.nn.silu(y)


def even_mixer(x, w_in, w_out, lam_params, subln_w, conv_w, a_log, dt_bias, gdn_norm_w, t5_bias, lam_init):
    Bsz, T, _ = x.shape
    proj = x @ w_in
    aq, ak, av, bqkv, bz, bb, ba = jnp.split(proj, EVEN_SPLITS, axis=-1)
    aq = aq.reshape(Bsz, T, A_HEADS, 2, A_HEAD_DIM).transpose(0, 2, 3, 1, 4)
    ak = ak.reshape(Bsz, T, A_HEADS, 2, A_HEAD_DIM).transpose(0, 2, 3, 1, 4)
    av = av.reshape(Bsz, T, A_HEADS, A_V_DIM).transpose(0, 2, 1, 3)
    lp = lam_params.astype(jnp.float32)
    lam = jnp.exp(jnp.sum(lp[0] * lp[1])) - jnp.exp(jnp.sum(lp[2] * lp[3])) + lam_init
    ao = diff_attention(aq, ak, av, lam, t5_bias)
    ao = rms_norm(ao, subln_w) * (1.0 - lam_init)
    ao = ao.transpose(0, 2, 1, 3).reshape(Bsz, T, A_V)
    bqkv = causal_conv_silu(bqkv, conv_w)
    bq, bk, bv = jnp.split(bqkv, 3, axis=-1)
    heads = lambda t: t.reshape(Bsz, T, B_HEADS, B_HEAD_DIM).transpose(0, 2, 1, 3)
    bq, bk, bv = l2_norm(heads(bq)), l2_norm(heads(bk)), heads(bv)
    beta = jax.nn.sigmoid(bb.astype(jnp.float32)).transpose(0, 2, 1)
    g = (-jnp.exp(a_log.astype(jnp.float32))
         * jax.nn.softplus(ba.astype(jnp.float32) + dt_bias.astype(jnp.float32))).transpose(0, 2, 1)
    bo = gated_delta_rule(bq, bk, bv, g, beta).astype(x.dtype)
    bo = rms_norm(bo, gdn_norm_w) * jax.nn.silu(heads(bz))
    bo = bo.transpose(0, 2, 1, 3).reshape(Bsz, T, B_W)
    return jnp.concatenate([ao, bo], axis=-1) @ w_out


def odd_mixer(x, w_in, w_out, qk_norm_w, forget_b):
    Bsz, T, _ = x.shape
    proj = x @ w_in
    q, k, v, gate, f = jnp.split(proj, ODD_SPLITS, axis=-1)
    heads = lambda t: t.reshape(Bsz, T, C_HEADS, C_HEAD_DIM).transpose(0, 2, 1, 3)
    q = rms_norm(heads(q), qk_norm_w[0])
    k = rms_norm(heads(k), qk_norm_w[1])
    logf = jax.nn.log_sigmoid(f.astype(jnp.float32) + forget_b.astype(jnp.float32)).transpose(0, 2, 1)
    o = forgetting_attention(q, k, heads(v), logf)
    o = o.transpose(0, 2, 1, 3).reshape(Bsz, T, C_W) * jax.nn.sigmoid(gate)
    return o @ w_out


def moe_ffn(x, router_w, router_b, w_gu, b_gu, w_down, b_down):
    Bsz, T, D = x.shape
    xt = x.reshape(-1, D)
    n_tok = xt.shape[0]
    logits = (xt @ router_w + router_b).astype(jnp.float32)
    top_val, top_idx = lax.top_k(logits, TOP_K)
    gates = jax.nn.softmax(top_val, axis=-1)
    n_assign = n_tok * TOP_K
    flat_e = top_idx.reshape(-1)
    flat_tok = jnp.repeat(jnp.arange(n_tok, dtype=jnp.int32), TOP_K)
    order = jnp.argsort(flat_e)
    e_sorted, tok_sorted, gate_sorted = flat_e[order], flat_tok[order], gates.reshape(-1)[order]
    counts = jnp.bincount(flat_e, length=N_EXPERTS)
    padded = (counts + MOE_BLOCK - 1) // MOE_BLOCK * MOE_BLOCK
    start = jnp.cumsum(counts) - counts
    pad_end = jnp.cumsum(padded)
    pad_start = pad_end - padded
    dest = pad_start[e_sorted] + jnp.arange(n_assign) - start[e_sorted]
    n_blocks = -(-n_assign // MOE_BLOCK) + N_EXPERTS
    n_rows = n_blocks * MOE_BLOCK
    row_tok = jnp.full((n_rows,), n_tok, jnp.int32).at[dest].set(tok_sorted)
    block_expert = jnp.minimum(jnp.searchsorted(pad_end, jnp.arange(n_blocks) * MOE_BLOCK, side='right'),
                               N_EXPERTS - 1)
    x_pad = jnp.concatenate([xt, jnp.zeros((1, D), xt.dtype)], axis=0)
    xb = x_pad[row_tok].reshape(n_blocks, MOE_BLOCK, D)

    def expert_block(args):
        xblk, e = args
        h = xblk @ w_gu[e] + b_gu[e]
        g, u = jnp.split(h, 2, axis=-1)
        g = jnp.minimum(g, SWIGLU_LIMIT)
        u = jnp.clip(u, -SWIGLU_LIMIT, SWIGLU_LIMIT)
        act = g * jax.nn.sigmoid(SWIGLU_ALPHA * g) * (u + 1.0)
        return act @ w_down[e] + b_down[e]

    yb = lax.map(expert_block, (xb, block_expert)).reshape(n_rows, D)
    y_assign = yb[dest] * gate_sorted[:, None].astype(yb.dtype)
    out = jnp.zeros_like(xt).at[tok_sorted].add(y_assign)
    return out.reshape(Bsz, T, D)


def setup_inputs(seed: int = 0) -> dict:
    key = jax.random.key(seed)
    ks = jax.random.split(key, 24)
    f32 = jnp.float32
    nrm = lambda k, shape, s: jax.random.normal(k, shape, f32) * s
    even_scale = jnp.ones((EVEN_IN,), f32)
    even_scale = even_scale.at[A_Q + A_K:A_Q + A_K + A_V].set(DEEPNORM_BETA)
    bv0 = A_Q + A_K + A_V + 2 * B_W
    even_scale = even_scale.at[bv0:bv0 + B_W].set(DEEPNORM_BETA)
    odd_scale = jnp.ones((ODD_IN,), f32).at[2 * C_W:3 * C_W].set(DEEPNORM_BETA)
    dt = jnp.exp(jax.random.uniform(ks[8], (N_EVEN, B_HEADS), f32, math.log(1e-3), math.log(1e-1)))
    return {
        'x': jax.random.normal(ks[0], (BATCH, SEQ, D_MODEL), f32),
        't5_bias': nrm(ks[1], (T5_BUCKETS, A_HEADS), 0.5),
        'even_w_in': nrm(ks[2], (N_EVEN, D_MODEL, EVEN_IN), D_MODEL ** -0.5) * even_scale,
        'even_w_out': nrm(ks[3], (N_EVEN, EVEN_OUT, D_MODEL), EVEN_OUT ** -0.5 * DEEPNORM_BETA),
        'diff_lambda': nrm(ks[4], (N_EVEN, 4, A_HEAD_DIM), 0.1),
        'diff_subln_w': 1.0 + nrm(ks[5], (N_EVEN, A_V_DIM), 0.02),
        'gdn_conv_w': nrm(ks[6], (N_EVEN, CONV_WIDTH, 3 * B_W), CONV_WIDTH ** -0.5),
        'gdn_a_log': jnp.log(jax.random.uniform(ks[7], (N_EVEN, B_HEADS), f32, 1.0, 16.0)),
        'gdn_dt_bias': dt + jnp.log(-jnp.expm1(-dt)),
        'gdn_norm_w': 1.0 + nrm(ks[9], (N_EVEN, B_HEAD_DIM), 0.02),
        'odd_w_in': nrm(ks[10], (N_ODD, D_MODEL, ODD_IN), D_MODEL ** -0.5) * odd_scale,
        'odd_w_out': nrm(ks[11], (N_ODD, ODD_OUT, D_MODEL), ODD_OUT ** -0.5 * DEEPNORM_BETA),
        'fox_qk_norm_w': 1.0 + nrm(ks[12], (N_ODD, 2, C_HEAD_DIM), 0.02),
        'fox_forget_b': 2.0 + nrm(ks[13], (N_ODD, C_HEADS), 0.1),
        'router_w': nrm(ks[14], (DEPTH, D_MODEL, N_EXPERTS), D_MODEL ** -0.5),
        'router_b': nrm(ks[15], (DEPTH, N_EXPERTS), 0.01),
        'moe_w_gate_up': nrm(ks[16], (DEPTH, N_EXPERTS, D_MODEL, 2 * D_FF), D_MODEL ** -0.5),
        'moe_b_gate_up': nrm(ks[17], (DEPTH, N_EXPERTS, 2 * D_FF), 0.01),
        'moe_w_down': nrm(ks[18], (DEPTH, N_EXPERTS, D_FF, D_MODEL), D_FF ** -0.5 * DEEPNORM_BETA),
        'moe_b_down': nrm(ks[19], (DEPTH, N_EXPERTS, D_MODEL), 0.01),
        'ln_mix_g': 1.0 + nrm(ks[20], (DEPTH, D_MODEL), 0.02),
        'ln_mix_b': nrm(ks[21], (DEPTH, D_MODEL), 0.02),
        'ln_ffn_g': 1.0 + nrm(ks[22], (DEPTH, D_MODEL), 0.02),
        'ln_ffn_b': nrm(ks[23], (DEPTH, D_MODEL), 0.02),
    }


def reference(x, t5_bias, even_w_in, even_w_out, diff_lambda, diff_subln_w, gdn_conv_w, gdn_a_log,
              gdn_dt_bias, gdn_norm_w, odd_w_in, odd_w_out, fox_qk_norm_w, fox_forget_b,
              router_w, router_b, moe_w_gate_up, moe_b_gate_up, moe_w_down, moe_b_down,
              ln_mix_g, ln_mix_b, ln_ffn_g, ln_ffn_b):
    for layer in range(DEPTH):
        i = layer // 2
        if layer % 2 == 0:
            lam_init = 0.8 - 0.6 * math.exp(-0.3 * layer)
            h = even_mixer(x, even_w_in[i], even_w_out[i], diff_lambda[i], diff_subln_w[i],
                           gdn_conv_w[i], gdn_a_log[i], gdn_dt_bias[i], gdn_norm_w[i], t5_bias, lam_init)
        else:
            h = odd_mixer(x, odd_w_in[i], odd_w_out[i], fox_qk_norm_w[i], fox_forget_b[i])
        x = layer_norm(DEEPNORM_ALPHA * x + h, ln_mix_g[layer], ln_mix_b[layer])
        h = moe_ffn(x, router_w[layer], router_b[layer], moe_w_gate_up[layer], moe_b_gate_up[layer],
                    moe_w_down[layer], moe_b_down[layer])
        x = layer_norm(DEEPNORM_ALPHA * x + h, ln_ffn_g[layer], ln_ffn_b[layer])
    return x
```

```python
import math
from contextlib import ExitStack
import numpy as np
import concourse.bass as bass
import concourse.mybir as mybir
from concourse.bass_utils import run_bass_kernel_spmd

F32 = mybir.dt.float32
BF16 = mybir.dt.bfloat16
I32 = mybir.dt.int32
U32 = mybir.dt.uint32
AF = mybir.ActivationFunctionType
ALU = mybir.AluOpType
AX = mybir.AxisListType

COMPUTE = ("pe", "act", "dve", "pool")


class Buf:
    def __init__(self, name, handle, is_dram=False):
        self.name = name
        self.h = handle
        self.is_dram = is_dram
        self.w = {}
        self.r = {}
        self.dsem = None
        self.dcnt = 0

    def __getitem__(self, idx):
        return self.h.ap()[idx] if self.is_dram else self.h[idx]

    def ap(self):
        return self.h.ap() if self.is_dram else self.h[:]


class Sched:
    def __init__(self, nc, es):
        self.nc = nc
        self.es = es
        self.streams = {k: [] for k in ("pe", "act", "dve", "pool", "sp")}
        self.sems = {}
        self.latest = {}
        self.cnt = {k: 0 for k in COMPUTE}
        self.waited = {k: {} for k in self.streams}
        self.nbuf = 0
        self.es_local = es
        self.dq = {}
        self.dq_i = {}
        self.all_bufs = []
        import os
        self.sw_thresh = int(os.environ.get("KSW", "30000"))
        for k in COMPUTE:
            self._sem("E_" + k)

    def _sem(self, key):
        if key not in self.sems:
            self.sems[key] = self.es.enter_context(self.nc.semaphore("s%d_%s" % (len(self.sems), key[:12])))
            self.latest[key] = 0
        return self.sems[key]

    def sbuf(self, name, shape, dt):
        self.nbuf += 1
        h = self.es_local.enter_context(self.nc.sbuf_tensor("%s_%d" % (name, self.nbuf), list(shape), dt))
        b = Buf(name, h)
        b.local = self.es_local is not self.es
        self.all_bufs.append(b)
        return b

    def psum(self, name, shape, dt=F32):
        self.nbuf += 1
        h = self.es_local.enter_context(self.nc.psum_tensor("%s_%d" % (name, self.nbuf), list(shape), dt))
        b = Buf(name, h)
        b.local = self.es_local is not self.es
        self.all_bufs.append(b)
        return b

    def ring(self, name, shape, dt, n, psum=False):
        return Ring([(self.psum if psum else self.sbuf)("%s%d" % (name, i), shape, dt) for i in range(n)])

    def _dq(self, kind):
        if kind not in self.dq:
            n = {"hw": 12, "sw": 8, "cc": 1}[kind]
            self.dq[kind] = ["D_%s_%d" % (kind, i) for i in range(n)]
            for k in self.dq[kind]:
                self._sem(k)
            self.dq_i[kind] = 0
        key = self.dq[kind][self.dq_i[kind] % len(self.dq[kind])]
        self.dq_i[kind] += 1
        return key

    def phase(self):
        return _Phase(self)

    def dram(self, name, shape, dt, kind=None):
        if kind is None:
            h = self.nc.dram_tensor(name, list(shape), dt)
        else:
            h = self.nc.dram_tensor(name, list(shape), dt, kind=kind)
        b = Buf(name, h, is_dram=True)
        self.all_bufs.append(b)
        return b

    def _need(self, eng, reads, writes, mykey, prev=None):
        need = {}

        def add(k, v):
            if k == mykey == "E_pe":
                return
            if need.get(k, 0) < v:
                need[k] = v
        if prev is not None and prev[1] > 0:
            add(*prev)
        for b in reads:
            for k, v in b.w.items():
                add(k, v)
        for b in writes:
            for k, v in b.w.items():
                add(k, v)
            for k, v in b.r.items():
                add(k, v)
        out = []
        wd = self.waited[eng]
        for k, v in need.items():
            if wd.get(k, 0) < v:
                wd[k] = v
                out.append((self.sems[k], v))
        return out

    def op(self, eng, fn, reads=(), writes=()):
        key = "E_" + eng
        waits = self._need(eng, reads, writes, key)
        self.cnt[eng] += 1
        val = self.cnt[eng]
        self.latest[key] = val
        sem = self.sems[key]
        st = self.streams[eng]
        for s, v in waits:
            st.append(lambda e, s=s, v=v: e.wait_ge(s, v))
        st.append(lambda e, fn=fn, sem=sem: fn(e).then_inc(sem, 1))
        for b in reads:
            if b.r.get(key, 0) < val:
                b.r[key] = val
        for b in writes:
            b.w = {key: val}
            b.r = {}

    def dma(self, q, out_ap, in_ap, dst, src, fn=None, extra=(), **kw):
        key = self._dq("sw" if q == "pool" else "hw")
        wr = [dst] if (dst.r or not all(k.startswith("D_") for k in dst.w)) else []
        waits = self._need(q, ([src] if src is not None else []) + list(extra), wr, key, prev=(key, self.latest[key]))
        val = self.latest[key] + 16
        self.latest[key] = val
        sem = self.sems[key]
        st = self.streams[q]
        for s, v in waits:
            st.append(lambda e, s=s, v=v: e.wait_ge(s, v))
        if fn is None:
            st.append(lambda e: e.dma_start(out=out_ap, in_=in_ap, **kw).then_inc(sem, 16))
        else:
            st.append(lambda e: fn(e).then_inc(sem, 16))
        for b in ([src] if src is not None else []) + list(extra):
            if b.r.get(key, 0) < val:
                b.r[key] = val
        if wr:
            dst.w = {key: val}
        else:
            dst.w[key] = val
        dst.r = {}

    def cc(self, kind, op, groups, src, dst, src_ap=None, dst_ap=None):
        sap = src.h.ap().opt() if src_ap is None else src_ap.opt()
        dap = dst.h.ap().opt() if dst_ap is None else dst_ap.opt()
        key = self._dq("cc")
        waits = self._need("pool", [src], [dst], key)
        val = self.latest[key] + 1
        self.latest[key] = val
        sem = self.sems[key]
        st = self.streams["pool"]
        for s, v in waits:
            st.append(lambda e, s=s, v=v: e.wait_ge(s, v))
        st.append(lambda e: e.collective_compute(kind, op, replica_groups=groups,
                                                 ins=[sap], outs=[dap]).then_inc(sem, 1))
        st.append(lambda e: e.wait_ge(sem, val))
        self.waited["pool"][key] = val
        src.r[key] = val
        dst.w = {key: val}
        dst.r = {}

    def barrier(self):
        for eng, st in self.streams.items():
            wd = self.waited[eng]
            for k, v in self.latest.items():
                if v > 0 and wd.get(k, 0) < v:
                    wd[k] = v
                    st.append(lambda e, s=self.sems[k], v=v: e.wait_ge(s, v))

    def emit(self):
        self.barrier()
        for eng in COMPUTE:
            key = "E_" + eng
            if self.cnt[eng] > self.sw_thresh:
                self.nsw = getattr(self, "nsw", 0) + 1
                self.sems[key] = self.es.enter_context(self.nc.semaphore("sw%d_%s" % (self.nsw, eng)))
                self.cnt[eng] = 0
                self.latest[key] = 0
                for wd in self.waited.values():
                    wd.pop(key, None)
                for b in self.all_bufs:
                    b.w.pop(key, None)
                    b.r.pop(key, None)
        streams = self.streams
        self.streams = {k: [] for k in streams}
        self.ninst = getattr(self, "ninst", 0) + sum(len(v) for v in streams.values())
        import os
        if os.environ.get("KCOUNT"):
            return
        self._emit(streams)

    def _emit(self, streams):
        class _S:
            pass
        self_ = _S()
        self_.streams = streams
        nc = self.nc
        self = self_
        with nc.Block() as block:
            @block.tensor
            def _(e):
                for f in self.streams["pe"]:
                    f(e)

            @block.scalar
            def _(e):
                for f in self.streams["act"]:
                    f(e)

            @block.vector
            def _(e):
                for f in self.streams["dve"]:
                    f(e)

            @block.gpsimd
            def _(e):
                for f in self.streams["pool"]:
                    f(e)

            @block.sync
            def _(e):
                for f in self.streams["sp"]:
                    f(e)


class Ring:
    def __init__(self, bufs):
        self.bufs = bufs
        self.i = 0

    def get(self):
        b = self.bufs[self.i % len(self.bufs)]
        self.i += 1
        return b


class _Phase:
    def __init__(self, S):
        self.S = S

    def __enter__(self):
        self.old = self.S.es_local
        self.stack = ExitStack()
        self.stack.__enter__()
        self.S.es_local = self.stack
        return self.S

    def __exit__(self, *a):
        S = self.S
        stop = False
        if a[0] is None:
            S.emit()

            S.nphase = getattr(S, "nphase", 0) + 1
            import os
            stop = S.nphase == int(os.environ.get("KSTOP", "0"))
        S.es_local = self.old
        self.stack.__exit__(*a)
        if stop:
            raise _StopBuild()
        return False


class _StopBuild(Exception):
    pass


D = 1024
ALPHA = (2 * 4) ** 0.25
LN_EPS = 1e-5
RMS_EPS = 1e-6
G4 = [[0, 1, 2, 3], [4, 5, 6, 7]]
G2 = [[0, 4], [1, 5], [2, 6], [3, 7]]
NEG = -30000.0


class Cfg:
    def __init__(self, SEQ=16384, NEXP=32, CAP=640, LAYERS=(0, 1, 2, 3), NLW=4, WL0=0, mixer=True, moe=True):
        self.SEQ, self.NEXP, self.CAP, self.LAYERS, self.NLW = SEQ, NEXP, CAP, tuple(LAYERS), NLW
        self.mixer, self.moe, self.WL0 = mixer, moe, WL0
        self.TOK = SEQ // 4
        self.EPC = NEXP // 8


def build(cfg):
    nc = bass.Bass("TRN2", target_bir_lowering=False)
    es = ExitStack()
    with es:
        S = Sched(nc, es)
        try:
            _build(S, cfg)
        except _StopBuild:
            pass
        print("kernel build: instructions incl. waits =", getattr(S, "ninst", 0), flush=True)
    return nc


def _build(S, cfg):
    SEQ, TOK, NEXP, C, EPC, NLW = cfg.SEQ, cfg.TOK, cfg.NEXP, cfg.CAP, cfg.EPC, cfg.NLW
    NBL = TOK // 128
    NTL = TOK // 512
    NBF = SEQ // 128
    NQT = SEQ // 512
    CB = C // 128
    TRASH = NEXP * C

    def MM(out, lhsT, rhs, start, stop, R, W):
        S.op("pe", lambda e: e.matmul(out, lhsT, rhs, start=start, stop=stop), reads=R, writes=W)

    def TR(out, in_, ident, R, W):
        S.op("pe", lambda e: e.transpose(out, in_, ident), reads=R, writes=W)

    def ACT(out, in_, func, R, W, bias=None, scale=None, accum=None):
        kw = {}
        if bias is not None:
            kw["bias"] = bias
        if scale is not None:
            kw["scale"] = scale
        if accum is not None:
            kw["accum_out"] = accum
        S.op("act", lambda e: e.activation(out=out, in_=in_, func=func, **kw), reads=R, writes=W)

    def TS(eng, out, in0, s1, op0, R, W, s2=None, op1=None, accum=None):
        kw = {}
        if op1 is not None:
            kw["op1"] = op1
        if accum is not None:
            kw["accum_out"] = accum
        S.op(eng, lambda e: e.tensor_scalar(out=out, in0=in0, scalar1=s1, scalar2=s2, op0=op0, **kw), reads=R, writes=W)

    def TT(eng, out, in0, in1, op, R, W):
        S.op(eng, lambda e: e.tensor_tensor(out=out, in0=in0, in1=in1, op=op), reads=R, writes=W)

    def STT(out, in0, scalar, in1, op0, op1, R, W, accum=None):
        kw = {}
        if accum is not None:
            kw["accum_out"] = accum
        S.op("dve", lambda e: e.scalar_tensor_tensor(out=out, in0=in0, scalar=scalar, in1=in1, op0=op0, op1=op1, **kw),
             reads=R, writes=W)

    def CP(eng, out, in_, R, W):
        if eng == "act":
            S.op("act", lambda e: e.copy(out=out, in_=in_), reads=R, writes=W)
        else:
            S.op(eng, lambda e: e.tensor_copy(out=out, in_=in_), reads=R, writes=W)

    def MEMSET(eng, ap, val, W):
        S.op(eng, lambda e: e.memset(ap, val), writes=W)

    def RECIP(out, in_, R, W):
        S.op("dve", lambda e: e.reciprocal(out=out, in_=in_), reads=R, writes=W)

    def ASEL(out, in_, pattern, cmp, fill, base, cm, R, W):
        S.op("pool", lambda e: e.affine_select(out=out, in_=in_, pattern=pattern, compare_op=cmp, fill=fill,
                                                base=base, channel_multiplier=cm), reads=R, writes=W)

    def din(name, shape, dt=F32):
        return S.dram(name, shape, dt, kind="ExternalInput")

    x_in = din("x_in", [TOK, D])
    lng = din("lng", [8, D])
    lnb = din("lnb", [8, D])
    out = S.dram("out", [TOK, D], F32, kind="ExternalOutput")
    if cfg.moe:
        router_w = din("router_w", [4, D, NEXP])
        router_b = din("router_b", [4, NEXP])
        bgu = din("bgu", [128, 4 * NEXP * 16])
        bdn = din("bdn", [4 * NEXP, D])
        wgu_sh = din("wgu_sh", [NLW * EPC * D, 2 * D])
        wdn_sh = din("wdn_sh", [NLW * EPC * D, D])
    has_even = cfg.mixer and any(L % 2 == 0 for L in cfg.LAYERS)
    has_odd = cfg.mixer and any(L % 2 == 1 for L in cfg.LAYERS)
    if has_even:
        we_in = din("we_in", [2, D, 898])
        we_out = din("we_out", [2, D, D])
        convw = din("convw", [2, 128, 12])
        lamp = din("lamp", [2, 256])
        subw = din("subw", [2, 128])
        gnw = din("gnw", [2, 128])
        alog = din("alog", [2, 1])
        dtb = din("dtb", [2, 1])
        t5col = din("t5col", [32, 1])
        t5oh = din("t5oh", [33, 2560])
    if has_odd:
        wo_in = din("wo_in", [2, D, 1028])
        wo_out = din("wo_out", [2, D, D])
        qkw = din("qkw", [2, 64, 2])
        fb = din("fb", [2, 4])

    XR = [S.dram("xres0", [TOK, D], F32), S.dram("xres1", [TOK, D], F32)]
    XR1 = S.dram("xr1", [TOK, D], F32)
    XTB = S.dram("xtb", [D, TOK], BF16)
    XTALL = S.dram("xtall", [4 * D, TOK], BF16)
    TC = min(2048, SEQ)
    if cfg.mixer:
        OTOK = S.dram("otok", [SEQ, 256], BF16)
        OALL = S.dram("oall", [4 * SEQ, 256], BF16)
        oidx = din("oidx", [128, NBL * 4], I32)
        CQ = S.dram("cq", [3, SEQ], BF16)
        O0S = S.dram("o0s", [SEQ, 128], F32)
        BV = S.dram("bv", [2560], F32)
        SKW = S.dram("skw", [16, 128 * 640], F32)
    if cfg.moe:
        XDISP = S.dram("xdisp", [NEXP * C + 128, D], BF16)
        YDISP = S.dram("ydisp", [NEXP * C + 128, D], F32)
        R1 = NLW * EPC * D
        WB1G = S.dram("wb1g", [R1, 2 * D], BF16)
        WB1D = S.dram("wb1d", [R1, D], BF16)
        RGG, RGD = 256, 512
        NCG, NCD = R1 // RGG, R1 // RGD
        WALLG = [S.dram("wallg%d" % i, [16 * 8 * RGG, 2 * D], BF16) for i in range((NCG + 15) // 16)]
        WALLD = [S.dram("walld%d" % i, [16 * 8 * RGD, D], BF16) for i in range((NCD + 15) // 16)]
        S1G = [S.dram("s1g%d" % i, [4 * RGG, 2 * D], BF16) for i in range(2)]
        S1D = [S.dram("s1d%d" % i, [4 * RGD, D], BF16) for i in range(2)]

    ident_f = S.sbuf("ident_f", [128, 128], F32)
    ident_b = S.sbuf("ident_b", [128, 128], BF16)
    ones_f = S.sbuf("ones_f", [128, 128], F32)
    ones_b = S.sbuf("ones_b", [128, 128], BF16)
    triU_f = S.sbuf("triU_f", [128, 128], F32)
    Ls_b = S.sbuf("Ls_b", [128, 128], BF16)
    DESTI = S.sbuf("DESTI", [128, NBL * 4], I32)
    GATE = S.sbuf("GATE", [128, NBL * 4], F32)
    CNT = S.sbuf("CNT", [128, NEXP], F32)
    EOFF = S.sbuf("EOFF", [128, NEXP], F32)
    with S.phase():
        tmpf = S.sbuf("tmpf", [128, 128], F32)
        tmpi = S.sbuf("tmpi", [128, NEXP], I32)
        MEMSET("pool", ident_f[:], 0.0, [ident_f])
        ASEL(ident_f[:], ident_f[:], [[-1, 128]], ALU.not_equal, 1.0, 0, 1, [ident_f], [ident_f])
        CP("dve", ident_b[:], ident_f[:], [ident_f], [ident_b])
        MEMSET("pool", ones_f[:], 1.0, [ones_f])
        MEMSET("pool", ones_b[:], 1.0, [ones_b])
        MEMSET("pool", triU_f[:], 1.0, [triU_f])
        ASEL(triU_f[:], triU_f[:], [[1, 128]], ALU.is_ge, 0.0, 0, -1, [triU_f], [triU_f])
        MEMSET("pool", tmpf[:], 1.0, [tmpf])
        ASEL(tmpf[:], tmpf[:], [[1, 128]], ALU.is_ge, 0.0, -1, -1, [tmpf], [tmpf])
        CP("dve", Ls_b[:], tmpf[:], [tmpf], [Ls_b])
        S.op("pool", lambda e: e.iota(tmpi[:], pattern=[[C, NEXP]], base=0, channel_multiplier=0), writes=[tmpi])
        CP("dve", EOFF[:], tmpi[:], [tmpi], [EOFF])

    if cfg.moe:
        with S.phase():
            st_r = S.ring("wst", [128, 2 * D], F32, 3)
            sb_r = S.ring("wsb", [128, 2 * D], BF16, 3)
            n = 0
            for (src, b1, ncol) in ((wgu_sh, WB1G, 2 * D), (wdn_sh, WB1D, D)):
                rows = 128 * (2 * D // ncol)
                for r0 in range(0, R1, rows):
                    st = st_r.get()
                    sb = sb_r.get()
                    k = rows // 128
                    S.dma("sp", st[:].rearrange("p (k n) -> p k n", k=k), src[r0:r0 + rows, :].rearrange("(k p) n -> p k n", p=128), st, src)
                    CP("act" if n % 2 == 0 else "dve", sb[:], st[:], [st], [sb])
                    S.dma("sp", b1[r0:r0 + rows, :].rearrange("(k p) n -> p k n", p=128), sb[:].rearrange("p (k n) -> p k n", k=k), b1, sb)
                    n += 1
            for (b1, s1s, walls, RG, NC_) in ((WB1G, S1G, WALLG, RGG, NCG), (WB1D, S1D, WALLD, RGD, NCD)):
                for i in range(NC_):
                    s1 = s1s[i % 2]
                    S.cc("AllGather", ALU.bypass, G4, b1, s1, src_ap=b1[i * RG:(i + 1) * RG, :])
                    wt = walls[i // 16]
                    for r4 in range(4):
                        base = (((i % 16) * 4 + r4) * 2) * RG
                        S.cc("AllGather", ALU.bypass, G2, s1, wt, src_ap=s1[r4 * RG:(r4 + 1) * RG, :],
                             dst_ap=wt[base:base + 2 * RG, :])
            zt = S.sbuf("zt", [128, D], F32)
            ztb = S.sbuf("ztb", [128, D], BF16)
            MEMSET("dve", zt[:], 0.0, [zt])
            MEMSET("dve", ztb[:], 0.0, [ztb])
            for r0 in range(0, NEXP * C + 128, 128):
                S.dma("sp", XDISP[r0:r0 + 128, :], ztb[:], XDISP, ztb)
                S.dma("sp", YDISP[r0:r0 + 128, :], zt[:], YDISP, zt)

    def wloc(L, e, RG, chunk):
        c, le = e // EPC, e % EPC
        g, r4 = c // 4, c % 4
        rr = ((L - cfg.WL0) * EPC + le) * D + chunk * RG
        i = rr // RG
        return i // 16, ((((i % 16) * 4 + r4) * 2 + g) * RG)

    XTBv = XTB.ap().rearrange("(k p) t -> p k t", p=128)

    def emit_xT_block(xt_f32, xT_tile, col, ps_ring, cp_eng):
        ps = ps_ring.get()
        for kc in range(8):
            TR(ps[:, kc * 128:(kc + 1) * 128], xt_f32[:, kc * 128:(kc + 1) * 128], ident_f[:], [xt_f32, ident_f], [ps])
        CP(cp_eng, xT_tile[:, :, col:col + 128], ps[:].rearrange("p (k t) -> p k t", k=8), [ps], [xT_tile])

    def xt_allgather():
        for kc in range(8):
            S.cc("AllGather", ALU.bypass, G4, XTB, XTALL, src_ap=XTB[kc * 128:(kc + 1) * 128, :],
                 dst_ap=XTALL[kc * 512:(kc + 1) * 512, :])

    def phase_x0():
        with S.phase():
            xin_r = S.ring("xin", [128, D], F32, 3)
            xT_r = S.ring("xT", [128, 8, 512], BF16, 2)
            ps_r = S.ring("psx", [128, D], F32, 2, psum=True)
            for tl in range(NTL):
                xT = xT_r.get()
                for b4 in range(4):
                    b = tl * 4 + b4
                    xt = xin_r.get()
                    S.dma("sp", xt[:], x_in[b * 128:(b + 1) * 128, :], xt, x_in)
                    emit_xT_block(xt, xT, b4 * 128, ps_r, "act" if b4 % 2 == 0 else "dve")
                S.dma("sp", XTBv[:, :, tl * 512:(tl + 1) * 512], xT[:], XTB, xT)
            xt_allgather()

    def layer_norm(z, outt, gt, bt, st_r, junk):
        st = st_r.get()
        ACT(junk[:], z[:], AF.Identity, [z], [junk, st], accum=st[:, 0:1])
        ACT(junk[:], z[:], AF.Square, [z], [junk, st], accum=st[:, 1:2])
        TS("dve", st[:, 2:3], st[:, 0:1], 1.0 / D, ALU.mult, [st], [st])
        TT("dve", st[:, 3:4], st[:, 2:3], st[:, 2:3], ALU.mult, [st], [st])
        STT(st[:, 4:5], st[:, 1:2], 1.0 / D, st[:, 3:4], ALU.mult, ALU.subtract, [st], [st])
        TS("dve", st[:, 4:5], st[:, 4:5], LN_EPS, ALU.add, [st], [st])
        ACT(st[:, 5:6], st[:, 4:5], AF.Sqrt, [st], [st])
        RECIP(st[:, 6:7], st[:, 5:6], [st], [st])
        TS("dve", outt[:], z[:], st[:, 2:3], ALU.subtract, [z, st], [outt], s2=st[:, 6:7], op1=ALU.mult)
        TT("dve", outt[:], outt[:], gt[:], ALU.mult, [outt, gt], [outt])
        TT("dve", outt[:], outt[:], bt[:], ALU.add, [outt, bt], [outt])

    def phase_ln_moe(L, xres, xnext, is_last):
        with S.phase():
            gt = S.sbuf("gt", [128, D], F32)
            bt = S.sbuf("bt", [128, D], F32)
            S.dma("sp", gt[:], lng.ap()[L, :].partition_broadcast(128), gt, lng)
            S.dma("sp", bt[:], lnb.ap()[L, :].partition_broadcast(128), bt, lnb)
            xr_r = S.ring("xr", [128, D], F32, 2)
            if cfg.mixer:
                wsrc = we_out if L % 2 == 0 else wo_out
                WO = S.sbuf("WO", [128, 8, D], BF16)
                for q4 in range(4):
                    S.dma("pool", WO[:, 2 * q4:2 * q4 + 2, :],
                          wsrc.ap()[L // 2, q4 * 256:(q4 + 1) * 256, :].rearrange("(k p) n -> p k n", p=128), WO, wsrc)
                OIDX = S.sbuf("OIDX", [128, NBL * 4], I32)
                S.dma("sp", OIDX[:], oidx[:, :], OIDX, oidx)
                og_r = S.ring("og", [128, 4, 256], BF16, 2)
                oT_r = S.ring("oT", [128, 8, 128], BF16, 2)
                pso_r = S.ring("pso", [128, D], BF16, 1, psum=True)
                psh_r = S.ring("psh", [128, 512], F32, 2, psum=True)
            z_r = S.ring("z", [128, D], F32, 2)
            x1_r = S.ring("x1", [128, D], F32, 2)
            x1b_r = S.ring("x1b", [128, D], BF16, 2)
            junk = S.sbuf("junk", [128, D], F32)
            st_r = S.ring("st", [128, 8], F32, 2)
            if cfg.moe:
                RW = S.sbuf("RW", [128, 8, NEXP], F32)
                RB = S.sbuf("RB", [128, NEXP], F32)
                S.dma("sp", RW[:], router_w.ap()[L].rearrange("(k p) e -> p k e", p=128), RW, router_w)
                S.dma("sp", RB[:], router_b.ap()[L, :].partition_broadcast(128), RB, router_b)
                x1T_r = S.ring("x1T", [128, 8, 128], F32, 2)
                psx_r = S.ring("psx", [128, D], F32, 1, psum=True)
                psl_r = S.ring("psl", [128, 512], F32, 1, psum=True)
                psp_r = S.ring("psp", [128, 512], F32, 2, psum=True)
                sm_r = S.ring("sm", [128, 12, NEXP], F32, 2)
                mb_r = S.ring("mb", [128, NEXP], BF16, 2)
                t8_r = S.ring("t8", [128, 8], F32, 2)
                sc_r = S.ring("sc", [128, 16], F32, 2)
                MEMSET("dve", CNT[:], 0.0, [CNT])
            for b in range(NBL):
                xr = xr_r.get()
                S.dma("sp", xr[:], xres[b * 128:(b + 1) * 128, :], xr, xres)
                z = z_r.get()
                if cfg.mixer:
                    og = og_r.get()
                    for s4 in range(4):
                        col = b * 4 + s4
                        S.dma("pool", None, None, og, OALL, extra=[OIDX],
                              fn=lambda e, col=col, og=og, s4=s4: e.indirect_dma_start(
                                  out=og[:, s4, :], out_offset=None, in_=OALL.ap()[:, :],
                                  in_offset=bass.IndirectOffsetOnAxis(ap=OIDX[:, col:col + 1], axis=0)))
                    pso = pso_r.get()
                    for kc in range(8):
                        TR(pso[:, kc * 128:(kc + 1) * 128], og[:, kc // 2, (kc % 2) * 128:(kc % 2 + 1) * 128], ident_b[:], [og, ident_b], [pso])
                    oT = oT_r.get()
                    CP("act", oT[:], pso[:].rearrange("p (k t) -> p k t", k=8), [pso], [oT])
                    for half in range(2):
                        psh = psh_r.get()
                        for kc in range(8):
                            MM(psh[:], oT[:, kc, :], WO[:, kc, half * 512:(half + 1) * 512], kc == 0, kc == 7, [oT, WO], [psh])
                        STT(z[:, half * 512:(half + 1) * 512], xr[:, half * 512:(half + 1) * 512], ALPHA, psh[:], ALU.mult, ALU.add, [xr, psh], [z])
                else:
                    TS("dve", z[:], xr[:], ALPHA, ALU.mult, [xr], [z])
                x1 = x1_r.get()
                layer_norm(z, x1, gt, bt, st_r, junk)
                S.dma("sp", XR1[b * 128:(b + 1) * 128, :], x1[:], XR1, x1)
                if not cfg.moe:
                    continue
                x1b = x1b_r.get()
                CP("act", x1b[:], x1[:], [x1], [x1b])
                x1T = x1T_r.get()
                ps = psx_r.get()
                for kc in range(8):
                    TR(ps[:, kc * 128:(kc + 1) * 128], x1[:, kc * 128:(kc + 1) * 128], ident_f[:], [x1, ident_f], [ps])
                CP("act", x1T[:], ps[:].rearrange("p (k t) -> p k t", k=8), [ps], [x1T])
                psl = psl_r.get()
                for kc in range(8):
                    MM(psl[:, 0:NEXP], x1T[:, kc, :], RW[:, kc, :], kc == 0, kc == 7, [x1T, RW], [psl])
                sm = sm_r.get()
                lg, ex, mk, gd, gates, pos, dfull, oh, jk = (sm[:, i, :] for i in range(9))
                t8 = t8_r.get()
                sc = sc_r.get()
                TT("dve", lg, psl[:, 0:NEXP], RB[:], ALU.add, [psl, RB], [sm])
                S.op("dve", lambda e, t8=t8, lg=lg: e.max(out=t8[:], in_=lg), reads=[sm], writes=[t8])
                TS("dve", sc[:, 0:1], t8[:, 0:1], -1.0, ALU.mult, [t8], [sc])
                ACT(ex, lg, AF.Exp, [sm, sc], [sm], bias=sc[:, 0:1])
                TS("dve", mk, lg, t8[:, 3:4], ALU.is_ge, [sm, t8], [sm])
                TT("dve", gd, ex, mk, ALU.mult, [sm], [sm])
                S.op("dve", lambda e, sc=sc, gd=gd: e.tensor_reduce(out=sc[:, 1:2], in_=gd, axis=AX.X, op=ALU.add),
                     reads=[sm], writes=[sc])
                RECIP(sc[:, 2:3], sc[:, 1:2], [sc], [sc])
                TS("dve", gates, gd, sc[:, 2:3], ALU.mult, [sm, sc], [sm])
                mb = mb_r.get()
                CP("dve", mb[:], mk, [sm], [mb])
                psp = psp_r.get()
                MM(psp[:, 0:NEXP], Ls_b[:], mb[:], True, True, [Ls_b, mb], [psp])
                TT("dve", pos, psp[:, 0:NEXP], CNT[:], ALU.add, [psp, CNT], [sm])
                psc = psp_r.get()
                MM(psc[:, 0:NEXP], ones_b[:], mb[:], True, True, [ones_b, mb], [psc])
                TT("dve", CNT[:], CNT[:], psc[:, 0:NEXP], ALU.add, [CNT, psc], [CNT])
                TT("dve", dfull, pos, EOFF[:], ALU.add, [sm, EOFF], [sm])
                for k in range(4):
                    TS("dve", oh, lg, t8[:, k:k + 1], ALU.is_equal, [sm, t8], [sm])
                    TT("dve", jk, oh, pos, ALU.mult, [sm], [sm])
                    S.op("dve", lambda e, sc=sc, jk=jk: e.tensor_reduce(out=sc[:, 4:5], in_=jk, axis=AX.X, op=ALU.add),
                         reads=[sm], writes=[sc])
                    TT("dve", jk, oh, dfull, ALU.mult, [sm], [sm])
                    S.op("dve", lambda e, sc=sc, jk=jk: e.tensor_reduce(out=sc[:, 5:6], in_=jk, axis=AX.X, op=ALU.add),
                         reads=[sm], writes=[sc])
                    TT("dve", jk, oh, gates, ALU.mult, [sm], [sm])
                    S.op("dve", lambda e, sc=sc, jk=jk: e.tensor_reduce(out=sc[:, 6:7], in_=jk, axis=AX.X, op=ALU.add),
                         reads=[sm], writes=[sc])
                    TS("dve", sc[:, 7:8], sc[:, 4:5], float(C), ALU.is_lt, [sc], [sc])
                    TS("dve", sc[:, 8:9], sc[:, 5:6], float(TRASH), ALU.subtract, [sc], [sc])
                    TT("dve", sc[:, 8:9], sc[:, 8:9], sc[:, 7:8], ALU.mult, [sc], [sc])
                    TS("dve", sc[:, 8:9], sc[:, 8:9], float(TRASH), ALU.add, [sc], [sc])
                    col = b * 4 + k
                    CP("dve", DESTI[:, col:col + 1], sc[:, 8:9], [sc], [DESTI])
                    TT("dve", GATE[:, col:col + 1], sc[:, 6:7], sc[:, 7:8], ALU.mult, [sc], [GATE])
                    S.dma("pool", None, None, XDISP, x1b, extra=[DESTI],
                          fn=lambda e, col=col, x1b=x1b: e.indirect_dma_start(
                              out=XDISP.ap()[:, :], out_offset=bass.IndirectOffsetOnAxis(ap=DESTI[:, col:col + 1], axis=0),
                              in_=x1b[:, :], in_offset=None))
        if cfg.moe:
            with S.phase():
                BG = S.sbuf("BG", [128, NEXP * 16], F32)
                S.dma("sp", BG[:], bgu[:, L * NEXP * 16:(L + 1) * NEXP * 16], BG, bgu)
                wgu_r = S.ring("wgu", [128, 8, 2 * D], BF16, 2)
                wdn_r = S.ring("wdn", [128, 8, D], BF16, 2)
                bd_r = S.ring("bd", [128, D], F32, 2)
                xs_r = S.ring("xs", [128, CB, D], BF16, 1)
                xsT_r = S.ring("xsT", [128, 8, C], BF16, 1)
                aT_r = S.ring("aT", [128, 8, C], BF16, 1)
                tmp_r = S.ring("tmp", [128, 5, 512], F32, 2)
                ys_r = S.ring("ys", [128, D], F32, 2)
                pst_r = S.ring("pst", [128, D], BF16, 2, psum=True)
                psg_r = S.ring("psg", [128, 512], F32, 2, psum=True)
                psu_r = S.ring("psu", [128, 512], F32, 2, psum=True)
                psd_r = S.ring("psd", [128, 512], F32, 2, psum=True)
                chunks = [(c0, min(512, C - c0)) for c0 in range(0, C, 512)]
                for e_ in range(NEXP):
                    wg = wgu_r.get()
                    wd = wdn_r.get()
                    for q4 in range(4):
                        ti, row = wloc(L, e_, RGG, q4)
                        S.dma("sp", wg[:, 2 * q4:2 * q4 + 2, :],
                              WALLG[ti].ap()[row:row + 256, :].rearrange("(k p) n -> p k n", p=128), wg, WALLG[ti])
                    for q4 in range(2):
                        ti, row = wloc(L, e_, RGD, q4)
                        S.dma("sp", wd[:, 4 * q4:4 * q4 + 4, :],
                              WALLD[ti].ap()[row:row + 512, :].rearrange("(k p) n -> p k n", p=128), wd, WALLD[ti])
                    bd = bd_r.get()
                    S.dma("sp", bd[:], bdn.ap()[L * NEXP + e_, :].partition_broadcast(128), bd, bdn)
                    xs = xs_r.get()
                    S.dma("sp", xs[:], XDISP.ap()[e_ * C:(e_ + 1) * C, :].rearrange("(c p) d -> p c d", p=128), xs, XDISP)
                    xsT = xsT_r.get()
                    for cb in range(CB):
                        pst = pst_r.get()
                        for kc in range(8):
                            TR(pst[:, kc * 128:(kc + 1) * 128], xs[:, cb, kc * 128:(kc + 1) * 128], ident_b[:], [xs, ident_b], [pst])
                        CP("act" if cb % 2 == 0 else "dve", xsT[:, :, cb * 128:(cb + 1) * 128],
                           pst[:].rearrange("p (k t) -> p k t", k=8), [pst], [xsT])
                    aT = aT_r.get()
                    for j in range(8):
                        for (c0, w) in chunks:
                            psg = psg_r.get()
                            psu = psu_r.get()
                            for kc in range(8):
                                MM(psg[:, 0:w], wg[:, kc, j * 128:(j + 1) * 128], xsT[:, kc, c0:c0 + w], kc == 0, kc == 7, [wg, xsT], [psg])
                            for kc in range(8):
                                MM(psu[:, 0:w], wg[:, kc, D + j * 128:D + (j + 1) * 128], xsT[:, kc, c0:c0 + w], kc == 0, kc == 7, [wg, xsT], [psu])
                            tmp = tmp_r.get()
                            g1, sg, u1, u2, tt_ = (tmp[:, i, 0:w] for i in range(5))
                            bgc = (e_ * 16 + j)
                            TS("dve", g1, psg[:, 0:w], BG[:, bgc:bgc + 1], ALU.add, [psg, BG], [tmp], s2=7.0, op1=ALU.min)
                            ACT(sg, g1, AF.Sigmoid, [tmp], [tmp], scale=1.702)
                            TS("dve", u1, psu[:, 0:w], BG[:, bgc + 8:bgc + 9], ALU.add, [psu, BG], [tmp], s2=7.0, op1=ALU.min)
                            TS("dve", u2, u1, -7.0, ALU.max, [tmp], [tmp], s2=1.0, op1=ALU.add)
                            TT("pool", tt_, g1, sg, ALU.mult, [tmp], [tmp])
                            TT("pool", aT[:, j, c0:c0 + w], tt_, u2, ALU.mult, [tmp], [aT])
                    for cb in range(CB):
                        ys = ys_r.get()
                        for half in range(2):
                            psd = psd_r.get()
                            for j in range(8):
                                MM(psd[:], aT[:, j, cb * 128:(cb + 1) * 128], wd[:, j, half * 512:(half + 1) * 512], j == 0, j == 7, [aT, wd], [psd])
                            TT("dve", ys[:, half * 512:(half + 1) * 512], psd[:], bd[:, half * 512:(half + 1) * 512], ALU.add, [psd, bd], [ys])
                        S.dma("sp", YDISP[e_ * C + cb * 128:e_ * C + (cb + 1) * 128, :], ys[:], YDISP, ys)
        with S.phase():
            gt = S.sbuf("gt", [128, D], F32)
            bt = S.sbuf("bt", [128, D], F32)
            S.dma("sp", gt[:], lng.ap()[4 + L, :].partition_broadcast(128), gt, lng)
            S.dma("sp", bt[:], lnb.ap()[4 + L, :].partition_broadcast(128), bt, lnb)
            x1_r = S.ring("x1", [128, D], F32, 2)
            acc_r = S.ring("acc", [128, D], F32, 2)
            yk_r = S.ring("yk", [128, D], F32, 4)
            x2_r = S.ring("x2", [128, D], F32, 2)
            junk = S.sbuf("junk", [128, D], F32)
            st_r = S.ring("st", [128, 8], F32, 2)
            xT_r = S.ring("xT", [128, 8, 512], BF16, 2)
            ps_r = S.ring("psx", [128, D], F32, 2, psum=True)
            dst = out if is_last else xnext
            xT = None
            for b in range(NBL):
                x1 = x1_r.get()
                S.dma("sp", x1[:], XR1[b * 128:(b + 1) * 128, :], x1, XR1)
                acc = acc_r.get()
                TS("dve", acc[:], x1[:], ALPHA, ALU.mult, [x1], [acc])
                if cfg.moe:
                    for k in range(4):
                        col = b * 4 + k
                        yk = yk_r.get()
                        S.dma("pool", None, None, yk, YDISP, extra=[DESTI],
                              fn=lambda e, col=col, yk=yk: e.indirect_dma_start(
                                  out=yk[:, :], out_offset=None, in_=YDISP.ap()[:, :],
                                  in_offset=bass.IndirectOffsetOnAxis(ap=DESTI[:, col:col + 1], axis=0)))
                        STT(acc[:], yk[:], GATE[:, col:col + 1], acc[:], ALU.mult, ALU.add, [yk, GATE, acc], [acc])
                x2 = x2_r.get()
                layer_norm(acc, x2, gt, bt, st_r, junk)
                S.dma("sp", dst[b * 128:(b + 1) * 128, :], x2[:], dst, x2)
                if not is_last:
                    if b % 4 == 0:
                        xT = xT_r.get()
                    emit_xT_block(x2, xT, (b % 4) * 128, ps_r, "act")
                    if b % 4 == 3:
                        tl = b // 4
                        S.dma("sp", XTBv[:, :, tl * 512:(tl + 1) * 512], xT[:], XTB, xT)
            if not is_last:
                xt_allgather()


    XTALLv = XTALL.ap().rearrange("(k r p) t -> r p k t", k=8, r=4) if True else None

    def load_xT(xT, tt):
        rank, lt = tt // NTL, tt % NTL
        S.dma("sp", xT[:], XTALLv[rank][:, :, lt * 512:(lt + 1) * 512], xT, XTALL)

    def o_allgather():
        for j in range(SEQ // TC):
            S.cc("AllGather", ALU.bypass, G4, OTOK, OALL, src_ap=OTOK[j * TC:(j + 1) * TC, :],
                 dst_ap=OALL[j * 4 * TC:(j + 1) * 4 * TC, :])

    def odd_mixer(L):
        i = L // 2
        with S.phase():
            Wb = S.sbuf("Wb", [128, 8, 1028], BF16)
            for q4 in range(4):
                S.dma("pool", Wb[:, 2 * q4:2 * q4 + 2, :],
                      wo_in.ap()[i, q4 * 256:(q4 + 1) * 256, :].rearrange("(k p) n -> p k n", p=128), Wb, wo_in)
            WQK = S.sbuf("WQK", [64, 2], F32)
            S.dma("sp", WQK[:], qkw.ap()[i], WQK, qkw)
            TS("dve", WQK[:, 0:1], WQK[:, 0:1], 0.125, ALU.mult, [WQK], [WQK])
            NFB = S.sbuf("NFB", [128, 4], F32)
            S.dma("sp", NFB[:], fb.ap()[i, :].partition_broadcast(128), NFB, fb)
            TS("dve", NFB[:], NFB[:], -1.0, ALU.mult, [NFB], [NFB])
            MKf = S.sbuf("MKf", [128, 4, 512], F32)
            MK = S.sbuf("MK", [128, 4, 512], BF16)
            MEMSET("pool", MKf[:], 0.0, [MKf])
            for j in range(4):
                ASEL(MKf[:, j, :], MKf[:, j, :], [[1, 512]], ALU.is_ge, NEG, -128 * j, -1, [MKf], [MKf])
            CP("dve", MK[:], MKf[:], [MKf], [MK])
            QT = S.sbuf("QT", [67, SEQ], BF16)
            KT = S.sbuf("KT", [67, SEQ], BF16)
            GTK = S.sbuf("GTK", [128, NBF, 64], BF16)
            VA = S.sbuf("VA", [128, NBF, 65], BF16)
            LF = S.sbuf("LF", [128, NBF], F32)
            CUM = S.sbuf("CUM", [128, NBF], F32)
            NEGCUM = S.sbuf("NEGCUM", [128, NBF], F32)
            scan = [S.sbuf("scan%d" % k, [128, NBF], F32) for k in range(2)]
            tot = S.sbuf("tot", [128, NBF], F32)
            ct = S.sbuf("ct", [128, 6, 128], F32)
            ctb = S.sbuf("ctb", [128, 3, 128], BF16)
            MEMSET("dve", VA[:, :, 64:65], 1.0, [VA])
            MEMSET("dve", KT[64:67, :], 1.0, [KT])
            xT_r = S.ring("xT", [128, 8, 512], BF16, 2)
            qf_r = S.ring("qf", [64, 512], F32, 2)
            sq_r = S.ring("sq", [64, 512], F32, 2)
            rs_r = S.ring("rs", [64, 512], F32, 2)
            sm_r = S.ring("smo", [128, 8], F32, 4)
            pt_r = S.ring("pt", [128, 512], BF16, 4)
            ot_r = S.ring("ot", [128, 4, 64], BF16, 2)
            osb_r = S.ring("osb", [128, 512], F32, 2)
            for b_ in osb_r.bufs:
                MEMSET("dve", b_[:], 0.0, [b_])
            psm_r = S.ring("psm", [128, 512], F32, 3, psum=True)
            pss_r = S.ring("pss", [128, 512], F32, 3, psum=True)
            pso_r = S.ring("pso", [128, 512], F32, 2, psum=True)
            import os
            KODD = int(os.environ.get("KODD", "9"))
            if KODD == 0:
                return
            for h in range(4):
                w0 = h * 257
                for tt in range(NQT):
                    xT = xT_r.get()
                    load_xT(xT, tt)
                    for (coff, dst, wi) in ((0, QT, 0), (64, KT, 1)):
                        psq = psm_r.get()
                        for kc in range(8):
                            MM(psq[0:64, :], Wb[:, kc, w0 + coff:w0 + coff + 64], xT[:, kc, :], kc == 0, kc == 7, [Wb, xT], [psq])
                        qf = qf_r.get()
                        CP("act", qf[:], psq[0:64, :], [psq], [qf])
                        sq = sq_r.get()
                        TT("pool", sq[:], qf[:], qf[:], ALU.mult, [qf], [sq])
                        pssum = psm_r.get()
                        MM(pssum[0:64, :], ones_f[0:64, 0:64], sq[:], True, True, [ones_f, sq], [pssum])
                        rs = rs_r.get()
                        TS("dve", rs[:], pssum[0:64, :], 1.0 / 64, ALU.mult, [pssum], [rs], s2=RMS_EPS, op1=ALU.add)
                        ACT(rs[:], rs[:], AF.Sqrt, [rs], [rs])
                        RECIP(rs[:], rs[:], [rs], [rs])
                        STT(dst[0:64, tt * 512:(tt + 1) * 512], qf[:], WQK[:, wi:wi + 1], rs[:], ALU.mult, ALU.mult, [qf, WQK, rs], [dst])
                    for b4 in range(4):
                        gb = tt * 4 + b4
                        ps = psm_r.get()
                        for kc in range(8):
                            MM(ps[:, 0:129], xT[:, kc, b4 * 128:(b4 + 1) * 128], Wb[:, kc, w0 + 128:w0 + 257], kc == 0, kc == 7, [xT, Wb], [ps])
                        sm = sm_r.get()
                        CP("dve", VA[:, gb, 0:64], ps[:, 0:64], [ps], [VA])
                        ACT(GTK[:, gb, :], ps[:, 65:129], AF.Sigmoid, [ps], [GTK])
                        ACT(sm[:, 0:1], ps[:, 64:65], AF.Exp, [ps, NFB], [sm], bias=NFB[:, h:h + 1], scale=-1.0)
                        ACT(sm[:, 1:2], sm[:, 0:1], AF.Ln, [sm], [sm], bias=1.0)
                        TS("dve", LF[:, gb:gb + 1], sm[:, 1:2], -1.0, ALU.mult, [sm], [LF])
                if KODD == 1:
                    return
                psw = psm_r.get()
                MM(psw[:, 0:NBF], triU_f[:], LF[:], True, True, [triU_f, LF], [psw])
                pstot = psm_r.get()
                MM(pstot[:, 0:NBF], ones_f[:], LF[:], True, True, [ones_f, LF], [pstot])
                CP("dve", tot[:], pstot[:, 0:NBF], [pstot], [tot])
                CP("dve", scan[0][:], tot[:], [tot], [scan[0]])
                a, bq = scan[0], scan[1]
                sft = 1
                while sft < NBF:
                    TT("dve", bq[:, sft:NBF], a[:, sft:NBF], a[:, 0:NBF - sft], ALU.add, [a], [bq])
                    CP("dve", bq[:, 0:sft], a[:, 0:sft], [a], [bq])
                    a, bq = bq, a
                    sft *= 2
                TT("dve", CUM[:], psw[:, 0:NBF], a[:], ALU.add, [psw, a], [CUM])
                TT("dve", CUM[:], CUM[:], tot[:], ALU.subtract, [CUM, tot], [CUM])
                TS("dve", NEGCUM[:], CUM[:], -1.0, ALU.mult, [CUM], [NEGCUM])
                pct = psm_r.get()
                TR(pct[0:NBF, 0:128], CUM[:, 0:NBF], ident_f[:], [CUM, ident_f], [pct])
                CP("dve", ct[0:NBF, 0, :], pct[0:NBF, 0:128], [pct], [ct])
                CP("dve", ctb[0:NBF, 0, :], ct[0:NBF, 0, :], [ct], [ctb])
                CP("dve", ct[0:NBF, 1, :], ctb[0:NBF, 0, :], [ctb], [ct])
                TT("dve", ct[0:NBF, 2, :], ct[0:NBF, 0, :], ct[0:NBF, 1, :], ALU.subtract, [ct], [ct])
                CP("dve", ctb[0:NBF, 1, :], ct[0:NBF, 2, :], [ct], [ctb])
                CP("dve", ct[0:NBF, 3, :], ctb[0:NBF, 1, :], [ctb], [ct])
                TT("dve", ct[0:NBF, 4, :], ct[0:NBF, 2, :], ct[0:NBF, 3, :], ALU.subtract, [ct], [ct])
                CP("dve", ctb[0:NBF, 2, :], ct[0:NBF, 4, :], [ct], [ctb])
                for j in range(3):
                    S.dma("sp", CQ.ap()[j, :].rearrange("(j t) -> j t", t=128), ctb[0:NBF, j, :], CQ, ctb)
                for j in range(3):
                    S.dma("sp", QT[64 + j:65 + j, :], CQ[j:j + 1, :], QT, CQ)
                if KODD == 2:
                    return
                for qt in range(NQT):
                    oacc = pso_r.get()
                    nkb = 4 * qt + 4
                    for kb in range(nkb):
                        pss = pss_r.get()
                        diag = kb >= 4 * qt
                        MM(pss[:], KT[0:67, kb * 128:(kb + 1) * 128], QT[0:67, qt * 512:(qt + 1) * 512], True, not diag, [KT, QT], [pss])
                        if diag:
                            MM(pss[:], ident_b[:], MK[:, kb - 4 * qt, :], False, True, [ident_b, MK], [pss])
                        pt = pt_r.get()
                        ACT(pt[:], pss[:], AF.Exp, [pss, NEGCUM], [pt], bias=NEGCUM[:, kb:kb + 1])
                        MM(oacc[0:65, :], VA[:, kb, :], pt[:], kb == 0, kb == nkb - 1, [VA, pt], [oacc])
                    osb = osb_r.get()
                    CP("dve", osb[0:65, :], oacc[0:65, :], [oacc], [osb])
                    ptr = psm_r.get()
                    for b4 in range(4):
                        TR(ptr[:, b4 * 128:(b4 + 1) * 128], osb[:, b4 * 128:(b4 + 1) * 128], ident_f[:], [osb, ident_f], [ptr])
                    ot = ot_r.get()
                    for b4 in range(4):
                        gb = qt * 4 + b4
                        sm = sm_r.get()
                        RECIP(sm[:, 0:1], ptr[:, b4 * 128 + 64:b4 * 128 + 65], [ptr], [sm])
                        STT(ot[:, b4, :], ptr[:, b4 * 128:b4 * 128 + 64], sm[:, 0:1], GTK[:, gb, :], ALU.mult, ALU.mult, [ptr, sm, GTK], [ot])
                    S.dma("sp", OTOK.ap()[qt * 512:(qt + 1) * 512, h * 64:(h + 1) * 64].rearrange("(b p) d -> p b d", p=128),
                          ot[:], OTOK, ot)
                if KODD == 3:
                    return
            o_allgather()

    def even_mixer(L):
        i = L // 2
        lam_init = 0.8 - 0.6 * math.exp(-0.3 * L)
        import os
        KEV = int(os.environ.get("KEV", "9"))
        with S.phase():
            Wd = S.sbuf("Wd", [128, 8, 384], BF16)
            for q4 in range(4):
                S.dma("pool", Wd[:, 2 * q4:2 * q4 + 2, :],
                      we_in.ap()[i, q4 * 256:(q4 + 1) * 256, 0:384].rearrange("(k p) n -> p k n", p=128), Wd, we_in)
            LP = S.sbuf("LP", [128, 256], F32)
            S.dma("sp", LP[:], lamp.ap()[i, :].partition_broadcast(128), LP, lamp)
            lw = S.sbuf("lw", [128, 8], F32)
            pr = S.sbuf("pr", [128, 128], F32)
            TT("dve", pr[:, 0:64], LP[:, 0:64], LP[:, 64:128], ALU.mult, [LP], [pr])
            TT("dve", pr[:, 64:128], LP[:, 128:192], LP[:, 192:256], ALU.mult, [LP], [pr])
            S.op("dve", lambda e: e.tensor_reduce(out=lw[:, 0:2], in_=pr[:].rearrange("p (g d) -> p g d", g=2), axis=AX.X, op=ALU.add),
                 reads=[pr], writes=[lw])
            ACT(lw[:, 2:4], lw[:, 0:2], AF.Exp, [lw], [lw])
            TT("dve", lw[:, 4:5], lw[:, 2:3], lw[:, 3:4], ALU.subtract, [lw], [lw])
            TS("dve", lw[:, 4:5], lw[:, 4:5], lam_init, ALU.add, [lw], [lw])
            TS("dve", lw[:, 5:6], lw[:, 4:5], -1.0, ALU.mult, [lw], [lw])
            SUBW = S.sbuf("SUBW", [128, 128], F32)
            S.dma("sp", SUBW[:], subw.ap()[i, :].partition_broadcast(128), SUBW, subw)
            TS("dve", SUBW[:], SUBW[:], 1.0 - lam_init, ALU.mult, [SUBW], [SUBW])
            T5C = S.sbuf("T5C", [33, 1], F32)
            S.dma("sp", T5C[0:32, :], t5col[:, :], T5C, t5col)
            MEMSET("dve", T5C[32:33, :], 1.0, [T5C])
            OH = S.sbuf("OH", [33, 2560], F32)
            S.dma("sp", OH[:], t5oh[:, :], OH, t5oh)
            bvs = S.sbuf("bvs", [1, 2560], F32)
            psm_r = S.ring("psm", [128, 512], F32, 3, psum=True)
            pss_r = S.ring("pss", [128, 512], F32, 3, psum=True)
            pso_r = S.ring("pso", [128, 512], F32, 2, psum=True)
            for c5 in range(5):
                ps = psm_r.get()
                MM(ps[0:1, :], T5C[:, 0:1], OH[:, c5 * 512:(c5 + 1) * 512], True, True, [T5C, OH], [ps])
                CP("dve", bvs[0:1, c5 * 512:(c5 + 1) * 512], ps[0:1, :], [ps], [bvs])
            S.dma("sp", BV.ap().rearrange("(o n) -> o n", o=1), bvs[:], BV, bvs)
            BT = S.sbuf("BT", [128, 16, 512], BF16)
            skb_r = S.ring("skb", [128, 640], F32, 2)
            for j in range(16):
                skb = skb_r.get()
                S.dma("sp", skb[:], BV.ap()[128 * j:128 * j + 640].partition_broadcast(128), skb, BV)
                S.dma("sp", SKW.ap()[j, :].rearrange("(p n) -> p n", n=640), skb[:], SKW, skb)
                S.dma("pool", BT[:, j, :], bass.AP(tensor=SKW.h, offset=j * 128 * 640 + 127, ap=[[639, 128], [1, 512]]), BT, SKW)
            B31 = S.sbuf("B31", [128, 1], F32)
            S.dma("sp", B31[:], BV.ap()[2559:2560].partition_broadcast(128), B31, BV)
            QT = S.sbuf("QT", [64, SEQ], BF16)
            KT = S.sbuf("KT", [64, SEQ], BF16)
            VA = S.sbuf("VA", [128, NBF, 129], BF16)
            MEMSET("dve", VA[:, :, 64:65], 1.0, [VA])
            xT_r = S.ring("xT", [128, 8, 512], BF16, 2)
            pt_r = S.ring("pt", [128, 512], BF16, 4)
            osb_r = S.ring("osbd", [128, 512], F32, 4)
            for b_ in osb_r.bufs:
                MEMSET("dve", b_[:], 0.0, [b_])
            om_r = S.ring("om", [128, 128], F32, 3)
            o0_r = S.ring("o0", [128, 128], F32, 2)
            a_r = S.ring("a", [128, 128], F32, 2)
            ao_r = S.ring("ao", [128, 128], BF16, 2)
            sm_r = S.ring("smd", [128, 8], F32, 4)
            junk = S.sbuf("junkd", [128, 128], F32)
            for m in range(2):
                for tt in range(NQT):
                    xT = xT_r.get()
                    load_xT(xT, tt)
                    psq = psm_r.get()
                    for kc in range(8):
                        MM(psq[0:64, :], Wd[:, kc, m * 64:(m + 1) * 64], xT[:, kc, :], kc == 0, kc == 7, [Wd, xT], [psq])
                    ACT(QT[:, tt * 512:(tt + 1) * 512], psq[0:64, :], AF.Identity, [psq], [QT], scale=0.125)
                    psk = psm_r.get()
                    for kc in range(8):
                        MM(psk[0:64, :], Wd[:, kc, 128 + m * 64:128 + (m + 1) * 64], xT[:, kc, :], kc == 0, kc == 7, [Wd, xT], [psk])
                    CP("dve", KT[:, tt * 512:(tt + 1) * 512], psk[0:64, :], [psk], [KT])
                    if m == 0:
                        for b4 in range(4):
                            gb = tt * 4 + b4
                            psv = psm_r.get()
                            for kc in range(8):
                                MM(psv[:, 0:128], xT[:, kc, b4 * 128:(b4 + 1) * 128], Wd[:, kc, 256:384], kc == 0, kc == 7, [xT, Wd], [psv])
                            CP("act", VA[:, gb, 0:64], psv[:, 0:64], [psv], [VA])
                            CP("act", VA[:, gb, 65:129], psv[:, 64:128], [psv], [VA])
                if KEV == 1:
                    return
                for qt in range(NQT):
                    oa = pso_r.get()
                    ob = pso_r.get()
                    nkb = 4 * qt + 4
                    for kb in range(nkb):
                        dj = 4 * qt - kb + 3
                        near = dj <= 15
                        pss = pss_r.get()
                        MM(pss[:], KT[:, kb * 128:(kb + 1) * 128], QT[:, qt * 512:(qt + 1) * 512], True, not near, [KT, QT], [pss])
                        if near:
                            MM(pss[:], ident_b[:], BT[:, dj, :], False, True, [ident_b, BT], [pss])
                        pt = pt_r.get()
                        if near:
                            ACT(pt[:], pss[:], AF.Exp, [pss], [pt])
                        else:
                            ACT(pt[:], pss[:], AF.Exp, [pss, B31], [pt], bias=B31[:, 0:1])
                        MM(oa[0:65, :], VA[:, kb, 0:65], pt[:], kb == 0, kb == nkb - 1, [VA, pt], [oa])
                        MM(ob[0:64, :], VA[:, kb, 65:129], pt[:], kb == 0, kb == nkb - 1, [VA, pt], [ob])
                    osa = osb_r.get()
                    osb = osb_r.get()
                    CP("dve", osa[0:65, :], oa[0:65, :], [oa], [osa])
                    CP("act", osb[0:64, :], ob[0:64, :], [ob], [osb])
                    ptra = psm_r.get()
                    ptrb = psm_r.get()
                    for b4 in range(4):
                        TR(ptra[:, b4 * 128:(b4 + 1) * 128], osa[:, b4 * 128:(b4 + 1) * 128], ident_f[:], [osa, ident_f], [ptra])
                        TR(ptrb[:, b4 * 128:(b4 + 1) * 128], osb[:, b4 * 128:(b4 + 1) * 128], ident_f[:], [osb, ident_f], [ptrb])
                    for b4 in range(4):
                        gb = qt * 4 + b4
                        sm = sm_r.get()
                        RECIP(sm[:, 0:1], ptra[:, b4 * 128 + 64:b4 * 128 + 65], [ptra], [sm])
                        om = om_r.get()
                        TS("dve", om[:, 0:64], ptra[:, b4 * 128:b4 * 128 + 64], sm[:, 0:1], ALU.mult, [ptra, sm], [om])
                        TS("dve", om[:, 64:128], ptrb[:, b4 * 128:b4 * 128 + 64], sm[:, 0:1], ALU.mult, [ptrb, sm], [om])
                        if m == 0:
                            S.dma("sp", O0S[gb * 128:(gb + 1) * 128, :], om[:], O0S, om)
                        else:
                            o0 = o0_r.get()
                            S.dma("sp", o0[:], O0S[gb * 128:(gb + 1) * 128, :], o0, O0S)
                            av = a_r.get()
                            STT(av[:], om[:], lw[:, 5:6], o0[:], ALU.mult, ALU.add, [om, lw, o0], [av])
                            ACT(junk[:], av[:], AF.Square, [av], [junk, sm], accum=sm[:, 1:2])
                            TS("dve", sm[:, 2:3], sm[:, 1:2], 1.0 / 128, ALU.mult, [sm], [sm], s2=RMS_EPS, op1=ALU.add)
                            ACT(sm[:, 3:4], sm[:, 2:3], AF.Sqrt, [sm], [sm])
                            RECIP(sm[:, 4:5], sm[:, 3:4], [sm], [sm])
                            ao = ao_r.get()
                            STT(ao[:], av[:], sm[:, 4:5], SUBW[:], ALU.mult, ALU.mult, [av, sm, SUBW], [ao])
                            S.dma("sp", OTOK[gb * 128:(gb + 1) * 128, 0:128], ao[:], OTOK, ao)
        if KEV <= 2:
            with S.phase():
                zb = S.sbuf("zb", [128, 128], BF16)
                MEMSET("dve", zb[:], 0.0, [zb])
                for gb in range(NBF):
                    S.dma("sp", OTOK[gb * 128:(gb + 1) * 128, 128:256], zb[:], OTOK, zb)
            o_allgather()
            return
        gdn_phase(L)
        o_allgather()

    def gdn_phase(L):
        i = L // 2
        SC = 128 ** -0.5
        with S.phase():
            Wg = S.sbuf("Wg", [128, 8, 514], BF16)
            for q4 in range(4):
                S.dma("pool", Wg[:, 2 * q4:2 * q4 + 2, :],
                      we_in.ap()[i, q4 * 256:(q4 + 1) * 256, 384:898].rearrange("(k p) n -> p k n", p=128), Wg, we_in)
            CW = S.sbuf("CW", [128, 12], F32)
            S.dma("sp", CW[:], convw.ap()[i], CW, convw)
            GNW = S.sbuf("GNW", [128, 128], F32)
            S.dma("sp", GNW[:], gnw.ap()[i, :].partition_broadcast(128), GNW, gnw)
            AD = S.sbuf("AD", [128, 4], F32)
            S.dma("sp", AD[:, 0:1], alog.ap()[i, :].partition_broadcast(128), AD, alog)
            S.dma("sp", AD[:, 1:2], dtb.ap()[i, :].partition_broadcast(128), AD, dtb)
            ACT(AD[:, 2:3], AD[:, 0:1], AF.Exp, [AD], [AD])
            TS("dve", AD[:, 2:3], AD[:, 2:3], -1.0, ALU.mult, [AD], [AD])
            MUs = S.sbuf("MUs", [128, 128], F32)
            MUi = S.sbuf("MUi", [128, 128], F32)
            MLs = S.sbuf("MLs", [128, 128], F32)
            for (t_, base, cm, pat) in ((MUs, -1, -1, 1), (MUi, 0, -1, 1), (MLs, -1, 1, -1)):
                MEMSET("pool", t_[:], 1.0, [t_])
                ASEL(t_[:], t_[:], [[pat, 128]], ALU.is_ge, 0.0, base, cm, [t_], [t_])
            St = [S.sbuf("St%d" % k, [128, 128], F32) for k in range(2)]
            MEMSET("dve", St[0][:], 0.0, [St[0]])
            cb = [S.sbuf("cb%d" % g, [128, 515], F32) for g in range(3)]
            for g in range(3):
                MEMSET("dve", cb[g][:], 0.0, [cb[g]])
            hal = S.sbuf("hal", [128, 3, 3], F32)
            xT_r = S.ring("xT", [128, 8, 512], BF16, 2)
            y_r = S.ring("y", [128, 512], F32, 2)
            fT = [S.ring("fT%d" % g, [128, 512], F32, 2) for g in range(3)]
            sq_r = S.ring("sqg", [128, 512], F32, 2)
            zt_r = S.ring("ztk", [128, 4, 128], F32, 2)
            gb_r = S.ring("gbt", [128, 12], F32, 2)
            w_r = S.ring("wk", [128, 128], F32, 44)
            w2_r = S.ring("wk2", [128, 256], F32, 13)
            c_r = S.ring("colg", [128, 8], F32, 9)
            bo_r = S.ring("bo", [128, 128], BF16, 2)
            ps_r = S.ring("psg", [128, 512], F32, 7, psum=True)
            pso_r = S.ring("psgo", [128, 512], F32, 1, psum=True)
            cur = 0
            for tt in range(NQT):
                xT = xT_r.get()
                load_xT(xT, tt)
                fts = []
                for g in range(3):
                    psf = ps_r.get()
                    for kc in range(8):
                        MM(psf[:], Wg[:, kc, g * 128:(g + 1) * 128], xT[:, kc, :], kc == 0, kc == 7, [Wg, xT], [psf])
                    CP("dve", hal[:, g, :], cb[g][:, 512:515], [cb[g]], [hal])
                    CP("act", cb[g][:, 3:515], psf[:], [psf], [cb[g]])
                    CP("dve", cb[g][:, 0:3], hal[:, g, :], [hal], [cb[g]])
                    y = y_r.get()
                    TS("dve", y[:], cb[g][:, 0:512], CW[:, g * 4:g * 4 + 1], ALU.mult, [cb[g], CW], [y])
                    for j in range(1, 4):
                        STT(y[:], cb[g][:, j:j + 512], CW[:, g * 4 + j:g * 4 + j + 1], y[:], ALU.mult, ALU.add, [cb[g], CW, y], [y])
                    ft = fT[g].get()
                    ACT(ft[:], y[:], AF.Silu, [y], [ft])
                    if g < 2:
                        sq = sq_r.get()
                        TT("pool", sq[:], ft[:], ft[:], ALU.mult, [ft], [sq])
                        pss = ps_r.get()
                        MM(pss[:], ones_f[:], sq[:], True, True, [ones_f, sq], [pss])
                        TS("dve", sq[:], pss[:], RMS_EPS, ALU.add, [pss], [sq])
                        ACT(sq[:], sq[:], AF.Sqrt, [sq], [sq])
                        RECIP(sq[:], sq[:], [sq], [sq])
                        TT("dve", ft[:], ft[:], sq[:], ALU.mult, [ft, sq], [ft])
                    fts.append(ft)
                qTt, kTt, vTt = fts
                zt = zt_r.get()
                gbt = gb_r.get()
                for b4 in range(4):
                    pz = ps_r.get()
                    for kc in range(8):
                        MM(pz[:, 0:130], xT[:, kc, b4 * 128:(b4 + 1) * 128], Wg[:, kc, 384:514], kc == 0, kc == 7, [xT, Wg], [pz])
                    ACT(zt[:, b4, :], pz[:, 0:128], AF.Silu, [pz], [zt])
                    ACT(gbt[:, 4 + b4:5 + b4], pz[:, 128:129], AF.Sigmoid, [pz], [gbt])
                    cg = c_r.get()
                    ACT(cg[:, 0:1], pz[:, 129:130], AF.Exp, [pz, AD], [cg], bias=AD[:, 1:2])
                    ACT(cg[:, 1:2], cg[:, 0:1], AF.Ln, [cg], [cg], bias=1.0)
                    TS("dve", gbt[:, b4:b4 + 1], cg[:, 1:2], AD[:, 2:3], ALU.mult, [cg, AD], [gbt])
                pgc = ps_r.get()
                MM(pgc[:, 0:4], triU_f[:], gbt[:, 0:4], True, True, [triU_f, gbt], [pgc])
                CP("dve", gbt[:, 8:12], pgc[:, 0:4], [pgc], [gbt])
                for b4 in range(4):
                    gbk = tt * 4 + b4
                    blk = slice(b4 * 128, (b4 + 1) * 128)
                    gc = gbt[:, 8 + b4:9 + b4]
                    beta = gbt[:, 4 + b4:5 + b4]
                    pk = ps_r.get()
                    TR(pk[:, 0:128], kTt[:, blk], ident_f[:], [kTt, ident_f], [pk])
                    Kc = w_r.get()
                    CP("act", Kc[:], pk[:, 0:128], [pk], [Kc])
                    pv = ps_r.get()
                    TR(pv[:, 0:128], vTt[:, blk], ident_f[:], [vTt, ident_f], [pv])
                    Vc = w_r.get()
                    CP("act", Vc[:], pv[:, 0:128], [pv], [Vc])
                    DG = w2_r.get()
                    TS("dve", DG[:, 0:128], ident_f[:], gc, ALU.mult, [ident_f, gbt], [DG])
                    TS("dve", DG[:, 128:256], ident_f[:], beta, ALU.mult, [ident_f, gbt], [DG])
                    pb = ps_r.get()
                    MM(pb[:, 0:256], ones_f[:], DG[:], True, True, [ones_f, DG], [pb])
                    GB = w2_r.get()
                    CP("act", GB[:], pb[:, 0:256], [pb], [GB])
                    dlt = w_r.get()
                    TS("dve", dlt[:], GB[:, 0:128], gc, ALU.subtract, [GB, gbt], [dlt])
                    ET = w_r.get()
                    TS("dve", ET[:], dlt[:], 0.0, ALU.min, [dlt], [ET])
                    ACT(ET[:], ET[:], AF.Exp, [ET], [ET])
                    E2 = w_r.get()
                    TS("dve", E2[:], dlt[:], -1.0, ALU.mult, [dlt], [E2], s2=0.0, op1=ALU.min)
                    ACT(E2[:], E2[:], AF.Exp, [E2], [E2])
                    EG = w_r.get()
                    ACT(EG[:], GB[:, 0:128], AF.Exp, [GB], [EG])
                    pkk = ps_r.get()
                    MM(pkk[:, 0:128], kTt[:, blk], kTt[:, blk], True, True, [kTt], [pkk])
                    Nj = w_r.get()
                    TT("dve", Nj[:], pkk[:, 0:128], GB[:, 128:256], ALU.mult, [pkk, GB], [Nj])
                    TT("dve", Nj[:], Nj[:], ET[:], ALU.mult, [Nj, ET], [Nj])
                    STT(Nj[:], Nj[:], -1.0, MUs[:], ALU.mult, ALU.mult, [Nj, MUs], [Nj])
                    Pj = w_r.get()
                    STT(Pj[:], pkk[:, 0:128], beta, E2[:], ALU.mult, ALU.mult, [pkk, gbt, E2], [Pj])
                    STT(Pj[:], Pj[:], -1.0, MLs[:], ALU.mult, ALU.mult, [Pj, MLs], [Pj])
                    cg = c_r.get()
                    ACT(cg[:, 0:1], gc, AF.Exp, [gbt], [cg])
                    TT("dve", cg[:, 1:2], cg[:, 0:1], beta, ALU.mult, [cg, gbt], [cg])
                    Rm = w2_r.get()
                    TS("dve", Rm[:, 0:128], Vc[:], beta, ALU.mult, [Vc, gbt], [Rm])
                    TS("dve", Rm[:, 128:256], Kc[:], cg[:, 1:2], ALU.mult, [Kc, cg], [Rm])
                    for j in range(7):
                        pr_ = ps_r.get()
                        MM(pr_[:, 0:256], Nj[:], Rm[:], True, True, [Nj, Rm], [pr_])
                        Rn = w2_r.get()
                        TT("dve", Rn[:], Rm[:], pr_[:, 0:256], ALU.add, [Rm, pr_], [Rn])
                        Rm = Rn
                        if j < 6:
                            pn = ps_r.get()
                            MM(pn[:, 0:128], Pj[:], Nj[:], True, True, [Pj, Nj], [pn])
                            pp = ps_r.get()
                            MM(pp[:, 0:128], Nj[:], Pj[:], True, True, [Nj, Pj], [pp])
                            Nn = w_r.get()
                            CP("act", Nn[:], pn[:, 0:128], [pn], [Nn])
                            Pn = w_r.get()
                            CP("act", Pn[:], pp[:, 0:128], [pp], [Pn])
                            Nj, Pj = Nn, Pn
                    pw = ps_r.get()
                    TR(pw[:, 0:128], Rm[:, 128:256], ident_f[:], [Rm, ident_f], [pw])
                    WT = w_r.get()
                    CP("act", WT[:], pw[:, 0:128], [pw], [WT])
                    Sc, Sn = St[cur], St[1 - cur]
                    pws = ps_r.get()
                    MM(pws[:, 0:128], WT[:], Sc[:], True, True, [WT, Sc], [pws])
                    Vn = w_r.get()
                    TT("dve", Vn[:], Rm[:, 0:128], pws[:, 0:128], ALU.subtract, [Rm, pws], [Vn])
                    pqk = ps_r.get()
                    MM(pqk[:, 0:128], kTt[:, blk], qTt[:, blk], True, True, [kTt, qTt], [pqk])
                    qki = w_r.get()
                    TT("dve", qki[:], pqk[:, 0:128], ET[:], ALU.mult, [pqk, ET], [qki])
                    STT(qki[:], qki[:], SC, MUi[:], ALU.mult, ALU.mult, [qki, MUi], [qki])
                    QdT = w_r.get()
                    STT(QdT[:], qTt[:, blk], SC, EG[:], ALU.mult, ALU.mult, [qTt, EG], [QdT])
                    po = pso_r.get()
                    MM(po[:, 0:128], QdT[:], Sc[:], True, False, [QdT, Sc], [po])
                    MM(po[:, 0:128], qki[:], Vn[:], False, True, [qki, Vn], [po])
                    TT("dve", cg[:, 2:3], GB[:, 127:128], gc, ALU.subtract, [GB, gbt], [cg])
                    ACT(cg[:, 3:4], cg[:, 2:3], AF.Exp, [cg], [cg])
                    Kd = w_r.get()
                    TS("dve", Kd[:], Kc[:], cg[:, 3:4], ALU.mult, [Kc, cg], [Kd])
                    psn = ps_r.get()
                    MM(psn[:, 0:128], Kd[:], Vn[:], True, True, [Kd, Vn], [psn])
                    STT(Sn[:], Sc[:], EG[:, 127:128], psn[:, 0:128], ALU.mult, ALU.add, [Sc, EG, psn], [Sn])
                    cur = 1 - cur
                    jk = w_r.get()
                    ACT(jk[:], po[:, 0:128], AF.Square, [po], [jk, cg], accum=cg[:, 4:5])
                    TS("dve", cg[:, 5:6], cg[:, 4:5], 1.0 / 128, ALU.mult, [cg], [cg], s2=RMS_EPS, op1=ALU.add)
                    ACT(cg[:, 6:7], cg[:, 5:6], AF.Sqrt, [cg], [cg])
                    RECIP(cg[:, 7:8], cg[:, 6:7], [cg], [cg])
                    STT(jk[:], po[:, 0:128], cg[:, 7:8], GNW[:], ALU.mult, ALU.mult, [po, cg, GNW], [jk])
                    bo = bo_r.get()
                    TT("dve", bo[:], jk[:], zt[:, b4, :], ALU.mult, [jk, zt], [bo])
                    S.dma("sp", OTOK[gbk * 128:(gbk + 1) * 128, 128:256], bo[:], OTOK, bo)


    phase_x0()
    xres = x_in
    for li, L in enumerate(cfg.LAYERS):
        is_last = li == len(cfg.LAYERS) - 1
        if cfg.mixer:
            if L % 2 == 0:
                even_mixer(L)
            else:
                odd_mixer(L)
        xnext = XR[li % 2]
        phase_ln_moe(L, xres, xnext, is_last)
        xres = xnext


def _even_mixer(S, cfg, L):
    raise NotImplementedError


def _odd_mixer(S, cfg, L):
    raise NotImplementedError


def t5_onehot():
    n = np.arange(2560) - 511
    nn = np.maximum(n, 0)
    nf = np.maximum(nn, 1).astype(np.float32)
    large = 16 + (np.log(nf / 16) / math.log(2048 / 16) * 16).astype(np.int32)
    large = np.minimum(large, 31)
    bucket = np.where(nn < 16, nn, large)
    oh = np.zeros((33, 2560), np.float32)
    valid = n >= 0
    oh[bucket[valid], np.nonzero(valid)[0]] = 1.0
    oh[32, ~valid] = NEG
    return oh


def prep_inputs(inp, cfg):
    SEQ, TOK, NEXP, EPC, NLW, WL0 = cfg.SEQ, cfg.TOK, cfg.NEXP, cfg.EPC, cfg.NLW, cfg.WL0
    f = lambda a: np.ascontiguousarray(np.asarray(a, dtype=np.float32))
    x = f(inp["x"])
    maps = []
    lng = f(np.concatenate([inp["ln_mix_g"], inp["ln_ffn_g"]], 0))
    lnb = f(np.concatenate([inp["ln_mix_b"], inp["ln_ffn_b"]], 0))
    if cfg.moe:
        bg = f(inp["moe_b_gate_up"]).reshape(4, NEXP, 16, 128)
        bgu = np.ascontiguousarray(bg.transpose(3, 0, 1, 2).reshape(128, 4 * NEXP * 16))
        bdn = f(inp["moe_b_down"]).reshape(4 * NEXP, D)
        wgu = np.asarray(inp["moe_w_gate_up"])
        wdn = np.asarray(inp["moe_w_down"])
    if cfg.mixer:
        ewi, ewo = f(inp["even_w_in"]), f(inp["even_w_out"])
        owi, owo = f(inp["odd_w_in"]), f(inp["odd_w_out"])
        cw = f(inp["gdn_conv_w"])
        oh = t5_onehot()
    for c in range(8):
        b, r = c // 4, c % 4
        m = {"x_in": np.ascontiguousarray(x[b, r * TOK:(r + 1) * TOK, :]), "lng": lng, "lnb": lnb}
        if cfg.moe:
            m["router_w"] = f(inp["router_w"])
            m["router_b"] = f(inp["router_b"])
            m["bgu"] = bgu
            m["bdn"] = bdn
            m["wgu_sh"] = np.ascontiguousarray(wgu[WL0:WL0 + NLW, c * EPC:(c + 1) * EPC], dtype=np.float32).reshape(NLW * EPC * D, 2 * D)
            m["wdn_sh"] = np.ascontiguousarray(wdn[WL0:WL0 + NLW, c * EPC:(c + 1) * EPC], dtype=np.float32).reshape(NLW * EPC * D, D)
        if cfg.mixer:
            h = r
            A = 512
            cols = np.concatenate([np.arange(h * 128, h * 128 + 128), A + np.arange(h * 128, h * 128 + 128),
                                   2 * A + np.arange(h * 128, h * 128 + 128),
                                   1536 + np.arange(h * 128, h * 128 + 128), 2048 + np.arange(h * 128, h * 128 + 128),
                                   2560 + np.arange(h * 128, h * 128 + 128), 3072 + np.arange(h * 128, h * 128 + 128),
                                   [3584 + h], [3588 + h]])
            m["we_in"] = np.ascontiguousarray(ewi[:, :, cols])
            perm = np.concatenate([np.concatenate([np.arange(s_ * 128, s_ * 128 + 128), 512 + np.arange(s_ * 128, s_ * 128 + 128)]) for s_ in range(4)])
            m["we_out"] = np.ascontiguousarray(ewo[:, perm, :])
            TC = min(2048, SEQ)
            gtok = r * TOK + np.arange(TOK)
            jj, tt_ = gtok // TC, gtok % TC
            oi = np.stack([(jj * 4 + s_) * TC + tt_ for s_ in range(4)], 1)
            m["oidx"] = np.ascontiguousarray(oi.reshape(TOK // 128, 128, 4).transpose(1, 0, 2).reshape(128, TOK // 32).astype(np.int32))
            ccols = np.concatenate([np.arange(h * 128, h * 128 + 128), 512 + np.arange(h * 128, h * 128 + 128), 1024 + np.arange(h * 128, h * 128 + 128)])
            m["convw"] = np.ascontiguousarray(cw[:, :, ccols].reshape(2, 4, 3, 128).transpose(0, 3, 2, 1).reshape(2, 128, 12))
            m["lamp"] = f(inp["diff_lambda"]).reshape(2, 256)
            m["subw"] = f(inp["diff_subln_w"])
            m["gnw"] = f(inp["gdn_norm_w"])
            m["alog"] = np.ascontiguousarray(f(inp["gdn_a_log"])[:, h:h + 1])
            m["dtb"] = np.ascontiguousarray(f(inp["gdn_dt_bias"])[:, h:h + 1])
            m["t5col"] = np.ascontiguousarray(f(inp["t5_bias"])[:, h:h + 1])
            m["t5oh"] = oh
            oc = []
            for hh in range(4 * r, 4 * r + 4):
                oc += [np.arange(hh * 64, hh * 64 + 64), 1024 + np.arange(hh * 64, hh * 64 + 64),
                       2048 + np.arange(hh * 64, hh * 64 + 64), [4096 + hh], 3072 + np.arange(hh * 64, hh * 64 + 64)]
            oc = np.concatenate(oc)
            m["wo_in"] = np.ascontiguousarray(owi[:, :, oc])
            m["wo_out"] = owo
            m["qkw"] = np.ascontiguousarray(f(inp["fox_qk_norm_w"]).transpose(0, 2, 1))
            m["fb"] = np.ascontiguousarray(f(inp["fox_forget_b"])[:, 4 * r:4 * r + 4])
        maps.append(m)
    return maps


_CACHE = {}


def run_cfg(inp, cfg):
    key = (cfg.SEQ, cfg.NEXP, cfg.CAP, cfg.LAYERS, cfg.NLW, cfg.WL0, cfg.mixer, cfg.moe)
    if key not in _CACHE:
        _CACHE[key] = build(cfg)
    nc = _CACHE[key]
    maps = prep_inputs(inp, cfg)
    has_even = cfg.mixer and any(L % 2 == 0 for L in cfg.LAYERS)
    has_odd = cfg.mixer and any(L % 2 == 1 for L in cfg.LAYERS)
    ev = ("we_in", "we_out", "convw", "lamp", "subw", "gnw", "alog", "dtb", "t5col", "t5oh")
    od = ("wo_in", "wo_out", "qkw", "fb")
    for m in maps:
        for k in list(m):
            if (k in ev and not has_even) or (k in od and not has_odd):
                del m[k]
    res = run_bass_kernel_spmd(nc, maps, core_ids=list(range(8)))
    outs = [res.results[c]["out"] for c in range(8)]
    TOK = cfg.TOK
    full = np.zeros((2, cfg.SEQ, D), np.float32)
    for c in range(8):
        full[c // 4, (c % 4) * TOK:(c % 4 + 1) * TOK, :] = outs[c]
    return full


def kernel(**inputs):
    return run_cfg(inputs, Cfg())
```

```python
import math
from contextlib import ExitStack
import numpy as np
import concourse.bass as bass
import concourse.mybir as mybir
from concourse.bass_utils import run_bass_kernel_spmd

F32 = mybir.dt.float32
BF16 = mybir.dt.bfloat16
I32 = mybir.dt.int32
U32 = mybir.dt.uint32
AF = mybir.ActivationFunctionType
ALU = mybir.AluOpType
AX = mybir.AxisListType

COMPUTE = ("pe", "act", "dve", "pool")


class Buf:
    def __init__(self, name, handle, is_dram=False):
        self.name = name
        self.h = handle
        self.is_dram = is_dram
        self.w = {}
        self.r = {}
        self.dsem = None
        self.dcnt = 0

    def __getitem__(self, idx):
        return self.h.ap()[idx] if self.is_dram else self.h[idx]

    def ap(self):
        return self.h.ap() if self.is_dram else self.h[:]


class Sched:
    def __init__(self, nc, es):
        self.nc = nc
        self.es = es
        self.streams = {k: [] for k in ("pe", "act", "dve", "pool", "sp")}
        self.sems = {}
        self.latest = {}
        self.cnt = {k: 0 for k in COMPUTE}
        self.waited = {k: {} for k in self.streams}
        self.nbuf = 0
        self.es_local = es
        self.dq = {}
        self.dq_i = {}
        self.all_bufs = []
        import os
        self.sw_thresh = int(os.environ.get("KSW", "30000"))
        for k in COMPUTE:
            self._sem("E_" + k)

    def _sem(self, key):
        if key not in self.sems:
            self.sems[key] = self.es.enter_context(self.nc.semaphore("s%d_%s" % (len(self.sems), key[:12])))
            self.latest[key] = 0
        return self.sems[key]

    def sbuf(self, name, shape, dt):
        self.nbuf += 1
        h = self.es_local.enter_context(self.nc.sbuf_tensor("%s_%d" % (name, self.nbuf), list(shape), dt))
        b = Buf(name, h)
        b.local = self.es_local is not self.es
        self.all_bufs.append(b)
        return b

    def psum(self, name, shape, dt=F32):
        self.nbuf += 1
        h = self.es_local.enter_context(self.nc.psum_tensor("%s_%d" % (name, self.nbuf), list(shape), dt))
        b = Buf(name, h)
        b.local = self.es_local is not self.es
        self.all_bufs.append(b)
        return b

    def ring(self, name, shape, dt, n, psum=False):
        return Ring([(self.psum if psum else self.sbuf)("%s%d" % (name, i), shape, dt) for i in range(n)])

    def _dq(self, kind):
        if kind not in self.dq:
            n = {"hw": 12, "sw": 8, "cc": 1}[kind]
            self.dq[kind] = ["D_%s_%d" % (kind, i) for i in range(n)]
            for k in self.dq[kind]:
                self._sem(k)
            self.dq_i[kind] = 0
        key = self.dq[kind][self.dq_i[kind] % len(self.dq[kind])]
        self.dq_i[kind] += 1
        return key

    def phase(self):
        return _Phase(self)

    def dram(self, name, shape, dt, kind=None):
        if kind is None:
            h = self.nc.dram_tensor(name, list(shape), dt)
        else:
            h = self.nc.dram_tensor(name, list(shape), dt, kind=kind)
        b = Buf(name, h, is_dram=True)
        self.all_bufs.append(b)
        return b

    def _need(self, eng, reads, writes, mykey, prev=None):
        need = {}

        def add(k, v):
            if k == mykey == "E_pe":
                return
            if need.get(k, 0) < v:
                need[k] = v
        if prev is not None and prev[1] > 0:
            add(*prev)
        for b in reads:
            for k, v in b.w.items():
                add(k, v)
        for b in writes:
            for k, v in b.w.items():
                add(k, v)
            for k, v in b.r.items():
                add(k, v)
        out = []
        wd = self.waited[eng]
        for k, v in need.items():
            if wd.get(k, 0) < v:
                wd[k] = v
                out.append((self.sems[k], v))
        return out

    def op(self, eng, fn, reads=(), writes=()):
        key = "E_" + eng
        waits = self._need(eng, reads, writes, key)
        self.cnt[eng] += 1
        val = self.cnt[eng]
        self.latest[key] = val
        sem = self.sems[key]
        st = self.streams[eng]
        for s, v in waits:
            st.append(lambda e, s=s, v=v: e.wait_ge(s, v))
        st.append(lambda e, fn=fn, sem=sem: fn(e).then_inc(sem, 1))
        for b in reads:
            if b.r.get(key, 0) < val:
                b.r[key] = val
        for b in writes:
            b.w = {key: val}
            b.r = {}

    def dma(self, q, out_ap, in_ap, dst, src, fn=None, extra=(), **kw):
        key = self._dq("sw" if q == "pool" else "hw")
        wr = [dst] if (dst.r or not all(k.startswith("D_") for k in dst.w)) else []
        waits = self._need(q, ([src] if src is not None else []) + list(extra), wr, key, prev=(key, self.latest[key]))
        val = self.latest[key] + 16
        self.latest[key] = val
        sem = self.sems[key]
        st = self.streams[q]
        for s, v in waits:
            st.append(lambda e, s=s, v=v: e.wait_ge(s, v))
        if fn is None:
            st.append(lambda e: e.dma_start(out=out_ap, in_=in_ap, **kw).then_inc(sem, 16))
        else:
            st.append(lambda e: fn(e).then_inc(sem, 16))
        for b in ([src] if src is not None else []) + list(extra):
            if b.r.get(key, 0) < val:
                b.r[key] = val
        if wr:
            dst.w = {key: val}
        else:
            dst.w[key] = val
        dst.r = {}

    def cc(self, kind, op, groups, src, dst, src_ap=None, dst_ap=None):
        sap = src.h.ap().opt() if src_ap is None else src_ap.opt()
        dap = dst.h.ap().opt() if dst_ap is None else dst_ap.opt()
        key = self._dq("cc")
        waits = self._need("pool", [src], [dst], key)
        val = self.latest[key] + 1
        self.latest[key] = val
        sem = self.sems[key]
        st = self.streams["pool"]
        for s, v in waits:
            st.append(lambda e, s=s, v=v: e.wait_ge(s, v))
        st.append(lambda e: e.collective_compute(kind, op, replica_groups=groups,
                                                 ins=[sap], outs=[dap]).then_inc(sem, 1))
        st.append(lambda e: e.wait_ge(sem, val))
        self.waited["pool"][key] = val
        src.r[key] = val
        dst.w = {key: val}
        dst.r = {}

    def barrier(self):
        for eng, st in self.streams.items():
            wd = self.waited[eng]
            for k, v in self.latest.items():
                if v > 0 and wd.get(k, 0) < v:
                    wd[k] = v
                    st.append(lambda e, s=self.sems[k], v=v: e.wait_ge(s, v))

    def emit(self):
        self.barrier()
        for eng in COMPUTE:
            key = "E_" + eng
            if self.cnt[eng] > self.sw_thresh:
                self.nsw = getattr(self, "nsw", 0) + 1
                self.sems[key] = self.es.enter_context(self.nc.semaphore("sw%d_%s" % (self.nsw, eng)))
                self.cnt[eng] = 0
                self.latest[key] = 0
                for wd in self.waited.values():
                    wd.pop(key, None)
                for b in self.all_bufs:
                    b.w.pop(key, None)
                    b.r.pop(key, None)
        streams = self.streams
        self.streams = {k: [] for k in streams}
        self.ninst = getattr(self, "ninst", 0) + sum(len(v) for v in streams.values())
        import os
        if os.environ.get("KCOUNT"):
            return
        self._emit(streams)

    def _emit(self, streams):
        class _S:
            pass
        self_ = _S()
        self_.streams = streams
        nc = self.nc
        self = self_
        with nc.Block() as block:
            @block.tensor
            def _(e):
                for f in self.streams["pe"]:
                    f(e)

            @block.scalar
            def _(e):
                for f in self.streams["act"]:
                    f(e)

            @block.vector
            def _(e):
                for f in self.streams["dve"]:
                    f(e)

            @block.gpsimd
            def _(e):
                for f in self.streams["pool"]:
                    f(e)

            @block.sync
            def _(e):
                for f in self.streams["sp"]:
                    f(e)


class Ring:
    def __init__(self, bufs):
        self.bufs = bufs
        self.i = 0

    def get(self):
        b = self.bufs[self.i % len(self.bufs)]
        self.i += 1
        return b


class _Phase:
    def __init__(self, S):
        self.S = S

    def __enter__(self):
        self.old = self.S.es_local
        self.stack = ExitStack()
        self.stack.__enter__()
        self.S.es_local = self.stack
        return self.S

    def __exit__(self, *a):
        S = self.S
        stop = False
        if a[0] is None:
            S.emit()

            S.nphase = getattr(S, "nphase", 0) + 1
            import os
            stop = S.nphase == int(os.environ.get("KSTOP", "0"))
        S.es_local = self.old
        self.stack.__exit__(*a)
        if stop:
            raise _StopBuild()
        return False


class _StopBuild(Exception):
    pass


D = 1024
ALPHA = (2 * 4) ** 0.25
LN_EPS = 1e-5
RMS_EPS = 1e-6
G4 = [[0, 1, 2, 3], [4, 5, 6, 7]]
G2 = [[0, 4], [1, 5], [2, 6], [3, 7]]
NEG = -30000.0


class Cfg:
    def __init__(self, SEQ=16384, NEXP=32, CAP=640, LAYERS=(0, 1, 2, 3), NLW=4, WL0=0, mixer=True, moe=True):
        self.SEQ, self.NEXP, self.CAP, self.LAYERS, self.NLW = SEQ, NEXP, CAP, tuple(LAYERS), NLW
        self.mixer, self.moe, self.WL0 = mixer, moe, WL0
        self.TOK = SEQ // 4
        self.EPC = NEXP // 8


def build(cfg):
    nc = bass.Bass("TRN2", target_bir_lowering=False)
    es = ExitStack()
    with es:
        S = Sched(nc, es)
        try:
            _build(S, cfg)
        except _StopBuild:
            pass
        print("kernel build: instructions incl. waits =", getattr(S, "ninst", 0), flush=True)
    return nc


def _build(S, cfg):
    SEQ, TOK, NEXP, C, EPC, NLW = cfg.SEQ, cfg.TOK, cfg.NEXP, cfg.CAP, cfg.EPC, cfg.NLW
    NBL = TOK // 128
    NTL = TOK // 512
    NBF = SEQ // 128
    NQT = SEQ // 512
    CB = C // 128
    TRASH = NEXP * C

    def MM(out, lhsT, rhs, start, stop, R, W):
        S.op("pe", lambda e: e.matmul(out, lhsT, rhs, start=start, stop=stop), reads=R, writes=W)

    def TR(out, in_, ident, R, W):
        S.op("pe", lambda e: e.transpose(out, in_, ident), reads=R, writes=W)

    def ACT(out, in_, func, R, W, bias=None, scale=None, accum=None):
        kw = {}
        if bias is not None:
            kw["bias"] = bias
        if scale is not None:
            kw["scale"] = scale
        if accum is not None:
            kw["accum_out"] = accum
        S.op("act", lambda e: e.activation(out=out, in_=in_, func=func, **kw), reads=R, writes=W)

    def TS(eng, out, in0, s1, op0, R, W, s2=None, op1=None, accum=None):
        kw = {}
        if op1 is not None:
            kw["op1"] = op1
        if accum is not None:
            kw["accum_out"] = accum
        S.op(eng, lambda e: e.tensor_scalar(out=out, in0=in0, scalar1=s1, scalar2=s2, op0=op0, **kw), reads=R, writes=W)

    def TT(eng, out, in0, in1, op, R, W):
        S.op(eng, lambda e: e.tensor_tensor(out=out, in0=in0, in1=in1, op=op), reads=R, writes=W)

    def STT(out, in0, scalar, in1, op0, op1, R, W, accum=None):
        kw = {}
        if accum is not None:
            kw["accum_out"] = accum
        S.op("dve", lambda e: e.scalar_tensor_tensor(out=out, in0=in0, scalar=scalar, in1=in1, op0=op0, op1=op1, **kw),
             reads=R, writes=W)

    def CP(eng, out, in_, R, W):
        if eng == "act":
            S.op("act", lambda e: e.copy(out=out, in_=in_), reads=R, writes=W)
        else:
            S.op(eng, lambda e: e.tensor_copy(out=out, in_=in_), reads=R, writes=W)

    def MEMSET(eng, ap, val, W):
        S.op(eng, lambda e: e.memset(ap, val), writes=W)

    def RECIP(out, in_, R, W):
        S.op("dve", lambda e: e.reciprocal(out=out, in_=in_), reads=R, writes=W)

    def ASEL(out, in_, pattern, cmp, fill, base, cm, R, W):
        S.op("pool", lambda e: e.affine_select(out=out, in_=in_, pattern=pattern, compare_op=cmp, fill=fill,
                                                base=base, channel_multiplier=cm), reads=R, writes=W)

    def din(name, shape, dt=F32):
        return S.dram(name, shape, dt, kind="ExternalInput")

    x_in = din("x_in", [TOK, D])
    lng = din("lng", [8, D])
    lnb = din("lnb", [8, D])
    out = S.dram("out", [TOK, D], F32, kind="ExternalOutput")
    if cfg.moe:
        router_w = din("router_w", [4, D, NEXP])
        router_b = din("router_b", [4, NEXP])
        bgu = din("bgu", [128, 4 * NEXP * 16])
        bdn = din("bdn", [4 * NEXP, D])
        wgu_sh = din("wgu_sh", [NLW * EPC * D, 2 * D])
        wdn_sh = din("wdn_sh", [NLW * EPC * D, D])
    has_even = cfg.mixer and any(L % 2 == 0 for L in cfg.LAYERS)
    has_odd = cfg.mixer and any(L % 2 == 1 for L in cfg.LAYERS)
    if has_even:
        we_in = din("we_in", [2, D, 898])
        we_out = din("we_out", [2, D, D])
        convw = din("convw", [2, 128, 12])
        lamp = din("lamp", [2, 256])
        subw = din("subw", [2, 128])
        gnw = din("gnw", [2, 128])
        alog = din("alog", [2, 1])
        dtb = din("dtb", [2, 1])
        t5col = din("t5col", [32, 1])
        t5oh = din("t5oh", [33, 2560])
    if has_odd:
        wo_in = din("wo_in", [2, D, 1028])
        wo_out = din("wo_out", [2, D, D])
        qkw = din("qkw", [2, 64, 2])
        fb = din("fb", [2, 4])

    XR = [S.dram("xres0", [TOK, D], F32), S.dram("xres1", [TOK, D], F32)]
    XR1 = S.dram("xr1", [TOK, D], F32)
    XTB = S.dram("xtb", [D, TOK], BF16)
    XTALL = S.dram("xtall", [4 * D, TOK], BF16)
    TC = min(2048, SEQ)
    if cfg.mixer:
        OTOK = S.dram("otok", [SEQ, 256], BF16)
        OALL = S.dram("oall", [4 * SEQ, 256], BF16)
        oidx = din("oidx", [128, NBL * 4], I32)
        CQ = S.dram("cq", [3, SEQ], BF16)
        O0S = S.dram("o0s", [SEQ, 128], F32)
        BV = S.dram("bv", [2560], F32)
        SKW = S.dram("skw", [16, 128 * 640], F32)
    if cfg.moe:
        XDISP = S.dram("xdisp", [NEXP * C + 128, D], BF16)
        YDISP = S.dram("ydisp", [NEXP * C + 128, D], F32)
        R1 = NLW * EPC * D
        WB1G = S.dram("wb1g", [R1, 2 * D], BF16)
        WB1D = S.dram("wb1d", [R1, D], BF16)
        RGG, RGD = 256, 512
        NCG, NCD = R1 // RGG, R1 // RGD
        WALLG = [S.dram("wallg%d" % i, [16 * 8 * RGG, 2 * D], BF16) for i in range((NCG + 15) // 16)]
        WALLD = [S.dram("walld%d" % i, [16 * 8 * RGD, D], BF16) for i in range((NCD + 15) // 16)]
        S1G = [S.dram("s1g%d" % i, [4 * RGG, 2 * D], BF16) for i in range(2)]
        S1D = [S.dram("s1d%d" % i, [4 * RGD, D], BF16) for i in range(2)]

    ident_f = S.sbuf("ident_f", [128, 128], F32)
    ident_b = S.sbuf("ident_b", [128, 128], BF16)
    ones_f = S.sbuf("ones_f", [128, 128], F32)
    ones_b = S.sbuf("ones_b", [128, 128], BF16)
    triU_f = S.sbuf("triU_f", [128, 128], F32)
    Ls_b = S.sbuf("Ls_b", [128, 128], BF16)
    DESTI = S.sbuf("DESTI", [128, NBL * 4], I32)
    GATE = S.sbuf("GATE", [128, NBL * 4], F32)
    CNT = S.sbuf("CNT", [128, NEXP], F32)
    EOFF = S.sbuf("EOFF", [128, NEXP], F32)
    with S.phase():
        tmpf = S.sbuf("tmpf", [128, 128], F32)
        tmpi = S.sbuf("tmpi", [128, NEXP], I32)
        MEMSET("pool", ident_f[:], 0.0, [ident_f])
        ASEL(ident_f[:], ident_f[:], [[-1, 128]], ALU.not_equal, 1.0, 0, 1, [ident_f], [ident_f])
        CP("dve", ident_b[:], ident_f[:], [ident_f], [ident_b])
        MEMSET("pool", ones_f[:], 1.0, [ones_f])
        MEMSET("pool", ones_b[:], 1.0, [ones_b])
        MEMSET("pool", triU_f[:], 1.0, [triU_f])
        ASEL(triU_f[:], triU_f[:], [[1, 128]], ALU.is_ge, 0.0, 0, -1, [triU_f], [triU_f])
        MEMSET("pool", tmpf[:], 1.0, [tmpf])
        ASEL(tmpf[:], tmpf[:], [[1, 128]], ALU.is_ge, 0.0, -1, -1, [tmpf], [tmpf])
        CP("dve", Ls_b[:], tmpf[:], [tmpf], [Ls_b])
        S.op("pool", lambda e: e.iota(tmpi[:], pattern=[[C, NEXP]], base=0, channel_multiplier=0), writes=[tmpi])
        CP("dve", EOFF[:], tmpi[:], [tmpi], [EOFF])

    if cfg.moe:
        with S.phase():
            st_r = S.ring("wst", [128, 2 * D], F32, 3)
            sb_r = S.ring("wsb", [128, 2 * D], BF16, 3)
            n = 0
            for (src, b1, ncol) in ((wgu_sh, WB1G, 2 * D), (wdn_sh, WB1D, D)):
                rows = 128 * (2 * D // ncol)
                for r0 in range(0, R1, rows):
                    st = st_r.get()
                    sb = sb_r.get()
                    k = rows // 128
                    S.dma("sp", st[:].rearrange("p (k n) -> p k n", k=k), src[r0:r0 + rows, :].rearrange("(k p) n -> p k n", p=128), st, src)
                    CP("act" if n % 2 == 0 else "dve", sb[:], st[:], [st], [sb])
                    S.dma("sp", b1[r0:r0 + rows, :].rearrange("(k p) n -> p k n", p=128), sb[:].rearrange("p (k n) -> p k n", k=k), b1, sb)
                    n += 1
            for (b1, s1s, walls, RG, NC_) in ((WB1G, S1G, WALLG, RGG, NCG), (WB1D, S1D, WALLD, RGD, NCD)):
                for i in range(NC_):
                    s1 = s1s[i % 2]
                    S.cc("AllGather", ALU.bypass, G4, b1, s1, src_ap=b1[i * RG:(i + 1) * RG, :])
                    wt = walls[i // 16]
                    for r4 in range(4):
                        base = (((i % 16) * 4 + r4) * 2) * RG
                        S.cc("AllGather", ALU.bypass, G2, s1, wt, src_ap=s1[r4 * RG:(r4 + 1) * RG, :],
                             dst_ap=wt[base:base + 2 * RG, :])
            zt = S.sbuf("zt", [128, D], F32)
            ztb = S.sbuf("ztb", [128, D], BF16)
            MEMSET("dve", zt[:], 0.0, [zt])
            MEMSET("dve", ztb[:], 0.0, [ztb])
            for r0 in range(0, NEXP * C + 128, 128):
                S.dma("sp", XDISP[r0:r0 + 128, :], ztb[:], XDISP, ztb)
                S.dma("sp", YDISP[r0:r0 + 128, :], zt[:], YDISP, zt)

    def wloc(L, e, RG, chunk):
        c, le = e // EPC, e % EPC
        g, r4 = c // 4, c % 4
        rr = ((L - cfg.WL0) * EPC + le) * D + chunk * RG
        i = rr // RG
        return i // 16, ((((i % 16) * 4 + r4) * 2 + g) * RG)

    XTBv = XTB.ap().rearrange("(k p) t -> p k t", p=128)

    def emit_xT_block(xt_f32, xT_tile, col, ps_ring, cp_eng):
        ps = ps_ring.get()
        for kc in range(8):
            TR(ps[:, kc * 128:(kc + 1) * 128], xt_f32[:, kc * 128:(kc + 1) * 128], ident_f[:], [xt_f32, ident_f], [ps])
        CP(cp_eng, xT_tile[:, :, col:col + 128], ps[:].rearrange("p (k t) -> p k t", k=8), [ps], [xT_tile])

    def xt_allgather():
        for kc in range(8):
            S.cc("AllGather", ALU.bypass, G4, XTB, XTALL, src_ap=XTB[kc * 128:(kc + 1) * 128, :],
                 dst_ap=XTALL[kc * 512:(kc + 1) * 512, :])

    def phase_x0():
        with S.phase():
            xin_r = S.ring("xin", [128, D], F32, 3)
            xT_r = S.ring("xT", [128, 8, 512], BF16, 2)
            ps_r = S.ring("psx", [128, D], F32, 2, psum=True)
            for tl in range(NTL):
                xT = xT_r.get()
                for b4 in range(4):
                    b = tl * 4 + b4
                    xt = xin_r.get()
                    S.dma("sp", xt[:], x_in[b * 128:(b + 1) * 128, :], xt, x_in)
                    emit_xT_block(xt, xT, b4 * 128, ps_r, "act" if b4 % 2 == 0 else "dve")
                S.dma("sp", XTBv[:, :, tl * 512:(tl + 1) * 512], xT[:], XTB, xT)
            xt_allgather()

    def layer_norm(z, outt, gt, bt, st_r, junk):
        st = st_r.get()
        ACT(junk[:], z[:], AF.Identity, [z], [junk, st], accum=st[:, 0:1])
        ACT(junk[:], z[:], AF.Square, [z], [junk, st], accum=st[:, 1:2])
        TS("dve", st[:, 2:3], st[:, 0:1], 1.0 / D, ALU.mult, [st], [st])
        TT("dve", st[:, 3:4], st[:, 2:3], st[:, 2:3], ALU.mult, [st], [st])
        STT(st[:, 4:5], st[:, 1:2], 1.0 / D, st[:, 3:4], ALU.mult, ALU.subtract, [st], [st])
        TS("dve", st[:, 4:5], st[:, 4:5], LN_EPS, ALU.add, [st], [st])
        ACT(st[:, 5:6], st[:, 4:5], AF.Sqrt, [st], [st])
        RECIP(st[:, 6:7], st[:, 5:6], [st], [st])
        TS("dve", outt[:], z[:], st[:, 2:3], ALU.subtract, [z, st], [outt], s2=st[:, 6:7], op1=ALU.mult)
        TT("dve", outt[:], outt[:], gt[:], ALU.mult, [outt, gt], [outt])
        TT("dve", outt[:], outt[:], bt[:], ALU.add, [outt, bt], [outt])

    def phase_ln_moe(L, xres, xnext, is_last):
        with S.phase():
            gt = S.sbuf("gt", [128, D], F32)
            bt = S.sbuf("bt", [128, D], F32)
            S.dma("sp", gt[:], lng.ap()[L, :].partition_broadcast(128), gt, lng)
            S.dma("sp", bt[:], lnb.ap()[L, :].partition_broadcast(128), bt, lnb)
            xr_r = S.ring("xr", [128, D], F32, 2)
            if cfg.mixer:
                wsrc = we_out if L % 2 == 0 else wo_out
                WO = S.sbuf("WO", [128, 8, D], BF16)
                for q4 in range(4):
                    S.dma("pool", WO[:, 2 * q4:2 * q4 + 2, :],
                          wsrc.ap()[L // 2, q4 * 256:(q4 + 1) * 256, :].rearrange("(k p) n -> p k n", p=128), WO, wsrc)
                OIDX = S.sbuf("OIDX", [128, NBL * 4], I32)
                S.dma("sp", OIDX[:], oidx[:, :], OIDX, oidx)
                og_r = S.ring("og", [128, 4, 256], BF16, 2)
                oT_r = S.ring("oT", [128, 8, 128], BF16, 2)
                pso_r = S.ring("pso", [128, D], BF16, 1, psum=True)
                psh_r = S.ring("psh", [128, 512], F32, 2, psum=True)
            z_r = S.ring("z", [128, D], F32, 2)
            x1_r = S.ring("x1", [128, D], F32, 2)
            x1b_r = S.ring("x1b", [128, D], BF16, 2)
            junk = S.sbuf("junk", [128, D], F32)
            st_r = S.ring("st", [128, 8], F32, 2)
            if cfg.moe:
                RW = S.sbuf("RW", [128, 8, NEXP], F32)
                RB = S.sbuf("RB", [128, NEXP], F32)
                S.dma("sp", RW[:], router_w.ap()[L].rearrange("(k p) e -> p k e", p=128), RW, router_w)
                S.dma("sp", RB[:], router_b.ap()[L, :].partition_broadcast(128), RB, router_b)
                x1T_r = S.ring("x1T", [128, 8, 128], F32, 2)
                psx_r = S.ring("psx", [128, D], F32, 1, psum=True)
                psl_r = S.ring("psl", [128, 512], F32, 1, psum=True)
                psp_r = S.ring("psp", [128, 512], F32, 2, psum=True)
                sm_r = S.ring("sm", [128, 12, NEXP], F32, 2)
                mb_r = S.ring("mb", [128, NEXP], BF16, 2)
                t8_r = S.ring("t8", [128, 8], F32, 2)
                sc_r = S.ring("sc", [128, 16], F32, 2)
                MEMSET("dve", CNT[:], 0.0, [CNT])
            for b in range(NBL):
                xr = xr_r.get()
                S.dma("sp", xr[:], xres[b * 128:(b + 1) * 128, :], xr, xres)
                z = z_r.get()
                if cfg.mixer:
                    og = og_r.get()
                    for s4 in range(4):
                        col = b * 4 + s4
                        S.dma("pool", None, None, og, OALL, extra=[OIDX],
                              fn=lambda e, col=col, og=og, s4=s4: e.indirect_dma_start(
                                  out=og[:, s4, :], out_offset=None, in_=OALL.ap()[:, :],
                                  in_offset=bass.IndirectOffsetOnAxis(ap=OIDX[:, col:col + 1], axis=0)))
                    pso = pso_r.get()
                    for kc in range(8):
                        TR(pso[:, kc * 128:(kc + 1) * 128], og[:, kc // 2, (kc % 2) * 128:(kc % 2 + 1) * 128], ident_b[:], [og, ident_b], [pso])
                    oT = oT_r.get()
                    CP("act", oT[:], pso[:].rearrange("p (k t) -> p k t", k=8), [pso], [oT])
                    for half in range(2):
                        psh = psh_r.get()
                        for kc in range(8):
                            MM(psh[:], oT[:, kc, :], WO[:, kc, half * 512:(half + 1) * 512], kc == 0, kc == 7, [oT, WO], [psh])
                        STT(z[:, half * 512:(half + 1) * 512], xr[:, half * 512:(half + 1) * 512], ALPHA, psh[:], ALU.mult, ALU.add, [xr, psh], [z])
                else:
                    TS("dve", z[:], xr[:], ALPHA, ALU.mult, [xr], [z])
                x1 = x1_r.get()
                layer_norm(z, x1, gt, bt, st_r, junk)
                S.dma("sp", XR1[b * 128:(b + 1) * 128, :], x1[:], XR1, x1)
                if not cfg.moe:
                    continue
                x1b = x1b_r.get()
                CP("act", x1b[:], x1[:], [x1], [x1b])
                x1T = x1T_r.get()
                ps = psx_r.get()
                for kc in range(8):
                    TR(ps[:, kc * 128:(kc + 1) * 128], x1[:, kc * 128:(kc + 1) * 128], ident_f[:], [x1, ident_f], [ps])
                CP("act", x1T[:], ps[:].rearrange("p (k t) -> p k t", k=8), [ps], [x1T])
                psl = psl_r.get()
                for kc in range(8):
                    MM(psl[:, 0:NEXP], x1T[:, kc, :], RW[:, kc, :], kc == 0, kc == 7, [x1T, RW], [psl])
                sm = sm_r.get()
                lg, ex, mk, gd, gates, pos, dfull, oh, jk = (sm[:, i, :] for i in range(9))
                t8 = t8_r.get()
                sc = sc_r.get()
                TT("dve", lg, psl[:, 0:NEXP], RB[:], ALU.add, [psl, RB], [sm])
                S.op("dve", lambda e, t8=t8, lg=lg: e.max(out=t8[:], in_=lg), reads=[sm], writes=[t8])
                TS("dve", sc[:, 0:1], t8[:, 0:1], -1.0, ALU.mult, [t8], [sc])
                ACT(ex, lg, AF.Exp, [sm, sc], [sm], bias=sc[:, 0:1])
                TS("dve", mk, lg, t8[:, 3:4], ALU.is_ge, [sm, t8], [sm])
                TT("dve", gd, ex, mk, ALU.mult, [sm], [sm])
                S.op("dve", lambda e, sc=sc, gd=gd: e.tensor_reduce(out=sc[:, 1:2], in_=gd, axis=AX.X, op=ALU.add),
                     reads=[sm], writes=[sc])
                RECIP(sc[:, 2:3], sc[:, 1:2], [sc], [sc])
                TS("dve", gates, gd, sc[:, 2:3], ALU.mult, [sm, sc], [sm])
                mb = mb_r.get()
                CP("dve", mb[:], mk, [sm], [mb])
                psp = psp_r.get()
                MM(psp[:, 0:NEXP], Ls_b[:], mb[:], True, True, [Ls_b, mb], [psp])
                TT("dve", pos, psp[:, 0:NEXP], CNT[:], ALU.add, [psp, CNT], [sm])
                psc = psp_r.get()
                MM(psc[:, 0:NEXP], ones_b[:], mb[:], True, True, [ones_b, mb], [psc])
                TT("dve", CNT[:], CNT[:], psc[:, 0:NEXP], ALU.add, [CNT, psc], [CNT])
                TT("dve", dfull, pos, EOFF[:], ALU.add, [sm, EOFF], [sm])
                for k in range(4):
                    TS("dve", oh, lg, t8[:, k:k + 1], ALU.is_equal, [sm, t8], [sm])
                    TT("dve", jk, oh, pos, ALU.mult, [sm], [sm])
                    S.op("dve", lambda e, sc=sc, jk=jk: e.tensor_reduce(out=sc[:, 4:5], in_=jk, axis=AX.X, op=ALU.add),
                         reads=[sm], writes=[sc])
                    TT("dve", jk, oh, dfull, ALU.mult, [sm], [sm])
                    S.op("dve", lambda e, sc=sc, jk=jk: e.tensor_reduce(out=sc[:, 5:6], in_=jk, axis=AX.X, op=ALU.add),
                         reads=[sm], writes=[sc])
                    TT("dve", jk, oh, gates, ALU.mult, [sm], [sm])
                    S.op("dve", lambda e, sc=sc, jk=jk: e.tensor_reduce(out=sc[:, 6:7], in_=jk, axis=AX.X, op=ALU.add),
                         reads=[sm], writes=[sc])
                    TS("dve", sc[:, 7:8], sc[:, 4:5], float(C), ALU.is_lt, [sc], [sc])
                    TS("dve", sc[:, 8:9], sc[:, 5:6], float(TRASH), ALU.subtract, [sc], [sc])
                    TT("dve", sc[:, 8:9], sc[:, 8:9], sc[:, 7:8], ALU.mult, [sc], [sc])
                    TS("dve", sc[:, 8:9], sc[:, 8:9], float(TRASH), ALU.add, [sc], [sc])
                    col = b * 4 + k
                    CP("dve", DESTI[:, col:col + 1], sc[:, 8:9], [sc], [DESTI])
                    TT("dve", GATE[:, col:col + 1], sc[:, 6:7], sc[:, 7:8], ALU.mult, [sc], [GATE])
                    S.dma("pool", None, None, XDISP, x1b, extra=[DESTI],
                          fn=lambda e, col=col, x1b=x1b: e.indirect_dma_start(
                              out=XDISP.ap()[:, :], out_offset=bass.IndirectOffsetOnAxis(ap=DESTI[:, col:col + 1], axis=0),
                              in_=x1b[:, :], in_offset=None))
        if cfg.moe:
            with S.phase():
                BG = S.sbuf("BG", [128, NEXP * 16], F32)
                S.dma("sp", BG[:], bgu[:, L * NEXP * 16:(L + 1) * NEXP * 16], BG, bgu)
                wgu_r = S.ring("wgu", [128, 8, 2 * D], BF16, 2)
                wdn_r = S.ring("wdn", [128, 8, D], BF16, 2)
                bd_r = S.ring("bd", [128, D], F32, 2)
                xs_r = S.ring("xs", [128, CB, D], BF16, 1)
                xsT_r = S.ring("xsT", [128, 8, C], BF16, 1)
                aT_r = S.ring("aT", [128, 8, C], BF16, 1)
                tmp_r = S.ring("tmp", [128, 5, 512], F32, 2)
                ys_r = S.ring("ys", [128, D], F32, 2)
                pst_r = S.ring("pst", [128, D], BF16, 2, psum=True)
                psg_r = S.ring("psg", [128, 512], F32, 2, psum=True)
                psu_r = S.ring("psu", [128, 512], F32, 2, psum=True)
                psd_r = S.ring("psd", [128, 512], F32, 2, psum=True)
                chunks = [(c0, min(512, C - c0)) for c0 in range(0, C, 512)]
                for e_ in range(NEXP):
                    wg = wgu_r.get()
                    wd = wdn_r.get()
                    for q4 in range(4):
                        ti, row = wloc(L, e_, RGG, q4)
                        S.dma("sp", wg[:, 2 * q4:2 * q4 + 2, :],
                              WALLG[ti].ap()[row:row + 256, :].rearrange("(k p) n -> p k n", p=128), wg, WALLG[ti])
                    for q4 in range(2):
                        ti, row = wloc(L, e_, RGD, q4)
                        S.dma("sp", wd[:, 4 * q4:4 * q4 + 4, :],
                              WALLD[ti].ap()[row:row + 512, :].rearrange("(k p) n -> p k n", p=128), wd, WALLD[ti])
                    bd = bd_r.get()
                    S.dma("sp", bd[:], bdn.ap()[L * NEXP + e_, :].partition_broadcast(128), bd, bdn)
                    xs = xs_r.get()
                    S.dma("sp", xs[:], XDISP.ap()[e_ * C:(e_ + 1) * C, :].rearrange("(c p) d -> p c d", p=128), xs, XDISP)
                    xsT = xsT_r.get()
                    for cb in range(CB):
                        pst = pst_r.get()
                        for kc in range(8):
                            TR(pst[:, kc * 128:(kc + 1) * 128], xs[:, cb, kc * 128:(kc + 1) * 128], ident_b[:], [xs, ident_b], [pst])
                        CP("act" if cb % 2 == 0 else "dve", xsT[:, :, cb * 128:(cb + 1) * 128],
                           pst[:].rearrange("p (k t) -> p k t", k=8), [pst], [xsT])
                    aT = aT_r.get()
                    for j in range(8):
                        for (c0, w) in chunks:
                            psg = psg_r.get()
                            psu = psu_r.get()
                            for kc in range(8):
                                MM(psg[:, 0:w], wg[:, kc, j * 128:(j + 1) * 128], xsT[:, kc, c0:c0 + w], kc == 0, kc == 7, [wg, xsT], [psg])
                            for kc in range(8):
                                MM(psu[:, 0:w], wg[:, kc, D + j * 128:D + (j + 1) * 128], xsT[:, kc, c0:c0 + w], kc == 0, kc == 7, [wg, xsT], [psu])
                            tmp = tmp_r.get()
                            g1, sg, u1, u2, tt_ = (tmp[:, i, 0:w] for i in range(5))
                            bgc = (e_ * 16 + j)
                            TS("dve", g1, psg[:, 0:w], BG[:, bgc:bgc + 1], ALU.add, [psg, BG], [tmp], s2=7.0, op1=ALU.min)
                            ACT(sg, g1, AF.Sigmoid, [tmp], [tmp], scale=1.702)
                            TS("dve", u1, psu[:, 0:w], BG[:, bgc + 8:bgc + 9], ALU.add, [psu, BG], [tmp], s2=7.0, op1=ALU.min)
                            TS("dve", u2, u1, -7.0, ALU.max, [tmp], [tmp], s2=1.0, op1=ALU.add)
                            TT("pool", tt_, g1, sg, ALU.mult, [tmp], [tmp])
                            TT("pool", aT[:, j, c0:c0 + w], tt_, u2, ALU.mult, [tmp], [aT])
                    for cb in range(CB):
                        ys = ys_r.get()
                        for half in range(2):
                            psd = psd_r.get()
                            for j in range(8):
                                MM(psd[:], aT[:, j, cb * 128:(cb + 1) * 128], wd[:, j, half * 512:(half + 1) * 512], j == 0, j == 7, [aT, wd], [psd])
                            TT("dve", ys[:, half * 512:(half + 1) * 512], psd[:], bd[:, half * 512:(half + 1) * 512], ALU.add, [psd, bd], [ys])
                        S.dma("sp", YDISP[e_ * C + cb * 128:e_ * C + (cb + 1) * 128, :], ys[:], YDISP, ys)
        with S.phase():
            gt = S.sbuf("gt", [128, D], F32)
            bt = S.sbuf("bt", [128, D], F32)
            S.dma("sp", gt[:], lng.ap()[4 + L, :].partition_broadcast(128), gt, lng)
            S.dma("sp", bt[:], lnb.ap()[4 + L, :].partition_broadcast(128), bt, lnb)
            x1_r = S.ring("x1", [128, D], F32, 2)
            acc_r = S.ring("acc", [128, D], F32, 2)
            yk_r = S.ring("yk", [128, D], F32, 4)
            x2_r = S.ring("x2", [128, D], F32, 2)
            junk = S.sbuf("junk", [128, D], F32)
            st_r = S.ring("st", [128, 8], F32, 2)
            xT_r = S.ring("xT", [128, 8, 512], BF16, 2)
            ps_r = S.ring("psx", [128, D], F32, 2, psum=True)
            dst = out if is_last else xnext
            xT = None
            for b in range(NBL):
                x1 = x1_r.get()
                S.dma("sp", x1[:], XR1[b * 128:(b + 1) * 128, :], x1, XR1)
                acc = acc_r.get()
                TS("dve", acc[:], x1[:], ALPHA, ALU.mult, [x1], [acc])
                if cfg.moe:
                    for k in range(4):
                        col = b * 4 + k
                        yk = yk_r.get()
                        S.dma("pool", None, None, yk, YDISP, extra=[DESTI],
                              fn=lambda e, col=col, yk=yk: e.indirect_dma_start(
                                  out=yk[:, :], out_offset=None, in_=YDISP.ap()[:, :],
                                  in_offset=bass.IndirectOffsetOnAxis(ap=DESTI[:, col:col + 1], axis=0)))
                        STT(acc[:], yk[:], GATE[:, col:col + 1], acc[:], ALU.mult, ALU.add, [yk, GATE, acc], [acc])
                x2 = x2_r.get()
                layer_norm(acc, x2, gt, bt, st_r, junk)
                S.dma("sp", dst[b * 128:(b + 1) * 128, :], x2[:], dst, x2)
                if not is_last:
                    if b % 4 == 0:
                        xT = xT_r.get()
                    emit_xT_block(x2, xT, (b % 4) * 128, ps_r, "act")
                    if b % 4 == 3:
                        tl = b // 4
                        S.dma("sp", XTBv[:, :, tl * 512:(tl + 1) * 512], xT[:], XTB, xT)
            if not is_last:
                xt_allgather()


    XTALLv = XTALL.ap().rearrange("(k r p) t -> r p k t", k=8, r=4) if True else None

    def load_xT(xT, tt):
        rank, lt = tt // NTL, tt % NTL
        S.dma("sp", xT[:], XTALLv[rank][:, :, lt * 512:(lt + 1) * 512], xT, XTALL)

    def o_allgather():
        for j in range(SEQ // TC):
            S.cc("AllGather", ALU.bypass, G4, OTOK, OALL, src_ap=OTOK[j * TC:(j + 1) * TC, :],
                 dst_ap=OALL[j * 4 * TC:(j + 1) * 4 * TC, :])

    def odd_mixer(L):
        i = L // 2
        with S.phase():
            Wb = S.sbuf("Wb", [128, 8, 1028], BF16)
            for q4 in range(4):
                S.dma("pool", Wb[:, 2 * q4:2 * q4 + 2, :],
                      wo_in.ap()[i, q4 * 256:(q4 + 1) * 256, :].rearrange("(k p) n -> p k n", p=128), Wb, wo_in)
            WQK = S.sbuf("WQK", [64, 2], F32)
            S.dma("sp", WQK[:], qkw.ap()[i], WQK, qkw)
            TS("dve", WQK[:, 0:1], WQK[:, 0:1], 0.125, ALU.mult, [WQK], [WQK])
            NFB = S.sbuf("NFB", [128, 4], F32)
            S.dma("sp", NFB[:], fb.ap()[i, :].partition_broadcast(128), NFB, fb)
            TS("dve", NFB[:], NFB[:], -1.0, ALU.mult, [NFB], [NFB])
            MKf = S.sbuf("MKf", [128, 4, 512], F32)
            MK = S.sbuf("MK", [128, 4, 512], BF16)
            MEMSET("pool", MKf[:], 0.0, [MKf])
            for j in range(4):
                ASEL(MKf[:, j, :], MKf[:, j, :], [[1, 512]], ALU.is_ge, NEG, -128 * j, -1, [MKf], [MKf])
            CP("dve", MK[:], MKf[:], [MKf], [MK])
            QT = S.sbuf("QT", [67, SEQ], BF16)
            KT = S.sbuf("KT", [67, SEQ], BF16)
            GTK = S.sbuf("GTK", [128, NBF, 64], BF16)
            VA = S.sbuf("VA", [128, NBF, 65], BF16)
            LF = S.sbuf("LF", [128, NBF], F32)
            CUM = S.sbuf("CUM", [128, NBF], F32)
            NEGCUM = S.sbuf("NEGCUM", [128, NBF], F32)
            scan = [S.sbuf("scan%d" % k, [128, NBF], F32) for k in range(2)]
            tot = S.sbuf("tot", [128, NBF], F32)
            ct = S.sbuf("ct", [128, 6, 128], F32)
            ctb = S.sbuf("ctb", [128, 3, 128], BF16)
            MEMSET("dve", VA[:, :, 64:65], 1.0, [VA])
            MEMSET("dve", KT[64:67, :], 1.0, [KT])
            xT_r = S.ring("xT", [128, 8, 512], BF16, 2)
            qf_r = S.ring("qf", [64, 512], F32, 2)
            sq_r = S.ring("sq", [64, 512], F32, 2)
            rs_r = S.ring("rs", [64, 512], F32, 2)
            sm_r = S.ring("smo", [128, 8], F32, 4)
            pt_r = S.ring("pt", [128, 512], BF16, 4)
            ot_r = S.ring("ot", [128, 4, 64], BF16, 2)
            osb_r = S.ring("osb", [128, 512], F32, 2)
            for b_ in osb_r.bufs:
                MEMSET("dve", b_[:], 0.0, [b_])
            psm_r = S.ring("psm", [128, 512], F32, 3, psum=True)
            pss_r = S.ring("pss", [128, 512], F32, 3, psum=True)
            pso_r = S.ring("pso", [128, 512], F32, 2, psum=True)
            import os
            KODD = int(os.environ.get("KODD", "9"))
            if KODD == 0:
                return
            for h in range(4):
                w0 = h * 257
                for tt in range(NQT):
                    xT = xT_r.get()
                    load_xT(xT, tt)
                    for (coff, dst, wi) in ((0, QT, 0), (64, KT, 1)):
                        psq = psm_r.get()
                        for kc in range(8):
                            MM(psq[0:64, :], Wb[:, kc, w0 + coff:w0 + coff + 64], xT[:, kc, :], kc == 0, kc == 7, [Wb, xT], [psq])
                        qf = qf_r.get()
                        CP("act", qf[:], psq[0:64, :], [psq], [qf])
                        sq = sq_r.get()
                        TT("pool", sq[:], qf[:], qf[:], ALU.mult, [qf], [sq])
                        pssum = psm_r.get()
                        MM(pssum[0:64, :], ones_f[0:64, 0:64], sq[:], True, True, [ones_f, sq], [pssum])
                        rs = rs_r.get()
                        TS("dve", rs[:], pssum[0:64, :], 1.0 / 64, ALU.mult, [pssum], [rs], s2=RMS_EPS, op1=ALU.add)
                        ACT(rs[:], rs[:], AF.Sqrt, [rs], [rs])
                        RECIP(rs[:], rs[:], [rs], [rs])
                        STT(dst[0:64, tt * 512:(tt + 1) * 512], qf[:], WQK[:, wi:wi + 1], rs[:], ALU.mult, ALU.mult, [qf, WQK, rs], [dst])
                    for b4 in range(4):
                        gb = tt * 4 + b4
                        ps = psm_r.get()
                        for kc in range(8):
                            MM(ps[:, 0:129], xT[:, kc, b4 * 128:(b4 + 1) * 128], Wb[:, kc, w0 + 128:w0 + 257], kc == 0, kc == 7, [xT, Wb], [ps])
                        sm = sm_r.get()
                        CP("dve", VA[:, gb, 0:64], ps[:, 0:64], [ps], [VA])
                        ACT(GTK[:, gb, :], ps[:, 65:129], AF.Sigmoid, [ps], [GTK])
                        ACT(sm[:, 0:1], ps[:, 64:65], AF.Exp, [ps, NFB], [sm], bias=NFB[:, h:h + 1], scale=-1.0)
                        ACT(sm[:, 1:2], sm[:, 0:1], AF.Ln, [sm], [sm], bias=1.0)
                        TS("dve", LF[:, gb:gb + 1], sm[:, 1:2], -1.0, ALU.mult, [sm], [LF])
                if KODD == 1:
                    return
                psw = psm_r.get()
                MM(psw[:, 0:NBF], triU_f[:], LF[:], True, True, [triU_f, LF], [psw])
                pstot = psm_r.get()
                MM(pstot[:, 0:NBF], ones_f[:], LF[:], True, True, [ones_f, LF], [pstot])
                CP("dve", tot[:], pstot[:, 0:NBF], [pstot], [tot])
                CP("dve", scan[0][:], tot[:], [tot], [scan[0]])
                a, bq = scan[0], scan[1]
                sft = 1
                while sft < NBF:
                    TT("dve", bq[:, sft:NBF], a[:, sft:NBF], a[:, 0:NBF - sft], ALU.add, [a], [bq])
                    CP("dve", bq[:, 0:sft], a[:, 0:sft], [a], [bq])
                    a, bq = bq, a
                    sft *= 2
                TT("dve", CUM[:], psw[:, 0:NBF], a[:], ALU.add, [psw, a], [CUM])
                TT("dve", CUM[:], CUM[:], tot[:], ALU.subtract, [CUM, tot], [CUM])
                TS("dve", NEGCUM[:], CUM[:], -1.0, ALU.mult, [CUM], [NEGCUM])
                pct = psm_r.get()
                TR(pct[0:NBF, 0:128], CUM[:, 0:NBF], ident_f[:], [CUM, ident_f], [pct])
                CP("dve", ct[0:NBF, 0, :], pct[0:NBF, 0:128], [pct], [ct])
                CP("dve", ctb[0:NBF, 0, :], ct[0:NBF, 0, :], [ct], [ctb])
                CP("dve", ct[0:NBF, 1, :], ctb[0:NBF, 0, :], [ctb], [ct])
                TT("dve", ct[0:NBF, 2, :], ct[0:NBF, 0, :], ct[0:NBF, 1, :], ALU.subtract, [ct], [ct])
                CP("dve", ctb[0:NBF, 1, :], ct[0:NBF, 2, :], [ct], [ctb])
                CP("dve", ct[0:NBF, 3, :], ctb[0:NBF, 1, :], [ctb], [ct])
                TT("dve", ct[0:NBF, 4, :], ct[0:NBF, 2, :], ct[0:NBF, 3, :], ALU.subtract, [ct], [ct])
                CP("dve", ctb[0:NBF, 2, :], ct[0:NBF, 4, :], [ct], [ctb])
                for j in range(3):
                    S.dma("sp", CQ.ap()[j, :].rearrange("(j t) -> j t", t=128), ctb[0:NBF, j, :], CQ, ctb)
                for j in range(3):
                    S.dma("sp", QT[64 + j:65 + j, :], CQ[j:j + 1, :], QT, CQ)
                if KODD == 2:
                    return
                acc_of = {}

                def stage_a(qt, kb):
                    pss = pss_r.get()
                    diag = kb >= 4 * qt
                    MM(pss[:], KT[0:67, kb * 128:(kb + 1) * 128], QT[0:67, qt * 512:(qt + 1) * 512], True, not diag, [KT, QT], [pss])
                    if diag:
                        MM(pss[:], ident_b[:], MK[:, kb - 4 * qt, :], False, True, [ident_b, MK], [pss])
                    pt = pt_r.get()
                    ACT(pt[:], pss[:], AF.Exp, [pss, NEGCUM], [pt], bias=NEGCUM[:, kb:kb + 1])
                    return pt

                def stage_b(qt, kb, pt):
                    nkb = 4 * qt + 4
                    if kb == 0:
                        acc_of[qt] = pso_r.get()
                    oacc = acc_of[qt]
                    MM(oacc[0:65, :], VA[:, kb, :], pt[:], kb == 0, kb == nkb - 1, [VA, pt], [oacc])
                    if kb < nkb - 1:
                        return
                    osb = osb_r.get()
                    CP("dve", osb[0:65, :], oacc[0:65, :], [oacc], [osb])
                    ptr = psm_r.get()
                    for b4 in range(4):
                        TR(ptr[:, b4 * 128:(b4 + 1) * 128], osb[:, b4 * 128:(b4 + 1) * 128], ident_f[:], [osb, ident_f], [ptr])
                    ot = ot_r.get()
                    for b4 in range(4):
                        gb = qt * 4 + b4
                        sm = sm_r.get()
                        RECIP(sm[:, 0:1], ptr[:, b4 * 128 + 64:b4 * 128 + 65], [ptr], [sm])
                        STT(ot[:, b4, :], ptr[:, b4 * 128:b4 * 128 + 64], sm[:, 0:1], GTK[:, gb, :], ALU.mult, ALU.mult, [ptr, sm, GTK], [ot])
                    S.dma("sp", OTOK.ap()[qt * 512:(qt + 1) * 512, h * 64:(h + 1) * 64].rearrange("(b p) d -> p b d", p=128),
                          ot[:], OTOK, ot)

                pend = []
                for qt in range(NQT):
                    for kb in range(4 * qt + 4):
                        pend.append((qt, kb, stage_a(qt, kb)))
                        if len(pend) > 2:
                            stage_b(*pend.pop(0))
                while pend:
                    stage_b(*pend.pop(0))
                if KODD == 3:
                    return
            o_allgather()

    def even_mixer(L):
        i = L // 2
        lam_init = 0.8 - 0.6 * math.exp(-0.3 * L)
        import os
        KEV = int(os.environ.get("KEV", "9"))
        with S.phase():
            Wd = S.sbuf("Wd", [128, 8, 384], BF16)
            for q4 in range(4):
                S.dma("pool", Wd[:, 2 * q4:2 * q4 + 2, :],
                      we_in.ap()[i, q4 * 256:(q4 + 1) * 256, 0:384].rearrange("(k p) n -> p k n", p=128), Wd, we_in)
            LP = S.sbuf("LP", [128, 256], F32)
            S.dma("sp", LP[:], lamp.ap()[i, :].partition_broadcast(128), LP, lamp)
            lw = S.sbuf("lw", [128, 8], F32)
            pr = S.sbuf("pr", [128, 128], F32)
            TT("dve", pr[:, 0:64], LP[:, 0:64], LP[:, 64:128], ALU.mult, [LP], [pr])
            TT("dve", pr[:, 64:128], LP[:, 128:192], LP[:, 192:256], ALU.mult, [LP], [pr])
            S.op("dve", lambda e: e.tensor_reduce(out=lw[:, 0:2], in_=pr[:].rearrange("p (g d) -> p g d", g=2), axis=AX.X, op=ALU.add),
                 reads=[pr], writes=[lw])
            ACT(lw[:, 2:4], lw[:, 0:2], AF.Exp, [lw], [lw])
            TT("dve", lw[:, 4:5], lw[:, 2:3], lw[:, 3:4], ALU.subtract, [lw], [lw])
            TS("dve", lw[:, 4:5], lw[:, 4:5], lam_init, ALU.add, [lw], [lw])
            TS("dve", lw[:, 5:6], lw[:, 4:5], -1.0, ALU.mult, [lw], [lw])
            SUBW = S.sbuf("SUBW", [128, 128], F32)
            S.dma("sp", SUBW[:], subw.ap()[i, :].partition_broadcast(128), SUBW, subw)
            TS("dve", SUBW[:], SUBW[:], 1.0 - lam_init, ALU.mult, [SUBW], [SUBW])
            T5C = S.sbuf("T5C", [33, 1], F32)
            S.dma("sp", T5C[0:32, :], t5col[:, :], T5C, t5col)
            MEMSET("dve", T5C[32:33, :], 1.0, [T5C])
            OH = S.sbuf("OH", [33, 2560], F32)
            S.dma("sp", OH[:], t5oh[:, :], OH, t5oh)
            bvs = S.sbuf("bvs", [1, 2560], F32)
            psm_r = S.ring("psm", [128, 512], F32, 3, psum=True)
            pss_r = S.ring("pss", [128, 512], F32, 3, psum=True)
            pso_r = S.ring("pso", [128, 512], F32, 2, psum=True)
            for c5 in range(5):
                ps = psm_r.get()
                MM(ps[0:1, :], T5C[:, 0:1], OH[:, c5 * 512:(c5 + 1) * 512], True, True, [T5C, OH], [ps])
                CP("dve", bvs[0:1, c5 * 512:(c5 + 1) * 512], ps[0:1, :], [ps], [bvs])
            S.dma("sp", BV.ap().rearrange("(o n) -> o n", o=1), bvs[:], BV, bvs)
            BT = S.sbuf("BT", [128, 16, 512], BF16)
            skb_r = S.ring("skb", [128, 640], F32, 2)
            for j in range(16):
                skb = skb_r.get()
                S.dma("sp", skb[:], BV.ap()[128 * j:128 * j + 640].partition_broadcast(128), skb, BV)
                S.dma("sp", SKW.ap()[j, :].rearrange("(p n) -> p n", n=640), skb[:], SKW, skb)
                S.dma("pool", BT[:, j, :], bass.AP(tensor=SKW.h, offset=j * 128 * 640 + 127, ap=[[639, 128], [1, 512]]), BT, SKW)
            B31 = S.sbuf("B31", [128, 1], F32)
            S.dma("sp", B31[:], BV.ap()[2559:2560].partition_broadcast(128), B31, BV)
            QT = S.sbuf("QT", [64, SEQ], BF16)
            KT = S.sbuf("KT", [64, SEQ], BF16)
            VA = S.sbuf("VA", [128, NBF, 129], BF16)
            MEMSET("dve", VA[:, :, 64:65], 1.0, [VA])
            xT_r = S.ring("xT", [128, 8, 512], BF16, 2)
            pt_r = S.ring("pt", [128, 512], BF16, 4)
            osb_r = S.ring("osbd", [128, 512], F32, 4)
            for b_ in osb_r.bufs:
                MEMSET("dve", b_[:], 0.0, [b_])
            om_r = S.ring("om", [128, 128], F32, 3)
            o0_r = S.ring("o0", [128, 128], F32, 2)
            a_r = S.ring("a", [128, 128], F32, 2)
            ao_r = S.ring("ao", [128, 128], BF16, 2)
            sm_r = S.ring("smd", [128, 8], F32, 4)
            junk = S.sbuf("junkd", [128, 128], F32)
            for m in range(2):
                for tt in range(NQT):
                    xT = xT_r.get()
                    load_xT(xT, tt)
                    psq = psm_r.get()
                    for kc in range(8):
                        MM(psq[0:64, :], Wd[:, kc, m * 64:(m + 1) * 64], xT[:, kc, :], kc == 0, kc == 7, [Wd, xT], [psq])
                    ACT(QT[:, tt * 512:(tt + 1) * 512], psq[0:64, :], AF.Identity, [psq], [QT], scale=0.125)
                    psk = psm_r.get()
                    for kc in range(8):
                        MM(psk[0:64, :], Wd[:, kc, 128 + m * 64:128 + (m + 1) * 64], xT[:, kc, :], kc == 0, kc == 7, [Wd, xT], [psk])
                    CP("dve", KT[:, tt * 512:(tt + 1) * 512], psk[0:64, :], [psk], [KT])
                    if m == 0:
                        for b4 in range(4):
                            gb = tt * 4 + b4
                            psv = psm_r.get()
                            for kc in range(8):
                                MM(psv[:, 0:128], xT[:, kc, b4 * 128:(b4 + 1) * 128], Wd[:, kc, 256:384], kc == 0, kc == 7, [xT, Wd], [psv])
                            CP("act", VA[:, gb, 0:64], psv[:, 0:64], [psv], [VA])
                            CP("act", VA[:, gb, 65:129], psv[:, 64:128], [psv], [VA])
                if KEV == 1:
                    return
                acc_of = {}

                def stage_a(qt, kb):
                    dj = 4 * qt - kb + 3
                    near = dj <= 15
                    pss = pss_r.get()
                    MM(pss[:], KT[:, kb * 128:(kb + 1) * 128], QT[:, qt * 512:(qt + 1) * 512], True, not near, [KT, QT], [pss])
                    if near:
                        MM(pss[:], ident_b[:], BT[:, dj, :], False, True, [ident_b, BT], [pss])
                    pt = pt_r.get()
                    if near:
                        ACT(pt[:], pss[:], AF.Exp, [pss], [pt])
                    else:
                        ACT(pt[:], pss[:], AF.Exp, [pss, B31], [pt], bias=B31[:, 0:1])
                    return pt

                def stage_b(qt, kb, pt, m=m):
                    nkb = 4 * qt + 4
                    if kb == 0:
                        acc_of[qt] = (pso_r.get(), pso_r.get())
                    oa, ob = acc_of[qt]
                    MM(oa[0:65, :], VA[:, kb, 0:65], pt[:], kb == 0, kb == nkb - 1, [VA, pt], [oa])
                    MM(ob[0:64, :], VA[:, kb, 65:129], pt[:], kb == 0, kb == nkb - 1, [VA, pt], [ob])
                    if kb < nkb - 1:
                        return
                    osa = osb_r.get()
                    osb = osb_r.get()
                    CP("dve", osa[0:65, :], oa[0:65, :], [oa], [osa])
                    CP("act", osb[0:64, :], ob[0:64, :], [ob], [osb])
                    ptra = psm_r.get()
                    ptrb = psm_r.get()
                    for b4 in range(4):
                        TR(ptra[:, b4 * 128:(b4 + 1) * 128], osa[:, b4 * 128:(b4 + 1) * 128], ident_f[:], [osa, ident_f], [ptra])
                        TR(ptrb[:, b4 * 128:(b4 + 1) * 128], osb[:, b4 * 128:(b4 + 1) * 128], ident_f[:], [osb, ident_f], [ptrb])
                    for b4 in range(4):
                        gb = qt * 4 + b4
                        sm = sm_r.get()
                        RECIP(sm[:, 0:1], ptra[:, b4 * 128 + 64:b4 * 128 + 65], [ptra], [sm])
                        om = om_r.get()
                        TS("dve", om[:, 0:64], ptra[:, b4 * 128:b4 * 128 + 64], sm[:, 0:1], ALU.mult, [ptra, sm], [om])
                        TS("dve", om[:, 64:128], ptrb[:, b4 * 128:b4 * 128 + 64], sm[:, 0:1], ALU.mult, [ptrb, sm], [om])
                        if m == 0:
                            S.dma("sp", O0S[gb * 128:(gb + 1) * 128, :], om[:], O0S, om)
                        else:
                            o0 = o0_r.get()
                            S.dma("sp", o0[:], O0S[gb * 128:(gb + 1) * 128, :], o0, O0S)
                            av = a_r.get()
                            STT(av[:], om[:], lw[:, 5:6], o0[:], ALU.mult, ALU.add, [om, lw, o0], [av])
                            ACT(junk[:], av[:], AF.Square, [av], [junk, sm], accum=sm[:, 1:2])
                            TS("dve", sm[:, 2:3], sm[:, 1:2], 1.0 / 128, ALU.mult, [sm], [sm], s2=RMS_EPS, op1=ALU.add)
                            ACT(sm[:, 3:4], sm[:, 2:3], AF.Sqrt, [sm], [sm])
                            RECIP(sm[:, 4:5], sm[:, 3:4], [sm], [sm])
                            ao = ao_r.get()
                            STT(ao[:], av[:], sm[:, 4:5], SUBW[:], ALU.mult, ALU.mult, [av, sm, SUBW], [ao])
                            S.dma("sp", OTOK[gb * 128:(gb + 1) * 128, 0:128], ao[:], OTOK, ao)

                pend = []
                for qt in range(NQT):
                    for kb in range(4 * qt + 4):
                        pend.append((qt, kb, stage_a(qt, kb)))
                        if len(pend) > 2:
                            stage_b(*pend.pop(0))
                while pend:
                    stage_b(*pend.pop(0))
        if KEV <= 2:
            with S.phase():
                zb = S.sbuf("zb", [128, 128], BF16)
                MEMSET("dve", zb[:], 0.0, [zb])
                for gb in range(NBF):
                    S.dma("sp", OTOK[gb * 128:(gb + 1) * 128, 128:256], zb[:], OTOK, zb)
            o_allgather()
            return
        gdn_phase(L)
        o_allgather()

    def gdn_phase(L):
        i = L // 2
        SC = 128 ** -0.5
        with S.phase():
            Wg = S.sbuf("Wg", [128, 8, 514], BF16)
            for q4 in range(4):
                S.dma("pool", Wg[:, 2 * q4:2 * q4 + 2, :],
                      we_in.ap()[i, q4 * 256:(q4 + 1) * 256, 384:898].rearrange("(k p) n -> p k n", p=128), Wg, we_in)
            CW = S.sbuf("CW", [128, 12], F32)
            S.dma("sp", CW[:], convw.ap()[i], CW, convw)
            GNW = S.sbuf("GNW", [128, 128], F32)
            S.dma("sp", GNW[:], gnw.ap()[i, :].partition_broadcast(128), GNW, gnw)
            AD = S.sbuf("AD", [128, 4], F32)
            S.dma("sp", AD[:, 0:1], alog.ap()[i, :].partition_broadcast(128), AD, alog)
            S.dma("sp", AD[:, 1:2], dtb.ap()[i, :].partition_broadcast(128), AD, dtb)
            ACT(AD[:, 2:3], AD[:, 0:1], AF.Exp, [AD], [AD])
            TS("dve", AD[:, 2:3], AD[:, 2:3], -1.0, ALU.mult, [AD], [AD])
            MUs = S.sbuf("MUs", [128, 128], F32)
            MUi = S.sbuf("MUi", [128, 128], F32)
            MLs = S.sbuf("MLs", [128, 128], F32)
            for (t_, base, cm, pat) in ((MUs, -1, -1, 1), (MUi, 0, -1, 1), (MLs, -1, 1, -1)):
                MEMSET("pool", t_[:], 1.0, [t_])
                ASEL(t_[:], t_[:], [[pat, 128]], ALU.is_ge, 0.0, base, cm, [t_], [t_])
            St = [S.sbuf("St%d" % k, [128, 128], F32) for k in range(2)]
            MEMSET("dve", St[0][:], 0.0, [St[0]])
            cb = [S.sbuf("cb%d" % g, [128, 515], F32) for g in range(3)]
            for g in range(3):
                MEMSET("dve", cb[g][:], 0.0, [cb[g]])
            hal = S.sbuf("hal", [128, 3, 3], F32)
            xT_r = S.ring("xT", [128, 8, 512], BF16, 2)
            y_r = S.ring("y", [128, 512], F32, 2)
            fT = [S.ring("fT%d" % g, [128, 512], F32, 2) for g in range(3)]
            sq_r = S.ring("sqg", [128, 512], F32, 2)
            zt_r = S.ring("ztk", [128, 4, 128], F32, 2)
            gb_r = S.ring("gbt", [128, 12], F32, 2)
            w_r = S.ring("wk", [128, 128], F32, 44)
            w2_r = S.ring("wk2", [128, 256], F32, 13)
            c_r = S.ring("colg", [128, 8], F32, 9)
            bo_r = S.ring("bo", [128, 128], BF16, 2)
            ps_r = S.ring("psg", [128, 512], F32, 7, psum=True)
            pso_r = S.ring("psgo", [128, 512], F32, 1, psum=True)
            cur = 0
            for tt in range(NQT):
                xT = xT_r.get()
                load_xT(xT, tt)
                fts = []
                for g in range(3):
                    psf = ps_r.get()
                    for kc in range(8):
                        MM(psf[:], Wg[:, kc, g * 128:(g + 1) * 128], xT[:, kc, :], kc == 0, kc == 7, [Wg, xT], [psf])
                    CP("dve", hal[:, g, :], cb[g][:, 512:515], [cb[g]], [hal])
                    CP("act", cb[g][:, 3:515], psf[:], [psf], [cb[g]])
                    CP("dve", cb[g][:, 0:3], hal[:, g, :], [hal], [cb[g]])
                    y = y_r.get()
                    TS("dve", y[:], cb[g][:, 0:512], CW[:, g * 4:g * 4 + 1], ALU.mult, [cb[g], CW], [y])
                    for j in range(1, 4):
                        STT(y[:], cb[g][:, j:j + 512], CW[:, g * 4 + j:g * 4 + j + 1], y[:], ALU.mult, ALU.add, [cb[g], CW, y], [y])
                    ft = fT[g].get()
                    ACT(ft[:], y[:], AF.Silu, [y], [ft])
                    if g < 2:
                        sq = sq_r.get()
                        TT("pool", sq[:], ft[:], ft[:], ALU.mult, [ft], [sq])
                        pss = ps_r.get()
                        MM(pss[:], ones_f[:], sq[:], True, True, [ones_f, sq], [pss])
                        TS("dve", sq[:], pss[:], RMS_EPS, ALU.add, [pss], [sq])
                        ACT(sq[:], sq[:], AF.Sqrt, [sq], [sq])
                        RECIP(sq[:], sq[:], [sq], [sq])
                        TT("dve", ft[:], ft[:], sq[:], ALU.mult, [ft, sq], [ft])
                    fts.append(ft)
                qTt, kTt, vTt = fts
                zt = zt_r.get()
                gbt = gb_r.get()
                for b4 in range(4):
                    pz = ps_r.get()
                    for kc in range(8):
                        MM(pz[:, 0:130], xT[:, kc, b4 * 128:(b4 + 1) * 128], Wg[:, kc, 384:514], kc == 0, kc == 7, [xT, Wg], [pz])
                    ACT(zt[:, b4, :], pz[:, 0:128], AF.Silu, [pz], [zt])
                    ACT(gbt[:, 4 + b4:5 + b4], pz[:, 128:129], AF.Sigmoid, [pz], [gbt])
                    cg = c_r.get()
                    ACT(cg[:, 0:1], pz[:, 129:130], AF.Exp, [pz, AD], [cg], bias=AD[:, 1:2])
                    ACT(cg[:, 1:2], cg[:, 0:1], AF.Ln, [cg], [cg], bias=1.0)
                    TS("dve", gbt[:, b4:b4 + 1], cg[:, 1:2], AD[:, 2:3], ALU.mult, [cg, AD], [gbt])
                pgc = ps_r.get()
                MM(pgc[:, 0:4], triU_f[:], gbt[:, 0:4], True, True, [triU_f, gbt], [pgc])
                CP("dve", gbt[:, 8:12], pgc[:, 0:4], [pgc], [gbt])
                for b4 in range(4):
                    gbk = tt * 4 + b4
                    blk = slice(b4 * 128, (b4 + 1) * 128)
                    gc = gbt[:, 8 + b4:9 + b4]
                    beta = gbt[:, 4 + b4:5 + b4]
                    pk = ps_r.get()
                    TR(pk[:, 0:128], kTt[:, blk], ident_f[:], [kTt, ident_f], [pk])
                    Kc = w_r.get()
                    CP("act", Kc[:], pk[:, 0:128], [pk], [Kc])
                    pv = ps_r.get()
                    TR(pv[:, 0:128], vTt[:, blk], ident_f[:], [vTt, ident_f], [pv])
                    Vc = w_r.get()
                    CP("act", Vc[:], pv[:, 0:128], [pv], [Vc])
                    DG = w2_r.get()
                    TS("dve", DG[:, 0:128], ident_f[:], gc, ALU.mult, [ident_f, gbt], [DG])
                    TS("dve", DG[:, 128:256], ident_f[:], beta, ALU.mult, [ident_f, gbt], [DG])
                    pb = ps_r.get()
                    MM(pb[:, 0:256], ones_f[:], DG[:], True, True, [ones_f, DG], [pb])
                    GB = w2_r.get()
                    CP("act", GB[:], pb[:, 0:256], [pb], [GB])
                    dlt = w_r.get()
                    TS("dve", dlt[:], GB[:, 0:128], gc, ALU.subtract, [GB, gbt], [dlt])
                    ET = w_r.get()
                    TS("dve", ET[:], dlt[:], 0.0, ALU.min, [dlt], [ET])
                    ACT(ET[:], ET[:], AF.Exp, [ET], [ET])
                    E2 = w_r.get()
                    TS("dve", E2[:], dlt[:], -1.0, ALU.mult, [dlt], [E2], s2=0.0, op1=ALU.min)
                    ACT(E2[:], E2[:], AF.Exp, [E2], [E2])
                    EG = w_r.get()
                    ACT(EG[:], GB[:, 0:128], AF.Exp, [GB], [EG])
                    pkk = ps_r.get()
                    MM(pkk[:, 0:128], kTt[:, blk], kTt[:, blk], True, True, [kTt], [pkk])
                    Nj = w_r.get()
                    TT("dve", Nj[:], pkk[:, 0:128], GB[:, 128:256], ALU.mult, [pkk, GB], [Nj])
                    TT("dve", Nj[:], Nj[:], ET[:], ALU.mult, [Nj, ET], [Nj])
                    STT(Nj[:], Nj[:], -1.0, MUs[:], ALU.mult, ALU.mult, [Nj, MUs], [Nj])
                    Pj = w_r.get()
                    STT(Pj[:], pkk[:, 0:128], beta, E2[:], ALU.mult, ALU.mult, [pkk, gbt, E2], [Pj])
                    STT(Pj[:], Pj[:], -1.0, MLs[:], ALU.mult, ALU.mult, [Pj, MLs], [Pj])
                    cg = c_r.get()
                    ACT(cg[:, 0:1], gc, AF.Exp, [gbt], [cg])
                    TT("dve", cg[:, 1:2], cg[:, 0:1], beta, ALU.mult, [cg, gbt], [cg])
                    Rm = w2_r.get()
                    TS("dve", Rm[:, 0:128], Vc[:], beta, ALU.mult, [Vc, gbt], [Rm])
                    TS("dve", Rm[:, 128:256], Kc[:], cg[:, 1:2], ALU.mult, [Kc, cg], [Rm])
                    for j in range(7):
                        pr_ = ps_r.get()
                        MM(pr_[:, 0:256], Nj[:], Rm[:], True, True, [Nj, Rm], [pr_])
                        Rn = w2_r.get()
                        TT("dve", Rn[:], Rm[:], pr_[:, 0:256], ALU.add, [Rm, pr_], [Rn])
                        Rm = Rn
                        if j < 6:
                            pn = ps_r.get()
                            MM(pn[:, 0:128], Pj[:], Nj[:], True, True, [Pj, Nj], [pn])
                            pp = ps_r.get()
                            MM(pp[:, 0:128], Nj[:], Pj[:], True, True, [Nj, Pj], [pp])
                            Nn = w_r.get()
                            CP("act", Nn[:], pn[:, 0:128], [pn], [Nn])
                            Pn = w_r.get()
                            CP("act", Pn[:], pp[:, 0:128], [pp], [Pn])
                            Nj, Pj = Nn, Pn
                    pw = ps_r.get()
                    TR(pw[:, 0:128], Rm[:, 128:256], ident_f[:], [Rm, ident_f], [pw])
                    WT = w_r.get()
                    CP("act", WT[:], pw[:, 0:128], [pw], [WT])
                    Sc, Sn = St[cur], St[1 - cur]
                    pws = ps_r.get()
                    MM(pws[:, 0:128], WT[:], Sc[:], True, True, [WT, Sc], [pws])
                    Vn = w_r.get()
                    TT("dve", Vn[:], Rm[:, 0:128], pws[:, 0:128], ALU.subtract, [Rm, pws], [Vn])
                    pqk = ps_r.get()
                    MM(pqk[:, 0:128], kTt[:, blk], qTt[:, blk], True, True, [kTt, qTt], [pqk])
                    qki = w_r.get()
                    TT("dve", qki[:], pqk[:, 0:128], ET[:], ALU.mult, [pqk, ET], [qki])
                    STT(qki[:], qki[:], SC, MUi[:], ALU.mult, ALU.mult, [qki, MUi], [qki])
                    QdT = w_r.get()
                    STT(QdT[:], qTt[:, blk], SC, EG[:], ALU.mult, ALU.mult, [qTt, EG], [QdT])
                    po = pso_r.get()
                    MM(po[:, 0:128], QdT[:], Sc[:], True, False, [QdT, Sc], [po])
                    MM(po[:, 0:128], qki[:], Vn[:], False, True, [qki, Vn], [po])
                    TT("dve", cg[:, 2:3], GB[:, 127:128], gc, ALU.subtract, [GB, gbt], [cg])
                    ACT(cg[:, 3:4], cg[:, 2:3], AF.Exp, [cg], [cg])
                    Kd = w_r.get()
                    TS("dve", Kd[:], Kc[:], cg[:, 3:4], ALU.mult, [Kc, cg], [Kd])
                    psn = ps_r.get()
                    MM(psn[:, 0:128], Kd[:], Vn[:], True, True, [Kd, Vn], [psn])
                    STT(Sn[:], Sc[:], EG[:, 127:128], psn[:, 0:128], ALU.mult, ALU.add, [Sc, EG, psn], [Sn])
                    cur = 1 - cur
                    jk = w_r.get()
                    ACT(jk[:], po[:, 0:128], AF.Square, [po], [jk, cg], accum=cg[:, 4:5])
                    TS("dve", cg[:, 5:6], cg[:, 4:5], 1.0 / 128, ALU.mult, [cg], [cg], s2=RMS_EPS, op1=ALU.add)
                    ACT(cg[:, 6:7], cg[:, 5:6], AF.Sqrt, [cg], [cg])
                    RECIP(cg[:, 7:8], cg[:, 6:7], [cg], [cg])
                    STT(jk[:], po[:, 0:128], cg[:, 7:8], GNW[:], ALU.mult, ALU.mult, [po, cg, GNW], [jk])
                    bo = bo_r.get()
                    TT("dve", bo[:], jk[:], zt[:, b4, :], ALU.mult, [jk, zt], [bo])
                    S.dma("sp", OTOK[gbk * 128:(gbk + 1) * 128, 128:256], bo[:], OTOK, bo)


    phase_x0()
    xres = x_in
    for li, L in enumerate(cfg.LAYERS):
        is_last = li == len(cfg.LAYERS) - 1
        if cfg.mixer:
            if L % 2 == 0:
                even_mixer(L)
            else:
                odd_mixer(L)
        xnext = XR[li % 2]
        phase_ln_moe(L, xres, xnext, is_last)
        xres = xnext


def _even_mixer(S, cfg, L):
    raise NotImplementedError


def _odd_mixer(S, cfg, L):
    raise NotImplementedError


def t5_onehot():
    n = np.arange(2560) - 511
    nn = np.maximum(n, 0)
    nf = np.maximum(nn, 1).astype(np.float32)
    large = 16 + (np.log(nf / 16) / math.log(2048 / 16) * 16).astype(np.int32)
    large = np.minimum(large, 31)
    bucket = np.where(nn < 16, nn, large)
    oh = np.zeros((33, 2560), np.float32)
    valid = n >= 0
    oh[bucket[valid], np.nonzero(valid)[0]] = 1.0
    oh[32, ~valid] = NEG
    return oh


def prep_inputs(inp, cfg):
    SEQ, TOK, NEXP, EPC, NLW, WL0 = cfg.SEQ, cfg.TOK, cfg.NEXP, cfg.EPC, cfg.NLW, cfg.WL0
    f = lambda a: np.ascontiguousarray(np.asarray(a, dtype=np.float32))
    x = f(inp["x"])
    maps = []
    lng = f(np.concatenate([inp["ln_mix_g"], inp["ln_ffn_g"]], 0))
    lnb = f(np.concatenate([inp["ln_mix_b"], inp["ln_ffn_b"]], 0))
    if cfg.moe:
        bg = f(inp["moe_b_gate_up"]).reshape(4, NEXP, 16, 128)
        bgu = np.ascontiguousarray(bg.transpose(3, 0, 1, 2).reshape(128, 4 * NEXP * 16))
        bdn = f(inp["moe_b_down"]).reshape(4 * NEXP, D)
        wgu = np.asarray(inp["moe_w_gate_up"])
        wdn = np.asarray(inp["moe_w_down"])
    if cfg.mixer:
        ewi, ewo = f(inp["even_w_in"]), f(inp["even_w_out"])
        owi, owo = f(inp["odd_w_in"]), f(inp["odd_w_out"])
        cw = f(inp["gdn_conv_w"])
        oh = t5_onehot()
    for c in range(8):
        b, r = c // 4, c % 4
        m = {"x_in": np.ascontiguousarray(x[b, r * TOK:(r + 1) * TOK, :]), "lng": lng, "lnb": lnb}
        if cfg.moe:
            m["router_w"] = f(inp["router_w"])
            m["router_b"] = f(inp["router_b"])
            m["bgu"] = bgu
            m["bdn"] = bdn
            m["wgu_sh"] = np.ascontiguousarray(wgu[WL0:WL0 + NLW, c * EPC:(c + 1) * EPC], dtype=np.float32).reshape(NLW * EPC * D, 2 * D)
            m["wdn_sh"] = np.ascontiguousarray(wdn[WL0:WL0 + NLW, c * EPC:(c + 1) * EPC], dtype=np.float32).reshape(NLW * EPC * D, D)
        if cfg.mixer:
            h = r
            A = 512
            cols = np.concatenate([np.arange(h * 128, h * 128 + 128), A + np.arange(h * 128, h * 128 + 128),
                                   2 * A + np.arange(h * 128, h * 128 + 128),
                                   1536 + np.arange(h * 128, h * 128 + 128), 2048 + np.arange(h * 128, h * 128 + 128),
                                   2560 + np.arange(h * 128, h * 128 + 128), 3072 + np.arange(h * 128, h * 128 + 128),
                                   [3584 + h], [3588 + h]])
            m["we_in"] = np.ascontiguousarray(ewi[:, :, cols])
            perm = np.concatenate([np.concatenate([np.arange(s_ * 128, s_ * 128 + 128), 512 + np.arange(s_ * 128, s_ * 128 + 128)]) for s_ in range(4)])
            m["we_out"] = np.ascontiguousarray(ewo[:, perm, :])
            TC = min(2048, SEQ)
            gtok = r * TOK + np.arange(TOK)
            jj, tt_ = gtok // TC, gtok % TC
            oi = np.stack([(jj * 4 + s_) * TC + tt_ for s_ in range(4)], 1)
            m["oidx"] = np.ascontiguousarray(oi.reshape(TOK // 128, 128, 4).transpose(1, 0, 2).reshape(128, TOK // 32).astype(np.int32))
            ccols = np.concatenate([np.arange(h * 128, h * 128 + 128), 512 + np.arange(h * 128, h * 128 + 128), 1024 + np.arange(h * 128, h * 128 + 128)])
            m["convw"] = np.ascontiguousarray(cw[:, :, ccols].reshape(2, 4, 3, 128).transpose(0, 3, 2, 1).reshape(2, 128, 12))
            m["lamp"] = f(inp["diff_lambda"]).reshape(2, 256)
            m["subw"] = f(inp["diff_subln_w"])
            m["gnw"] = f(inp["gdn_norm_w"])
            m["alog"] = np.ascontiguousarray(f(inp["gdn_a_log"])[:, h:h + 1])
            m["dtb"] = np.ascontiguousarray(f(inp["gdn_dt_bias"])[:, h:h + 1])
            m["t5col"] = np.ascontiguousarray(f(inp["t5_bias"])[:, h:h + 1])
            m["t5oh"] = oh
            oc = []
            for hh in range(4 * r, 4 * r + 4):
                oc += [np.arange(hh * 64, hh * 64 + 64), 1024 + np.arange(hh * 64, hh * 64 + 64),
                       2048 + np.arange(hh * 64, hh * 64 + 64), [4096 + hh], 3072 + np.arange(hh * 64, hh * 64 + 64)]
            oc = np.concatenate(oc)
            m["wo_in"] = np.ascontiguousarray(owi[:, :, oc])
            m["wo_out"] = owo
            m["qkw"] = np.ascontiguousarray(f(inp["fox_qk_norm_w"]).transpose(0, 2, 1))
            m["fb"] = np.ascontiguousarray(f(inp["fox_forget_b"])[:, 4 * r:4 * r + 4])
        maps.append(m)
    return maps


_CACHE = {}


def run_cfg(inp, cfg):
    key = (cfg.SEQ, cfg.NEXP, cfg.CAP, cfg.LAYERS, cfg.NLW, cfg.WL0, cfg.mixer, cfg.moe)
    if key not in _CACHE:
        _CACHE[key] = build(cfg)
    nc = _CACHE[key]
    maps = prep_inputs(inp, cfg)
    has_even = cfg.mixer and any(L % 2 == 0 for L in cfg.LAYERS)
    has_odd = cfg.mixer and any(L % 2 == 1 for L in cfg.LAYERS)
    ev = ("we_in", "we_out", "convw", "lamp", "subw", "gnw", "alog", "dtb", "t5col", "t5oh")
    od = ("wo_in", "wo_out", "qkw", "fb")
    for m in maps:
        for k in list(m):
            if (k in ev and not has_even) or (k in od and not has_odd):
                del m[k]
    res = run_bass_kernel_spmd(nc, maps, core_ids=list(range(8)))
    outs = [res.results[c]["out"] for c in range(8)]
    TOK = cfg.TOK
    full = np.zeros((2, cfg.SEQ, D), np.float32)
    for c in range(8):
        full[c // 4, (c % 4) * TOK:(c % 4 + 1) * TOK, :] = outs[c]
    return full


def kernel(**inputs):
    return run_cfg(inputs, Cfg())
```

```python
import math
from contextlib import ExitStack
import numpy as np
import concourse.bass as bass
import concourse.mybir as mybir
from concourse.bass_utils import run_bass_kernel_spmd

F32 = mybir.dt.float32
BF16 = mybir.dt.bfloat16
I32 = mybir.dt.int32
U32 = mybir.dt.uint32
AF = mybir.ActivationFunctionType
ALU = mybir.AluOpType
AX = mybir.AxisListType

COMPUTE = ("pe", "act", "dve", "pool")


class Buf:
    def __init__(self, name, handle, is_dram=False):
        self.name = name
        self.h = handle
        self.is_dram = is_dram
        self.w = {}
        self.r = {}
        self.dsem = None
        self.dcnt = 0

    def __getitem__(self, idx):
        return self.h.ap()[idx] if self.is_dram else self.h[idx]

    def ap(self):
        return self.h.ap() if self.is_dram else self.h[:]


class Sched:
    def __init__(self, nc, es):
        self.nc = nc
        self.es = es
        self.streams = {k: [] for k in ("pe", "act", "dve", "pool", "sp")}
        self.sems = {}
        self.latest = {}
        self.cnt = {k: 0 for k in COMPUTE}
        self.waited = {k: {} for k in self.streams}
        self.nbuf = 0
        self.es_local = es
        self.dq = {}
        self.dq_i = {}
        self.all_bufs = []
        import os
        self.sw_thresh = int(os.environ.get("KSW", "30000"))
        for k in COMPUTE:
            self._sem("E_" + k)

    def _sem(self, key):
        if key not in self.sems:
            self.sems[key] = self.es.enter_context(self.nc.semaphore("s%d_%s" % (len(self.sems), key[:12])))
            self.latest[key] = 0
        return self.sems[key]

    def sbuf(self, name, shape, dt):
        self.nbuf += 1
        h = self.es_local.enter_context(self.nc.sbuf_tensor("%s_%d" % (name, self.nbuf), list(shape), dt))
        b = Buf(name, h)
        b.local = self.es_local is not self.es
        self.all_bufs.append(b)
        return b

    def psum(self, name, shape, dt=F32):
        self.nbuf += 1
        h = self.es_local.enter_context(self.nc.psum_tensor("%s_%d" % (name, self.nbuf), list(shape), dt))
        b = Buf(name, h)
        b.local = self.es_local is not self.es
        self.all_bufs.append(b)
        return b

    def ring(self, name, shape, dt, n, psum=False):
        return Ring([(self.psum if psum else self.sbuf)("%s%d" % (name, i), shape, dt) for i in range(n)])

    def _dq(self, kind):
        if kind not in self.dq:
            n = {"hw": 12, "sw": 8, "cc": 1}[kind]
            self.dq[kind] = ["D_%s_%d" % (kind, i) for i in range(n)]
            for k in self.dq[kind]:
                self._sem(k)
            self.dq_i[kind] = 0
        key = self.dq[kind][self.dq_i[kind] % len(self.dq[kind])]
        self.dq_i[kind] += 1
        return key

    def phase(self):
        return _Phase(self)

    def dram(self, name, shape, dt, kind=None):
        if kind is None:
            h = self.nc.dram_tensor(name, list(shape), dt)
        else:
            h = self.nc.dram_tensor(name, list(shape), dt, kind=kind)
        b = Buf(name, h, is_dram=True)
        self.all_bufs.append(b)
        return b

    def _need(self, eng, reads, writes, mykey, prev=None):
        need = {}

        def add(k, v):
            if k == mykey == "E_pe":
                return
            if need.get(k, 0) < v:
                need[k] = v
        if prev is not None and prev[1] > 0:
            add(*prev)
        for b in reads:
            for k, v in b.w.items():
                add(k, v)
        for b in writes:
            for k, v in b.w.items():
                add(k, v)
            for k, v in b.r.items():
                add(k, v)
        out = []
        wd = self.waited[eng]
        for k, v in need.items():
            if wd.get(k, 0) < v:
                wd[k] = v
                out.append((self.sems[k], v))
        return out

    def op(self, eng, fn, reads=(), writes=()):
        key = "E_" + eng
        waits = self._need(eng, reads, writes, key)
        self.cnt[eng] += 1
        val = self.cnt[eng]
        self.latest[key] = val
        sem = self.sems[key]
        st = self.streams[eng]
        for s, v in waits:
            st.append(lambda e, s=s, v=v: e.wait_ge(s, v))
        st.append(lambda e, fn=fn, sem=sem: fn(e).then_inc(sem, 1))
        for b in reads:
            if b.r.get(key, 0) < val:
                b.r[key] = val
        for b in writes:
            b.w = {key: val}
            b.r = {}

    def dma(self, q, out_ap, in_ap, dst, src, fn=None, extra=(), **kw):
        key = self._dq("sw" if q == "pool" else "hw")
        wr = [dst] if (dst.r or not all(k.startswith("D_") for k in dst.w)) else []
        waits = self._need(q, ([src] if src is not None else []) + list(extra), wr, key, prev=(key, self.latest[key]))
        val = self.latest[key] + 16
        self.latest[key] = val
        sem = self.sems[key]
        st = self.streams[q]
        for s, v in waits:
            st.append(lambda e, s=s, v=v: e.wait_ge(s, v))
        if fn is None:
            st.append(lambda e: e.dma_start(out=out_ap, in_=in_ap, **kw).then_inc(sem, 16))
        else:
            st.append(lambda e: fn(e).then_inc(sem, 16))
        for b in ([src] if src is not None else []) + list(extra):
            if b.r.get(key, 0) < val:
                b.r[key] = val
        if wr:
            dst.w = {key: val}
        else:
            dst.w[key] = val
        dst.r = {}

    def cc(self, kind, op, groups, src, dst, src_ap=None, dst_ap=None):
        sap = src.h.ap().opt() if src_ap is None else src_ap.opt()
        dap = dst.h.ap().opt() if dst_ap is None else dst_ap.opt()
        key = self._dq("cc")
        waits = self._need("pool", [src], [dst], key)
        val = self.latest[key] + 1
        self.latest[key] = val
        sem = self.sems[key]
        st = self.streams["pool"]
        for s, v in waits:
            st.append(lambda e, s=s, v=v: e.wait_ge(s, v))
        st.append(lambda e: e.collective_compute(kind, op, replica_groups=groups,
                                                 ins=[sap], outs=[dap]).then_inc(sem, 1))
        st.append(lambda e: e.wait_ge(sem, val))
        self.waited["pool"][key] = val
        src.r[key] = val
        dst.w = {key: val}
        dst.r = {}

    def barrier(self):
        for eng, st in self.streams.items():
            wd = self.waited[eng]
            for k, v in self.latest.items():
                if v > 0 and wd.get(k, 0) < v:
                    wd[k] = v
                    st.append(lambda e, s=self.sems[k], v=v: e.wait_ge(s, v))

    def emit(self):
        self.barrier()
        for eng in COMPUTE:
            key = "E_" + eng
            if self.cnt[eng] > self.sw_thresh:
                self.nsw = getattr(self, "nsw", 0) + 1
                self.sems[key] = self.es.enter_context(self.nc.semaphore("sw%d_%s" % (self.nsw, eng)))
                self.cnt[eng] = 0
                self.latest[key] = 0
                for wd in self.waited.values():
                    wd.pop(key, None)
                for b in self.all_bufs:
                    b.w.pop(key, None)
                    b.r.pop(key, None)
        streams = self.streams
        self.streams = {k: [] for k in streams}
        self.ninst = getattr(self, "ninst", 0) + sum(len(v) for v in streams.values())
        import os
        if os.environ.get("KCOUNT"):
            return
        self._emit(streams)

    def _emit(self, streams):
        class _S:
            pass
        self_ = _S()
        self_.streams = streams
        nc = self.nc
        self = self_
        with nc.Block() as block:
            @block.tensor
            def _(e):
                for f in self.streams["pe"]:
                    f(e)

            @block.scalar
            def _(e):
                for f in self.streams["act"]:
                    f(e)

            @block.vector
            def _(e):
                for f in self.streams["dve"]:
                    f(e)

            @block.gpsimd
            def _(e):
                for f in self.streams["pool"]:
                    f(e)

            @block.sync
            def _(e):
                for f in self.streams["sp"]:
                    f(e)


class Ring:
    def __init__(self, bufs):
        self.bufs = bufs
        self.i = 0

    def get(self):
        b = self.bufs[self.i % len(self.bufs)]
        self.i += 1
        return b


class _Phase:
    def __init__(self, S):
        self.S = S

    def __enter__(self):
        self.old = self.S.es_local
        self.stack = ExitStack()
        self.stack.__enter__()
        self.S.es_local = self.stack
        return self.S

    def __exit__(self, *a):
        S = self.S
        stop = False
        if a[0] is None:
            S.emit()

            S.nphase = getattr(S, "nphase", 0) + 1
            import os
            stop = S.nphase == int(os.environ.get("KSTOP", "0"))
        S.es_local = self.old
        self.stack.__exit__(*a)
        if stop:
            raise _StopBuild()
        return False


class _StopBuild(Exception):
    pass


D = 1024
ALPHA = (2 * 4) ** 0.25
LN_EPS = 1e-5
RMS_EPS = 1e-6
G4 = [[0, 1, 2, 3], [4, 5, 6, 7]]
G2 = [[0, 4], [1, 5], [2, 6], [3, 7]]
NEG = -30000.0


class Cfg:
    def __init__(self, SEQ=16384, NEXP=32, CAP=640, LAYERS=(0, 1, 2, 3), NLW=4, WL0=0, mixer=True, moe=True):
        self.SEQ, self.NEXP, self.CAP, self.LAYERS, self.NLW = SEQ, NEXP, CAP, tuple(LAYERS), NLW
        self.mixer, self.moe, self.WL0 = mixer, moe, WL0
        self.TOK = SEQ // 4
        self.EPC = NEXP // 8


def build(cfg):
    nc = bass.Bass("TRN2", target_bir_lowering=False)
    es = ExitStack()
    with es:
        S = Sched(nc, es)
        try:
            _build(S, cfg)
        except _StopBuild:
            pass
        print("kernel build: instructions incl. waits =", getattr(S, "ninst", 0), flush=True)
    return nc


def _build(S, cfg):
    SEQ, TOK, NEXP, C, EPC, NLW = cfg.SEQ, cfg.TOK, cfg.NEXP, cfg.CAP, cfg.EPC, cfg.NLW
    NBL = TOK // 128
    NTL = TOK // 512
    NBF = SEQ // 128
    NQT = SEQ // 512
    CB = C // 128
    TRASH = NEXP * C

    def MM(out, lhsT, rhs, start, stop, R, W):
        S.op("pe", lambda e: e.matmul(out, lhsT, rhs, start=start, stop=stop), reads=R, writes=W)

    def TR(out, in_, ident, R, W):
        S.op("pe", lambda e: e.transpose(out, in_, ident), reads=R, writes=W)

    def ACT(out, in_, func, R, W, bias=None, scale=None, accum=None):
        kw = {}
        if bias is not None:
            kw["bias"] = bias
        if scale is not None:
            kw["scale"] = scale
        if accum is not None:
            kw["accum_out"] = accum
        S.op("act", lambda e: e.activation(out=out, in_=in_, func=func, **kw), reads=R, writes=W)

    def TS(eng, out, in0, s1, op0, R, W, s2=None, op1=None, accum=None):
        kw = {}
        if op1 is not None:
            kw["op1"] = op1
        if accum is not None:
            kw["accum_out"] = accum
        S.op(eng, lambda e: e.tensor_scalar(out=out, in0=in0, scalar1=s1, scalar2=s2, op0=op0, **kw), reads=R, writes=W)

    def TT(eng, out, in0, in1, op, R, W):
        S.op(eng, lambda e: e.tensor_tensor(out=out, in0=in0, in1=in1, op=op), reads=R, writes=W)

    def STT(out, in0, scalar, in1, op0, op1, R, W, accum=None):
        kw = {}
        if accum is not None:
            kw["accum_out"] = accum
        S.op("dve", lambda e: e.scalar_tensor_tensor(out=out, in0=in0, scalar=scalar, in1=in1, op0=op0, op1=op1, **kw),
             reads=R, writes=W)

    def CP(eng, out, in_, R, W):
        if eng == "act":
            S.op("act", lambda e: e.copy(out=out, in_=in_), reads=R, writes=W)
        else:
            S.op(eng, lambda e: e.tensor_copy(out=out, in_=in_), reads=R, writes=W)

    def MEMSET(eng, ap, val, W):
        S.op(eng, lambda e: e.memset(ap, val), writes=W)

    def RECIP(out, in_, R, W):
        S.op("dve", lambda e: e.reciprocal(out=out, in_=in_), reads=R, writes=W)

    def ASEL(out, in_, pattern, cmp, fill, base, cm, R, W):
        S.op("pool", lambda e: e.affine_select(out=out, in_=in_, pattern=pattern, compare_op=cmp, fill=fill,
                                                base=base, channel_multiplier=cm), reads=R, writes=W)

    def din(name, shape, dt=F32):
        return S.dram(name, shape, dt, kind="ExternalInput")

    x_in = din("x_in", [TOK, D])
    lng = din("lng", [8, D])
    lnb = din("lnb", [8, D])
    out = S.dram("out", [TOK, D], F32, kind="ExternalOutput")
    if cfg.moe:
        router_w = din("router_w", [4, D, NEXP])
        router_b = din("router_b", [4, NEXP])
        bgu = din("bgu", [128, 4 * NEXP * 16])
        bdn = din("bdn", [4 * NEXP, D])
        wgu_sh = din("wgu_sh", [NLW * EPC * D, 2 * D])
        wdn_sh = din("wdn_sh", [NLW * EPC * D, D])
    has_even = cfg.mixer and any(L % 2 == 0 for L in cfg.LAYERS)
    has_odd = cfg.mixer and any(L % 2 == 1 for L in cfg.LAYERS)
    if has_even:
        we_in = din("we_in", [2, D, 898])
        we_out = din("we_out", [2, D, D])
        convw = din("convw", [2, 128, 12])
        lamp = din("lamp", [2, 256])
        subw = din("subw", [2, 128])
        gnw = din("gnw", [2, 128])
        alog = din("alog", [2, 1])
        dtb = din("dtb", [2, 1])
        t5col = din("t5col", [32, 1])
        t5oh = din("t5oh", [33, 2560])
    if has_odd:
        wo_in = din("wo_in", [2, D, 1028])
        wo_out = din("wo_out", [2, D, D])
        qkw = din("qkw", [2, 64, 2])
        fb = din("fb", [2, 4])

    XR = [S.dram("xres0", [TOK, D], F32), S.dram("xres1", [TOK, D], F32)]
    XR1 = S.dram("xr1", [TOK, D], F32)
    XTB = S.dram("xtb", [D, TOK], BF16)
    XTALL = S.dram("xtall", [4 * D, TOK], BF16)
    TC = min(2048, SEQ)
    if cfg.mixer:
        OTOK = S.dram("otok", [SEQ, 256], BF16)
        OALL = S.dram("oall", [4 * SEQ, 256], BF16)
        oidx = din("oidx", [128, NBL * 4], I32)
        CQ = S.dram("cq", [3, SEQ], BF16)
        O0S = S.dram("o0s", [SEQ, 128], F32)
        BV = S.dram("bv", [2560], F32)
        SKW = S.dram("skw", [16, 128 * 640], F32)
    if cfg.moe:
        XDISP = S.dram("xdisp", [NEXP * C + 128, D], BF16)
        YDISP = S.dram("ydisp", [NEXP * C + 128, D], F32)
        R1 = NLW * EPC * D
        WB1G = S.dram("wb1g", [R1, 2 * D], BF16)
        WB1D = S.dram("wb1d", [R1, D], BF16)
        RGG, RGD = 256, 512
        NCG, NCD = R1 // RGG, R1 // RGD
        WALLG = [S.dram("wallg%d" % i, [16 * 8 * RGG, 2 * D], BF16) for i in range((NCG + 15) // 16)]
        WALLD = [S.dram("walld%d" % i, [16 * 8 * RGD, D], BF16) for i in range((NCD + 15) // 16)]
        S1G = [S.dram("s1g%d" % i, [2 * RGG, 2 * D], BF16) for i in range(2)]
        S1D = [S.dram("s1d%d" % i, [2 * RGD, D], BF16) for i in range(2)]

    ident_f = S.sbuf("ident_f", [128, 128], F32)
    ident_b = S.sbuf("ident_b", [128, 128], BF16)
    ones_f = S.sbuf("ones_f", [128, 128], F32)
    ones_b = S.sbuf("ones_b", [128, 128], BF16)
    triU_f = S.sbuf("triU_f", [128, 128], F32)
    Ls_b = S.sbuf("Ls_b", [128, 128], BF16)
    DESTI = S.sbuf("DESTI", [128, NBL * 4], I32)
    GATE = S.sbuf("GATE", [128, NBL * 4], F32)
    CNT = S.sbuf("CNT", [128, NEXP], F32)
    EOFF = S.sbuf("EOFF", [128, NEXP], F32)
    with S.phase():
        tmpf = S.sbuf("tmpf", [128, 128], F32)
        tmpi = S.sbuf("tmpi", [128, NEXP], I32)
        MEMSET("pool", ident_f[:], 0.0, [ident_f])
        ASEL(ident_f[:], ident_f[:], [[-1, 128]], ALU.not_equal, 1.0, 0, 1, [ident_f], [ident_f])
        CP("dve", ident_b[:], ident_f[:], [ident_f], [ident_b])
        MEMSET("pool", ones_f[:], 1.0, [ones_f])
        MEMSET("pool", ones_b[:], 1.0, [ones_b])
        MEMSET("pool", triU_f[:], 1.0, [triU_f])
        ASEL(triU_f[:], triU_f[:], [[1, 128]], ALU.is_ge, 0.0, 0, -1, [triU_f], [triU_f])
        MEMSET("pool", tmpf[:], 1.0, [tmpf])
        ASEL(tmpf[:], tmpf[:], [[1, 128]], ALU.is_ge, 0.0, -1, -1, [tmpf], [tmpf])
        CP("dve", Ls_b[:], tmpf[:], [tmpf], [Ls_b])
        S.op("pool", lambda e: e.iota(tmpi[:], pattern=[[C, NEXP]], base=0, channel_multiplier=0), writes=[tmpi])
        CP("dve", EOFF[:], tmpi[:], [tmpi], [EOFF])

    if cfg.moe:
        with S.phase():
            st_r = S.ring("wst", [128, 2 * D], F32, 3)
            sb_r = S.ring("wsb", [128, 2 * D], BF16, 3)
            n = 0
            for (src, b1, ncol) in ((wgu_sh, WB1G, 2 * D), (wdn_sh, WB1D, D)):
                rows = 128 * (2 * D // ncol)
                for r0 in range(0, R1, rows):
                    st = st_r.get()
                    sb = sb_r.get()
                    k = rows // 128
                    S.dma("sp", st[:].rearrange("p (k n) -> p k n", k=k), src[r0:r0 + rows, :].rearrange("(k p) n -> p k n", p=128), st, src)
                    CP("act" if n % 2 == 0 else "dve", sb[:], st[:], [st], [sb])
                    S.dma("sp", b1[r0:r0 + rows, :].rearrange("(k p) n -> p k n", p=128), sb[:].rearrange("p (k n) -> p k n", k=k), b1, sb)
                    n += 1
            zt = S.sbuf("zt", [128, D], F32)
            ztb = S.sbuf("ztb", [128, D], BF16)
            MEMSET("dve", zt[:], 0.0, [zt])
            MEMSET("dve", ztb[:], 0.0, [ztb])
            for r0 in range(0, NEXP * C + 128, 128):
                S.dma("sp", XDISP[r0:r0 + 128, :], ztb[:], XDISP, ztb)
                S.dma("sp", YDISP[r0:r0 + 128, :], zt[:], YDISP, zt)

    def w_collectives():
        for (b1, s1s, walls, RG, NC_) in ((WB1G, S1G, WALLG, RGG, NCG), (WB1D, S1D, WALLD, RGD, NCD)):
            for i in range(NC_):
                s1 = s1s[i % 2]
                S.cc("AllGather", ALU.bypass, G2, b1, s1, src_ap=b1[i * RG:(i + 1) * RG, :])
                wt = walls[i // 16]
                for g in range(2):
                    base = (((i % 16) * 2 + g) * 4) * RG
                    S.cc("AllGather", ALU.bypass, G4, s1, wt, src_ap=s1[g * RG:(g + 1) * RG, :],
                         dst_ap=wt[base:base + 4 * RG, :])

    S.w_pending = cfg.moe

    def wloc(L, e, RG, chunk):
        c, le = e // EPC, e % EPC
        g, r4 = c // 4, c % 4
        rr = ((L - cfg.WL0) * EPC + le) * D + chunk * RG
        i = rr // RG
        return i // 16, ((((i % 16) * 2 + g) * 4 + r4) * RG)

    XTBv = XTB.ap().rearrange("(k p) t -> p k t", p=128)

    def emit_xT_block(xt_f32, xT_tile, col, ps_ring, cp_eng):
        ps = ps_ring.get()
        for kc in range(8):
            TR(ps[:, kc * 128:(kc + 1) * 128], xt_f32[:, kc * 128:(kc + 1) * 128], ident_f[:], [xt_f32, ident_f], [ps])
        CP(cp_eng, xT_tile[:, :, col:col + 128], ps[:].rearrange("p (k t) -> p k t", k=8), [ps], [xT_tile])

    def xt_allgather():
        for kc in range(8):
            S.cc("AllGather", ALU.bypass, G4, XTB, XTALL, src_ap=XTB[kc * 128:(kc + 1) * 128, :],
                 dst_ap=XTALL[kc * 512:(kc + 1) * 512, :])

    def phase_x0():
        with S.phase():
            xin_r = S.ring("xin", [128, D], F32, 3)
            xT_r = S.ring("xT", [128, 8, 512], BF16, 2)
            ps_r = S.ring("psx", [128, D], F32, 2, psum=True)
            for tl in range(NTL):
                xT = xT_r.get()
                for b4 in range(4):
                    b = tl * 4 + b4
                    xt = xin_r.get()
                    S.dma("sp", xt[:], x_in[b * 128:(b + 1) * 128, :], xt, x_in)
                    emit_xT_block(xt, xT, b4 * 128, ps_r, "act" if b4 % 2 == 0 else "dve")
                S.dma("sp", XTBv[:, :, tl * 512:(tl + 1) * 512], xT[:], XTB, xT)
            xt_allgather()

    def layer_norm(z, outt, gt, bt, st_r, junk):
        st = st_r.get()
        ACT(junk[:], z[:], AF.Identity, [z], [junk, st], accum=st[:, 0:1])
        ACT(junk[:], z[:], AF.Square, [z], [junk, st], accum=st[:, 1:2])
        TS("dve", st[:, 2:3], st[:, 0:1], 1.0 / D, ALU.mult, [st], [st])
        TT("dve", st[:, 3:4], st[:, 2:3], st[:, 2:3], ALU.mult, [st], [st])
        STT(st[:, 4:5], st[:, 1:2], 1.0 / D, st[:, 3:4], ALU.mult, ALU.subtract, [st], [st])
        TS("dve", st[:, 4:5], st[:, 4:5], LN_EPS, ALU.add, [st], [st])
        ACT(st[:, 5:6], st[:, 4:5], AF.Sqrt, [st], [st])
        RECIP(st[:, 6:7], st[:, 5:6], [st], [st])
        TS("dve", outt[:], z[:], st[:, 2:3], ALU.subtract, [z, st], [outt], s2=st[:, 6:7], op1=ALU.mult)
        TT("dve", outt[:], outt[:], gt[:], ALU.mult, [outt, gt], [outt])
        TT("dve", outt[:], outt[:], bt[:], ALU.add, [outt, bt], [outt])

    def phase_ln_moe(L, xres, xnext, is_last):
        with S.phase():
            gt = S.sbuf("gt", [128, D], F32)
            bt = S.sbuf("bt", [128, D], F32)
            S.dma("sp", gt[:], lng.ap()[L, :].partition_broadcast(128), gt, lng)
            S.dma("sp", bt[:], lnb.ap()[L, :].partition_broadcast(128), bt, lnb)
            xr_r = S.ring("xr", [128, D], F32, 2)
            if cfg.mixer:
                wsrc = we_out if L % 2 == 0 else wo_out
                WO = S.sbuf("WO", [128, 8, D], BF16)
                for q4 in range(4):
                    S.dma("pool", WO[:, 2 * q4:2 * q4 + 2, :],
                          wsrc.ap()[L // 2, q4 * 256:(q4 + 1) * 256, :].rearrange("(k p) n -> p k n", p=128), WO, wsrc)
                OIDX = S.sbuf("OIDX", [128, NBL * 4], I32)
                S.dma("sp", OIDX[:], oidx[:, :], OIDX, oidx)
                og_r = S.ring("og", [128, 4, 256], BF16, 2)
                oT_r = S.ring("oT", [128, 8, 128], BF16, 2)
                pso_r = S.ring("pso", [128, D], BF16, 1, psum=True)
                psh_r = S.ring("psh", [128, 512], F32, 2, psum=True)
            z_r = S.ring("z", [128, D], F32, 2)
            x1_r = S.ring("x1", [128, D], F32, 2)
            x1b_r = S.ring("x1b", [128, D], BF16, 2)
            junk = S.sbuf("junk", [128, D], F32)
            st_r = S.ring("st", [128, 8], F32, 2)
            if cfg.moe:
                RW = S.sbuf("RW", [128, 8, NEXP], F32)
                RB = S.sbuf("RB", [128, NEXP], F32)
                S.dma("sp", RW[:], router_w.ap()[L].rearrange("(k p) e -> p k e", p=128), RW, router_w)
                S.dma("sp", RB[:], router_b.ap()[L, :].partition_broadcast(128), RB, router_b)
                x1T_r = S.ring("x1T", [128, 8, 128], F32, 2)
                psx_r = S.ring("psx", [128, D], F32, 1, psum=True)
                psl_r = S.ring("psl", [128, 512], F32, 1, psum=True)
                psp_r = S.ring("psp", [128, 512], F32, 2, psum=True)
                sm_r = S.ring("sm", [128, 12, NEXP], F32, 2)
                mb_r = S.ring("mb", [128, NEXP], BF16, 2)
                t8_r = S.ring("t8", [128, 8], F32, 2)
                sc_r = S.ring("sc", [128, 16], F32, 2)
                MEMSET("dve", CNT[:], 0.0, [CNT])
            for b in range(NBL):
                xr = xr_r.get()
                S.dma("sp", xr[:], xres[b * 128:(b + 1) * 128, :], xr, xres)
                z = z_r.get()
                if cfg.mixer:
                    og = og_r.get()
                    for s4 in range(4):
                        col = b * 4 + s4
                        S.dma("pool", None, None, og, OALL, extra=[OIDX],
                              fn=lambda e, col=col, og=og, s4=s4: e.indirect_dma_start(
                                  out=og[:, s4, :], out_offset=None, in_=OALL.ap()[:, :],
                                  in_offset=bass.IndirectOffsetOnAxis(ap=OIDX[:, col:col + 1], axis=0)))
                    pso = pso_r.get()
                    for kc in range(8):
                        TR(pso[:, kc * 128:(kc + 1) * 128], og[:, kc // 2, (kc % 2) * 128:(kc % 2 + 1) * 128], ident_b[:], [og, ident_b], [pso])
                    oT = oT_r.get()
                    CP("act", oT[:], pso[:].rearrange("p (k t) -> p k t", k=8), [pso], [oT])
                    for half in range(2):
                        psh = psh_r.get()
                        for kc in range(8):
                            MM(psh[:], oT[:, kc, :], WO[:, kc, half * 512:(half + 1) * 512], kc == 0, kc == 7, [oT, WO], [psh])
                        STT(z[:, half * 512:(half + 1) * 512], xr[:, half * 512:(half + 1) * 512], ALPHA, psh[:], ALU.mult, ALU.add, [xr, psh], [z])
                else:
                    TS("dve", z[:], xr[:], ALPHA, ALU.mult, [xr], [z])
                x1 = x1_r.get()
                layer_norm(z, x1, gt, bt, st_r, junk)
                S.dma("sp", XR1[b * 128:(b + 1) * 128, :], x1[:], XR1, x1)
                if not cfg.moe:
                    continue
                x1b = x1b_r.get()
                CP("act", x1b[:], x1[:], [x1], [x1b])
                x1T = x1T_r.get()
                ps = psx_r.get()
                for kc in range(8):
                    TR(ps[:, kc * 128:(kc + 1) * 128], x1[:, kc * 128:(kc + 1) * 128], ident_f[:], [x1, ident_f], [ps])
                CP("act", x1T[:], ps[:].rearrange("p (k t) -> p k t", k=8), [ps], [x1T])
                psl = psl_r.get()
                for kc in range(8):
                    MM(psl[:, 0:NEXP], x1T[:, kc, :], RW[:, kc, :], kc == 0, kc == 7, [x1T, RW], [psl])
                sm = sm_r.get()
                lg, ex, mk, gd, gates, pos, dfull, oh, jk = (sm[:, i, :] for i in range(9))
                t8 = t8_r.get()
                sc = sc_r.get()
                TT("dve", lg, psl[:, 0:NEXP], RB[:], ALU.add, [psl, RB], [sm])
                S.op("dve", lambda e, t8=t8, lg=lg: e.max(out=t8[:], in_=lg), reads=[sm], writes=[t8])
                TS("dve", sc[:, 0:1], t8[:, 0:1], -1.0, ALU.mult, [t8], [sc])
                ACT(ex, lg, AF.Exp, [sm, sc], [sm], bias=sc[:, 0:1])
                TS("dve", mk, lg, t8[:, 3:4], ALU.is_ge, [sm, t8], [sm])
                TT("dve", gd, ex, mk, ALU.mult, [sm], [sm])
                S.op("dve", lambda e, sc=sc, gd=gd: e.tensor_reduce(out=sc[:, 1:2], in_=gd, axis=AX.X, op=ALU.add),
                     reads=[sm], writes=[sc])
                RECIP(sc[:, 2:3], sc[:, 1:2], [sc], [sc])
                TS("dve", gates, gd, sc[:, 2:3], ALU.mult, [sm, sc], [sm])
                mb = mb_r.get()
                CP("dve", mb[:], mk, [sm], [mb])
                psp = psp_r.get()
                MM(psp[:, 0:NEXP], Ls_b[:], mb[:], True, True, [Ls_b, mb], [psp])
                TT("dve", pos, psp[:, 0:NEXP], CNT[:], ALU.add, [psp, CNT], [sm])
                psc = psp_r.get()
                MM(psc[:, 0:NEXP], ones_b[:], mb[:], True, True, [ones_b, mb], [psc])
                TT("dve", CNT[:], CNT[:], psc[:, 0:NEXP], ALU.add, [CNT, psc], [CNT])
                TT("dve", dfull, pos, EOFF[:], ALU.add, [sm, EOFF], [sm])
                for k in range(4):
                    TS("dve", oh, lg, t8[:, k:k + 1], ALU.is_equal, [sm, t8], [sm])
                    TT("dve", jk, oh, pos, ALU.mult, [sm], [sm])
                    S.op("dve", lambda e, sc=sc, jk=jk: e.tensor_reduce(out=sc[:, 4:5], in_=jk, axis=AX.X, op=ALU.add),
                         reads=[sm], writes=[sc])
                    TT("dve", jk, oh, dfull, ALU.mult, [sm], [sm])
                    S.op("dve", lambda e, sc=sc, jk=jk: e.tensor_reduce(out=sc[:, 5:6], in_=jk, axis=AX.X, op=ALU.add),
                         reads=[sm], writes=[sc])
                    TT("dve", jk, oh, gates, ALU.mult, [sm], [sm])
                    S.op("dve", lambda e, sc=sc, jk=jk: e.tensor_reduce(out=sc[:, 6:7], in_=jk, axis=AX.X, op=ALU.add),
                         reads=[sm], writes=[sc])
                    TS("dve", sc[:, 7:8], sc[:, 4:5], float(C), ALU.is_lt, [sc], [sc])
                    TS("dve", sc[:, 8:9], sc[:, 5:6], float(TRASH), ALU.subtract, [sc], [sc])
                    TT("dve", sc[:, 8:9], sc[:, 8:9], sc[:, 7:8], ALU.mult, [sc], [sc])
                    TS("dve", sc[:, 8:9], sc[:, 8:9], float(TRASH), ALU.add, [sc], [sc])
                    col = b * 4 + k
                    CP("dve", DESTI[:, col:col + 1], sc[:, 8:9], [sc], [DESTI])
                    TT("dve", GATE[:, col:col + 1], sc[:, 6:7], sc[:, 7:8], ALU.mult, [sc], [GATE])
                    S.dma("pool", None, None, XDISP, x1b, extra=[DESTI],
                          fn=lambda e, col=col, x1b=x1b: e.indirect_dma_start(
                              out=XDISP.ap()[:, :], out_offset=bass.IndirectOffsetOnAxis(ap=DESTI[:, col:col + 1], axis=0),
                              in_=x1b[:, :], in_offset=None))
        if cfg.moe:
            with S.phase():
                BG = S.sbuf("BG", [128, NEXP * 16], F32)
                S.dma("sp", BG[:], bgu[:, L * NEXP * 16:(L + 1) * NEXP * 16], BG, bgu)
                wgu_r = S.ring("wgu", [128, 8, 2 * D], BF16, 2)
                wdn_r = S.ring("wdn", [128, 8, D], BF16, 2)
                bd_r = S.ring("bd", [128, D], F32, 2)
                xs_r = S.ring("xs", [128, CB, D], BF16, 1)
                xsT_r = S.ring("xsT", [128, 8, C], BF16, 1)
                aT_r = S.ring("aT", [128, 8, C], BF16, 1)
                tmp_r = S.ring("tmp", [128, 5, 512], F32, 2)
                ys_r = S.ring("ys", [128, D], F32, 2)
                pst_r = S.ring("pst", [128, D], BF16, 2, psum=True)
                psg_r = S.ring("psg", [128, 512], F32, 2, psum=True)
                psu_r = S.ring("psu", [128, 512], F32, 2, psum=True)
                psd_r = S.ring("psd", [128, 512], F32, 2, psum=True)
                chunks = [(c0, min(512, C - c0)) for c0 in range(0, C, 512)]
                for e_ in range(NEXP):
                    wg = wgu_r.get()
                    wd = wdn_r.get()
                    for q4 in range(4):
                        ti, row = wloc(L, e_, RGG, q4)
                        S.dma("sp", wg[:, 2 * q4:2 * q4 + 2, :],
                              WALLG[ti].ap()[row:row + 256, :].rearrange("(k p) n -> p k n", p=128), wg, WALLG[ti])
                    for q4 in range(2):
                        ti, row = wloc(L, e_, RGD, q4)
                        S.dma("sp", wd[:, 4 * q4:4 * q4 + 4, :],
                              WALLD[ti].ap()[row:row + 512, :].rearrange("(k p) n -> p k n", p=128), wd, WALLD[ti])
                    bd = bd_r.get()
                    S.dma("sp", bd[:], bdn.ap()[L * NEXP + e_, :].partition_broadcast(128), bd, bdn)
                    xs = xs_r.get()
                    S.dma("sp", xs[:], XDISP.ap()[e_ * C:(e_ + 1) * C, :].rearrange("(c p) d -> p c d", p=128), xs, XDISP)
                    xsT = xsT_r.get()
                    for cb in range(CB):
                        pst = pst_r.get()
                        for kc in range(8):
                            TR(pst[:, kc * 128:(kc + 1) * 128], xs[:, cb, kc * 128:(kc + 1) * 128], ident_b[:], [xs, ident_b], [pst])
                        CP("act" if cb % 2 == 0 else "dve", xsT[:, :, cb * 128:(cb + 1) * 128],
                           pst[:].rearrange("p (k t) -> p k t", k=8), [pst], [xsT])
                    aT = aT_r.get()
                    for j in range(8):
                        for (c0, w) in chunks:
                            psg = psg_r.get()
                            psu = psu_r.get()
                            for kc in range(8):
                                MM(psg[:, 0:w], wg[:, kc, j * 128:(j + 1) * 128], xsT[:, kc, c0:c0 + w], kc == 0, kc == 7, [wg, xsT], [psg])
                            for kc in range(8):
                                MM(psu[:, 0:w], wg[:, kc, D + j * 128:D + (j + 1) * 128], xsT[:, kc, c0:c0 + w], kc == 0, kc == 7, [wg, xsT], [psu])
                            tmp = tmp_r.get()
                            g1, sg, u1, u2, tt_ = (tmp[:, i, 0:w] for i in range(5))
                            bgc = (e_ * 16 + j)
                            TS("dve", g1, psg[:, 0:w], BG[:, bgc:bgc + 1], ALU.add, [psg, BG], [tmp], s2=7.0, op1=ALU.min)
                            ACT(sg, g1, AF.Sigmoid, [tmp], [tmp], scale=1.702)
                            TS("dve", u1, psu[:, 0:w], BG[:, bgc + 8:bgc + 9], ALU.add, [psu, BG], [tmp], s2=7.0, op1=ALU.min)
                            TS("dve", u2, u1, -7.0, ALU.max, [tmp], [tmp], s2=1.0, op1=ALU.add)
                            TT("pool", tt_, g1, sg, ALU.mult, [tmp], [tmp])
                            TT("pool", aT[:, j, c0:c0 + w], tt_, u2, ALU.mult, [tmp], [aT])
                    for cb in range(CB):
                        ys = ys_r.get()
                        for half in range(2):
                            psd = psd_r.get()
                            for j in range(8):
                                MM(psd[:], aT[:, j, cb * 128:(cb + 1) * 128], wd[:, j, half * 512:(half + 1) * 512], j == 0, j == 7, [aT, wd], [psd])
                            TT("dve", ys[:, half * 512:(half + 1) * 512], psd[:], bd[:, half * 512:(half + 1) * 512], ALU.add, [psd, bd], [ys])
                        S.dma("sp", YDISP[e_ * C + cb * 128:e_ * C + (cb + 1) * 128, :], ys[:], YDISP, ys)
        with S.phase():
            gt = S.sbuf("gt", [128, D], F32)
            bt = S.sbuf("bt", [128, D], F32)
            S.dma("sp", gt[:], lng.ap()[4 + L, :].partition_broadcast(128), gt, lng)
            S.dma("sp", bt[:], lnb.ap()[4 + L, :].partition_broadcast(128), bt, lnb)
            x1_r = S.ring("x1", [128, D], F32, 2)
            acc_r = S.ring("acc", [128, D], F32, 2)
            yk_r = S.ring("yk", [128, D], F32, 4)
            x2_r = S.ring("x2", [128, D], F32, 2)
            junk = S.sbuf("junk", [128, D], F32)
            st_r = S.ring("st", [128, 8], F32, 2)
            xT_r = S.ring("xT", [128, 8, 512], BF16, 2)
            ps_r = S.ring("psx", [128, D], F32, 2, psum=True)
            dst = out if is_last else xnext
            xT = None
            for b in range(NBL):
                x1 = x1_r.get()
                S.dma("sp", x1[:], XR1[b * 128:(b + 1) * 128, :], x1, XR1)
                acc = acc_r.get()
                TS("dve", acc[:], x1[:], ALPHA, ALU.mult, [x1], [acc])
                if cfg.moe:
                    for k in range(4):
                        col = b * 4 + k
                        yk = yk_r.get()
                        S.dma("pool", None, None, yk, YDISP, extra=[DESTI],
                              fn=lambda e, col=col, yk=yk: e.indirect_dma_start(
                                  out=yk[:, :], out_offset=None, in_=YDISP.ap()[:, :],
                                  in_offset=bass.IndirectOffsetOnAxis(ap=DESTI[:, col:col + 1], axis=0)))
                        STT(acc[:], yk[:], GATE[:, col:col + 1], acc[:], ALU.mult, ALU.add, [yk, GATE, acc], [acc])
                x2 = x2_r.get()
                layer_norm(acc, x2, gt, bt, st_r, junk)
                S.dma("sp", dst[b * 128:(b + 1) * 128, :], x2[:], dst, x2)
                if not is_last:
                    if b % 4 == 0:
                        xT = xT_r.get()
                    emit_xT_block(x2, xT, (b % 4) * 128, ps_r, "act")
                    if b % 4 == 3:
                        tl = b // 4
                        S.dma("sp", XTBv[:, :, tl * 512:(tl + 1) * 512], xT[:], XTB, xT)
            if not is_last:
                xt_allgather()


    XTALLv = XTALL.ap().rearrange("(k r p) t -> r p k t", k=8, r=4) if True else None

    def load_xT(xT, tt):
        rank, lt = tt // NTL, tt % NTL
        S.dma("sp", xT[:], XTALLv[rank][:, :, lt * 512:(lt + 1) * 512], xT, XTALL)

    def o_allgather():
        for j in range(SEQ // TC):
            S.cc("AllGather", ALU.bypass, G4, OTOK, OALL, src_ap=OTOK[j * TC:(j + 1) * TC, :],
                 dst_ap=OALL[j * 4 * TC:(j + 1) * 4 * TC, :])

    def odd_mixer(L):
        i = L // 2
        with S.phase():
            Wb = S.sbuf("Wb", [128, 8, 1028], BF16)
            for q4 in range(4):
                S.dma("pool", Wb[:, 2 * q4:2 * q4 + 2, :],
                      wo_in.ap()[i, q4 * 256:(q4 + 1) * 256, :].rearrange("(k p) n -> p k n", p=128), Wb, wo_in)
            WQK = S.sbuf("WQK", [64, 2], F32)
            S.dma("sp", WQK[:], qkw.ap()[i], WQK, qkw)
            TS("dve", WQK[:, 0:1], WQK[:, 0:1], 0.125, ALU.mult, [WQK], [WQK])
            NFB = S.sbuf("NFB", [128, 4], F32)
            S.dma("sp", NFB[:], fb.ap()[i, :].partition_broadcast(128), NFB, fb)
            TS("dve", NFB[:], NFB[:], -1.0, ALU.mult, [NFB], [NFB])
            MKf = S.sbuf("MKf", [128, 4, 512], F32)
            MK = S.sbuf("MK", [128, 4, 512], BF16)
            MEMSET("pool", MKf[:], 0.0, [MKf])
            for j in range(4):
                ASEL(MKf[:, j, :], MKf[:, j, :], [[1, 512]], ALU.is_ge, NEG, -128 * j, -1, [MKf], [MKf])
            CP("dve", MK[:], MKf[:], [MKf], [MK])
            QT = S.sbuf("QT", [67, SEQ], BF16)
            KT = S.sbuf("KT", [67, SEQ], BF16)
            GTK = S.sbuf("GTK", [128, NBF, 64], BF16)
            VA = S.sbuf("VA", [128, NBF, 65], BF16)
            LF = S.sbuf("LF", [128, NBF], F32)
            CUM = S.sbuf("CUM", [128, NBF], F32)
            NEGCUM = S.sbuf("NEGCUM", [128, NBF], F32)
            scan = [S.sbuf("scan%d" % k, [128, NBF], F32) for k in range(2)]
            tot = S.sbuf("tot", [128, NBF], F32)
            ct = S.sbuf("ct", [128, 6, 128], F32)
            ctb = S.sbuf("ctb", [128, 3, 128], BF16)
            MEMSET("dve", VA[:, :, 64:65], 1.0, [VA])
            MEMSET("dve", KT[64:67, :], 1.0, [KT])
            xT_r = S.ring("xT", [128, 8, 512], BF16, 2)
            qf_r = S.ring("qf", [64, 512], F32, 2)
            sq_r = S.ring("sq", [64, 512], F32, 2)
            rs_r = S.ring("rs", [64, 512], F32, 2)
            sm_r = S.ring("smo", [128, 8], F32, 4)
            pt_r = S.ring("pt", [128, 512], BF16, 4)
            ot_r = S.ring("ot", [128, 4, 64], BF16, 2)
            osb_r = S.ring("osb", [128, 512], F32, 2)
            for b_ in osb_r.bufs:
                MEMSET("dve", b_[:], 0.0, [b_])
            psm_r = S.ring("psm", [128, 512], F32, 3, psum=True)
            pss_r = S.ring("pss", [128, 512], F32, 3, psum=True)
            pso_r = S.ring("pso", [128, 512], F32, 2, psum=True)
            import os
            KODD = int(os.environ.get("KODD", "9"))
            if KODD == 0:
                return
            for h in range(4):
                w0 = h * 257
                for tt in range(NQT):
                    xT = xT_r.get()
                    load_xT(xT, tt)
                    for (coff, dst, wi) in ((0, QT, 0), (64, KT, 1)):
                        psq = psm_r.get()
                        for kc in range(8):
                            MM(psq[0:64, :], Wb[:, kc, w0 + coff:w0 + coff + 64], xT[:, kc, :], kc == 0, kc == 7, [Wb, xT], [psq])
                        qf = qf_r.get()
                        CP("act", qf[:], psq[0:64, :], [psq], [qf])
                        sq = sq_r.get()
                        TT("pool", sq[:], qf[:], qf[:], ALU.mult, [qf], [sq])
                        pssum = psm_r.get()
                        MM(pssum[0:64, :], ones_f[0:64, 0:64], sq[:], True, True, [ones_f, sq], [pssum])
                        rs = rs_r.get()
                        TS("dve", rs[:], pssum[0:64, :], 1.0 / 64, ALU.mult, [pssum], [rs], s2=RMS_EPS, op1=ALU.add)
                        ACT(rs[:], rs[:], AF.Sqrt, [rs], [rs])
                        RECIP(rs[:], rs[:], [rs], [rs])
                        STT(dst[0:64, tt * 512:(tt + 1) * 512], qf[:], WQK[:, wi:wi + 1], rs[:], ALU.mult, ALU.mult, [qf, WQK, rs], [dst])
                    for b4 in range(4):
                        gb = tt * 4 + b4
                        ps = psm_r.get()
                        for kc in range(8):
                            MM(ps[:, 0:129], xT[:, kc, b4 * 128:(b4 + 1) * 128], Wb[:, kc, w0 + 128:w0 + 257], kc == 0, kc == 7, [xT, Wb], [ps])
                        sm = sm_r.get()
                        CP("dve", VA[:, gb, 0:64], ps[:, 0:64], [ps], [VA])
                        ACT(GTK[:, gb, :], ps[:, 65:129], AF.Sigmoid, [ps], [GTK])
                        ACT(sm[:, 0:1], ps[:, 64:65], AF.Exp, [ps, NFB], [sm], bias=NFB[:, h:h + 1], scale=-1.0)
                        ACT(sm[:, 1:2], sm[:, 0:1], AF.Ln, [sm], [sm], bias=1.0)
                        TS("dve", LF[:, gb:gb + 1], sm[:, 1:2], -1.0, ALU.mult, [sm], [LF])
                if KODD == 1:
                    return
                psw = psm_r.get()
                MM(psw[:, 0:NBF], triU_f[:], LF[:], True, True, [triU_f, LF], [psw])
                pstot = psm_r.get()
                MM(pstot[:, 0:NBF], ones_f[:], LF[:], True, True, [ones_f, LF], [pstot])
                CP("dve", tot[:], pstot[:, 0:NBF], [pstot], [tot])
                CP("dve", scan[0][:], tot[:], [tot], [scan[0]])
                a, bq = scan[0], scan[1]
                sft = 1
                while sft < NBF:
                    TT("dve", bq[:, sft:NBF], a[:, sft:NBF], a[:, 0:NBF - sft], ALU.add, [a], [bq])
                    CP("dve", bq[:, 0:sft], a[:, 0:sft], [a], [bq])
                    a, bq = bq, a
                    sft *= 2
                TT("dve", CUM[:], psw[:, 0:NBF], a[:], ALU.add, [psw, a], [CUM])
                TT("dve", CUM[:], CUM[:], tot[:], ALU.subtract, [CUM, tot], [CUM])
                TS("dve", NEGCUM[:], CUM[:], -1.0, ALU.mult, [CUM], [NEGCUM])
                pct = psm_r.get()
                TR(pct[0:NBF, 0:128], CUM[:, 0:NBF], ident_f[:], [CUM, ident_f], [pct])
                CP("dve", ct[0:NBF, 0, :], pct[0:NBF, 0:128], [pct], [ct])
                CP("dve", ctb[0:NBF, 0, :], ct[0:NBF, 0, :], [ct], [ctb])
                CP("dve", ct[0:NBF, 1, :], ctb[0:NBF, 0, :], [ctb], [ct])
                TT("dve", ct[0:NBF, 2, :], ct[0:NBF, 0, :], ct[0:NBF, 1, :], ALU.subtract, [ct], [ct])
                CP("dve", ctb[0:NBF, 1, :], ct[0:NBF, 2, :], [ct], [ctb])
                CP("dve", ct[0:NBF, 3, :], ctb[0:NBF, 1, :], [ctb], [ct])
                TT("dve", ct[0:NBF, 4, :], ct[0:NBF, 2, :], ct[0:NBF, 3, :], ALU.subtract, [ct], [ct])
                CP("dve", ctb[0:NBF, 2, :], ct[0:NBF, 4, :], [ct], [ctb])
                for j in range(3):
                    S.dma("sp", CQ.ap()[j, :].rearrange("(j t) -> j t", t=128), ctb[0:NBF, j, :], CQ, ctb)
                for j in range(3):
                    S.dma("sp", QT[64 + j:65 + j, :], CQ[j:j + 1, :], QT, CQ)
                if KODD == 2:
                    return
                acc_of = {}

                def stage_a(qt, kb):
                    pss = pss_r.get()
                    diag = kb >= 4 * qt
                    MM(pss[:], KT[0:67, kb * 128:(kb + 1) * 128], QT[0:67, qt * 512:(qt + 1) * 512], True, not diag, [KT, QT], [pss])
                    if diag:
                        MM(pss[:], ident_b[:], MK[:, kb - 4 * qt, :], False, True, [ident_b, MK], [pss])
                    pt = pt_r.get()
                    ACT(pt[:], pss[:], AF.Exp, [pss, NEGCUM], [pt], bias=NEGCUM[:, kb:kb + 1])
                    return pt

                def stage_b(qt, kb, pt):
                    nkb = 4 * qt + 4
                    if kb == 0:
                        acc_of[qt] = pso_r.get()
                    oacc = acc_of[qt]
                    MM(oacc[0:65, :], VA[:, kb, :], pt[:], kb == 0, kb == nkb - 1, [VA, pt], [oacc])
                    if kb < nkb - 1:
                        return
                    osb = osb_r.get()
                    CP("dve", osb[0:65, :], oacc[0:65, :], [oacc], [osb])
                    ptr = psm_r.get()
                    for b4 in range(4):
                        TR(ptr[:, b4 * 128:(b4 + 1) * 128], osb[:, b4 * 128:(b4 + 1) * 128], ident_f[:], [osb, ident_f], [ptr])
                    ot = ot_r.get()
                    for b4 in range(4):
                        gb = qt * 4 + b4
                        sm = sm_r.get()
                        RECIP(sm[:, 0:1], ptr[:, b4 * 128 + 64:b4 * 128 + 65], [ptr], [sm])
                        STT(ot[:, b4, :], ptr[:, b4 * 128:b4 * 128 + 64], sm[:, 0:1], GTK[:, gb, :], ALU.mult, ALU.mult, [ptr, sm, GTK], [ot])
                    S.dma("sp", OTOK.ap()[qt * 512:(qt + 1) * 512, h * 64:(h + 1) * 64].rearrange("(b p) d -> p b d", p=128),
                          ot[:], OTOK, ot)

                pend = []
                for qt in range(NQT):
                    for kb in range(4 * qt + 4):
                        pend.append((qt, kb, stage_a(qt, kb)))
                        if len(pend) > 2:
                            stage_b(*pend.pop(0))
                while pend:
                    stage_b(*pend.pop(0))
                if KODD == 3:
                    return
            o_allgather()

    def even_mixer(L):
        i = L // 2
        lam_init = 0.8 - 0.6 * math.exp(-0.3 * L)
        import os
        KEV = int(os.environ.get("KEV", "9"))
        with S.phase():
            Wd = S.sbuf("Wd", [128, 8, 384], BF16)
            for q4 in range(4):
                S.dma("pool", Wd[:, 2 * q4:2 * q4 + 2, :],
                      we_in.ap()[i, q4 * 256:(q4 + 1) * 256, 0:384].rearrange("(k p) n -> p k n", p=128), Wd, we_in)
            LP = S.sbuf("LP", [128, 256], F32)
            S.dma("sp", LP[:], lamp.ap()[i, :].partition_broadcast(128), LP, lamp)
            lw = S.sbuf("lw", [128, 8], F32)
            pr = S.sbuf("pr", [128, 128], F32)
            TT("dve", pr[:, 0:64], LP[:, 0:64], LP[:, 64:128], ALU.mult, [LP], [pr])
            TT("dve", pr[:, 64:128], LP[:, 128:192], LP[:, 192:256], ALU.mult, [LP], [pr])
            S.op("dve", lambda e: e.tensor_reduce(out=lw[:, 0:2], in_=pr[:].rearrange("p (g d) -> p g d", g=2), axis=AX.X, op=ALU.add),
                 reads=[pr], writes=[lw])
            ACT(lw[:, 2:4], lw[:, 0:2], AF.Exp, [lw], [lw])
            TT("dve", lw[:, 4:5], lw[:, 2:3], lw[:, 3:4], ALU.subtract, [lw], [lw])
            TS("dve", lw[:, 4:5], lw[:, 4:5], lam_init, ALU.add, [lw], [lw])
            TS("dve", lw[:, 5:6], lw[:, 4:5], -1.0, ALU.mult, [lw], [lw])
            SUBW = S.sbuf("SUBW", [128, 128], F32)
            S.dma("sp", SUBW[:], subw.ap()[i, :].partition_broadcast(128), SUBW, subw)
            TS("dve", SUBW[:], SUBW[:], 1.0 - lam_init, ALU.mult, [SUBW], [SUBW])
            T5C = S.sbuf("T5C", [33, 1], F32)
            S.dma("sp", T5C[0:32, :], t5col[:, :], T5C, t5col)
            MEMSET("dve", T5C[32:33, :], 1.0, [T5C])
            OH = S.sbuf("OH", [33, 2560], F32)
            S.dma("sp", OH[:], t5oh[:, :], OH, t5oh)
            bvs = S.sbuf("bvs", [1, 2560], F32)
            psm_r = S.ring("psm", [128, 512], F32, 3, psum=True)
            pss_r = S.ring("pss", [128, 512], F32, 3, psum=True)
            pso_r = S.ring("pso", [128, 512], F32, 2, psum=True)
            for c5 in range(5):
                ps = psm_r.get()
                MM(ps[0:1, :], T5C[:, 0:1], OH[:, c5 * 512:(c5 + 1) * 512], True, True, [T5C, OH], [ps])
                CP("dve", bvs[0:1, c5 * 512:(c5 + 1) * 512], ps[0:1, :], [ps], [bvs])
            S.dma("sp", BV.ap().rearrange("(o n) -> o n", o=1), bvs[:], BV, bvs)
            BT = S.sbuf("BT", [128, 16, 512], BF16)
            skb_r = S.ring("skb", [128, 640], F32, 2)
            for j in range(16):
                skb = skb_r.get()
                S.dma("sp", skb[:], BV.ap()[128 * j:128 * j + 640].partition_broadcast(128), skb, BV)
                S.dma("sp", SKW.ap()[j, :].rearrange("(p n) -> p n", n=640), skb[:], SKW, skb)
                S.dma("pool", BT[:, j, :], bass.AP(tensor=SKW.h, offset=j * 128 * 640 + 127, ap=[[639, 128], [1, 512]]), BT, SKW)
            B31 = S.sbuf("B31", [128, 1], F32)
            S.dma("sp", B31[:], BV.ap()[2559:2560].partition_broadcast(128), B31, BV)
            if S.w_pending:
                S.w_pending = False
                w_collectives()
            QT = S.sbuf("QT", [64, SEQ], BF16)
            KT = S.sbuf("KT", [64, SEQ], BF16)
            VA = S.sbuf("VA", [128, NBF, 129], BF16)
            MEMSET("dve", VA[:, :, 64:65], 1.0, [VA])
            xT_r = S.ring("xT", [128, 8, 512], BF16, 2)
            pt_r = S.ring("pt", [128, 512], BF16, 4)
            osb_r = S.ring("osbd", [128, 512], F32, 4)
            for b_ in osb_r.bufs:
                MEMSET("dve", b_[:], 0.0, [b_])
            om_r = S.ring("om", [128, 128], F32, 3)
            o0_r = S.ring("o0", [128, 128], F32, 2)
            a_r = S.ring("a", [128, 128], F32, 2)
            ao_r = S.ring("ao", [128, 128], BF16, 2)
            sm_r = S.ring("smd", [128, 8], F32, 4)
            junk = S.sbuf("junkd", [128, 128], F32)
            for m in range(2):
                for tt in range(NQT):
                    xT = xT_r.get()
                    load_xT(xT, tt)
                    psq = psm_r.get()
                    for kc in range(8):
                        MM(psq[0:64, :], Wd[:, kc, m * 64:(m + 1) * 64], xT[:, kc, :], kc == 0, kc == 7, [Wd, xT], [psq])
                    ACT(QT[:, tt * 512:(tt + 1) * 512], psq[0:64, :], AF.Identity, [psq], [QT], scale=0.125)
                    psk = psm_r.get()
                    for kc in range(8):
                        MM(psk[0:64, :], Wd[:, kc, 128 + m * 64:128 + (m + 1) * 64], xT[:, kc, :], kc == 0, kc == 7, [Wd, xT], [psk])
                    CP("dve", KT[:, tt * 512:(tt + 1) * 512], psk[0:64, :], [psk], [KT])
                    if m == 0:
                        for b4 in range(4):
                            gb = tt * 4 + b4
                            psv = psm_r.get()
                            for kc in range(8):
                                MM(psv[:, 0:128], xT[:, kc, b4 * 128:(b4 + 1) * 128], Wd[:, kc, 256:384], kc == 0, kc == 7, [xT, Wd], [psv])
                            CP("act", VA[:, gb, 0:64], psv[:, 0:64], [psv], [VA])
                            CP("act", VA[:, gb, 65:129], psv[:, 64:128], [psv], [VA])
                if KEV == 1:
                    return
                acc_of = {}

                def stage_a(qt, kb):
                    dj = 4 * qt - kb + 3
                    near = dj <= 15
                    pss = pss_r.get()
                    MM(pss[:], KT[:, kb * 128:(kb + 1) * 128], QT[:, qt * 512:(qt + 1) * 512], True, not near, [KT, QT], [pss])
                    if near:
                        MM(pss[:], ident_b[:], BT[:, dj, :], False, True, [ident_b, BT], [pss])
                    pt = pt_r.get()
                    if near:
                        ACT(pt[:], pss[:], AF.Exp, [pss], [pt])
                    else:
                        ACT(pt[:], pss[:], AF.Exp, [pss, B31], [pt], bias=B31[:, 0:1])
                    return pt

                def stage_b(qt, kb, pt, m=m):
                    nkb = 4 * qt + 4
                    if kb == 0:
                        acc_of[qt] = (pso_r.get(), pso_r.get())
                    oa, ob = acc_of[qt]
                    MM(oa[0:65, :], VA[:, kb, 0:65], pt[:], kb == 0, kb == nkb - 1, [VA, pt], [oa])
                    MM(ob[0:64, :], VA[:, kb, 65:129], pt[:], kb == 0, kb == nkb - 1, [VA, pt], [ob])
                    if kb < nkb - 1:
                        return
                    osa = osb_r.get()
                    osb = osb_r.get()
                    CP("dve", osa[0:65, :], oa[0:65, :], [oa], [osa])
                    CP("act", osb[0:64, :], ob[0:64, :], [ob], [osb])
                    ptra = psm_r.get()
                    ptrb = psm_r.get()
                    for b4 in range(4):
                        TR(ptra[:, b4 * 128:(b4 + 1) * 128], osa[:, b4 * 128:(b4 + 1) * 128], ident_f[:], [osa, ident_f], [ptra])
                        TR(ptrb[:, b4 * 128:(b4 + 1) * 128], osb[:, b4 * 128:(b4 + 1) * 128], ident_f[:], [osb, ident_f], [ptrb])
                    for b4 in range(4):
                        gb = qt * 4 + b4
                        sm = sm_r.get()
                        RECIP(sm[:, 0:1], ptra[:, b4 * 128 + 64:b4 * 128 + 65], [ptra], [sm])
                        om = om_r.get()
                        TS("dve", om[:, 0:64], ptra[:, b4 * 128:b4 * 128 + 64], sm[:, 0:1], ALU.mult, [ptra, sm], [om])
                        TS("dve", om[:, 64:128], ptrb[:, b4 * 128:b4 * 128 + 64], sm[:, 0:1], ALU.mult, [ptrb, sm], [om])
                        if m == 0:
                            S.dma("sp", O0S[gb * 128:(gb + 1) * 128, :], om[:], O0S, om)
                        else:
                            o0 = o0_r.get()
                            S.dma("sp", o0[:], O0S[gb * 128:(gb + 1) * 128, :], o0, O0S)
                            av = a_r.get()
                            STT(av[:], om[:], lw[:, 5:6], o0[:], ALU.mult, ALU.add, [om, lw, o0], [av])
                            ACT(junk[:], av[:], AF.Square, [av], [junk, sm], accum=sm[:, 1:2])
                            TS("dve", sm[:, 2:3], sm[:, 1:2], 1.0 / 128, ALU.mult, [sm], [sm], s2=RMS_EPS, op1=ALU.add)
                            ACT(sm[:, 3:4], sm[:, 2:3], AF.Sqrt, [sm], [sm])
                            RECIP(sm[:, 4:5], sm[:, 3:4], [sm], [sm])
                            ao = ao_r.get()
                            STT(ao[:], av[:], sm[:, 4:5], SUBW[:], ALU.mult, ALU.mult, [av, sm, SUBW], [ao])
                            S.dma("sp", OTOK[gb * 128:(gb + 1) * 128, 0:128], ao[:], OTOK, ao)

                pend = []
                for qt in range(NQT):
                    for kb in range(4 * qt + 4):
                        pend.append((qt, kb, stage_a(qt, kb)))
                        if len(pend) > 2:
                            stage_b(*pend.pop(0))
                while pend:
                    stage_b(*pend.pop(0))
        if KEV <= 2:
            with S.phase():
                zb = S.sbuf("zb", [128, 128], BF16)
                MEMSET("dve", zb[:], 0.0, [zb])
                for gb in range(NBF):
                    S.dma("sp", OTOK[gb * 128:(gb + 1) * 128, 128:256], zb[:], OTOK, zb)
            o_allgather()
            return
        gdn_phase(L)
        o_allgather()

    def gdn_phase(L):
        i = L // 2
        SC = 128 ** -0.5
        with S.phase():
            Wg = S.sbuf("Wg", [128, 8, 514], BF16)
            for q4 in range(4):
                S.dma("pool", Wg[:, 2 * q4:2 * q4 + 2, :],
                      we_in.ap()[i, q4 * 256:(q4 + 1) * 256, 384:898].rearrange("(k p) n -> p k n", p=128), Wg, we_in)
            CW = S.sbuf("CW", [128, 12], F32)
            S.dma("sp", CW[:], convw.ap()[i], CW, convw)
            GNW = S.sbuf("GNW", [128, 128], F32)
            S.dma("sp", GNW[:], gnw.ap()[i, :].partition_broadcast(128), GNW, gnw)
            AD = S.sbuf("AD", [128, 4], F32)
            S.dma("sp", AD[:, 0:1], alog.ap()[i, :].partition_broadcast(128), AD, alog)
            S.dma("sp", AD[:, 1:2], dtb.ap()[i, :].partition_broadcast(128), AD, dtb)
            ACT(AD[:, 2:3], AD[:, 0:1], AF.Exp, [AD], [AD])
            TS("dve", AD[:, 2:3], AD[:, 2:3], -1.0, ALU.mult, [AD], [AD])
            MUs = S.sbuf("MUs", [128, 128], F32)
            MUi = S.sbuf("MUi", [128, 128], F32)
            MLs = S.sbuf("MLs", [128, 128], F32)
            for (t_, base, cm, pat) in ((MUs, -1, -1, 1), (MUi, 0, -1, 1), (MLs, -1, 1, -1)):
                MEMSET("pool", t_[:], 1.0, [t_])
                ASEL(t_[:], t_[:], [[pat, 128]], ALU.is_ge, 0.0, base, cm, [t_], [t_])
            St = [S.sbuf("St%d" % k, [128, 128], F32) for k in range(2)]
            MEMSET("dve", St[0][:], 0.0, [St[0]])
            cb = [S.sbuf("cb%d" % g, [128, 515], F32) for g in range(3)]
            for g in range(3):
                MEMSET("dve", cb[g][:], 0.0, [cb[g]])
            hal = S.sbuf("hal", [128, 3, 3], F32)
            xT_r = S.ring("xT", [128, 8, 512], BF16, 2)
            y_r = S.ring("y", [128, 512], F32, 2)
            fT = [S.ring("fT%d" % g, [128, 512], F32, 2) for g in range(3)]
            sq_r = S.ring("sqg", [128, 512], F32, 2)
            zt_r = S.ring("ztk", [128, 4, 128], F32, 2)
            gb_r = S.ring("gbt", [128, 12], F32, 2)
            w_r = S.ring("wk", [128, 128], F32, 44)
            w2_r = S.ring("wk2", [128, 256], F32, 13)
            c_r = S.ring("colg", [128, 8], F32, 9)
            bo_r = S.ring("bo", [128, 128], BF16, 2)
            ps_r = S.ring("psg", [128, 512], F32, 7, psum=True)
            pso_r = S.ring("psgo", [128, 512], F32, 1, psum=True)
            cur = 0
            for tt in range(NQT):
                xT = xT_r.get()
                load_xT(xT, tt)
                fts = []
                for g in range(3):
                    psf = ps_r.get()
                    for kc in range(8):
                        MM(psf[:], Wg[:, kc, g * 128:(g + 1) * 128], xT[:, kc, :], kc == 0, kc == 7, [Wg, xT], [psf])
                    CP("dve", hal[:, g, :], cb[g][:, 512:515], [cb[g]], [hal])
                    CP("act", cb[g][:, 3:515], psf[:], [psf], [cb[g]])
                    CP("dve", cb[g][:, 0:3], hal[:, g, :], [hal], [cb[g]])
                    y = y_r.get()
                    TS("dve", y[:], cb[g][:, 0:512], CW[:, g * 4:g * 4 + 1], ALU.mult, [cb[g], CW], [y])
                    for j in range(1, 4):
                        STT(y[:], cb[g][:, j:j + 512], CW[:, g * 4 + j:g * 4 + j + 1], y[:], ALU.mult, ALU.add, [cb[g], CW, y], [y])
                    ft = fT[g].get()
                    ACT(ft[:], y[:], AF.Silu, [y], [ft])
                    if g < 2:
                        sq = sq_r.get()
                        TT("pool", sq[:], ft[:], ft[:], ALU.mult, [ft], [sq])
                        pss = ps_r.get()
                        MM(pss[:], ones_f[:], sq[:], True, True, [ones_f, sq], [pss])
                        TS("dve", sq[:], pss[:], RMS_EPS, ALU.add, [pss], [sq])
                        ACT(sq[:], sq[:], AF.Sqrt, [sq], [sq])
                        RECIP(sq[:], sq[:], [sq], [sq])
                        TT("dve", ft[:], ft[:], sq[:], ALU.mult, [ft, sq], [ft])
                    fts.append(ft)
                qTt, kTt, vTt = fts
                zt = zt_r.get()
                gbt = gb_r.get()
                for b4 in range(4):
                    pz = ps_r.get()
                    for kc in range(8):
                        MM(pz[:, 0:130], xT[:, kc, b4 * 128:(b4 + 1) * 128], Wg[:, kc, 384:514], kc == 0, kc == 7, [xT, Wg], [pz])
                    ACT(zt[:, b4, :], pz[:, 0:128], AF.Silu, [pz], [zt])
                    ACT(gbt[:, 4 + b4:5 + b4], pz[:, 128:129], AF.Sigmoid, [pz], [gbt])
                    cg = c_r.get()
                    ACT(cg[:, 0:1], pz[:, 129:130], AF.Exp, [pz, AD], [cg], bias=AD[:, 1:2])
                    ACT(cg[:, 1:2], cg[:, 0:1], AF.Ln, [cg], [cg], bias=1.0)
                    TS("dve", gbt[:, b4:b4 + 1], cg[:, 1:2], AD[:, 2:3], ALU.mult, [cg, AD], [gbt])
                pgc = ps_r.get()
                MM(pgc[:, 0:4], triU_f[:], gbt[:, 0:4], True, True, [triU_f, gbt], [pgc])
                CP("dve", gbt[:, 8:12], pgc[:, 0:4], [pgc], [gbt])
                for b4 in range(4):
                    gbk = tt * 4 + b4
                    blk = slice(b4 * 128, (b4 + 1) * 128)
                    gc = gbt[:, 8 + b4:9 + b4]
                    beta = gbt[:, 4 + b4:5 + b4]
                    pk = ps_r.get()
                    TR(pk[:, 0:128], kTt[:, blk], ident_f[:], [kTt, ident_f], [pk])
                    Kc = w_r.get()
                    CP("act", Kc[:], pk[:, 0:128], [pk], [Kc])
                    pv = ps_r.get()
                    TR(pv[:, 0:128], vTt[:, blk], ident_f[:], [vTt, ident_f], [pv])
                    Vc = w_r.get()
                    CP("act", Vc[:], pv[:, 0:128], [pv], [Vc])
                    DG = w2_r.get()
                    TS("dve", DG[:, 0:128], ident_f[:], gc, ALU.mult, [ident_f, gbt], [DG])
                    TS("dve", DG[:, 128:256], ident_f[:], beta, ALU.mult, [ident_f, gbt], [DG])
                    pb = ps_r.get()
                    MM(pb[:, 0:256], ones_f[:], DG[:], True, True, [ones_f, DG], [pb])
                    GB = w2_r.get()
                    CP("act", GB[:], pb[:, 0:256], [pb], [GB])
                    dlt = w_r.get()
                    TS("dve", dlt[:], GB[:, 0:128], gc, ALU.subtract, [GB, gbt], [dlt])
                    ET = w_r.get()
                    TS("dve", ET[:], dlt[:], 0.0, ALU.min, [dlt], [ET])
                    ACT(ET[:], ET[:], AF.Exp, [ET], [ET])
                    E2 = w_r.get()
                    TS("dve", E2[:], dlt[:], -1.0, ALU.mult, [dlt], [E2], s2=0.0, op1=ALU.min)
                    ACT(E2[:], E2[:], AF.Exp, [E2], [E2])
                    EG = w_r.get()
                    ACT(EG[:], GB[:, 0:128], AF.Exp, [GB], [EG])
                    pkk = ps_r.get()
                    MM(pkk[:, 0:128], kTt[:, blk], kTt[:, blk], True, True, [kTt], [pkk])
                    Nj = w_r.get()
                    TT("dve", Nj[:], pkk[:, 0:128], GB[:, 128:256], ALU.mult, [pkk, GB], [Nj])
                    TT("dve", Nj[:], Nj[:], ET[:], ALU.mult, [Nj, ET], [Nj])
                    STT(Nj[:], Nj[:], -1.0, MUs[:], ALU.mult, ALU.mult, [Nj, MUs], [Nj])
                    Pj = w_r.get()
                    STT(Pj[:], pkk[:, 0:128], beta, E2[:], ALU.mult, ALU.mult, [pkk, gbt, E2], [Pj])
                    STT(Pj[:], Pj[:], -1.0, MLs[:], ALU.mult, ALU.mult, [Pj, MLs], [Pj])
                    cg = c_r.get()
                    ACT(cg[:, 0:1], gc, AF.Exp, [gbt], [cg])
                    TT("dve", cg[:, 1:2], cg[:, 0:1], beta, ALU.mult, [cg, gbt], [cg])
                    Rm = w2_r.get()
                    TS("dve", Rm[:, 0:128], Vc[:], beta, ALU.mult, [Vc, gbt], [Rm])
                    TS("dve", Rm[:, 128:256], Kc[:], cg[:, 1:2], ALU.mult, [Kc, cg], [Rm])
                    for j in range(7):
                        pr_ = ps_r.get()
                        MM(pr_[:, 0:256], Nj[:], Rm[:], True, True, [Nj, Rm], [pr_])
                        Rn = w2_r.get()
                        TT("dve", Rn[:], Rm[:], pr_[:, 0:256], ALU.add, [Rm, pr_], [Rn])
                        Rm = Rn
                        if j < 6:
                            pn = ps_r.get()
                            MM(pn[:, 0:128], Pj[:], Nj[:], True, True, [Pj, Nj], [pn])
                            pp = ps_r.get()
                            MM(pp[:, 0:128], Nj[:], Pj[:], True, True, [Nj, Pj], [pp])
                            Nn = w_r.get()
                            CP("act", Nn[:], pn[:, 0:128], [pn], [Nn])
                            Pn = w_r.get()
                            CP("act", Pn[:], pp[:, 0:128], [pp], [Pn])
                            Nj, Pj = Nn, Pn
                    pw = ps_r.get()
                    TR(pw[:, 0:128], Rm[:, 128:256], ident_f[:], [Rm, ident_f], [pw])
                    WT = w_r.get()
                    CP("act", WT[:], pw[:, 0:128], [pw], [WT])
                    Sc, Sn = St[cur], St[1 - cur]
                    pws = ps_r.get()
                    MM(pws[:, 0:128], WT[:], Sc[:], True, True, [WT, Sc], [pws])
                    Vn = w_r.get()
                    TT("dve", Vn[:], Rm[:, 0:128], pws[:, 0:128], ALU.subtract, [Rm, pws], [Vn])
                    pqk = ps_r.get()
                    MM(pqk[:, 0:128], kTt[:, blk], qTt[:, blk], True, True, [kTt, qTt], [pqk])
                    qki = w_r.get()
                    TT("dve", qki[:], pqk[:, 0:128], ET[:], ALU.mult, [pqk, ET], [qki])
                    STT(qki[:], qki[:], SC, MUi[:], ALU.mult, ALU.mult, [qki, MUi], [qki])
                    QdT = w_r.get()
                    STT(QdT[:], qTt[:, blk], SC, EG[:], ALU.mult, ALU.mult, [qTt, EG], [QdT])
                    po = pso_r.get()
                    MM(po[:, 0:128], QdT[:], Sc[:], True, False, [QdT, Sc], [po])
                    MM(po[:, 0:128], qki[:], Vn[:], False, True, [qki, Vn], [po])
                    TT("dve", cg[:, 2:3], GB[:, 127:128], gc, ALU.subtract, [GB, gbt], [cg])
                    ACT(cg[:, 3:4], cg[:, 2:3], AF.Exp, [cg], [cg])
                    Kd = w_r.get()
                    TS("dve", Kd[:], Kc[:], cg[:, 3:4], ALU.mult, [Kc, cg], [Kd])
                    psn = ps_r.get()
                    MM(psn[:, 0:128], Kd[:], Vn[:], True, True, [Kd, Vn], [psn])
                    STT(Sn[:], Sc[:], EG[:, 127:128], psn[:, 0:128], ALU.mult, ALU.add, [Sc, EG, psn], [Sn])
                    cur = 1 - cur
                    jk = w_r.get()
                    ACT(jk[:], po[:, 0:128], AF.Square, [po], [jk, cg], accum=cg[:, 4:5])
                    TS("dve", cg[:, 5:6], cg[:, 4:5], 1.0 / 128, ALU.mult, [cg], [cg], s2=RMS_EPS, op1=ALU.add)
                    ACT(cg[:, 6:7], cg[:, 5:6], AF.Sqrt, [cg], [cg])
                    RECIP(cg[:, 7:8], cg[:, 6:7], [cg], [cg])
                    STT(jk[:], po[:, 0:128], cg[:, 7:8], GNW[:], ALU.mult, ALU.mult, [po, cg, GNW], [jk])
                    bo = bo_r.get()
                    TT("dve", bo[:], jk[:], zt[:, b4, :], ALU.mult, [jk, zt], [bo])
                    S.dma("sp", OTOK[gbk * 128:(gbk + 1) * 128, 128:256], bo[:], OTOK, bo)


    phase_x0()
    xres = x_in
    if S.w_pending and not (cfg.mixer and cfg.LAYERS[0] % 2 == 0):
        S.w_pending = False
        with S.phase():
            w_collectives()
    for li, L in enumerate(cfg.LAYERS):
        is_last = li == len(cfg.LAYERS) - 1
        if cfg.mixer:
            if L % 2 == 0:
                even_mixer(L)
            else:
                odd_mixer(L)
        xnext = XR[li % 2]
        phase_ln_moe(L, xres, xnext, is_last)
        xres = xnext


def _even_mixer(S, cfg, L):
    raise NotImplementedError


def _odd_mixer(S, cfg, L):
    raise NotImplementedError


def t5_onehot():
    n = np.arange(2560) - 511
    nn = np.maximum(n, 0)
    nf = np.maximum(nn, 1).astype(np.float32)
    large = 16 + (np.log(nf / 16) / math.log(2048 / 16) * 16).astype(np.int32)
    large = np.minimum(large, 31)
    bucket = np.where(nn < 16, nn, large)
    oh = np.zeros((33, 2560), np.float32)
    valid = n >= 0
    oh[bucket[valid], np.nonzero(valid)[0]] = 1.0
    oh[32, ~valid] = NEG
    return oh


def prep_inputs(inp, cfg):
    SEQ, TOK, NEXP, EPC, NLW, WL0 = cfg.SEQ, cfg.TOK, cfg.NEXP, cfg.EPC, cfg.NLW, cfg.WL0
    f = lambda a: np.ascontiguousarray(np.asarray(a, dtype=np.float32))
    x = f(inp["x"])
    maps = []
    lng = f(np.concatenate([inp["ln_mix_g"], inp["ln_ffn_g"]], 0))
    lnb = f(np.concatenate([inp["ln_mix_b"], inp["ln_ffn_b"]], 0))
    if cfg.moe:
        bg = f(inp["moe_b_gate_up"]).reshape(4, NEXP, 16, 128)
        bgu = np.ascontiguousarray(bg.transpose(3, 0, 1, 2).reshape(128, 4 * NEXP * 16))
        bdn = f(inp["moe_b_down"]).reshape(4 * NEXP, D)
        wgu = np.asarray(inp["moe_w_gate_up"])
        wdn = np.asarray(inp["moe_w_down"])
    if cfg.mixer:
        ewi, ewo = f(inp["even_w_in"]), f(inp["even_w_out"])
        owi, owo = f(inp["odd_w_in"]), f(inp["odd_w_out"])
        cw = f(inp["gdn_conv_w"])
        oh = t5_onehot()
    for c in range(8):
        b, r = c // 4, c % 4
        m = {"x_in": np.ascontiguousarray(x[b, r * TOK:(r + 1) * TOK, :]), "lng": lng, "lnb": lnb}
        if cfg.moe:
            m["router_w"] = f(inp["router_w"])
            m["router_b"] = f(inp["router_b"])
            m["bgu"] = bgu
            m["bdn"] = bdn
            m["wgu_sh"] = np.ascontiguousarray(wgu[WL0:WL0 + NLW, c * EPC:(c + 1) * EPC], dtype=np.float32).reshape(NLW * EPC * D, 2 * D)
            m["wdn_sh"] = np.ascontiguousarray(wdn[WL0:WL0 + NLW, c * EPC:(c + 1) * EPC], dtype=np.float32).reshape(NLW * EPC * D, D)
        if cfg.mixer:
            h = r
            A = 512
            cols = np.concatenate([np.arange(h * 128, h * 128 + 128), A + np.arange(h * 128, h * 128 + 128),
                                   2 * A + np.arange(h * 128, h * 128 + 128),
                                   1536 + np.arange(h * 128, h * 128 + 128), 2048 + np.arange(h * 128, h * 128 + 128),
                                   2560 + np.arange(h * 128, h * 128 + 128), 3072 + np.arange(h * 128, h * 128 + 128),
                                   [3584 + h], [3588 + h]])
            m["we_in"] = np.ascontiguousarray(ewi[:, :, cols])
            perm = np.concatenate([np.concatenate([np.arange(s_ * 128, s_ * 128 + 128), 512 + np.arange(s_ * 128, s_ * 128 + 128)]) for s_ in range(4)])
            m["we_out"] = np.ascontiguousarray(ewo[:, perm, :])
            TC = min(2048, SEQ)
            gtok = r * TOK + np.arange(TOK)
            jj, tt_ = gtok // TC, gtok % TC
            oi = np.stack([(jj * 4 + s_) * TC + tt_ for s_ in range(4)], 1)
            m["oidx"] = np.ascontiguousarray(oi.reshape(TOK // 128, 128, 4).transpose(1, 0, 2).reshape(128, TOK // 32).astype(np.int32))
            ccols = np.concatenate([np.arange(h * 128, h * 128 + 128), 512 + np.arange(h * 128, h * 128 + 128), 1024 + np.arange(h * 128, h * 128 + 128)])
            m["convw"] = np.ascontiguousarray(cw[:, :, ccols].reshape(2, 4, 3, 128).transpose(0, 3, 2, 1).reshape(2, 128, 12))
            m["lamp"] = f(inp["diff_lambda"]).reshape(2, 256)
            m["subw"] = f(inp["diff_subln_w"])
            m["gnw"] = f(inp["gdn_norm_w"])
            m["alog"] = np.ascontiguousarray(f(inp["gdn_a_log"])[:, h:h + 1])
            m["dtb"] = np.ascontiguousarray(f(inp["gdn_dt_bias"])[:, h:h + 1])
            m["t5col"] = np.ascontiguousarray(f(inp["t5_bias"])[:, h:h + 1])
            m["t5oh"] = oh
            oc = []
            for hh in range(4 * r, 4 * r + 4):
                oc += [np.arange(hh * 64, hh * 64 + 64), 1024 + np.arange(hh * 64, hh * 64 + 64),
                       2048 + np.arange(hh * 64, hh * 64 + 64), [4096 + hh], 3072 + np.arange(hh * 64, hh * 64 + 64)]
            oc = np.concatenate(oc)
            m["wo_in"] = np.ascontiguousarray(owi[:, :, oc])
            m["wo_out"] = owo
            m["qkw"] = np.ascontiguousarray(f(inp["fox_qk_norm_w"]).transpose(0, 2, 1))
            m["fb"] = np.ascontiguousarray(f(inp["fox_forget_b"])[:, 4 * r:4 * r + 4])
        maps.append(m)
    return maps


_CACHE = {}


def run_cfg(inp, cfg):
    key = (cfg.SEQ, cfg.NEXP, cfg.CAP, cfg.LAYERS, cfg.NLW, cfg.WL0, cfg.mixer, cfg.moe)
    if key not in _CACHE:
        _CACHE[key] = build(cfg)
    nc = _CACHE[key]
    maps = prep_inputs(inp, cfg)
    has_even = cfg.mixer and any(L % 2 == 0 for L in cfg.LAYERS)
    has_odd = cfg.mixer and any(L % 2 == 1 for L in cfg.LAYERS)
    ev = ("we_in", "we_out", "convw", "lamp", "subw", "gnw", "alog", "dtb", "t5col", "t5oh")
    od = ("wo_in", "wo_out", "qkw", "fb")
    for m in maps:
        for k in list(m):
            if (k in ev and not has_even) or (k in od and not has_odd):
                del m[k]
    res = run_bass_kernel_spmd(nc, maps, core_ids=list(range(8)))
    outs = [res.results[c]["out"] for c in range(8)]
    TOK = cfg.TOK
    full = np.zeros((2, cfg.SEQ, D), np.float32)
    for c in range(8):
        full[c // 4, (c % 4) * TOK:(c % 4 + 1) * TOK, :] = outs[c]
    return full


def kernel(**inputs):
    return run_cfg(inputs, Cfg())
```

```python
import math
from contextlib import ExitStack
import numpy as np
import concourse.bass as bass
import concourse.mybir as mybir
from concourse.bass_utils import run_bass_kernel_spmd

F32 = mybir.dt.float32
BF16 = mybir.dt.bfloat16
I32 = mybir.dt.int32
U32 = mybir.dt.uint32
AF = mybir.ActivationFunctionType
ALU = mybir.AluOpType
AX = mybir.AxisListType

COMPUTE = ("pe", "act", "dve", "pool")


class Buf:
    def __init__(self, name, handle, is_dram=False):
        self.name = name
        self.h = handle
        self.is_dram = is_dram
        self.w = {}
        self.r = {}
        self.dsem = None
        self.dcnt = 0

    def __getitem__(self, idx):
        return self.h.ap()[idx] if self.is_dram else self.h[idx]

    def ap(self):
        return self.h.ap() if self.is_dram else self.h[:]


class Sched:
    def __init__(self, nc, es):
        self.nc = nc
        self.es = es
        self.streams = {k: [] for k in ("pe", "act", "dve", "pool", "sp")}
        self.sems = {}
        self.latest = {}
        self.cnt = {k: 0 for k in COMPUTE}
        self.waited = {k: {} for k in self.streams}
        self.nbuf = 0
        self.es_local = es
        self.dq = {}
        self.dq_i = {}
        self.all_bufs = []
        import os
        self.sw_thresh = int(os.environ.get("KSW", "30000"))
        for k in COMPUTE:
            self._sem("E_" + k)

    def _sem(self, key):
        if key not in self.sems:
            self.sems[key] = self.es.enter_context(self.nc.semaphore("s%d_%s" % (len(self.sems), key[:12])))
            self.latest[key] = 0
        return self.sems[key]

    def sbuf(self, name, shape, dt):
        self.nbuf += 1
        h = self.es_local.enter_context(self.nc.sbuf_tensor("%s_%d" % (name, self.nbuf), list(shape), dt))
        b = Buf(name, h)
        b.local = self.es_local is not self.es
        self.all_bufs.append(b)
        return b

    def psum(self, name, shape, dt=F32):
        self.nbuf += 1
        h = self.es_local.enter_context(self.nc.psum_tensor("%s_%d" % (name, self.nbuf), list(shape), dt))
        b = Buf(name, h)
        b.local = self.es_local is not self.es
        self.all_bufs.append(b)
        return b

    def ring(self, name, shape, dt, n, psum=False):
        return Ring([(self.psum if psum else self.sbuf)("%s%d" % (name, i), shape, dt) for i in range(n)])

    def _dq(self, kind):
        if kind not in self.dq:
            n = {"hw": 12, "sw": 8, "cc": 1}[kind]
            self.dq[kind] = ["D_%s_%d" % (kind, i) for i in range(n)]
            for k in self.dq[kind]:
                self._sem(k)
            self.dq_i[kind] = 0
        key = self.dq[kind][self.dq_i[kind] % len(self.dq[kind])]
        self.dq_i[kind] += 1
        return key

    def phase(self):
        return _Phase(self)

    def dram(self, name, shape, dt, kind=None):
        if kind is None:
            h = self.nc.dram_tensor(name, list(shape), dt)
        else:
            h = self.nc.dram_tensor(name, list(shape), dt, kind=kind)
        b = Buf(name, h, is_dram=True)
        self.all_bufs.append(b)
        return b

    def _need(self, eng, reads, writes, mykey, prev=None):
        need = {}

        def add(k, v):
            if k == mykey == "E_pe":
                return
            if need.get(k, 0) < v:
                need[k] = v
        if prev is not None and prev[1] > 0:
            add(*prev)
        for b in reads:
            for k, v in b.w.items():
                add(k, v)
        for b in writes:
            for k, v in b.w.items():
                add(k, v)
            for k, v in b.r.items():
                add(k, v)
        out = []
        wd = self.waited[eng]
        for k, v in need.items():
            if wd.get(k, 0) < v:
                wd[k] = v
                out.append((self.sems[k], v))
        return out

    def op(self, eng, fn, reads=(), writes=()):
        key = "E_" + eng
        waits = self._need(eng, reads, writes, key)
        self.cnt[eng] += 1
        val = self.cnt[eng]
        self.latest[key] = val
        sem = self.sems[key]
        st = self.streams[eng]
        for s, v in waits:
            st.append(lambda e, s=s, v=v: e.wait_ge(s, v))
        st.append(lambda e, fn=fn, sem=sem: fn(e).then_inc(sem, 1))
        for b in reads:
            if b.r.get(key, 0) < val:
                b.r[key] = val
        for b in writes:
            b.w = {key: val}
            b.r = {}

    def dma(self, q, out_ap, in_ap, dst, src, fn=None, extra=(), **kw):
        key = self._dq("sw" if q == "pool" else "hw")
        wr = [dst] if (dst.r or not all(k.startswith("D_") for k in dst.w)) else []
        waits = self._need(q, ([src] if src is not None else []) + list(extra), wr, key, prev=(key, self.latest[key]))
        val = self.latest[key] + 16
        self.latest[key] = val
        sem = self.sems[key]
        st = self.streams[q]
        for s, v in waits:
            st.append(lambda e, s=s, v=v: e.wait_ge(s, v))
        if fn is None:
            st.append(lambda e: e.dma_start(out=out_ap, in_=in_ap, **kw).then_inc(sem, 16))
        else:
            st.append(lambda e: fn(e).then_inc(sem, 16))
        for b in ([src] if src is not None else []) + list(extra):
            if b.r.get(key, 0) < val:
                b.r[key] = val
        if wr:
            dst.w = {key: val}
        else:
            dst.w[key] = val
        dst.r = {}

    def cc(self, kind, op, groups, src, dst, src_ap=None, dst_ap=None):
        sap = src.h.ap().opt() if src_ap is None else src_ap.opt()
        dap = dst.h.ap().opt() if dst_ap is None else dst_ap.opt()
        key = self._dq("cc")
        waits = self._need("pool", [src], [dst], key)
        val = self.latest[key] + 1
        self.latest[key] = val
        sem = self.sems[key]
        st = self.streams["pool"]
        for s, v in waits:
            st.append(lambda e, s=s, v=v: e.wait_ge(s, v))
        st.append(lambda e: e.collective_compute(kind, op, replica_groups=groups,
                                                 ins=[sap], outs=[dap]).then_inc(sem, 1))
        st.append(lambda e: e.wait_ge(sem, val))
        self.waited["pool"][key] = val
        src.r[key] = val
        dst.w = {key: val}
        dst.r = {}

    def barrier(self):
        for eng, st in self.streams.items():
            wd = self.waited[eng]
            for k, v in self.latest.items():
                if v > 0 and wd.get(k, 0) < v:
                    wd[k] = v
                    st.append(lambda e, s=self.sems[k], v=v: e.wait_ge(s, v))

    def emit(self):
        self.barrier()
        for eng in COMPUTE:
            key = "E_" + eng
            if self.cnt[eng] > self.sw_thresh:
                self.nsw = getattr(self, "nsw", 0) + 1
                self.sems[key] = self.es.enter_context(self.nc.semaphore("sw%d_%s" % (self.nsw, eng)))
                self.cnt[eng] = 0
                self.latest[key] = 0
                for wd in self.waited.values():
                    wd.pop(key, None)
                for b in self.all_bufs:
                    b.w.pop(key, None)
                    b.r.pop(key, None)
        streams = self.streams
        self.streams = {k: [] for k in streams}
        self.ninst = getattr(self, "ninst", 0) + sum(len(v) for v in streams.values())
        import os
        if os.environ.get("KCOUNT"):
            return
        self._emit(streams)

    def _emit(self, streams):
        class _S:
            pass
        self_ = _S()
        self_.streams = streams
        nc = self.nc
        self = self_
        with nc.Block() as block:
            @block.tensor
            def _(e):
                for f in self.streams["pe"]:
                    f(e)

            @block.scalar
            def _(e):
                for f in self.streams["act"]:
                    f(e)

            @block.vector
            def _(e):
                for f in self.streams["dve"]:
                    f(e)

            @block.gpsimd
            def _(e):
                for f in self.streams["pool"]:
                    f(e)

            @block.sync
            def _(e):
                for f in self.streams["sp"]:
                    f(e)


class Ring:
    def __init__(self, bufs):
        self.bufs = bufs
        self.i = 0

    def get(self):
        b = self.bufs[self.i % len(self.bufs)]
        self.i += 1
        return b


class _Phase:
    def __init__(self, S):
        self.S = S

    def __enter__(self):
        self.old = self.S.es_local
        self.stack = ExitStack()
        self.stack.__enter__()
        self.S.es_local = self.stack
        return self.S

    def __exit__(self, *a):
        S = self.S
        stop = False
        if a[0] is None:
            S.emit()

            S.nphase = getattr(S, "nphase", 0) + 1
            import os
            stop = S.nphase == int(os.environ.get("KSTOP", "0"))
        S.es_local = self.old
        self.stack.__exit__(*a)
        if stop:
            raise _StopBuild()
        return False


class _StopBuild(Exception):
    pass


D = 1024
ALPHA = (2 * 4) ** 0.25
LN_EPS = 1e-5
RMS_EPS = 1e-6
G4 = [[0, 1, 2, 3], [4, 5, 6, 7]]
G2 = [[0, 4], [1, 5], [2, 6], [3, 7]]
NEG = -30000.0


class Cfg:
    def __init__(self, SEQ=16384, NEXP=32, CAP=640, LAYERS=(0, 1, 2, 3), NLW=4, WL0=0, mixer=True, moe=True):
        self.SEQ, self.NEXP, self.CAP, self.LAYERS, self.NLW = SEQ, NEXP, CAP, tuple(LAYERS), NLW
        self.mixer, self.moe, self.WL0 = mixer, moe, WL0
        self.TOK = SEQ // 4
        self.EPC = NEXP // 8


def build(cfg):
    nc = bass.Bass("TRN2", target_bir_lowering=False)
    es = ExitStack()
    with es:
        S = Sched(nc, es)
        try:
            _build(S, cfg)
        except _StopBuild:
            pass
        print("kernel build: instructions incl. waits =", getattr(S, "ninst", 0), flush=True)
    return nc


def _build(S, cfg):
    SEQ, TOK, NEXP, C, EPC, NLW = cfg.SEQ, cfg.TOK, cfg.NEXP, cfg.CAP, cfg.EPC, cfg.NLW
    NBL = TOK // 128
    NTL = TOK // 512
    NBF = SEQ // 128
    NQT = SEQ // 512
    CB = C // 128
    TRASH = NEXP * C

    def MM(out, lhsT, rhs, start, stop, R, W):
        S.op("pe", lambda e: e.matmul(out, lhsT, rhs, start=start, stop=stop), reads=R, writes=W)

    def TR(out, in_, ident, R, W):
        S.op("pe", lambda e: e.transpose(out, in_, ident), reads=R, writes=W)

    def ACT(out, in_, func, R, W, bias=None, scale=None, accum=None):
        kw = {}
        if bias is not None:
            kw["bias"] = bias
        if scale is not None:
            kw["scale"] = scale
        if accum is not None:
            kw["accum_out"] = accum
        S.op("act", lambda e: e.activation(out=out, in_=in_, func=func, **kw), reads=R, writes=W)

    def TS(eng, out, in0, s1, op0, R, W, s2=None, op1=None, accum=None):
        kw = {}
        if op1 is not None:
            kw["op1"] = op1
        if accum is not None:
            kw["accum_out"] = accum
        S.op(eng, lambda e: e.tensor_scalar(out=out, in0=in0, scalar1=s1, scalar2=s2, op0=op0, **kw), reads=R, writes=W)

    def TT(eng, out, in0, in1, op, R, W):
        S.op(eng, lambda e: e.tensor_tensor(out=out, in0=in0, in1=in1, op=op), reads=R, writes=W)

    def STT(out, in0, scalar, in1, op0, op1, R, W, accum=None):
        kw = {}
        if accum is not None:
            kw["accum_out"] = accum
        S.op("dve", lambda e: e.scalar_tensor_tensor(out=out, in0=in0, scalar=scalar, in1=in1, op0=op0, op1=op1, **kw),
             reads=R, writes=W)

    def CP(eng, out, in_, R, W):
        if eng == "act":
            S.op("act", lambda e: e.copy(out=out, in_=in_), reads=R, writes=W)
        else:
            S.op(eng, lambda e: e.tensor_copy(out=out, in_=in_), reads=R, writes=W)

    def MEMSET(eng, ap, val, W):
        S.op(eng, lambda e: e.memset(ap, val), writes=W)

    def RECIP(out, in_, R, W):
        S.op("dve", lambda e: e.reciprocal(out=out, in_=in_), reads=R, writes=W)

    def ASEL(out, in_, pattern, cmp, fill, base, cm, R, W):
        S.op("pool", lambda e: e.affine_select(out=out, in_=in_, pattern=pattern, compare_op=cmp, fill=fill,
                                                base=base, channel_multiplier=cm), reads=R, writes=W)

    def din(name, shape, dt=F32):
        return S.dram(name, shape, dt, kind="ExternalInput")

    x_in = din("x_in", [TOK, D])
    lng = din("lng", [8, D])
    lnb = din("lnb", [8, D])
    out = S.dram("out", [TOK, D], F32, kind="ExternalOutput")
    if cfg.moe:
        router_w = din("router_w", [4, D, NEXP])
        router_b = din("router_b", [4, NEXP])
        bgu = din("bgu", [128, 4 * NEXP * 16])
        bdn = din("bdn", [4 * NEXP, D])
        wgu_sh = din("wgu_sh", [NLW * EPC * D, 2 * D])
        wdn_sh = din("wdn_sh", [NLW * EPC * D, D])
    has_even = cfg.mixer and any(L % 2 == 0 for L in cfg.LAYERS)
    has_odd = cfg.mixer and any(L % 2 == 1 for L in cfg.LAYERS)
    if has_even:
        we_in = din("we_in", [2, D, 898])
        we_out = din("we_out", [2, D, D])
        convw = din("convw", [2, 128, 12])
        lamp = din("lamp", [2, 256])
        subw = din("subw", [2, 128])
        gnw = din("gnw", [2, 128])
        alog = din("alog", [2, 1])
        dtb = din("dtb", [2, 1])
        t5col = din("t5col", [32, 1])
        t5oh = din("t5oh", [33, 2560])
    if has_odd:
        wo_in = din("wo_in", [2, D, 1028])
        wo_out = din("wo_out", [2, D, D])
        qkw = din("qkw", [2, 64, 2])
        fb = din("fb", [2, 4])

    XR = [S.dram("xres0", [TOK, D], F32), S.dram("xres1", [TOK, D], F32)]
    XR1 = S.dram("xr1", [TOK, D], F32)
    XTB = S.dram("xtb", [D, TOK], BF16)
    XTALL = S.dram("xtall", [4 * D, TOK], BF16)
    TC = min(2048, SEQ)
    if cfg.mixer:
        OTOK = S.dram("otok", [SEQ, 256], BF16)
        OALL = S.dram("oall", [4 * SEQ, 256], BF16)
        oidx = din("oidx", [128, NBL * 4], I32)
        CQ = S.dram("cq", [3, SEQ], BF16)
        O0S = S.dram("o0s", [SEQ, 128], F32)
        BV = S.dram("bv", [2560], F32)
        SKW = S.dram("skw", [16, 128 * 640], F32)
    if cfg.moe:
        XDISP = S.dram("xdisp", [NEXP * C + 128, D], BF16)
        YDISP = S.dram("ydisp", [NEXP * C + 128, D], F32)
        R1 = NLW * EPC * D
        WB1G = S.dram("wb1g", [R1, 2 * D], BF16)
        WB1D = S.dram("wb1d", [R1, D], BF16)
        RGG, RGD = 256, 512
        NCG, NCD = R1 // RGG, R1 // RGD
        WALLG = [S.dram("wallg%d" % i, [16 * 8 * RGG, 2 * D], BF16) for i in range((NCG + 15) // 16)]
        WALLD = [S.dram("walld%d" % i, [16 * 8 * RGD, D], BF16) for i in range((NCD + 15) // 16)]
        S1G = [S.dram("s1g%d" % i, [2 * RGG, 2 * D], BF16) for i in range(2)]
        S1D = [S.dram("s1d%d" % i, [2 * RGD, D], BF16) for i in range(2)]

    ident_f = S.sbuf("ident_f", [128, 128], F32)
    ident_b = S.sbuf("ident_b", [128, 128], BF16)
    ones_f = S.sbuf("ones_f", [128, 128], F32)
    ones_b = S.sbuf("ones_b", [128, 128], BF16)
    triU_f = S.sbuf("triU_f", [128, 128], F32)
    Ls_b = S.sbuf("Ls_b", [128, 128], BF16)
    DESTI = S.sbuf("DESTI", [128, NBL * 4], I32)
    GATE = S.sbuf("GATE", [128, NBL * 4], F32)
    CNT = S.sbuf("CNT", [128, NEXP], F32)
    EOFF = S.sbuf("EOFF", [128, NEXP], F32)
    with S.phase():
        tmpf = S.sbuf("tmpf", [128, 128], F32)
        tmpi = S.sbuf("tmpi", [128, NEXP], I32)
        MEMSET("pool", ident_f[:], 0.0, [ident_f])
        ASEL(ident_f[:], ident_f[:], [[-1, 128]], ALU.not_equal, 1.0, 0, 1, [ident_f], [ident_f])
        CP("dve", ident_b[:], ident_f[:], [ident_f], [ident_b])
        MEMSET("pool", ones_f[:], 1.0, [ones_f])
        MEMSET("pool", ones_b[:], 1.0, [ones_b])
        MEMSET("pool", triU_f[:], 1.0, [triU_f])
        ASEL(triU_f[:], triU_f[:], [[1, 128]], ALU.is_ge, 0.0, 0, -1, [triU_f], [triU_f])
        MEMSET("pool", tmpf[:], 1.0, [tmpf])
        ASEL(tmpf[:], tmpf[:], [[1, 128]], ALU.is_ge, 0.0, -1, -1, [tmpf], [tmpf])
        CP("dve", Ls_b[:], tmpf[:], [tmpf], [Ls_b])
        S.op("pool", lambda e: e.iota(tmpi[:], pattern=[[C, NEXP]], base=0, channel_multiplier=0), writes=[tmpi])
        CP("dve", EOFF[:], tmpi[:], [tmpi], [EOFF])

    if cfg.moe:
        with S.phase():
            st_r = S.ring("wst", [128, 2 * D], F32, 3)
            sb_r = S.ring("wsb", [128, 2 * D], BF16, 3)
            n = 0
            for (src, b1, ncol) in ((wgu_sh, WB1G, 2 * D), (wdn_sh, WB1D, D)):
                rows = 128 * (2 * D // ncol)
                for r0 in range(0, R1, rows):
                    st = st_r.get()
                    sb = sb_r.get()
                    k = rows // 128
                    S.dma("sp", st[:].rearrange("p (k n) -> p k n", k=k), src[r0:r0 + rows, :].rearrange("(k p) n -> p k n", p=128), st, src)
                    CP("act" if n % 2 == 0 else "dve", sb[:], st[:], [st], [sb])
                    S.dma("sp", b1[r0:r0 + rows, :].rearrange("(k p) n -> p k n", p=128), sb[:].rearrange("p (k n) -> p k n", k=k), b1, sb)
                    n += 1
            zt = S.sbuf("zt", [128, D], F32)
            ztb = S.sbuf("ztb", [128, D], BF16)
            MEMSET("dve", zt[:], 0.0, [zt])
            MEMSET("dve", ztb[:], 0.0, [ztb])
            for r0 in range(0, NEXP * C + 128, 128):
                S.dma("sp", XDISP[r0:r0 + 128, :], ztb[:], XDISP, ztb)
                S.dma("sp", YDISP[r0:r0 + 128, :], zt[:], YDISP, zt)

    def w_collectives():
        for (b1, s1s, walls, RG, NC_) in ((WB1G, S1G, WALLG, RGG, NCG), (WB1D, S1D, WALLD, RGD, NCD)):
            for i in range(NC_):
                s1 = s1s[i % 2]
                S.cc("AllGather", ALU.bypass, G2, b1, s1, src_ap=b1[i * RG:(i + 1) * RG, :])
                wt = walls[i // 16]
                for g in range(2):
                    base = (((i % 16) * 2 + g) * 4) * RG
                    S.cc("AllGather", ALU.bypass, G4, s1, wt, src_ap=s1[g * RG:(g + 1) * RG, :],
                         dst_ap=wt[base:base + 4 * RG, :])

    S.w_pending = cfg.moe

    def wloc(L, e, RG, chunk):
        c, le = e // EPC, e % EPC
        g, r4 = c // 4, c % 4
        rr = ((L - cfg.WL0) * EPC + le) * D + chunk * RG
        i = rr // RG
        return i // 16, ((((i % 16) * 2 + g) * 4 + r4) * RG)

    XTBv = XTB.ap().rearrange("(k p) t -> p k t", p=128)

    def emit_xT_block(xt_f32, xT_tile, col, ps_ring, cp_eng):
        ps = ps_ring.get()
        for kc in range(8):
            TR(ps[:, kc * 128:(kc + 1) * 128], xt_f32[:, kc * 128:(kc + 1) * 128], ident_f[:], [xt_f32, ident_f], [ps])
        CP(cp_eng, xT_tile[:, :, col:col + 128], ps[:].rearrange("p (k t) -> p k t", k=8), [ps], [xT_tile])

    def xt_allgather():
        for kc in range(8):
            S.cc("AllGather", ALU.bypass, G4, XTB, XTALL, src_ap=XTB[kc * 128:(kc + 1) * 128, :],
                 dst_ap=XTALL[kc * 512:(kc + 1) * 512, :])

    def phase_x0():
        with S.phase():
            xin_r = S.ring("xin", [128, D], F32, 3)
            xT_r = S.ring("xT", [128, 8, 512], BF16, 2)
            ps_r = S.ring("psx", [128, D], F32, 2, psum=True)
            for tl in range(NTL):
                xT = xT_r.get()
                for b4 in range(4):
                    b = tl * 4 + b4
                    xt = xin_r.get()
                    S.dma("sp", xt[:], x_in[b * 128:(b + 1) * 128, :], xt, x_in)
                    emit_xT_block(xt, xT, b4 * 128, ps_r, "act" if b4 % 2 == 0 else "dve")
                S.dma("sp", XTBv[:, :, tl * 512:(tl + 1) * 512], xT[:], XTB, xT)
            xt_allgather()

    def layer_norm(z, outt, gt, bt, st_r, junk):
        st = st_r.get()
        ACT(junk[:], z[:], AF.Identity, [z], [junk, st], accum=st[:, 0:1])
        ACT(junk[:], z[:], AF.Square, [z], [junk, st], accum=st[:, 1:2])
        TS("dve", st[:, 2:3], st[:, 0:1], 1.0 / D, ALU.mult, [st], [st])
        TT("dve", st[:, 3:4], st[:, 2:3], st[:, 2:3], ALU.mult, [st], [st])
        STT(st[:, 4:5], st[:, 1:2], 1.0 / D, st[:, 3:4], ALU.mult, ALU.subtract, [st], [st])
        TS("dve", st[:, 4:5], st[:, 4:5], LN_EPS, ALU.add, [st], [st])
        ACT(st[:, 5:6], st[:, 4:5], AF.Sqrt, [st], [st])
        RECIP(st[:, 6:7], st[:, 5:6], [st], [st])
        TS("dve", outt[:], z[:], st[:, 2:3], ALU.subtract, [z, st], [outt], s2=st[:, 6:7], op1=ALU.mult)
        TT("dve", outt[:], outt[:], gt[:], ALU.mult, [outt, gt], [outt])
        TT("dve", outt[:], outt[:], bt[:], ALU.add, [outt, bt], [outt])

    def phase_ln_moe(L, xres, xnext, is_last):
        with S.phase():
            gt = S.sbuf("gt", [128, D], F32)
            bt = S.sbuf("bt", [128, D], F32)
            S.dma("sp", gt[:], lng.ap()[L, :].partition_broadcast(128), gt, lng)
            S.dma("sp", bt[:], lnb.ap()[L, :].partition_broadcast(128), bt, lnb)
            xr_r = S.ring("xr", [128, D], F32, 2)
            if cfg.mixer:
                wsrc = we_out if L % 2 == 0 else wo_out
                WO = S.sbuf("WO", [128, 8, D], BF16)
                for q4 in range(4):
                    S.dma("pool", WO[:, 2 * q4:2 * q4 + 2, :],
                          wsrc.ap()[L // 2, q4 * 256:(q4 + 1) * 256, :].rearrange("(k p) n -> p k n", p=128), WO, wsrc)
                OIDX = S.sbuf("OIDX", [128, NBL * 4], I32)
                S.dma("sp", OIDX[:], oidx[:, :], OIDX, oidx)
                og_r = S.ring("og", [128, 4, 256], BF16, 2)
                oT_r = S.ring("oT", [128, 8, 128], BF16, 2)
                pso_r = S.ring("pso", [128, D], BF16, 1, psum=True)
                psh_r = S.ring("psh", [128, 512], F32, 2, psum=True)
            z_r = S.ring("z", [128, D], F32, 2)
            x1_r = S.ring("x1", [128, D], F32, 2)
            x1b_r = S.ring("x1b", [128, D], BF16, 2)
            junk = S.sbuf("junk", [128, D], F32)
            st_r = S.ring("st", [128, 8], F32, 2)
            if cfg.moe:
                RW = S.sbuf("RW", [128, 8, NEXP], F32)
                RB = S.sbuf("RB", [128, NEXP], F32)
                S.dma("sp", RW[:], router_w.ap()[L].rearrange("(k p) e -> p k e", p=128), RW, router_w)
                S.dma("sp", RB[:], router_b.ap()[L, :].partition_broadcast(128), RB, router_b)
                x1T_r = S.ring("x1T", [128, 8, 128], F32, 2)
                psx_r = S.ring("psx", [128, D], F32, 1, psum=True)
                psl_r = S.ring("psl", [128, 512], F32, 1, psum=True)
                psp_r = S.ring("psp", [128, 512], F32, 2, psum=True)
                sm_r = S.ring("sm", [128, 12, NEXP], F32, 2)
                mb_r = S.ring("mb", [128, NEXP], BF16, 2)
                t8_r = S.ring("t8", [128, 8], F32, 2)
                sc_r = S.ring("sc", [128, 16], F32, 2)
                MEMSET("dve", CNT[:], 0.0, [CNT])
            for b in range(NBL):
                xr = xr_r.get()
                S.dma("sp", xr[:], xres[b * 128:(b + 1) * 128, :], xr, xres)
                z = z_r.get()
                if cfg.mixer:
                    og = og_r.get()
                    for s4 in range(4):
                        col = b * 4 + s4
                        S.dma("pool", None, None, og, OALL, extra=[OIDX],
                              fn=lambda e, col=col, og=og, s4=s4: e.indirect_dma_start(
                                  out=og[:, s4, :], out_offset=None, in_=OALL.ap()[:, :],
                                  in_offset=bass.IndirectOffsetOnAxis(ap=OIDX[:, col:col + 1], axis=0)))
                    pso = pso_r.get()
                    for kc in range(8):
                        TR(pso[:, kc * 128:(kc + 1) * 128], og[:, kc // 2, (kc % 2) * 128:(kc % 2 + 1) * 128], ident_b[:], [og, ident_b], [pso])
                    oT = oT_r.get()
                    CP("act", oT[:], pso[:].rearrange("p (k t) -> p k t", k=8), [pso], [oT])
                    for half in range(2):
                        psh = psh_r.get()
                        for kc in range(8):
                            MM(psh[:], oT[:, kc, :], WO[:, kc, half * 512:(half + 1) * 512], kc == 0, kc == 7, [oT, WO], [psh])
                        STT(z[:, half * 512:(half + 1) * 512], xr[:, half * 512:(half + 1) * 512], ALPHA, psh[:], ALU.mult, ALU.add, [xr, psh], [z])
                else:
                    TS("dve", z[:], xr[:], ALPHA, ALU.mult, [xr], [z])
                x1 = x1_r.get()
                layer_norm(z, x1, gt, bt, st_r, junk)
                S.dma("sp", XR1[b * 128:(b + 1) * 128, :], x1[:], XR1, x1)
                if not cfg.moe:
                    continue
                x1b = x1b_r.get()
                CP("act", x1b[:], x1[:], [x1], [x1b])
                x1T = x1T_r.get()
                ps = psx_r.get()
                for kc in range(8):
                    TR(ps[:, kc * 128:(kc + 1) * 128], x1[:, kc * 128:(kc + 1) * 128], ident_f[:], [x1, ident_f], [ps])
                CP("act", x1T[:], ps[:].rearrange("p (k t) -> p k t", k=8), [ps], [x1T])
                psl = psl_r.get()
                for kc in range(8):
                    MM(psl[:, 0:NEXP], x1T[:, kc, :], RW[:, kc, :], kc == 0, kc == 7, [x1T, RW], [psl])
                sm = sm_r.get()
                lg, ex, mk, gd, gates, pos, dfull, oh, jk = (sm[:, i, :] for i in range(9))
                t8 = t8_r.get()
                sc = sc_r.get()
                TT("dve", lg, psl[:, 0:NEXP], RB[:], ALU.add, [psl, RB], [sm])
                S.op("dve", lambda e, t8=t8, lg=lg: e.max(out=t8[:], in_=lg), reads=[sm], writes=[t8])
                TS("dve", sc[:, 0:1], t8[:, 0:1], -1.0, ALU.mult, [t8], [sc])
                ACT(ex, lg, AF.Exp, [sm, sc], [sm], bias=sc[:, 0:1])
                TS("dve", mk, lg, t8[:, 3:4], ALU.is_ge, [sm, t8], [sm])
                TT("dve", gd, ex, mk, ALU.mult, [sm], [sm])
                S.op("dve", lambda e, sc=sc, gd=gd: e.tensor_reduce(out=sc[:, 1:2], in_=gd, axis=AX.X, op=ALU.add),
                     reads=[sm], writes=[sc])
                RECIP(sc[:, 2:3], sc[:, 1:2], [sc], [sc])
                TS("dve", gates, gd, sc[:, 2:3], ALU.mult, [sm, sc], [sm])
                mb = mb_r.get()
                CP("dve", mb[:], mk, [sm], [mb])
                psp = psp_r.get()
                MM(psp[:, 0:NEXP], Ls_b[:], mb[:], True, True, [Ls_b, mb], [psp])
                TT("dve", pos, psp[:, 0:NEXP], CNT[:], ALU.add, [psp, CNT], [sm])
                psc = psp_r.get()
                MM(psc[:, 0:NEXP], ones_b[:], mb[:], True, True, [ones_b, mb], [psc])
                TT("dve", CNT[:], CNT[:], psc[:, 0:NEXP], ALU.add, [CNT, psc], [CNT])
                TT("dve", dfull, pos, EOFF[:], ALU.add, [sm, EOFF], [sm])
                for k in range(4):
                    TS("dve", oh, lg, t8[:, k:k + 1], ALU.is_equal, [sm, t8], [sm])
                    TT("dve", jk, oh, pos, ALU.mult, [sm], [sm])
                    S.op("dve", lambda e, sc=sc, jk=jk: e.tensor_reduce(out=sc[:, 4:5], in_=jk, axis=AX.X, op=ALU.add),
                         reads=[sm], writes=[sc])
                    TT("dve", jk, oh, dfull, ALU.mult, [sm], [sm])
                    S.op("dve", lambda e, sc=sc, jk=jk: e.tensor_reduce(out=sc[:, 5:6], in_=jk, axis=AX.X, op=ALU.add),
                         reads=[sm], writes=[sc])
                    TT("dve", jk, oh, gates, ALU.mult, [sm], [sm])
                    S.op("dve", lambda e, sc=sc, jk=jk: e.tensor_reduce(out=sc[:, 6:7], in_=jk, axis=AX.X, op=ALU.add),
                         reads=[sm], writes=[sc])
                    TS("dve", sc[:, 7:8], sc[:, 4:5], float(C), ALU.is_lt, [sc], [sc])
                    TS("dve", sc[:, 8:9], sc[:, 5:6], float(TRASH), ALU.subtract, [sc], [sc])
                    TT("dve", sc[:, 8:9], sc[:, 8:9], sc[:, 7:8], ALU.mult, [sc], [sc])
                    TS("dve", sc[:, 8:9], sc[:, 8:9], float(TRASH), ALU.add, [sc], [sc])
                    col = b * 4 + k
                    CP("dve", DESTI[:, col:col + 1], sc[:, 8:9], [sc], [DESTI])
                    TT("dve", GATE[:, col:col + 1], sc[:, 6:7], sc[:, 7:8], ALU.mult, [sc], [GATE])
                    S.dma("pool", None, None, XDISP, x1b, extra=[DESTI],
                          fn=lambda e, col=col, x1b=x1b: e.indirect_dma_start(
                              out=XDISP.ap()[:, :], out_offset=bass.IndirectOffsetOnAxis(ap=DESTI[:, col:col + 1], axis=0),
                              in_=x1b[:, :], in_offset=None))
        if cfg.moe:
            with S.phase():
                BG = S.sbuf("BG", [128, NEXP * 16], F32)
                S.dma("sp", BG[:], bgu[:, L * NEXP * 16:(L + 1) * NEXP * 16], BG, bgu)
                wgu_r = S.ring("wgu", [128, 8, 2 * D], BF16, 2)
                wdn_r = S.ring("wdn", [128, 8, D], BF16, 2)
                bd_r = S.ring("bd", [128, D], F32, 2)
                xs_r = S.ring("xs", [128, CB, D], BF16, 2)
                xsT_r = S.ring("xsT", [128, 8, C], BF16, 1)
                aT_r = S.ring("aT", [128, 8, C], BF16, 1)
                tmp_r = S.ring("tmp", [128, 5, 512], F32, 2)
                ys_r = S.ring("ys", [128, D], F32, 2)
                pst_r = S.ring("pst", [128, D], BF16, 2, psum=True)
                psg_r = S.ring("psg", [128, 512], F32, 2, psum=True)
                psu_r = S.ring("psu", [128, 512], F32, 2, psum=True)
                psd_r = S.ring("psd", [128, 512], F32, 2, psum=True)
                chunks = [(c0, min(512, C - c0)) for c0 in range(0, C, 512)]
                def load_expert(e_):
                    wg = wgu_r.get()
                    wd = wdn_r.get()
                    for q4 in range(4):
                        ti, row = wloc(L, e_, RGG, q4)
                        S.dma("sp", wg[:, 2 * q4:2 * q4 + 2, :],
                              WALLG[ti].ap()[row:row + 256, :].rearrange("(k p) n -> p k n", p=128), wg, WALLG[ti])
                    for q4 in range(2):
                        ti, row = wloc(L, e_, RGD, q4)
                        S.dma("sp", wd[:, 4 * q4:4 * q4 + 4, :],
                              WALLD[ti].ap()[row:row + 512, :].rearrange("(k p) n -> p k n", p=128), wd, WALLD[ti])
                    bd = bd_r.get()
                    S.dma("sp", bd[:], bdn.ap()[L * NEXP + e_, :].partition_broadcast(128), bd, bdn)
                    xs = xs_r.get()
                    S.dma("sp", xs[:], XDISP.ap()[e_ * C:(e_ + 1) * C, :].rearrange("(c p) d -> p c d", p=128), xs, XDISP)
                    return wg, wd, bd, xs

                nxt = load_expert(0)
                for e_ in range(NEXP):
                    wg, wd, bd, xs = nxt
                    if e_ + 1 < NEXP:
                        nxt = load_expert(e_ + 1)
                    xsT = xsT_r.get()
                    for cb in range(CB):
                        pst = pst_r.get()
                        for kc in range(8):
                            TR(pst[:, kc * 128:(kc + 1) * 128], xs[:, cb, kc * 128:(kc + 1) * 128], ident_b[:], [xs, ident_b], [pst])
                        CP("act" if cb % 2 == 0 else "dve", xsT[:, :, cb * 128:(cb + 1) * 128],
                           pst[:].rearrange("p (k t) -> p k t", k=8), [pst], [xsT])
                    aT = aT_r.get()
                    for j in range(8):
                        for (c0, w) in chunks:
                            psg = psg_r.get()
                            psu = psu_r.get()
                            for kc in range(8):
                                MM(psg[:, 0:w], wg[:, kc, j * 128:(j + 1) * 128], xsT[:, kc, c0:c0 + w], kc == 0, kc == 7, [wg, xsT], [psg])
                            for kc in range(8):
                                MM(psu[:, 0:w], wg[:, kc, D + j * 128:D + (j + 1) * 128], xsT[:, kc, c0:c0 + w], kc == 0, kc == 7, [wg, xsT], [psu])
                            tmp = tmp_r.get()
                            g1, sg, u1, u2, tt_ = (tmp[:, i, 0:w] for i in range(5))
                            bgc = (e_ * 16 + j)
                            TS("dve", g1, psg[:, 0:w], BG[:, bgc:bgc + 1], ALU.add, [psg, BG], [tmp], s2=7.0, op1=ALU.min)
                            ACT(sg, g1, AF.Sigmoid, [tmp], [tmp], scale=1.702)
                            TS("dve", u1, psu[:, 0:w], BG[:, bgc + 8:bgc + 9], ALU.add, [psu, BG], [tmp], s2=7.0, op1=ALU.min)
                            TS("dve", u2, u1, -7.0, ALU.max, [tmp], [tmp], s2=1.0, op1=ALU.add)
                            TT("pool", tt_, g1, sg, ALU.mult, [tmp], [tmp])
                            TT("pool", aT[:, j, c0:c0 + w], tt_, u2, ALU.mult, [tmp], [aT])
                    for cb in range(CB):
                        ys = ys_r.get()
                        for half in range(2):
                            psd = psd_r.get()
                            for j in range(8):
                                MM(psd[:], aT[:, j, cb * 128:(cb + 1) * 128], wd[:, j, half * 512:(half + 1) * 512], j == 0, j == 7, [aT, wd], [psd])
                            TT("dve", ys[:, half * 512:(half + 1) * 512], psd[:], bd[:, half * 512:(half + 1) * 512], ALU.add, [psd, bd], [ys])
                        S.dma("sp", YDISP[e_ * C + cb * 128:e_ * C + (cb + 1) * 128, :], ys[:], YDISP, ys)
        with S.phase():
            gt = S.sbuf("gt", [128, D], F32)
            bt = S.sbuf("bt", [128, D], F32)
            S.dma("sp", gt[:], lng.ap()[4 + L, :].partition_broadcast(128), gt, lng)
            S.dma("sp", bt[:], lnb.ap()[4 + L, :].partition_broadcast(128), bt, lnb)
            x1_r = S.ring("x1", [128, D], F32, 2)
            acc_r = S.ring("acc", [128, D], F32, 2)
            yk_r = S.ring("yk", [128, D], F32, 4)
            x2_r = S.ring("x2", [128, D], F32, 2)
            junk = S.sbuf("junk", [128, D], F32)
            st_r = S.ring("st", [128, 8], F32, 2)
            xT_r = S.ring("xT", [128, 8, 512], BF16, 2)
            ps_r = S.ring("psx", [128, D], F32, 2, psum=True)
            dst = out if is_last else xnext
            xT = None
            for b in range(NBL):
                x1 = x1_r.get()
                S.dma("sp", x1[:], XR1[b * 128:(b + 1) * 128, :], x1, XR1)
                acc = acc_r.get()
                TS("dve", acc[:], x1[:], ALPHA, ALU.mult, [x1], [acc])
                if cfg.moe:
                    for k in range(4):
                        col = b * 4 + k
                        yk = yk_r.get()
                        S.dma("pool", None, None, yk, YDISP, extra=[DESTI],
                              fn=lambda e, col=col, yk=yk: e.indirect_dma_start(
                                  out=yk[:, :], out_offset=None, in_=YDISP.ap()[:, :],
                                  in_offset=bass.IndirectOffsetOnAxis(ap=DESTI[:, col:col + 1], axis=0)))
                        STT(acc[:], yk[:], GATE[:, col:col + 1], acc[:], ALU.mult, ALU.add, [yk, GATE, acc], [acc])
                x2 = x2_r.get()
                layer_norm(acc, x2, gt, bt, st_r, junk)
                S.dma("sp", dst[b * 128:(b + 1) * 128, :], x2[:], dst, x2)
                if not is_last:
                    if b % 4 == 0:
                        xT = xT_r.get()
                    emit_xT_block(x2, xT, (b % 4) * 128, ps_r, "act")
                    if b % 4 == 3:
                        tl = b // 4
                        S.dma("sp", XTBv[:, :, tl * 512:(tl + 1) * 512], xT[:], XTB, xT)
            if not is_last:
                xt_allgather()


    XTALLv = XTALL.ap().rearrange("(k r p) t -> r p k t", k=8, r=4) if True else None

    def load_xT(xT, tt):
        rank, lt = tt // NTL, tt % NTL
        S.dma("sp", xT[:], XTALLv[rank][:, :, lt * 512:(lt + 1) * 512], xT, XTALL)

    def o_allgather():
        for j in range(SEQ // TC):
            S.cc("AllGather", ALU.bypass, G4, OTOK, OALL, src_ap=OTOK[j * TC:(j + 1) * TC, :],
                 dst_ap=OALL[j * 4 * TC:(j + 1) * 4 * TC, :])

    def odd_mixer(L):
        i = L // 2
        with S.phase():
            Wb = S.sbuf("Wb", [128, 8, 1028], BF16)
            for q4 in range(4):
                S.dma("pool", Wb[:, 2 * q4:2 * q4 + 2, :],
                      wo_in.ap()[i, q4 * 256:(q4 + 1) * 256, :].rearrange("(k p) n -> p k n", p=128), Wb, wo_in)
            WQK = S.sbuf("WQK", [64, 2], F32)
            S.dma("sp", WQK[:], qkw.ap()[i], WQK, qkw)
            TS("dve", WQK[:, 0:1], WQK[:, 0:1], 0.125, ALU.mult, [WQK], [WQK])
            NFB = S.sbuf("NFB", [128, 4], F32)
            S.dma("sp", NFB[:], fb.ap()[i, :].partition_broadcast(128), NFB, fb)
            TS("dve", NFB[:], NFB[:], -1.0, ALU.mult, [NFB], [NFB])
            MKf = S.sbuf("MKf", [128, 4, 512], F32)
            MK = S.sbuf("MK", [128, 4, 512], BF16)
            MEMSET("pool", MKf[:], 0.0, [MKf])
            for j in range(4):
                ASEL(MKf[:, j, :], MKf[:, j, :], [[1, 512]], ALU.is_ge, NEG, -128 * j, -1, [MKf], [MKf])
            CP("dve", MK[:], MKf[:], [MKf], [MK])
            QT = S.sbuf("QT", [67, SEQ], BF16)
            KT = S.sbuf("KT", [67, SEQ], BF16)
            GTK = S.sbuf("GTK", [128, NBF, 64], BF16)
            VA = S.sbuf("VA", [128, NBF, 65], BF16)
            LF = S.sbuf("LF", [128, NBF], F32)
            CUM = S.sbuf("CUM", [128, NBF], F32)
            NEGCUM = S.sbuf("NEGCUM", [128, NBF], F32)
            scan = [S.sbuf("scan%d" % k, [128, NBF], F32) for k in range(2)]
            tot = S.sbuf("tot", [128, NBF], F32)
            ct = S.sbuf("ct", [128, 6, 128], F32)
            ctb = S.sbuf("ctb", [128, 3, 128], BF16)
            MEMSET("dve", VA[:, :, 64:65], 1.0, [VA])
            MEMSET("dve", KT[64:67, :], 1.0, [KT])
            xT_r = S.ring("xT", [128, 8, 512], BF16, 2)
            qf_r = S.ring("qf", [64, 512], F32, 2)
            sq_r = S.ring("sq", [64, 512], F32, 2)
            rs_r = S.ring("rs", [64, 512], F32, 2)
            g1_r = S.ring("g1", [128, 64], F32, 3)
            sm_r = S.ring("smo", [128, 8], F32, 4)
            pt_r = S.ring("pt", [128, 512], BF16, 4)
            ot_r = S.ring("ot", [128, 4, 64], BF16, 2)
            osb_r = S.ring("osb", [128, 512], F32, 2)
            for b_ in osb_r.bufs:
                MEMSET("dve", b_[:], 0.0, [b_])
            psm_r = S.ring("psm", [128, 512], F32, 3, psum=True)
            pss_r = S.ring("pss", [128, 512], F32, 3, psum=True)
            pso_r = S.ring("pso", [128, 512], F32, 2, psum=True)
            import os
            KODD = int(os.environ.get("KODD", "9"))
            if KODD == 0:
                return
            for h in range(4):
                w0 = h * 257
                for tt in range(NQT):
                    xT = xT_r.get()
                    load_xT(xT, tt)
                    for (coff, dst, wi) in ((0, QT, 0), (64, KT, 1)):
                        psq = psm_r.get()
                        for kc in range(8):
                            MM(psq[0:64, :], Wb[:, kc, w0 + coff:w0 + coff + 64], xT[:, kc, :], kc == 0, kc == 7, [Wb, xT], [psq])
                        qf = qf_r.get()
                        CP("act", qf[:], psq[0:64, :], [psq], [qf])
                        sq = sq_r.get()
                        TT("pool", sq[:], qf[:], qf[:], ALU.mult, [qf], [sq])
                        pssum = psm_r.get()
                        MM(pssum[0:64, :], ones_f[0:64, 0:64], sq[:], True, True, [ones_f, sq], [pssum])
                        rs = rs_r.get()
                        TS("dve", rs[:], pssum[0:64, :], 1.0 / 64, ALU.mult, [pssum], [rs], s2=RMS_EPS, op1=ALU.add)
                        ACT(rs[:], rs[:], AF.Ln, [rs], [rs])
                        ACT(rs[:], rs[:], AF.Exp, [rs], [rs], scale=-0.5)
                        STT(dst[0:64, tt * 512:(tt + 1) * 512], qf[:], WQK[:, wi:wi + 1], rs[:], ALU.mult, ALU.mult, [qf, WQK, rs], [dst])
                    for b4 in range(4):
                        gb = tt * 4 + b4
                        ps = psm_r.get()
                        for kc in range(8):
                            MM(ps[:, 0:129], xT[:, kc, b4 * 128:(b4 + 1) * 128], Wb[:, kc, w0 + 128:w0 + 257], kc == 0, kc == 7, [xT, Wb], [ps])
                        sm = sm_r.get()
                        CP("dve", VA[:, gb, 0:64], ps[:, 0:64], [ps], [VA])
                        g1 = g1_r.get()
                        ACT(g1[:], ps[:, 65:129], AF.Exp, [ps], [g1], scale=-1.0)
                        TS("pool", g1[:], g1[:], 1.0, ALU.add, [g1], [g1])
                        RECIP(g1[:], g1[:], [g1], [g1])
                        CP("pool", GTK[:, gb, :], g1[:], [g1], [GTK])
                        ACT(sm[:, 0:1], ps[:, 64:65], AF.Exp, [ps, NFB], [sm], bias=NFB[:, h:h + 1], scale=-1.0)
                        ACT(sm[:, 1:2], sm[:, 0:1], AF.Ln, [sm], [sm], bias=1.0)
                        TS("dve", LF[:, gb:gb + 1], sm[:, 1:2], -1.0, ALU.mult, [sm], [LF])
                if KODD == 1:
                    return
                psw = psm_r.get()
                MM(psw[:, 0:NBF], triU_f[:], LF[:], True, True, [triU_f, LF], [psw])
                pstot = psm_r.get()
                MM(pstot[:, 0:NBF], ones_f[:], LF[:], True, True, [ones_f, LF], [pstot])
                CP("dve", tot[:], pstot[:, 0:NBF], [pstot], [tot])
                CP("dve", scan[0][:], tot[:], [tot], [scan[0]])
                a, bq = scan[0], scan[1]
                sft = 1
                while sft < NBF:
                    TT("dve", bq[:, sft:NBF], a[:, sft:NBF], a[:, 0:NBF - sft], ALU.add, [a], [bq])
                    CP("dve", bq[:, 0:sft], a[:, 0:sft], [a], [bq])
                    a, bq = bq, a
                    sft *= 2
                TT("dve", CUM[:], psw[:, 0:NBF], a[:], ALU.add, [psw, a], [CUM])
                TT("dve", CUM[:], CUM[:], tot[:], ALU.subtract, [CUM, tot], [CUM])
                TS("dve", NEGCUM[:], CUM[:], -1.0, ALU.mult, [CUM], [NEGCUM])
                pct = psm_r.get()
                TR(pct[0:NBF, 0:128], CUM[:, 0:NBF], ident_f[:], [CUM, ident_f], [pct])
                CP("dve", ct[0:NBF, 0, :], pct[0:NBF, 0:128], [pct], [ct])
                CP("dve", ctb[0:NBF, 0, :], ct[0:NBF, 0, :], [ct], [ctb])
                CP("dve", ct[0:NBF, 1, :], ctb[0:NBF, 0, :], [ctb], [ct])
                TT("dve", ct[0:NBF, 2, :], ct[0:NBF, 0, :], ct[0:NBF, 1, :], ALU.subtract, [ct], [ct])
                CP("dve", ctb[0:NBF, 1, :], ct[0:NBF, 2, :], [ct], [ctb])
                CP("dve", ct[0:NBF, 3, :], ctb[0:NBF, 1, :], [ctb], [ct])
                TT("dve", ct[0:NBF, 4, :], ct[0:NBF, 2, :], ct[0:NBF, 3, :], ALU.subtract, [ct], [ct])
                CP("dve", ctb[0:NBF, 2, :], ct[0:NBF, 4, :], [ct], [ctb])
                for j in range(3):
                    S.dma("sp", CQ.ap()[j, :].rearrange("(j t) -> j t", t=128), ctb[0:NBF, j, :], CQ, ctb)
                for j in range(3):
                    S.dma("sp", QT[64 + j:65 + j, :], CQ[j:j + 1, :], QT, CQ)
                if KODD == 2:
                    return
                acc_of = {}

                def stage_a(qt, kb):
                    pss = pss_r.get()
                    diag = kb >= 4 * qt
                    MM(pss[:], KT[0:67, kb * 128:(kb + 1) * 128], QT[0:67, qt * 512:(qt + 1) * 512], True, not diag, [KT, QT], [pss])
                    if diag:
                        MM(pss[:], ident_b[:], MK[:, kb - 4 * qt, :], False, True, [ident_b, MK], [pss])
                    pt = pt_r.get()
                    ACT(pt[:], pss[:], AF.Exp, [pss, NEGCUM], [pt], bias=NEGCUM[:, kb:kb + 1])
                    return pt

                def stage_b(qt, kb, pt):
                    nkb = 4 * qt + 4
                    if kb == 0:
                        acc_of[qt] = pso_r.get()
                    oacc = acc_of[qt]
                    MM(oacc[0:65, :], VA[:, kb, :], pt[:], kb == 0, kb == nkb - 1, [VA, pt], [oacc])
                    if kb < nkb - 1:
                        return
                    osb = osb_r.get()
                    CP("dve", osb[0:65, :], oacc[0:65, :], [oacc], [osb])
                    ptr = psm_r.get()
                    for b4 in range(4):
                        TR(ptr[:, b4 * 128:(b4 + 1) * 128], osb[:, b4 * 128:(b4 + 1) * 128], ident_f[:], [osb, ident_f], [ptr])
                    ot = ot_r.get()
                    for b4 in range(4):
                        gb = qt * 4 + b4
                        sm = sm_r.get()
                        RECIP(sm[:, 0:1], ptr[:, b4 * 128 + 64:b4 * 128 + 65], [ptr], [sm])
                        STT(ot[:, b4, :], ptr[:, b4 * 128:b4 * 128 + 64], sm[:, 0:1], GTK[:, gb, :], ALU.mult, ALU.mult, [ptr, sm, GTK], [ot])
                    S.dma("sp", OTOK.ap()[qt * 512:(qt + 1) * 512, h * 64:(h + 1) * 64].rearrange("(b p) d -> p b d", p=128),
                          ot[:], OTOK, ot)

                pend = []
                for qt in range(NQT):
                    for kb in range(4 * qt + 4):
                        pend.append((qt, kb, stage_a(qt, kb)))
                        if len(pend) > 2:
                            stage_b(*pend.pop(0))
                while pend:
                    stage_b(*pend.pop(0))
                if KODD == 3:
                    return
            o_allgather()

    def even_mixer(L):
        i = L // 2
        lam_init = 0.8 - 0.6 * math.exp(-0.3 * L)
        import os
        KEV = int(os.environ.get("KEV", "9"))
        with S.phase():
            Wd = S.sbuf("Wd", [128, 8, 384], BF16)
            for q4 in range(4):
                S.dma("pool", Wd[:, 2 * q4:2 * q4 + 2, :],
                      we_in.ap()[i, q4 * 256:(q4 + 1) * 256, 0:384].rearrange("(k p) n -> p k n", p=128), Wd, we_in)
            LP = S.sbuf("LP", [128, 256], F32)
            S.dma("sp", LP[:], lamp.ap()[i, :].partition_broadcast(128), LP, lamp)
            lw = S.sbuf("lw", [128, 8], F32)
            pr = S.sbuf("pr", [128, 128], F32)
            TT("dve", pr[:, 0:64], LP[:, 0:64], LP[:, 64:128], ALU.mult, [LP], [pr])
            TT("dve", pr[:, 64:128], LP[:, 128:192], LP[:, 192:256], ALU.mult, [LP], [pr])
            S.op("dve", lambda e: e.tensor_reduce(out=lw[:, 0:2], in_=pr[:].rearrange("p (g d) -> p g d", g=2), axis=AX.X, op=ALU.add),
                 reads=[pr], writes=[lw])
            ACT(lw[:, 2:4], lw[:, 0:2], AF.Exp, [lw], [lw])
            TT("dve", lw[:, 4:5], lw[:, 2:3], lw[:, 3:4], ALU.subtract, [lw], [lw])
            TS("dve", lw[:, 4:5], lw[:, 4:5], lam_init, ALU.add, [lw], [lw])
            TS("dve", lw[:, 5:6], lw[:, 4:5], -1.0, ALU.mult, [lw], [lw])
            SUBW = S.sbuf("SUBW", [128, 128], F32)
            S.dma("sp", SUBW[:], subw.ap()[i, :].partition_broadcast(128), SUBW, subw)
            TS("dve", SUBW[:], SUBW[:], 1.0 - lam_init, ALU.mult, [SUBW], [SUBW])
            T5C = S.sbuf("T5C", [33, 1], F32)
            S.dma("sp", T5C[0:32, :], t5col[:, :], T5C, t5col)
            MEMSET("dve", T5C[32:33, :], 1.0, [T5C])
            OH = S.sbuf("OH", [33, 2560], F32)
            S.dma("sp", OH[:], t5oh[:, :], OH, t5oh)
            bvs = S.sbuf("bvs", [1, 2560], F32)
            psm_r = S.ring("psm", [128, 512], F32, 3, psum=True)
            pss_r = S.ring("pss", [128, 512], F32, 3, psum=True)
            pso_r = S.ring("pso", [128, 512], F32, 2, psum=True)
            for c5 in range(5):
                ps = psm_r.get()
                MM(ps[0:1, :], T5C[:, 0:1], OH[:, c5 * 512:(c5 + 1) * 512], True, True, [T5C, OH], [ps])
                CP("dve", bvs[0:1, c5 * 512:(c5 + 1) * 512], ps[0:1, :], [ps], [bvs])
            S.dma("sp", BV.ap().rearrange("(o n) -> o n", o=1), bvs[:], BV, bvs)
            BT = S.sbuf("BT", [128, 16, 512], BF16)
            skb_r = S.ring("skb", [128, 640], F32, 2)
            for j in range(16):
                skb = skb_r.get()
                S.dma("sp", skb[:], BV.ap()[128 * j:128 * j + 640].partition_broadcast(128), skb, BV)
                S.dma("sp", SKW.ap()[j, :].rearrange("(p n) -> p n", n=640), skb[:], SKW, skb)
                S.dma("pool", BT[:, j, :], bass.AP(tensor=SKW.h, offset=j * 128 * 640 + 127, ap=[[639, 128], [1, 512]]), BT, SKW)
            B31 = S.sbuf("B31", [128, 1], F32)
            S.dma("sp", B31[:], BV.ap()[2559:2560].partition_broadcast(128), B31, BV)
            if S.w_pending:
                S.w_pending = False
                w_collectives()
            QT = S.sbuf("QT", [64, SEQ], BF16)
            KT = S.sbuf("KT", [64, SEQ], BF16)
            VA = S.sbuf("VA", [128, NBF, 129], BF16)
            MEMSET("dve", VA[:, :, 64:65], 1.0, [VA])
            xT_r = S.ring("xT", [128, 8, 512], BF16, 2)
            pt_r = S.ring("pt", [128, 512], BF16, 4)
            osb_r = S.ring("osbd", [128, 512], F32, 4)
            for b_ in osb_r.bufs:
                MEMSET("dve", b_[:], 0.0, [b_])
            om_r = S.ring("om", [128, 128], F32, 3)
            o0_r = S.ring("o0", [128, 128], F32, 2)
            a_r = S.ring("a", [128, 128], F32, 2)
            ao_r = S.ring("ao", [128, 128], BF16, 2)
            sm_r = S.ring("smd", [128, 8], F32, 4)
            junk = S.sbuf("junkd", [128, 128], F32)
            for m in range(2):
                for tt in range(NQT):
                    xT = xT_r.get()
                    load_xT(xT, tt)
                    psq = psm_r.get()
                    for kc in range(8):
                        MM(psq[0:64, :], Wd[:, kc, m * 64:(m + 1) * 64], xT[:, kc, :], kc == 0, kc == 7, [Wd, xT], [psq])
                    ACT(QT[:, tt * 512:(tt + 1) * 512], psq[0:64, :], AF.Identity, [psq], [QT], scale=0.125)
                    psk = psm_r.get()
                    for kc in range(8):
                        MM(psk[0:64, :], Wd[:, kc, 128 + m * 64:128 + (m + 1) * 64], xT[:, kc, :], kc == 0, kc == 7, [Wd, xT], [psk])
                    CP("dve", KT[:, tt * 512:(tt + 1) * 512], psk[0:64, :], [psk], [KT])
                    if m == 0:
                        for b4 in range(4):
                            gb = tt * 4 + b4
                            psv = psm_r.get()
                            for kc in range(8):
                                MM(psv[:, 0:128], xT[:, kc, b4 * 128:(b4 + 1) * 128], Wd[:, kc, 256:384], kc == 0, kc == 7, [xT, Wd], [psv])
                            CP("act", VA[:, gb, 0:64], psv[:, 0:64], [psv], [VA])
                            CP("act", VA[:, gb, 65:129], psv[:, 64:128], [psv], [VA])
                if KEV == 1:
                    return
                acc_of = {}

                def stage_a(qt, kb):
                    dj = 4 * qt - kb + 3
                    near = dj <= 15
                    pss = pss_r.get()
                    MM(pss[:], KT[:, kb * 128:(kb + 1) * 128], QT[:, qt * 512:(qt + 1) * 512], True, not near, [KT, QT], [pss])
                    if near:
                        MM(pss[:], ident_b[:], BT[:, dj, :], False, True, [ident_b, BT], [pss])
                    pt = pt_r.get()
                    if near:
                        ACT(pt[:], pss[:], AF.Exp, [pss], [pt])
                    else:
                        ACT(pt[:], pss[:], AF.Exp, [pss, B31], [pt], bias=B31[:, 0:1])
                    return pt

                def stage_b(qt, kb, pt, m=m):
                    nkb = 4 * qt + 4
                    if kb == 0:
                        acc_of[qt] = (pso_r.get(), pso_r.get())
                    oa, ob = acc_of[qt]
                    MM(oa[0:65, :], VA[:, kb, 0:65], pt[:], kb == 0, kb == nkb - 1, [VA, pt], [oa])
                    MM(ob[0:64, :], VA[:, kb, 65:129], pt[:], kb == 0, kb == nkb - 1, [VA, pt], [ob])
                    if kb < nkb - 1:
                        return
                    osa = osb_r.get()
                    osb = osb_r.get()
                    CP("dve", osa[0:65, :], oa[0:65, :], [oa], [osa])
                    CP("act", osb[0:64, :], ob[0:64, :], [ob], [osb])
                    ptra = psm_r.get()
                    ptrb = psm_r.get()
                    for b4 in range(4):
                        TR(ptra[:, b4 * 128:(b4 + 1) * 128], osa[:, b4 * 128:(b4 + 1) * 128], ident_f[:], [osa, ident_f], [ptra])
                        TR(ptrb[:, b4 * 128:(b4 + 1) * 128], osb[:, b4 * 128:(b4 + 1) * 128], ident_f[:], [osb, ident_f], [ptrb])
                    for b4 in range(4):
                        gb = qt * 4 + b4
                        sm = sm_r.get()
                        RECIP(sm[:, 0:1], ptra[:, b4 * 128 + 64:b4 * 128 + 65], [ptra], [sm])
                        om = om_r.get()
                        TS("dve", om[:, 0:64], ptra[:, b4 * 128:b4 * 128 + 64], sm[:, 0:1], ALU.mult, [ptra, sm], [om])
                        TS("dve", om[:, 64:128], ptrb[:, b4 * 128:b4 * 128 + 64], sm[:, 0:1], ALU.mult, [ptrb, sm], [om])
                        if m == 0:
                            S.dma("sp", O0S[gb * 128:(gb + 1) * 128, :], om[:], O0S, om)
                        else:
                            o0 = o0_r.get()
                            S.dma("sp", o0[:], O0S[gb * 128:(gb + 1) * 128, :], o0, O0S)
                            av = a_r.get()
                            STT(av[:], om[:], lw[:, 5:6], o0[:], ALU.mult, ALU.add, [om, lw, o0], [av])
                            ACT(junk[:], av[:], AF.Square, [av], [junk, sm], accum=sm[:, 1:2])
                            TS("dve", sm[:, 2:3], sm[:, 1:2], 1.0 / 128, ALU.mult, [sm], [sm], s2=RMS_EPS, op1=ALU.add)
                            ACT(sm[:, 3:4], sm[:, 2:3], AF.Sqrt, [sm], [sm])
                            RECIP(sm[:, 4:5], sm[:, 3:4], [sm], [sm])
                            ao = ao_r.get()
                            STT(ao[:], av[:], sm[:, 4:5], SUBW[:], ALU.mult, ALU.mult, [av, sm, SUBW], [ao])
                            S.dma("sp", OTOK[gb * 128:(gb + 1) * 128, 0:128], ao[:], OTOK, ao)

                pend = []
                for qt in range(NQT):
                    for kb in range(4 * qt + 4):
                        pend.append((qt, kb, stage_a(qt, kb)))
                        if len(pend) > 2:
                            stage_b(*pend.pop(0))
                while pend:
                    stage_b(*pend.pop(0))
        if KEV <= 2:
            with S.phase():
                zb = S.sbuf("zb", [128, 128], BF16)
                MEMSET("dve", zb[:], 0.0, [zb])
                for gb in range(NBF):
                    S.dma("sp", OTOK[gb * 128:(gb + 1) * 128, 128:256], zb[:], OTOK, zb)
            o_allgather()
            return
        gdn_phase(L)
        o_allgather()

    def gdn_phase(L):
        i = L // 2
        SC = 128 ** -0.5
        with S.phase():
            Wg = S.sbuf("Wg", [128, 8, 514], BF16)
            for q4 in range(4):
                S.dma("pool", Wg[:, 2 * q4:2 * q4 + 2, :],
                      we_in.ap()[i, q4 * 256:(q4 + 1) * 256, 384:898].rearrange("(k p) n -> p k n", p=128), Wg, we_in)
            CW = S.sbuf("CW", [128, 12], F32)
            S.dma("sp", CW[:], convw.ap()[i], CW, convw)
            GNW = S.sbuf("GNW", [128, 128], F32)
            S.dma("sp", GNW[:], gnw.ap()[i, :].partition_broadcast(128), GNW, gnw)
            AD = S.sbuf("AD", [128, 4], F32)
            S.dma("sp", AD[:, 0:1], alog.ap()[i, :].partition_broadcast(128), AD, alog)
            S.dma("sp", AD[:, 1:2], dtb.ap()[i, :].partition_broadcast(128), AD, dtb)
            ACT(AD[:, 2:3], AD[:, 0:1], AF.Exp, [AD], [AD])
            TS("dve", AD[:, 2:3], AD[:, 2:3], -1.0, ALU.mult, [AD], [AD])
            MUs = S.sbuf("MUs", [128, 128], F32)
            MUi = S.sbuf("MUi", [128, 128], F32)
            MLs = S.sbuf("MLs", [128, 128], F32)
            for (t_, base, cm, pat) in ((MUs, -1, -1, 1), (MUi, 0, -1, 1), (MLs, -1, 1, -1)):
                MEMSET("pool", t_[:], 1.0, [t_])
                ASEL(t_[:], t_[:], [[pat, 128]], ALU.is_ge, 0.0, base, cm, [t_], [t_])
            St = [S.sbuf("St%d" % k, [128, 128], F32) for k in range(2)]
            MEMSET("dve", St[0][:], 0.0, [St[0]])
            cb = [S.sbuf("cb%d" % g, [128, 515], F32) for g in range(3)]
            for g in range(3):
                MEMSET("dve", cb[g][:], 0.0, [cb[g]])
            hal = S.sbuf("hal", [128, 3, 3], F32)
            xT_r = S.ring("xT", [128, 8, 512], BF16, 2)
            y_r = S.ring("y", [128, 512], F32, 2)
            fT = [S.ring("fT%d" % g, [128, 512], F32, 2) for g in range(3)]
            sq_r = S.ring("sqg", [128, 512], F32, 2)
            zt_r = S.ring("ztk", [128, 4, 128], F32, 2)
            gb_r = S.ring("gbt", [128, 12], F32, 2)
            w_r = S.ring("wk", [128, 128], F32, 44)
            w2_r = S.ring("wk2", [128, 256], F32, 13)
            c_r = S.ring("colg", [128, 8], F32, 9)
            bo_r = S.ring("bo", [128, 128], BF16, 2)
            ps_r = S.ring("psg", [128, 512], F32, 7, psum=True)
            pso_r = S.ring("psgo", [128, 512], F32, 1, psum=True)
            cur = 0
            for tt in range(NQT):
                xT = xT_r.get()
                load_xT(xT, tt)
                fts = []
                for g in range(3):
                    psf = ps_r.get()
                    for kc in range(8):
                        MM(psf[:], Wg[:, kc, g * 128:(g + 1) * 128], xT[:, kc, :], kc == 0, kc == 7, [Wg, xT], [psf])
                    CP("dve", hal[:, g, :], cb[g][:, 512:515], [cb[g]], [hal])
                    CP("act", cb[g][:, 3:515], psf[:], [psf], [cb[g]])
                    CP("dve", cb[g][:, 0:3], hal[:, g, :], [hal], [cb[g]])
                    y = y_r.get()
                    TS("dve", y[:], cb[g][:, 0:512], CW[:, g * 4:g * 4 + 1], ALU.mult, [cb[g], CW], [y])
                    for j in range(1, 4):
                        STT(y[:], cb[g][:, j:j + 512], CW[:, g * 4 + j:g * 4 + j + 1], y[:], ALU.mult, ALU.add, [cb[g], CW, y], [y])
                    ft = fT[g].get()
                    ACT(ft[:], y[:], AF.Silu, [y], [ft])
                    if g < 2:
                        sq = sq_r.get()
                        TT("pool", sq[:], ft[:], ft[:], ALU.mult, [ft], [sq])
                        pss = ps_r.get()
                        MM(pss[:], ones_f[:], sq[:], True, True, [ones_f, sq], [pss])
                        TS("dve", sq[:], pss[:], RMS_EPS, ALU.add, [pss], [sq])
                        ACT(sq[:], sq[:], AF.Sqrt, [sq], [sq])
                        RECIP(sq[:], sq[:], [sq], [sq])
                        TT("dve", ft[:], ft[:], sq[:], ALU.mult, [ft, sq], [ft])
                    fts.append(ft)
                qTt, kTt, vTt = fts
                zt = zt_r.get()
                gbt = gb_r.get()
                for b4 in range(4):
                    pz = ps_r.get()
                    for kc in range(8):
                        MM(pz[:, 0:130], xT[:, kc, b4 * 128:(b4 + 1) * 128], Wg[:, kc, 384:514], kc == 0, kc == 7, [xT, Wg], [pz])
                    ACT(zt[:, b4, :], pz[:, 0:128], AF.Silu, [pz], [zt])
                    ACT(gbt[:, 4 + b4:5 + b4], pz[:, 128:129], AF.Sigmoid, [pz], [gbt])
                    cg = c_r.get()
                    ACT(cg[:, 0:1], pz[:, 129:130], AF.Exp, [pz, AD], [cg], bias=AD[:, 1:2])
                    ACT(cg[:, 1:2], cg[:, 0:1], AF.Ln, [cg], [cg], bias=1.0)
                    TS("dve", gbt[:, b4:b4 + 1], cg[:, 1:2], AD[:, 2:3], ALU.mult, [cg, AD], [gbt])
                pgc = ps_r.get()
                MM(pgc[:, 0:4], triU_f[:], gbt[:, 0:4], True, True, [triU_f, gbt], [pgc])
                CP("dve", gbt[:, 8:12], pgc[:, 0:4], [pgc], [gbt])
                for b4 in range(4):
                    gbk = tt * 4 + b4
                    blk = slice(b4 * 128, (b4 + 1) * 128)
                    gc = gbt[:, 8 + b4:9 + b4]
                    beta = gbt[:, 4 + b4:5 + b4]
                    pk = ps_r.get()
                    TR(pk[:, 0:128], kTt[:, blk], ident_f[:], [kTt, ident_f], [pk])
                    Kc = w_r.get()
                    CP("act", Kc[:], pk[:, 0:128], [pk], [Kc])
                    pv = ps_r.get()
                    TR(pv[:, 0:128], vTt[:, blk], ident_f[:], [vTt, ident_f], [pv])
                    Vc = w_r.get()
                    CP("act", Vc[:], pv[:, 0:128], [pv], [Vc])
                    DG = w2_r.get()
                    TS("dve", DG[:, 0:128], ident_f[:], gc, ALU.mult, [ident_f, gbt], [DG])
                    TS("dve", DG[:, 128:256], ident_f[:], beta, ALU.mult, [ident_f, gbt], [DG])
                    pb = ps_r.get()
                    MM(pb[:, 0:256], ones_f[:], DG[:], True, True, [ones_f, DG], [pb])
                    GB = w2_r.get()
                    CP("act", GB[:], pb[:, 0:256], [pb], [GB])
                    dlt = w_r.get()
                    TS("dve", dlt[:], GB[:, 0:128], gc, ALU.subtract, [GB, gbt], [dlt])
                    ET = w_r.get()
                    TS("dve", ET[:], dlt[:], 0.0, ALU.min, [dlt], [ET])
                    ACT(ET[:], ET[:], AF.Exp, [ET], [ET])
                    E2 = w_r.get()
                    TS("dve", E2[:], dlt[:], -1.0, ALU.mult, [dlt], [E2], s2=0.0, op1=ALU.min)
                    ACT(E2[:], E2[:], AF.Exp, [E2], [E2])
                    EG = w_r.get()
                    ACT(EG[:], GB[:, 0:128], AF.Exp, [GB], [EG])
                    pkk = ps_r.get()
                    MM(pkk[:, 0:128], kTt[:, blk], kTt[:, blk], True, True, [kTt], [pkk])
                    Nj = w_r.get()
                    TT("dve", Nj[:], pkk[:, 0:128], GB[:, 128:256], ALU.mult, [pkk, GB], [Nj])
                    TT("dve", Nj[:], Nj[:], ET[:], ALU.mult, [Nj, ET], [Nj])
                    STT(Nj[:], Nj[:], -1.0, MUs[:], ALU.mult, ALU.mult, [Nj, MUs], [Nj])
                    Pj = w_r.get()
                    STT(Pj[:], pkk[:, 0:128], beta, E2[:], ALU.mult, ALU.mult, [pkk, gbt, E2], [Pj])
                    STT(Pj[:], Pj[:], -1.0, MLs[:], ALU.mult, ALU.mult, [Pj, MLs], [Pj])
                    cg = c_r.get()
                    ACT(cg[:, 0:1], gc, AF.Exp, [gbt], [cg])
                    TT("dve", cg[:, 1:2], cg[:, 0:1], beta, ALU.mult, [cg, gbt], [cg])
                    Rm = w2_r.get()
                    TS("dve", Rm[:, 0:128], Vc[:], beta, ALU.mult, [Vc, gbt], [Rm])
                    TS("dve", Rm[:, 128:256], Kc[:], cg[:, 1:2], ALU.mult, [Kc, cg], [Rm])
                    for j in range(7):
                        pr_ = ps_r.get()
                        MM(pr_[:, 0:256], Nj[:], Rm[:], True, True, [Nj, Rm], [pr_])
                        Rn = w2_r.get()
                        TT("dve", Rn[:], Rm[:], pr_[:, 0:256], ALU.add, [Rm, pr_], [Rn])
                        Rm = Rn
                        if j < 6:
                            pn = ps_r.get()
                            MM(pn[:, 0:128], Pj[:], Nj[:], True, True, [Pj, Nj], [pn])
                            pp = ps_r.get()
                            MM(pp[:, 0:128], Nj[:], Pj[:], True, True, [Nj, Pj], [pp])
                            Nn = w_r.get()
                            CP("act", Nn[:], pn[:, 0:128], [pn], [Nn])
                            Pn = w_r.get()
                            CP("act", Pn[:], pp[:, 0:128], [pp], [Pn])
                            Nj, Pj = Nn, Pn
                    pw = ps_r.get()
                    TR(pw[:, 0:128], Rm[:, 128:256], ident_f[:], [Rm, ident_f], [pw])
                    WT = w_r.get()
                    CP("act", WT[:], pw[:, 0:128], [pw], [WT])
                    Sc, Sn = St[cur], St[1 - cur]
                    pws = ps_r.get()
                    MM(pws[:, 0:128], WT[:], Sc[:], True, True, [WT, Sc], [pws])
                    Vn = w_r.get()
                    TT("dve", Vn[:], Rm[:, 0:128], pws[:, 0:128], ALU.subtract, [Rm, pws], [Vn])
                    pqk = ps_r.get()
                    MM(pqk[:, 0:128], kTt[:, blk], qTt[:, blk], True, True, [kTt, qTt], [pqk])
                    qki = w_r.get()
                    TT("dve", qki[:], pqk[:, 0:128], ET[:], ALU.mult, [pqk, ET], [qki])
                    STT(qki[:], qki[:], SC, MUi[:], ALU.mult, ALU.mult, [qki, MUi], [qki])
                    QdT = w_r.get()
                    STT(QdT[:], qTt[:, blk], SC, EG[:], ALU.mult, ALU.mult, [qTt, EG], [QdT])
                    po = pso_r.get()
                    MM(po[:, 0:128], QdT[:], Sc[:], True, False, [QdT, Sc], [po])
                    MM(po[:, 0:128], qki[:], Vn[:], False, True, [qki, Vn], [po])
                    TT("dve", cg[:, 2:3], GB[:, 127:128], gc, ALU.subtract, [GB, gbt], [cg])
                    ACT(cg[:, 3:4], cg[:, 2:3], AF.Exp, [cg], [cg])
                    Kd = w_r.get()
                    TS("dve", Kd[:], Kc[:], cg[:, 3:4], ALU.mult, [Kc, cg], [Kd])
                    psn = ps_r.get()
                    MM(psn[:, 0:128], Kd[:], Vn[:], True, True, [Kd, Vn], [psn])
                    STT(Sn[:], Sc[:], EG[:, 127:128], psn[:, 0:128], ALU.mult, ALU.add, [Sc, EG, psn], [Sn])
                    cur = 1 - cur
                    jk = w_r.get()
                    ACT(jk[:], po[:, 0:128], AF.Square, [po], [jk, cg], accum=cg[:, 4:5])
                    TS("dve", cg[:, 5:6], cg[:, 4:5], 1.0 / 128, ALU.mult, [cg], [cg], s2=RMS_EPS, op1=ALU.add)
                    ACT(cg[:, 6:7], cg[:, 5:6], AF.Sqrt, [cg], [cg])
                    RECIP(cg[:, 7:8], cg[:, 6:7], [cg], [cg])
                    STT(jk[:], po[:, 0:128], cg[:, 7:8], GNW[:], ALU.mult, ALU.mult, [po, cg, GNW], [jk])
                    bo = bo_r.get()
                    TT("dve", bo[:], jk[:], zt[:, b4, :], ALU.mult, [jk, zt], [bo])
                    S.dma("sp", OTOK[gbk * 128:(gbk + 1) * 128, 128:256], bo[:], OTOK, bo)


    phase_x0()
    xres = x_in
    if S.w_pending and not (cfg.mixer and cfg.LAYERS[0] % 2 == 0):
        S.w_pending = False
        with S.phase():
            w_collectives()
    for li, L in enumerate(cfg.LAYERS):
        is_last = li == len(cfg.LAYERS) - 1
        if cfg.mixer:
            if L % 2 == 0:
                even_mixer(L)
            else:
                odd_mixer(L)
        xnext = XR[li % 2]
        phase_ln_moe(L, xres, xnext, is_last)
        xres = xnext


def _even_mixer(S, cfg, L):
    raise NotImplementedError


def _odd_mixer(S, cfg, L):
    raise NotImplementedError


def t5_onehot():
    n = np.arange(2560) - 511
    nn = np.maximum(n, 0)
    nf = np.maximum(nn, 1).astype(np.float32)
    large = 16 + (np.log(nf / 16) / math.log(2048 / 16) * 16).astype(np.int32)
    large = np.minimum(large, 31)
    bucket = np.where(nn < 16, nn, large)
    oh = np.zeros((33, 2560), np.float32)
    valid = n >= 0
    oh[bucket[valid], np.nonzero(valid)[0]] = 1.0
    oh[32, ~valid] = NEG
    return oh


def prep_inputs(inp, cfg):
    SEQ, TOK, NEXP, EPC, NLW, WL0 = cfg.SEQ, cfg.TOK, cfg.NEXP, cfg.EPC, cfg.NLW, cfg.WL0
    f = lambda a: np.ascontiguousarray(np.asarray(a, dtype=np.float32))
    x = f(inp["x"])
    maps = []
    lng = f(np.concatenate([inp["ln_mix_g"], inp["ln_ffn_g"]], 0))
    lnb = f(np.concatenate([inp["ln_mix_b"], inp["ln_ffn_b"]], 0))
    if cfg.moe:
        bg = f(inp["moe_b_gate_up"]).reshape(4, NEXP, 16, 128)
        bgu = np.ascontiguousarray(bg.transpose(3, 0, 1, 2).reshape(128, 4 * NEXP * 16))
        bdn = f(inp["moe_b_down"]).reshape(4 * NEXP, D)
        wgu = np.asarray(inp["moe_w_gate_up"])
        wdn = np.asarray(inp["moe_w_down"])
    if cfg.mixer:
        ewi, ewo = f(inp["even_w_in"]), f(inp["even_w_out"])
        owi, owo = f(inp["odd_w_in"]), f(inp["odd_w_out"])
        cw = f(inp["gdn_conv_w"])
        oh = t5_onehot()
    for c in range(8):
        b, r = c // 4, c % 4
        m = {"x_in": np.ascontiguousarray(x[b, r * TOK:(r + 1) * TOK, :]), "lng": lng, "lnb": lnb}
        if cfg.moe:
            m["router_w"] = f(inp["router_w"])
            m["router_b"] = f(inp["router_b"])
            m["bgu"] = bgu
            m["bdn"] = bdn
            m["wgu_sh"] = np.ascontiguousarray(wgu[WL0:WL0 + NLW, c * EPC:(c + 1) * EPC], dtype=np.float32).reshape(NLW * EPC * D, 2 * D)
            m["wdn_sh"] = np.ascontiguousarray(wdn[WL0:WL0 + NLW, c * EPC:(c + 1) * EPC], dtype=np.float32).reshape(NLW * EPC * D, D)
        if cfg.mixer:
            h = r
            A = 512
            cols = np.concatenate([np.arange(h * 128, h * 128 + 128), A + np.arange(h * 128, h * 128 + 128),
                                   2 * A + np.arange(h * 128, h * 128 + 128),
                                   1536 + np.arange(h * 128, h * 128 + 128), 2048 + np.arange(h * 128, h * 128 + 128),
                                   2560 + np.arange(h * 128, h * 128 + 128), 3072 + np.arange(h * 128, h * 128 + 128),
                                   [3584 + h], [3588 + h]])
            m["we_in"] = np.ascontiguousarray(ewi[:, :, cols])
            perm = np.concatenate([np.concatenate([np.arange(s_ * 128, s_ * 128 + 128), 512 + np.arange(s_ * 128, s_ * 128 + 128)]) for s_ in range(4)])
            m["we_out"] = np.ascontiguousarray(ewo[:, perm, :])
            TC = min(2048, SEQ)
            gtok = r * TOK + np.arange(TOK)
            jj, tt_ = gtok // TC, gtok % TC
            oi = np.stack([(jj * 4 + s_) * TC + tt_ for s_ in range(4)], 1)
            m["oidx"] = np.ascontiguousarray(oi.reshape(TOK // 128, 128, 4).transpose(1, 0, 2).reshape(128, TOK // 32).astype(np.int32))
            ccols = np.concatenate([np.arange(h * 128, h * 128 + 128), 512 + np.arange(h * 128, h * 128 + 128), 1024 + np.arange(h * 128, h * 128 + 128)])
            m["convw"] = np.ascontiguousarray(cw[:, :, ccols].reshape(2, 4, 3, 128).transpose(0, 3, 2, 1).reshape(2, 128, 12))
            m["lamp"] = f(inp["diff_lambda"]).reshape(2, 256)
            m["subw"] = f(inp["diff_subln_w"])
            m["gnw"] = f(inp["gdn_norm_w"])
            m["alog"] = np.ascontiguousarray(f(inp["gdn_a_log"])[:, h:h + 1])
            m["dtb"] = np.ascontiguousarray(f(inp["gdn_dt_bias"])[:, h:h + 1])
            m["t5col"] = np.ascontiguousarray(f(inp["t5_bias"])[:, h:h + 1])
            m["t5oh"] = oh
            oc = []
            for hh in range(4 * r, 4 * r + 4):
                oc += [np.arange(hh * 64, hh * 64 + 64), 1024 + np.arange(hh * 64, hh * 64 + 64),
                       2048 + np.arange(hh * 64, hh * 64 + 64), [4096 + hh], 3072 + np.arange(hh * 64, hh * 64 + 64)]
            oc = np.concatenate(oc)
            m["wo_in"] = np.ascontiguousarray(owi[:, :, oc])
            m["wo_out"] = owo
            m["qkw"] = np.ascontiguousarray(f(inp["fox_qk_norm_w"]).transpose(0, 2, 1))
            m["fb"] = np.ascontiguousarray(f(inp["fox_forget_b"])[:, 4 * r:4 * r + 4])
        maps.append(m)
    return maps


_CACHE = {}


def run_cfg(inp, cfg):
    key = (cfg.SEQ, cfg.NEXP, cfg.CAP, cfg.LAYERS, cfg.NLW, cfg.WL0, cfg.mixer, cfg.moe)
    if key not in _CACHE:
        _CACHE[key] = build(cfg)
    nc = _CACHE[key]
    maps = prep_inputs(inp, cfg)
    has_even = cfg.mixer and any(L % 2 == 0 for L in cfg.LAYERS)
    has_odd = cfg.mixer and any(L % 2 == 1 for L in cfg.LAYERS)
    ev = ("we_in", "we_out", "convw", "lamp", "subw", "gnw", "alog", "dtb", "t5col", "t5oh")
    od = ("wo_in", "wo_out", "qkw", "fb")
    for m in maps:
        for k in list(m):
            if (k in ev and not has_even) or (k in od and not has_odd):
                del m[k]
    res = run_bass_kernel_spmd(nc, maps, core_ids=list(range(8)))
    outs = [res.results[c]["out"] for c in range(8)]
    TOK = cfg.TOK
    full = np.zeros((2, cfg.SEQ, D), np.float32)
    for c in range(8):
        full[c // 4, (c % 4) * TOK:(c % 4 + 1) * TOK, :] = outs[c]
    return full


def kernel(**inputs):
    return run_cfg(inputs, Cfg())
```

```python
import math
from contextlib import ExitStack
import numpy as np
import concourse.bass as bass
import concourse.mybir as mybir
from concourse.bass_utils import run_bass_kernel_spmd

F32 = mybir.dt.float32
BF16 = mybir.dt.bfloat16
I32 = mybir.dt.int32
U32 = mybir.dt.uint32
AF = mybir.ActivationFunctionType
ALU = mybir.AluOpType
AX = mybir.AxisListType

COMPUTE = ("pe", "act", "dve", "pool")


class Buf:
    def __init__(self, name, handle, is_dram=False):
        self.name = name
        self.h = handle
        self.is_dram = is_dram
        self.w = {}
        self.r = {}
        self.dsem = None
        self.dcnt = 0

    def __getitem__(self, idx):
        return self.h.ap()[idx] if self.is_dram else self.h[idx]

    def ap(self):
        return self.h.ap() if self.is_dram else self.h[:]


class Sched:
    def __init__(self, nc, es):
        self.nc = nc
        self.es = es
        self.streams = {k: [] for k in ("pe", "act", "dve", "pool", "sp")}
        self.sems = {}
        self.latest = {}
        self.cnt = {k: 0 for k in COMPUTE}
        self.waited = {k: {} for k in self.streams}
        self.nbuf = 0
        self.es_local = es
        self.dq = {}
        self.dq_i = {}
        self.all_bufs = []
        import os
        self.sw_thresh = int(os.environ.get("KSW", "30000"))
        for k in COMPUTE:
            self._sem("E_" + k)

    def _sem(self, key):
        if key not in self.sems:
            self.sems[key] = self.es.enter_context(self.nc.semaphore("s%d_%s" % (len(self.sems), key[:12])))
            self.latest[key] = 0
        return self.sems[key]

    def sbuf(self, name, shape, dt):
        self.nbuf += 1
        h = self.es_local.enter_context(self.nc.sbuf_tensor("%s_%d" % (name, self.nbuf), list(shape), dt))
        b = Buf(name, h)
        b.local = self.es_local is not self.es
        self.all_bufs.append(b)
        return b

    def psum(self, name, shape, dt=F32):
        self.nbuf += 1
        h = self.es_local.enter_context(self.nc.psum_tensor("%s_%d" % (name, self.nbuf), list(shape), dt))
        b = Buf(name, h)
        b.local = self.es_local is not self.es
        self.all_bufs.append(b)
        return b

    def ring(self, name, shape, dt, n, psum=False):
        return Ring([(self.psum if psum else self.sbuf)("%s%d" % (name, i), shape, dt) for i in range(n)])

    def _dq(self, kind):
        if kind not in self.dq:
            n = {"hw": 12, "sw": 8, "cc": 1}[kind]
            self.dq[kind] = ["D_%s_%d" % (kind, i) for i in range(n)]
            for k in self.dq[kind]:
                self._sem(k)
            self.dq_i[kind] = 0
        key = self.dq[kind][self.dq_i[kind] % len(self.dq[kind])]
        self.dq_i[kind] += 1
        return key

    def phase(self):
        return _Phase(self)

    def dram(self, name, shape, dt, kind=None):
        if kind is None:
            h = self.nc.dram_tensor(name, list(shape), dt)
        else:
            h = self.nc.dram_tensor(name, list(shape), dt, kind=kind)
        b = Buf(name, h, is_dram=True)
        self.all_bufs.append(b)
        return b

    def _need(self, eng, reads, writes, mykey, prev=None):
        need = {}

        def add(k, v):
            if k == mykey == "E_pe":
                return
            if need.get(k, 0) < v:
                need[k] = v
        if prev is not None and prev[1] > 0:
            add(*prev)
        for b in reads:
            for k, v in b.w.items():
                add(k, v)
        for b in writes:
            for k, v in b.w.items():
                add(k, v)
            for k, v in b.r.items():
                add(k, v)
        out = []
        wd = self.waited[eng]
        for k, v in need.items():
            if wd.get(k, 0) < v:
                wd[k] = v
                out.append((self.sems[k], v))
        return out

    def op(self, eng, fn, reads=(), writes=()):
        key = "E_" + eng
        waits = self._need(eng, reads, writes, key)
        self.cnt[eng] += 1
        val = self.cnt[eng]
        self.latest[key] = val
        sem = self.sems[key]
        st = self.streams[eng]
        for s, v in waits:
            st.append(lambda e, s=s, v=v: e.wait_ge(s, v))
        st.append(lambda e, fn=fn, sem=sem: fn(e).then_inc(sem, 1))
        for b in reads:
            if b.r.get(key, 0) < val:
                b.r[key] = val
        for b in writes:
            b.w = {key: val}
            b.r = {}

    def dma(self, q, out_ap, in_ap, dst, src, fn=None, extra=(), **kw):
        key = self._dq("sw" if q == "pool" else "hw")
        wr = [dst] if (dst.r or not all(k.startswith("D_") for k in dst.w)) else []
        waits = self._need(q, ([src] if src is not None else []) + list(extra), wr, key, prev=(key, self.latest[key]))
        val = self.latest[key] + 16
        self.latest[key] = val
        sem = self.sems[key]
        st = self.streams[q]
        for s, v in waits:
            st.append(lambda e, s=s, v=v: e.wait_ge(s, v))
        if fn is None:
            st.append(lambda e: e.dma_start(out=out_ap, in_=in_ap, **kw).then_inc(sem, 16))
        else:
            st.append(lambda e: fn(e).then_inc(sem, 16))
        for b in ([src] if src is not None else []) + list(extra):
            if b.r.get(key, 0) < val:
                b.r[key] = val
        if wr:
            dst.w = {key: val}
        else:
            dst.w[key] = val
        dst.r = {}

    def cc(self, kind, op, groups, src, dst, src_ap=None, dst_ap=None):
        sap = src.h.ap().opt() if src_ap is None else src_ap.opt()
        dap = dst.h.ap().opt() if dst_ap is None else dst_ap.opt()
        key = self._dq("cc")
        waits = self._need("pool", [src], [dst], key)
        val = self.latest[key] + 1
        self.latest[key] = val
        sem = self.sems[key]
        st = self.streams["pool"]
        for s, v in waits:
            st.append(lambda e, s=s, v=v: e.wait_ge(s, v))
        st.append(lambda e: e.collective_compute(kind, op, replica_groups=groups,
                                                 ins=[sap], outs=[dap]).then_inc(sem, 1))
        st.append(lambda e: e.wait_ge(sem, val))
        self.waited["pool"][key] = val
        src.r[key] = val
        dst.w = {key: val}
        dst.r = {}

    def barrier(self):
        for eng, st in self.streams.items():
            wd = self.waited[eng]
            for k, v in self.latest.items():
                if v > 0 and wd.get(k, 0) < v:
                    wd[k] = v
                    st.append(lambda e, s=self.sems[k], v=v: e.wait_ge(s, v))

    def emit(self):
        self.barrier()
        for eng in COMPUTE:
            key = "E_" + eng
            if self.cnt[eng] > self.sw_thresh:
                self.nsw = getattr(self, "nsw", 0) + 1
                self.sems[key] = self.es.enter_context(self.nc.semaphore("sw%d_%s" % (self.nsw, eng)))
                self.cnt[eng] = 0
                self.latest[key] = 0
                for wd in self.waited.values():
                    wd.pop(key, None)
                for b in self.all_bufs:
                    b.w.pop(key, None)
                    b.r.pop(key, None)
        streams = self.streams
        self.streams = {k: [] for k in streams}
        self.ninst = getattr(self, "ninst", 0) + sum(len(v) for v in streams.values())
        import os
        if os.environ.get("KCOUNT"):
            return
        self._emit(streams)

    def _emit(self, streams):
        class _S:
            pass
        self_ = _S()
        self_.streams = streams
        nc = self.nc
        self = self_
        with nc.Block() as block:
            @block.tensor
            def _(e):
                for f in self.streams["pe"]:
                    f(e)

            @block.scalar
            def _(e):
                for f in self.streams["act"]:
                    f(e)

            @block.vector
            def _(e):
                for f in self.streams["dve"]:
                    f(e)

            @block.gpsimd
            def _(e):
                for f in self.streams["pool"]:
                    f(e)

            @block.sync
            def _(e):
                for f in self.streams["sp"]:
                    f(e)


class Ring:
    def __init__(self, bufs):
        self.bufs = bufs
        self.i = 0

    def get(self):
        b = self.bufs[self.i % len(self.bufs)]
        self.i += 1
        return b


class _Phase:
    def __init__(self, S):
        self.S = S

    def __enter__(self):
        self.old = self.S.es_local
        self.stack = ExitStack()
        self.stack.__enter__()
        self.S.es_local = self.stack
        return self.S

    def __exit__(self, *a):
        S = self.S
        stop = False
        if a[0] is None:
            S.emit()

            S.nphase = getattr(S, "nphase", 0) + 1
            import os
            stop = S.nphase == int(os.environ.get("KSTOP", "0"))
        S.es_local = self.old
        self.stack.__exit__(*a)
        if stop:
            raise _StopBuild()
        return False


class _StopBuild(Exception):
    pass


D = 1024
ALPHA = (2 * 4) ** 0.25
LN_EPS = 1e-5
RMS_EPS = 1e-6
G4 = [[0, 1, 2, 3], [4, 5, 6, 7]]
G2 = [[0, 4], [1, 5], [2, 6], [3, 7]]
NEG = -30000.0


class Cfg:
    def __init__(self, SEQ=16384, NEXP=32, CAP=640, LAYERS=(0, 1, 2, 3), NLW=4, WL0=0, mixer=True, moe=True):
        self.SEQ, self.NEXP, self.CAP, self.LAYERS, self.NLW = SEQ, NEXP, CAP, tuple(LAYERS), NLW
        self.mixer, self.moe, self.WL0 = mixer, moe, WL0
        self.TOK = SEQ // 4
        self.EPC = NEXP // 8


def build(cfg):
    nc = bass.Bass("TRN2", target_bir_lowering=False)
    es = ExitStack()
    with es:
        S = Sched(nc, es)
        try:
            _build(S, cfg)
        except _StopBuild:
            pass
        print("kernel build: instructions incl. waits =", getattr(S, "ninst", 0), flush=True)
    return nc


def _build(S, cfg):
    SEQ, TOK, NEXP, C, EPC, NLW = cfg.SEQ, cfg.TOK, cfg.NEXP, cfg.CAP, cfg.EPC, cfg.NLW
    NBL = TOK // 128
    NTL = TOK // 512
    NBF = SEQ // 128
    NQT = SEQ // 512
    CB = C // 128
    TRASH = NEXP * C

    def MM(out, lhsT, rhs, start, stop, R, W):
        S.op("pe", lambda e: e.matmul(out, lhsT, rhs, start=start, stop=stop), reads=R, writes=W)

    def TR(out, in_, ident, R, W):
        S.op("pe", lambda e: e.transpose(out, in_, ident), reads=R, writes=W)

    def ACT(out, in_, func, R, W, bias=None, scale=None, accum=None):
        kw = {}
        if bias is not None:
            kw["bias"] = bias
        if scale is not None:
            kw["scale"] = scale
        if accum is not None:
            kw["accum_out"] = accum
        S.op("act", lambda e: e.activation(out=out, in_=in_, func=func, **kw), reads=R, writes=W)

    def TS(eng, out, in0, s1, op0, R, W, s2=None, op1=None, accum=None):
        kw = {}
        if op1 is not None:
            kw["op1"] = op1
        if accum is not None:
            kw["accum_out"] = accum
        S.op(eng, lambda e: e.tensor_scalar(out=out, in0=in0, scalar1=s1, scalar2=s2, op0=op0, **kw), reads=R, writes=W)

    def TT(eng, out, in0, in1, op, R, W):
        S.op(eng, lambda e: e.tensor_tensor(out=out, in0=in0, in1=in1, op=op), reads=R, writes=W)

    def STT(out, in0, scalar, in1, op0, op1, R, W, accum=None):
        kw = {}
        if accum is not None:
            kw["accum_out"] = accum
        S.op("dve", lambda e: e.scalar_tensor_tensor(out=out, in0=in0, scalar=scalar, in1=in1, op0=op0, op1=op1, **kw),
             reads=R, writes=W)

    def CP(eng, out, in_, R, W):
        if eng == "act":
            S.op("act", lambda e: e.copy(out=out, in_=in_), reads=R, writes=W)
        else:
            S.op(eng, lambda e: e.tensor_copy(out=out, in_=in_), reads=R, writes=W)

    def MEMSET(eng, ap, val, W):
        S.op(eng, lambda e: e.memset(ap, val), writes=W)

    def RECIP(out, in_, R, W):
        S.op("dve", lambda e: e.reciprocal(out=out, in_=in_), reads=R, writes=W)

    def ASEL(out, in_, pattern, cmp, fill, base, cm, R, W):
        S.op("pool", lambda e: e.affine_select(out=out, in_=in_, pattern=pattern, compare_op=cmp, fill=fill,
                                                base=base, channel_multiplier=cm), reads=R, writes=W)

    def din(name, shape, dt=F32):
        return S.dram(name, shape, dt, kind="ExternalInput")

    x_in = din("x_in", [TOK, D])
    lng = din("lng", [8, D])
    lnb = din("lnb", [8, D])
    out = S.dram("out", [TOK, D], F32, kind="ExternalOutput")
    if cfg.moe:
        router_w = din("router_w", [4, D, NEXP])
        router_b = din("router_b", [4, NEXP])
        bgu = din("bgu", [128, 4 * NEXP * 16])
        bdn = din("bdn", [4 * NEXP, D])
        wgu_sh = din("wgu_sh", [NLW * EPC * D, 2 * D])
        wdn_sh = din("wdn_sh", [NLW * EPC * D, D])
    has_even = cfg.mixer and any(L % 2 == 0 for L in cfg.LAYERS)
    has_odd = cfg.mixer and any(L % 2 == 1 for L in cfg.LAYERS)
    if has_even:
        we_in = din("we_in", [2, D, 898])
        we_out = din("we_out", [2, D, D])
        convw = din("convw", [2, 128, 12])
        lamp = din("lamp", [2, 256])
        subw = din("subw", [2, 128])
        gnw = din("gnw", [2, 128])
        alog = din("alog", [2, 1])
        dtb = din("dtb", [2, 1])
        t5col = din("t5col", [32, 1])
        t5oh = din("t5oh", [33, 2560])
    if has_odd:
        wo_in = din("wo_in", [2, D, 1028])
        wo_out = din("wo_out", [2, D, D])
        qkw = din("qkw", [2, 64, 2])
        fb = din("fb", [2, 4])

    XR = [S.dram("xres0", [TOK, D], F32), S.dram("xres1", [TOK, D], F32)]
    XR1 = S.dram("xr1", [TOK, D], F32)
    XTB = S.dram("xtb", [D, TOK], BF16)
    XTALL = S.dram("xtall", [4 * D, TOK], BF16)
    TC = min(2048, SEQ)
    if cfg.mixer:
        OTOK = S.dram("otok", [SEQ, 256], BF16)
        OALL = S.dram("oall", [4 * SEQ, 256], BF16)
        oidx = din("oidx", [128, NBL * 4], I32)
        CQ = S.dram("cq", [3, SEQ], BF16)
        O0S = S.dram("o0s", [SEQ, 128], F32)
        BV = S.dram("bv", [2560], F32)
        SKW = S.dram("skw", [16, 128 * 640], F32)
    if cfg.moe:
        XDISP = S.dram("xdisp", [NEXP * C + 128, D], BF16)
        YDISP = S.dram("ydisp", [NEXP * C + 128, D], F32)
        R1 = NLW * EPC * D
        WB1G = S.dram("wb1g", [R1, 2 * D], BF16)
        WB1D = S.dram("wb1d", [R1, D], BF16)
        RGG, RGD = 256, 512
        NCG, NCD = R1 // RGG, R1 // RGD
        WALLG = [S.dram("wallg%d" % i, [16 * 8 * RGG, 2 * D], BF16) for i in range((NCG + 15) // 16)]
        WALLD = [S.dram("walld%d" % i, [16 * 8 * RGD, D], BF16) for i in range((NCD + 15) // 16)]
        S1G = [S.dram("s1g%d" % i, [2 * RGG, 2 * D], BF16) for i in range(2)]
        S1D = [S.dram("s1d%d" % i, [2 * RGD, D], BF16) for i in range(2)]

    ident_f = S.sbuf("ident_f", [128, 128], F32)
    ident_b = S.sbuf("ident_b", [128, 128], BF16)
    ones_f = S.sbuf("ones_f", [128, 128], F32)
    ones_b = S.sbuf("ones_b", [128, 128], BF16)
    triU_f = S.sbuf("triU_f", [128, 128], F32)
    Ls_b = S.sbuf("Ls_b", [128, 128], BF16)
    DESTI = S.sbuf("DESTI", [128, NBL * 4], I32)
    GATE = S.sbuf("GATE", [128, NBL * 4], F32)
    CNT = S.sbuf("CNT", [128, NEXP], F32)
    EOFF = S.sbuf("EOFF", [128, NEXP], F32)
    with S.phase():
        tmpf = S.sbuf("tmpf", [128, 128], F32)
        tmpi = S.sbuf("tmpi", [128, NEXP], I32)
        MEMSET("pool", ident_f[:], 0.0, [ident_f])
        ASEL(ident_f[:], ident_f[:], [[-1, 128]], ALU.not_equal, 1.0, 0, 1, [ident_f], [ident_f])
        CP("dve", ident_b[:], ident_f[:], [ident_f], [ident_b])
        MEMSET("pool", ones_f[:], 1.0, [ones_f])
        MEMSET("pool", ones_b[:], 1.0, [ones_b])
        MEMSET("pool", triU_f[:], 1.0, [triU_f])
        ASEL(triU_f[:], triU_f[:], [[1, 128]], ALU.is_ge, 0.0, 0, -1, [triU_f], [triU_f])
        MEMSET("pool", tmpf[:], 1.0, [tmpf])
        ASEL(tmpf[:], tmpf[:], [[1, 128]], ALU.is_ge, 0.0, -1, -1, [tmpf], [tmpf])
        CP("dve", Ls_b[:], tmpf[:], [tmpf], [Ls_b])
        S.op("pool", lambda e: e.iota(tmpi[:], pattern=[[C, NEXP]], base=0, channel_multiplier=0), writes=[tmpi])
        CP("dve", EOFF[:], tmpi[:], [tmpi], [EOFF])

    if cfg.moe:
        with S.phase():
            st_r = S.ring("wst", [128, 2 * D], F32, 3)
            sb_r = S.ring("wsb", [128, 2 * D], BF16, 3)
            n = 0
            for (src, b1, ncol) in ((wgu_sh, WB1G, 2 * D), (wdn_sh, WB1D, D)):
                rows = 128 * (2 * D // ncol)
                for r0 in range(0, R1, rows):
                    st = st_r.get()
                    sb = sb_r.get()
                    k = rows // 128
                    S.dma("sp", st[:].rearrange("p (k n) -> p k n", k=k), src[r0:r0 + rows, :].rearrange("(k p) n -> p k n", p=128), st, src)
                    CP("act" if n % 2 == 0 else "dve", sb[:], st[:], [st], [sb])
                    S.dma("sp", b1[r0:r0 + rows, :].rearrange("(k p) n -> p k n", p=128), sb[:].rearrange("p (k n) -> p k n", k=k), b1, sb)
                    n += 1
            zt = S.sbuf("zt", [128, D], F32)
            ztb = S.sbuf("ztb", [128, D], BF16)
            MEMSET("dve", zt[:], 0.0, [zt])
            MEMSET("dve", ztb[:], 0.0, [ztb])
            for r0 in range(0, NEXP * C + 128, 128):
                S.dma("sp", XDISP[r0:r0 + 128, :], ztb[:], XDISP, ztb)
                S.dma("sp", YDISP[r0:r0 + 128, :], zt[:], YDISP, zt)

    def w_collectives():
        for (b1, s1s, walls, RG, NC_) in ((WB1G, S1G, WALLG, RGG, NCG), (WB1D, S1D, WALLD, RGD, NCD)):
            for i in range(NC_):
                s1 = s1s[i % 2]
                S.cc("AllGather", ALU.bypass, G2, b1, s1, src_ap=b1[i * RG:(i + 1) * RG, :])
                wt = walls[i // 16]
                for g in range(2):
                    base = (((i % 16) * 2 + g) * 4) * RG
                    S.cc("AllGather", ALU.bypass, G4, s1, wt, src_ap=s1[g * RG:(g + 1) * RG, :],
                         dst_ap=wt[base:base + 4 * RG, :])

    S.w_pending = cfg.moe

    def wloc(L, e, RG, chunk):
        c, le = e // EPC, e % EPC
        g, r4 = c // 4, c % 4
        rr = ((L - cfg.WL0) * EPC + le) * D + chunk * RG
        i = rr // RG
        return i // 16, ((((i % 16) * 2 + g) * 4 + r4) * RG)

    XTBv = XTB.ap().rearrange("(k p) t -> p k t", p=128)

    def emit_xT_block(xt_f32, xT_tile, col, ps_ring, cp_eng):
        ps = ps_ring.get()
        for kc in range(8):
            TR(ps[:, kc * 128:(kc + 1) * 128], xt_f32[:, kc * 128:(kc + 1) * 128], ident_f[:], [xt_f32, ident_f], [ps])
        CP(cp_eng, xT_tile[:, :, col:col + 128], ps[:].rearrange("p (k t) -> p k t", k=8), [ps], [xT_tile])

    def xt_allgather():
        for kc in range(8):
            S.cc("AllGather", ALU.bypass, G4, XTB, XTALL, src_ap=XTB[kc * 128:(kc + 1) * 128, :],
                 dst_ap=XTALL[kc * 512:(kc + 1) * 512, :])

    def phase_x0():
        with S.phase():
            xin_r = S.ring("xin", [128, D], F32, 3)
            xT_r = S.ring("xT", [128, 8, 512], BF16, 2)
            ps_r = S.ring("psx", [128, D], F32, 2, psum=True)
            for tl in range(NTL):
                xT = xT_r.get()
                for b4 in range(4):
                    b = tl * 4 + b4
                    xt = xin_r.get()
                    S.dma("sp", xt[:], x_in[b * 128:(b + 1) * 128, :], xt, x_in)
                    emit_xT_block(xt, xT, b4 * 128, ps_r, "act" if b4 % 2 == 0 else "dve")
                S.dma("sp", XTBv[:, :, tl * 512:(tl + 1) * 512], xT[:], XTB, xT)
            xt_allgather()

    def layer_norm(z, outt, gt, bt, st_r, junk):
        st = st_r.get()
        ACT(junk[:], z[:], AF.Identity, [z], [junk, st], accum=st[:, 0:1])
        ACT(junk[:], z[:], AF.Square, [z], [junk, st], accum=st[:, 1:2])
        TS("dve", st[:, 2:3], st[:, 0:1], 1.0 / D, ALU.mult, [st], [st])
        TT("dve", st[:, 3:4], st[:, 2:3], st[:, 2:3], ALU.mult, [st], [st])
        STT(st[:, 4:5], st[:, 1:2], 1.0 / D, st[:, 3:4], ALU.mult, ALU.subtract, [st], [st])
        TS("dve", st[:, 4:5], st[:, 4:5], LN_EPS, ALU.add, [st], [st])
        ACT(st[:, 5:6], st[:, 4:5], AF.Sqrt, [st], [st])
        RECIP(st[:, 6:7], st[:, 5:6], [st], [st])
        TS("dve", outt[:], z[:], st[:, 2:3], ALU.subtract, [z, st], [outt], s2=st[:, 6:7], op1=ALU.mult)
        TT("dve", outt[:], outt[:], gt[:], ALU.mult, [outt, gt], [outt])
        TT("dve", outt[:], outt[:], bt[:], ALU.add, [outt, bt], [outt])

    def phase_ln_moe(L, xres, xnext, is_last):
        with S.phase():
            gt = S.sbuf("gt", [128, D], F32)
            bt = S.sbuf("bt", [128, D], F32)
            S.dma("sp", gt[:], lng.ap()[L, :].partition_broadcast(128), gt, lng)
            S.dma("sp", bt[:], lnb.ap()[L, :].partition_broadcast(128), bt, lnb)
            xr_r = S.ring("xr", [128, D], F32, 2)
            if cfg.mixer:
                wsrc = we_out if L % 2 == 0 else wo_out
                WO = S.sbuf("WO", [128, 8, D], BF16)
                for q4 in range(4):
                    S.dma("pool", WO[:, 2 * q4:2 * q4 + 2, :],
                          wsrc.ap()[L // 2, q4 * 256:(q4 + 1) * 256, :].rearrange("(k p) n -> p k n", p=128), WO, wsrc)
                OIDX = S.sbuf("OIDX", [128, NBL * 4], I32)
                S.dma("sp", OIDX[:], oidx[:, :], OIDX, oidx)
                og_r = S.ring("og", [128, 4, 256], BF16, 2)
                oT_r = S.ring("oT", [128, 8, 128], BF16, 2)
                pso_r = S.ring("pso", [128, D], BF16, 1, psum=True)
                psh_r = S.ring("psh", [128, 512], F32, 2, psum=True)
            z_r = S.ring("z", [128, D], F32, 2)
            x1_r = S.ring("x1", [128, D], F32, 2)
            x1b_r = S.ring("x1b", [128, D], BF16, 2)
            junk = S.sbuf("junk", [128, D], F32)
            st_r = S.ring("st", [128, 8], F32, 2)
            if cfg.moe:
                RW = S.sbuf("RW", [128, 8, NEXP], F32)
                RB = S.sbuf("RB", [128, NEXP], F32)
                S.dma("sp", RW[:], router_w.ap()[L].rearrange("(k p) e -> p k e", p=128), RW, router_w)
                S.dma("sp", RB[:], router_b.ap()[L, :].partition_broadcast(128), RB, router_b)
                x1T_r = S.ring("x1T", [128, 8, 128], F32, 2)
                psx_r = S.ring("psx", [128, D], F32, 1, psum=True)
                psl_r = S.ring("psl", [128, 512], F32, 1, psum=True)
                psp_r = S.ring("psp", [128, 512], F32, 2, psum=True)
                sm_r = S.ring("sm", [128, 12, NEXP], F32, 2)
                mb_r = S.ring("mb", [128, NEXP], BF16, 2)
                t8_r = S.ring("t8", [128, 8], F32, 2)
                sc_r = S.ring("sc", [128, 16], F32, 2)
                MEMSET("dve", CNT[:], 0.0, [CNT])
            for b in range(NBL):
                xr = xr_r.get()
                S.dma("sp", xr[:], xres[b * 128:(b + 1) * 128, :], xr, xres)
                z = z_r.get()
                if cfg.mixer:
                    og = og_r.get()
                    for s4 in range(4):
                        col = b * 4 + s4
                        S.dma("pool", None, None, og, OALL, extra=[OIDX],
                              fn=lambda e, col=col, og=og, s4=s4: e.indirect_dma_start(
                                  out=og[:, s4, :], out_offset=None, in_=OALL.ap()[:, :],
                                  in_offset=bass.IndirectOffsetOnAxis(ap=OIDX[:, col:col + 1], axis=0)))
                    pso = pso_r.get()
                    for kc in range(8):
                        TR(pso[:, kc * 128:(kc + 1) * 128], og[:, kc // 2, (kc % 2) * 128:(kc % 2 + 1) * 128], ident_b[:], [og, ident_b], [pso])
                    oT = oT_r.get()
                    CP("act", oT[:], pso[:].rearrange("p (k t) -> p k t", k=8), [pso], [oT])
                    for half in range(2):
                        psh = psh_r.get()
                        for kc in range(8):
                            MM(psh[:], oT[:, kc, :], WO[:, kc, half * 512:(half + 1) * 512], kc == 0, kc == 7, [oT, WO], [psh])
                        STT(z[:, half * 512:(half + 1) * 512], xr[:, half * 512:(half + 1) * 512], ALPHA, psh[:], ALU.mult, ALU.add, [xr, psh], [z])
                else:
                    TS("dve", z[:], xr[:], ALPHA, ALU.mult, [xr], [z])
                x1 = x1_r.get()
                layer_norm(z, x1, gt, bt, st_r, junk)
                S.dma("sp", XR1[b * 128:(b + 1) * 128, :], x1[:], XR1, x1)
                if not cfg.moe:
                    continue
                x1b = x1b_r.get()
                CP("act", x1b[:], x1[:], [x1], [x1b])
                x1T = x1T_r.get()
                ps = psx_r.get()
                for kc in range(8):
                    TR(ps[:, kc * 128:(kc + 1) * 128], x1[:, kc * 128:(kc + 1) * 128], ident_f[:], [x1, ident_f], [ps])
                CP("act", x1T[:], ps[:].rearrange("p (k t) -> p k t", k=8), [ps], [x1T])
                psl = psl_r.get()
                for kc in range(8):
                    MM(psl[:, 0:NEXP], x1T[:, kc, :], RW[:, kc, :], kc == 0, kc == 7, [x1T, RW], [psl])
                sm = sm_r.get()
                lg, ex, mk, gd, gates, pos, dfull, oh, jk = (sm[:, i, :] for i in range(9))
                t8 = t8_r.get()
                sc = sc_r.get()
                TT("dve", lg, psl[:, 0:NEXP], RB[:], ALU.add, [psl, RB], [sm])
                S.op("dve", lambda e, t8=t8, lg=lg: e.max(out=t8[:], in_=lg), reads=[sm], writes=[t8])
                TS("dve", sc[:, 0:1], t8[:, 0:1], -1.0, ALU.mult, [t8], [sc])
                ACT(ex, lg, AF.Exp, [sm, sc], [sm], bias=sc[:, 0:1])
                TS("dve", mk, lg, t8[:, 3:4], ALU.is_ge, [sm, t8], [sm])
                TT("dve", gd, ex, mk, ALU.mult, [sm], [sm])
                S.op("dve", lambda e, sc=sc, gd=gd: e.tensor_reduce(out=sc[:, 1:2], in_=gd, axis=AX.X, op=ALU.add),
                     reads=[sm], writes=[sc])
                RECIP(sc[:, 2:3], sc[:, 1:2], [sc], [sc])
                TS("dve", gates, gd, sc[:, 2:3], ALU.mult, [sm, sc], [sm])
                mb = mb_r.get()
                CP("dve", mb[:], mk, [sm], [mb])
                psp = psp_r.get()
                MM(psp[:, 0:NEXP], Ls_b[:], mb[:], True, True, [Ls_b, mb], [psp])
                TT("dve", pos, psp[:, 0:NEXP], CNT[:], ALU.add, [psp, CNT], [sm])
                psc = psp_r.get()
                MM(psc[:, 0:NEXP], ones_b[:], mb[:], True, True, [ones_b, mb], [psc])
                TT("dve", CNT[:], CNT[:], psc[:, 0:NEXP], ALU.add, [CNT, psc], [CNT])
                TT("dve", dfull, pos, EOFF[:], ALU.add, [sm, EOFF], [sm])
                for k in range(4):
                    TS("dve", oh, lg, t8[:, k:k + 1], ALU.is_equal, [sm, t8], [sm])
                    TT("dve", jk, oh, pos, ALU.mult, [sm], [sm])
                    S.op("dve", lambda e, sc=sc, jk=jk: e.tensor_reduce(out=sc[:, 4:5], in_=jk, axis=AX.X, op=ALU.add),
                         reads=[sm], writes=[sc])
                    TT("dve", jk, oh, dfull, ALU.mult, [sm], [sm])
                    S.op("dve", lambda e, sc=sc, jk=jk: e.tensor_reduce(out=sc[:, 5:6], in_=jk, axis=AX.X, op=ALU.add),
                         reads=[sm], writes=[sc])
                    TT("dve", jk, oh, gates, ALU.mult, [sm], [sm])
                    S.op("dve", lambda e, sc=sc, jk=jk: e.tensor_reduce(out=sc[:, 6:7], in_=jk, axis=AX.X, op=ALU.add),
                         reads=[sm], writes=[sc])
                    TS("dve", sc[:, 7:8], sc[:, 4:5], float(C), ALU.is_lt, [sc], [sc])
                    TS("dve", sc[:, 8:9], sc[:, 5:6], float(TRASH), ALU.subtract, [sc], [sc])
                    TT("dve", sc[:, 8:9], sc[:, 8:9], sc[:, 7:8], ALU.mult, [sc], [sc])
                    TS("dve", sc[:, 8:9], sc[:, 8:9], float(TRASH), ALU.add, [sc], [sc])
                    col = b * 4 + k
                    CP("dve", DESTI[:, col:col + 1], sc[:, 8:9], [sc], [DESTI])
                    TT("dve", GATE[:, col:col + 1], sc[:, 6:7], sc[:, 7:8], ALU.mult, [sc], [GATE])
                    S.dma("pool", None, None, XDISP, x1b, extra=[DESTI],
                          fn=lambda e, col=col, x1b=x1b: e.indirect_dma_start(
                              out=XDISP.ap()[:, :], out_offset=bass.IndirectOffsetOnAxis(ap=DESTI[:, col:col + 1], axis=0),
                              in_=x1b[:, :], in_offset=None))
        if cfg.moe:
            with S.phase():
                BG = S.sbuf("BG", [128, NEXP * 16], F32)
                S.dma("sp", BG[:], bgu[:, L * NEXP * 16:(L + 1) * NEXP * 16], BG, bgu)
                wgu_r = S.ring("wgu", [128, 8, 2 * D], BF16, 2)
                wdn_r = S.ring("wdn", [128, 8, D], BF16, 2)
                bd_r = S.ring("bd", [128, D], F32, 2)
                xs_r = S.ring("xs", [128, CB, D], BF16, 2)
                xsT_r = S.ring("xsT", [128, 8, C], BF16, 1)
                aT_r = S.ring("aT", [128, 8, C], BF16, 1)
                tmp_r = S.ring("tmp", [128, 5, 512], F32, 2)
                ys_r = S.ring("ys", [128, D], F32, 2)
                pst_r = S.ring("pst", [128, D], BF16, 2, psum=True)
                psg_r = S.ring("psg", [128, 512], F32, 2, psum=True)
                psu_r = S.ring("psu", [128, 512], F32, 2, psum=True)
                psd_r = S.ring("psd", [128, 512], F32, 2, psum=True)
                chunks = [(c0, min(512, C - c0)) for c0 in range(0, C, 512)]
                def load_expert(e_):
                    wg = wgu_r.get()
                    wd = wdn_r.get()
                    for q4 in range(4):
                        ti, row = wloc(L, e_, RGG, q4)
                        S.dma("sp", wg[:, 2 * q4:2 * q4 + 2, :],
                              WALLG[ti].ap()[row:row + 256, :].rearrange("(k p) n -> p k n", p=128), wg, WALLG[ti])
                    for q4 in range(2):
                        ti, row = wloc(L, e_, RGD, q4)
                        S.dma("sp", wd[:, 4 * q4:4 * q4 + 4, :],
                              WALLD[ti].ap()[row:row + 512, :].rearrange("(k p) n -> p k n", p=128), wd, WALLD[ti])
                    bd = bd_r.get()
                    S.dma("sp", bd[:], bdn.ap()[L * NEXP + e_, :].partition_broadcast(128), bd, bdn)
                    xs = xs_r.get()
                    S.dma("sp", xs[:], XDISP.ap()[e_ * C:(e_ + 1) * C, :].rearrange("(c p) d -> p c d", p=128), xs, XDISP)
                    return wg, wd, bd, xs

                nxt = load_expert(0)
                for e_ in range(NEXP):
                    wg, wd, bd, xs = nxt
                    if e_ + 1 < NEXP:
                        nxt = load_expert(e_ + 1)
                    xsT = xsT_r.get()
                    for cb in range(CB):
                        pst = pst_r.get()
                        for kc in range(8):
                            TR(pst[:, kc * 128:(kc + 1) * 128], xs[:, cb, kc * 128:(kc + 1) * 128], ident_b[:], [xs, ident_b], [pst])
                        CP("act" if cb % 2 == 0 else "dve", xsT[:, :, cb * 128:(cb + 1) * 128],
                           pst[:].rearrange("p (k t) -> p k t", k=8), [pst], [xsT])
                    aT = aT_r.get()
                    for j in range(8):
                        for (c0, w) in chunks:
                            psg = psg_r.get()
                            psu = psu_r.get()
                            for kc in range(8):
                                MM(psg[:, 0:w], wg[:, kc, j * 128:(j + 1) * 128], xsT[:, kc, c0:c0 + w], kc == 0, kc == 7, [wg, xsT], [psg])
                            for kc in range(8):
                                MM(psu[:, 0:w], wg[:, kc, D + j * 128:D + (j + 1) * 128], xsT[:, kc, c0:c0 + w], kc == 0, kc == 7, [wg, xsT], [psu])
                            tmp = tmp_r.get()
                            g1, sg, u1, u2, tt_ = (tmp[:, i, 0:w] for i in range(5))
                            bgc = (e_ * 16 + j)
                            TS("dve", g1, psg[:, 0:w], BG[:, bgc:bgc + 1], ALU.add, [psg, BG], [tmp], s2=7.0, op1=ALU.min)
                            ACT(sg, g1, AF.Sigmoid, [tmp], [tmp], scale=1.702)
                            TS("dve", u1, psu[:, 0:w], BG[:, bgc + 8:bgc + 9], ALU.add, [psu, BG], [tmp], s2=7.0, op1=ALU.min)
                            TS("dve", u2, u1, -7.0, ALU.max, [tmp], [tmp], s2=1.0, op1=ALU.add)
                            TT("pool", tt_, g1, sg, ALU.mult, [tmp], [tmp])
                            TT("pool", aT[:, j, c0:c0 + w], tt_, u2, ALU.mult, [tmp], [aT])
                    for cb in range(CB):
                        ys = ys_r.get()
                        for half in range(2):
                            psd = psd_r.get()
                            for j in range(8):
                                MM(psd[:], aT[:, j, cb * 128:(cb + 1) * 128], wd[:, j, half * 512:(half + 1) * 512], j == 0, j == 7, [aT, wd], [psd])
                            TT("dve", ys[:, half * 512:(half + 1) * 512], psd[:], bd[:, half * 512:(half + 1) * 512], ALU.add, [psd, bd], [ys])
                        S.dma("sp", YDISP[e_ * C + cb * 128:e_ * C + (cb + 1) * 128, :], ys[:], YDISP, ys)
        with S.phase():
            gt = S.sbuf("gt", [128, D], F32)
            bt = S.sbuf("bt", [128, D], F32)
            S.dma("sp", gt[:], lng.ap()[4 + L, :].partition_broadcast(128), gt, lng)
            S.dma("sp", bt[:], lnb.ap()[4 + L, :].partition_broadcast(128), bt, lnb)
            x1_r = S.ring("x1", [128, D], F32, 2)
            acc_r = S.ring("acc", [128, D], F32, 2)
            yk_r = S.ring("yk", [128, D], F32, 4)
            x2_r = S.ring("x2", [128, D], F32, 2)
            junk = S.sbuf("junk", [128, D], F32)
            st_r = S.ring("st", [128, 8], F32, 2)
            xT_r = S.ring("xT", [128, 8, 512], BF16, 2)
            ps_r = S.ring("psx", [128, D], F32, 2, psum=True)
            dst = out if is_last else xnext
            xT = None
            for b in range(NBL):
                x1 = x1_r.get()
                S.dma("sp", x1[:], XR1[b * 128:(b + 1) * 128, :], x1, XR1)
                acc = acc_r.get()
                TS("dve", acc[:], x1[:], ALPHA, ALU.mult, [x1], [acc])
                if cfg.moe:
                    for k in range(4):
                        col = b * 4 + k
                        yk = yk_r.get()
                        S.dma("pool", None, None, yk, YDISP, extra=[DESTI],
                              fn=lambda e, col=col, yk=yk: e.indirect_dma_start(
                                  out=yk[:, :], out_offset=None, in_=YDISP.ap()[:, :],
                                  in_offset=bass.IndirectOffsetOnAxis(ap=DESTI[:, col:col + 1], axis=0)))
                        STT(acc[:], yk[:], GATE[:, col:col + 1], acc[:], ALU.mult, ALU.add, [yk, GATE, acc], [acc])
                x2 = x2_r.get()
                layer_norm(acc, x2, gt, bt, st_r, junk)
                S.dma("sp", dst[b * 128:(b + 1) * 128, :], x2[:], dst, x2)
                if not is_last:
                    if b % 4 == 0:
                        xT = xT_r.get()
                    emit_xT_block(x2, xT, (b % 4) * 128, ps_r, "act")
                    if b % 4 == 3:
                        tl = b // 4
                        S.dma("sp", XTBv[:, :, tl * 512:(tl + 1) * 512], xT[:], XTB, xT)
            if not is_last:
                xt_allgather()


    XTALLv = XTALL.ap().rearrange("(k r p) t -> r p k t", k=8, r=4) if True else None

    def load_xT(xT, tt):
        rank, lt = tt // NTL, tt % NTL
        S.dma("sp", xT[:], XTALLv[rank][:, :, lt * 512:(lt + 1) * 512], xT, XTALL)

    def o_allgather():
        for j in range(SEQ // TC):
            S.cc("AllGather", ALU.bypass, G4, OTOK, OALL, src_ap=OTOK[j * TC:(j + 1) * TC, :],
                 dst_ap=OALL[j * 4 * TC:(j + 1) * 4 * TC, :])

    def odd_mixer(L):
        i = L // 2
        with S.phase():
            Wb = S.sbuf("Wb", [128, 8, 1028], BF16)
            for q4 in range(4):
                S.dma("pool", Wb[:, 2 * q4:2 * q4 + 2, :],
                      wo_in.ap()[i, q4 * 256:(q4 + 1) * 256, :].rearrange("(k p) n -> p k n", p=128), Wb, wo_in)
            WQK = S.sbuf("WQK", [64, 2], F32)
            S.dma("sp", WQK[:], qkw.ap()[i], WQK, qkw)
            TS("dve", WQK[:, 0:1], WQK[:, 0:1], 0.125, ALU.mult, [WQK], [WQK])
            NFB = S.sbuf("NFB", [128, 4], F32)
            S.dma("sp", NFB[:], fb.ap()[i, :].partition_broadcast(128), NFB, fb)
            TS("dve", NFB[:], NFB[:], -1.0, ALU.mult, [NFB], [NFB])
            MKf = S.sbuf("MKf", [128, 4, 512], F32)
            MK = S.sbuf("MK", [128, 4, 512], BF16)
            MEMSET("pool", MKf[:], 0.0, [MKf])
            for j in range(4):
                ASEL(MKf[:, j, :], MKf[:, j, :], [[1, 512]], ALU.is_ge, NEG, -128 * j, -1, [MKf], [MKf])
            CP("dve", MK[:], MKf[:], [MKf], [MK])
            QT = S.sbuf("QT", [67, SEQ], BF16)
            KT = S.sbuf("KT", [67, SEQ], BF16)
            GTK = S.sbuf("GTK", [128, NBF, 64], BF16)
            VA = S.sbuf("VA", [128, NBF, 65], BF16)
            LF = S.sbuf("LF", [128, NBF], F32)
            CUM = S.sbuf("CUM", [128, NBF], F32)
            NEGCUM = S.sbuf("NEGCUM", [128, NBF], F32)
            scan = [S.sbuf("scan%d" % k, [128, NBF], F32) for k in range(2)]
            tot = S.sbuf("tot", [128, NBF], F32)
            ct = S.sbuf("ct", [128, 6, 128], F32)
            ctb = S.sbuf("ctb", [128, 3, 128], BF16)
            MEMSET("dve", VA[:, :, 64:65], 1.0, [VA])
            MEMSET("dve", KT[64:67, :], 1.0, [KT])
            xT_r = S.ring("xT", [128, 8, 512], BF16, 2)
            qf_r = S.ring("qf", [64, 512], F32, 2)
            sq_r = S.ring("sq", [64, 512], F32, 2)
            rs_r = S.ring("rs", [64, 512], F32, 2)
            g1_r = S.ring("g1", [128, 64], F32, 3)
            sm_r = S.ring("smo", [128, 8], F32, 4)
            pt_r = S.ring("pt", [128, 512], BF16, 4)
            ot_r = S.ring("ot", [128, 4, 64], BF16, 2)
            osb_r = S.ring("osb", [128, 512], F32, 2)
            for b_ in osb_r.bufs:
                MEMSET("dve", b_[:], 0.0, [b_])
            psm_r = S.ring("psm", [128, 512], F32, 3, psum=True)
            pss_r = S.ring("pss", [128, 512], F32, 3, psum=True)
            pso_r = S.ring("pso", [128, 512], F32, 2, psum=True)
            import os
            KODD = int(os.environ.get("KODD", "9"))
            if KODD == 0:
                return
            for h in range(4):
                w0 = h * 257
                for tt in range(NQT):
                    xT = xT_r.get()
                    load_xT(xT, tt)
                    for (coff, dst, wi) in ((0, QT, 0), (64, KT, 1)):
                        psq = psm_r.get()
                        for kc in range(8):
                            MM(psq[0:64, :], Wb[:, kc, w0 + coff:w0 + coff + 64], xT[:, kc, :], kc == 0, kc == 7, [Wb, xT], [psq])
                        qf = qf_r.get()
                        CP("act", qf[:], psq[0:64, :], [psq], [qf])
                        sq = sq_r.get()
                        TT("pool", sq[:], qf[:], qf[:], ALU.mult, [qf], [sq])
                        pssum = psm_r.get()
                        MM(pssum[0:64, :], ones_f[0:64, 0:64], sq[:], True, True, [ones_f, sq], [pssum])
                        rs = rs_r.get()
                        TS("dve", rs[:], pssum[0:64, :], 1.0 / 64, ALU.mult, [pssum], [rs], s2=RMS_EPS, op1=ALU.add)
                        ACT(rs[:], rs[:], AF.Ln, [rs], [rs])
                        ACT(rs[:], rs[:], AF.Exp, [rs], [rs], scale=-0.5)
                        STT(dst[0:64, tt * 512:(tt + 1) * 512], qf[:], WQK[:, wi:wi + 1], rs[:], ALU.mult, ALU.mult, [qf, WQK, rs], [dst])
                    for b4 in range(4):
                        gb = tt * 4 + b4
                        ps = psm_r.get()
                        for kc in range(8):
                            MM(ps[:, 0:129], xT[:, kc, b4 * 128:(b4 + 1) * 128], Wb[:, kc, w0 + 128:w0 + 257], kc == 0, kc == 7, [xT, Wb], [ps])
                        sm = sm_r.get()
                        CP("dve", VA[:, gb, 0:64], ps[:, 0:64], [ps], [VA])
                        g1 = g1_r.get()
                        ACT(g1[:], ps[:, 65:129], AF.Exp, [ps], [g1], scale=-1.0)
                        TS("pool", g1[:], g1[:], 1.0, ALU.add, [g1], [g1])
                        RECIP(g1[:], g1[:], [g1], [g1])
                        CP("pool", GTK[:, gb, :], g1[:], [g1], [GTK])
                        ACT(sm[:, 0:1], ps[:, 64:65], AF.Exp, [ps, NFB], [sm], bias=NFB[:, h:h + 1], scale=-1.0)
                        ACT(sm[:, 1:2], sm[:, 0:1], AF.Ln, [sm], [sm], bias=1.0)
                        TS("dve", LF[:, gb:gb + 1], sm[:, 1:2], -1.0, ALU.mult, [sm], [LF])
                if KODD == 1:
                    return
                psw = psm_r.get()
                MM(psw[:, 0:NBF], triU_f[:], LF[:], True, True, [triU_f, LF], [psw])
                pstot = psm_r.get()
                MM(pstot[:, 0:NBF], ones_f[:], LF[:], True, True, [ones_f, LF], [pstot])
                CP("dve", tot[:], pstot[:, 0:NBF], [pstot], [tot])
                CP("dve", scan[0][:], tot[:], [tot], [scan[0]])
                a, bq = scan[0], scan[1]
                sft = 1
                while sft < NBF:
                    TT("dve", bq[:, sft:NBF], a[:, sft:NBF], a[:, 0:NBF - sft], ALU.add, [a], [bq])
                    CP("dve", bq[:, 0:sft], a[:, 0:sft], [a], [bq])
                    a, bq = bq, a
                    sft *= 2
                TT("dve", CUM[:], psw[:, 0:NBF], a[:], ALU.add, [psw, a], [CUM])
                TT("dve", CUM[:], CUM[:], tot[:], ALU.subtract, [CUM, tot], [CUM])
                TS("dve", NEGCUM[:], CUM[:], -1.0, ALU.mult, [CUM], [NEGCUM])
                pct = psm_r.get()
                TR(pct[0:NBF, 0:128], CUM[:, 0:NBF], ident_f[:], [CUM, ident_f], [pct])
                CP("dve", ct[0:NBF, 0, :], pct[0:NBF, 0:128], [pct], [ct])
                CP("dve", ctb[0:NBF, 0, :], ct[0:NBF, 0, :], [ct], [ctb])
                CP("dve", ct[0:NBF, 1, :], ctb[0:NBF, 0, :], [ctb], [ct])
                TT("dve", ct[0:NBF, 2, :], ct[0:NBF, 0, :], ct[0:NBF, 1, :], ALU.subtract, [ct], [ct])
                CP("dve", ctb[0:NBF, 1, :], ct[0:NBF, 2, :], [ct], [ctb])
                CP("dve", ct[0:NBF, 3, :], ctb[0:NBF, 1, :], [ctb], [ct])
                TT("dve", ct[0:NBF, 4, :], ct[0:NBF, 2, :], ct[0:NBF, 3, :], ALU.subtract, [ct], [ct])
                CP("dve", ctb[0:NBF, 2, :], ct[0:NBF, 4, :], [ct], [ctb])
                for j in range(3):
                    S.dma("sp", CQ.ap()[j, :].rearrange("(j t) -> j t", t=128), ctb[0:NBF, j, :], CQ, ctb)
                for j in range(3):
                    S.dma("sp", QT[64 + j:65 + j, :], CQ[j:j + 1, :], QT, CQ)
                if KODD == 2:
                    return
                acc_of = {}

                def stage_a(qt, kb):
                    pss = pss_r.get()
                    diag = kb >= 4 * qt
                    MM(pss[:], KT[0:67, kb * 128:(kb + 1) * 128], QT[0:67, qt * 512:(qt + 1) * 512], True, not diag, [KT, QT], [pss])
                    if diag:
                        MM(pss[:], ident_b[:], MK[:, kb - 4 * qt, :], False, True, [ident_b, MK], [pss])
                    pt = pt_r.get()
                    ACT(pt[:], pss[:], AF.Exp, [pss, NEGCUM], [pt], bias=NEGCUM[:, kb:kb + 1])
                    return pt

                def stage_b(qt, kb, pt):
                    nkb = 4 * qt + 4
                    if kb == 0:
                        acc_of[qt] = pso_r.get()
                    oacc = acc_of[qt]
                    MM(oacc[0:65, :], VA[:, kb, :], pt[:], kb == 0, kb == nkb - 1, [VA, pt], [oacc])
                    if kb < nkb - 1:
                        return
                    osb = osb_r.get()
                    CP("dve", osb[0:65, :], oacc[0:65, :], [oacc], [osb])
                    ptr = psm_r.get()
                    for b4 in range(4):
                        TR(ptr[:, b4 * 128:(b4 + 1) * 128], osb[:, b4 * 128:(b4 + 1) * 128], ident_f[:], [osb, ident_f], [ptr])
                    ot = ot_r.get()
                    for b4 in range(4):
                        gb = qt * 4 + b4
                        sm = sm_r.get()
                        RECIP(sm[:, 0:1], ptr[:, b4 * 128 + 64:b4 * 128 + 65], [ptr], [sm])
                        STT(ot[:, b4, :], ptr[:, b4 * 128:b4 * 128 + 64], sm[:, 0:1], GTK[:, gb, :], ALU.mult, ALU.mult, [ptr, sm, GTK], [ot])
                    S.dma("sp", OTOK.ap()[qt * 512:(qt + 1) * 512, h * 64:(h + 1) * 64].rearrange("(b p) d -> p b d", p=128),
                          ot[:], OTOK, ot)

                pend = []
                for qt in range(NQT):
                    for kb in range(4 * qt + 4):
                        pend.append((qt, kb, stage_a(qt, kb)))
                        if len(pend) > 2:
                            stage_b(*pend.pop(0))
                while pend:
                    stage_b(*pend.pop(0))
                if KODD == 3:
                    return
            o_allgather()

    def even_mixer(L):
        i = L // 2
        lam_init = 0.8 - 0.6 * math.exp(-0.3 * L)
        import os
        KEV = int(os.environ.get("KEV", "9"))
        with S.phase():
            Wd = S.sbuf("Wd", [128, 8, 384], BF16)
            for q4 in range(4):
                S.dma("pool", Wd[:, 2 * q4:2 * q4 + 2, :],
                      we_in.ap()[i, q4 * 256:(q4 + 1) * 256, 0:384].rearrange("(k p) n -> p k n", p=128), Wd, we_in)
            LP = S.sbuf("LP", [128, 256], F32)
            S.dma("sp", LP[:], lamp.ap()[i, :].partition_broadcast(128), LP, lamp)
            lw = S.sbuf("lw", [128, 8], F32)
            pr = S.sbuf("pr", [128, 128], F32)
            TT("dve", pr[:, 0:64], LP[:, 0:64], LP[:, 64:128], ALU.mult, [LP], [pr])
            TT("dve", pr[:, 64:128], LP[:, 128:192], LP[:, 192:256], ALU.mult, [LP], [pr])
            S.op("dve", lambda e: e.tensor_reduce(out=lw[:, 0:2], in_=pr[:].rearrange("p (g d) -> p g d", g=2), axis=AX.X, op=ALU.add),
                 reads=[pr], writes=[lw])
            ACT(lw[:, 2:4], lw[:, 0:2], AF.Exp, [lw], [lw])
            TT("dve", lw[:, 4:5], lw[:, 2:3], lw[:, 3:4], ALU.subtract, [lw], [lw])
            TS("dve", lw[:, 4:5], lw[:, 4:5], lam_init, ALU.add, [lw], [lw])
            TS("dve", lw[:, 5:6], lw[:, 4:5], -1.0, ALU.mult, [lw], [lw])
            SUBW = S.sbuf("SUBW", [128, 128], F32)
            S.dma("sp", SUBW[:], subw.ap()[i, :].partition_broadcast(128), SUBW, subw)
            TS("dve", SUBW[:], SUBW[:], 1.0 - lam_init, ALU.mult, [SUBW], [SUBW])
            T5C = S.sbuf("T5C", [33, 1], F32)
            S.dma("sp", T5C[0:32, :], t5col[:, :], T5C, t5col)
            MEMSET("dve", T5C[32:33, :], 1.0, [T5C])
            OH = S.sbuf("OH", [33, 2560], F32)
            S.dma("sp", OH[:], t5oh[:, :], OH, t5oh)
            bvs = S.sbuf("bvs", [1, 2560], F32)
            psm_r = S.ring("psm", [128, 512], F32, 3, psum=True)
            pss_r = S.ring("pss", [128, 512], F32, 3, psum=True)
            pso_r = S.ring("pso", [128, 512], F32, 2, psum=True)
            for c5 in range(5):
                ps = psm_r.get()
                MM(ps[0:1, :], T5C[:, 0:1], OH[:, c5 * 512:(c5 + 1) * 512], True, True, [T5C, OH], [ps])
                CP("dve", bvs[0:1, c5 * 512:(c5 + 1) * 512], ps[0:1, :], [ps], [bvs])
            S.dma("sp", BV.ap().rearrange("(o n) -> o n", o=1), bvs[:], BV, bvs)
            BT = S.sbuf("BT", [128, 16, 512], BF16)
            skb_r = S.ring("skb", [128, 640], F32, 2)
            for j in range(16):
                skb = skb_r.get()
                S.dma("sp", skb[:], BV.ap()[128 * j:128 * j + 640].partition_broadcast(128), skb, BV)
                S.dma("sp", SKW.ap()[j, :].rearrange("(p n) -> p n", n=640), skb[:], SKW, skb)
                S.dma("pool", BT[:, j, :], bass.AP(tensor=SKW.h, offset=j * 128 * 640 + 127, ap=[[639, 128], [1, 512]]), BT, SKW)
            B31 = S.sbuf("B31", [128, 1], F32)
            S.dma("sp", B31[:], BV.ap()[2559:2560].partition_broadcast(128), B31, BV)
            if S.w_pending:
                S.w_pending = False
                w_collectives()
            QT = S.sbuf("QT", [64, SEQ], BF16)
            KT = S.sbuf("KT", [64, SEQ], BF16)
            VA = S.sbuf("VA", [128, NBF, 129], BF16)
            MEMSET("dve", VA[:, :, 64:65], 1.0, [VA])
            xT_r = S.ring("xT", [128, 8, 512], BF16, 2)
            pt_r = S.ring("pt", [128, 512], BF16, 4)
            osb_r = S.ring("osbd", [128, 512], F32, 4)
            for b_ in osb_r.bufs:
                MEMSET("dve", b_[:], 0.0, [b_])
            om_r = S.ring("om", [128, 128], F32, 3)
            o0_r = S.ring("o0", [128, 128], F32, 2)
            a_r = S.ring("a", [128, 128], F32, 2)
            ao_r = S.ring("ao", [128, 128], BF16, 2)
            sm_r = S.ring("smd", [128, 8], F32, 4)
            junk = S.sbuf("junkd", [128, 128], F32)
            for m in range(2):
                for tt in range(NQT):
                    xT = xT_r.get()
                    load_xT(xT, tt)
                    psq = psm_r.get()
                    for kc in range(8):
                        MM(psq[0:64, :], Wd[:, kc, m * 64:(m + 1) * 64], xT[:, kc, :], kc == 0, kc == 7, [Wd, xT], [psq])
                    ACT(QT[:, tt * 512:(tt + 1) * 512], psq[0:64, :], AF.Identity, [psq], [QT], scale=0.125)
                    psk = psm_r.get()
                    for kc in range(8):
                        MM(psk[0:64, :], Wd[:, kc, 128 + m * 64:128 + (m + 1) * 64], xT[:, kc, :], kc == 0, kc == 7, [Wd, xT], [psk])
                    CP("dve", KT[:, tt * 512:(tt + 1) * 512], psk[0:64, :], [psk], [KT])
                    if m == 0:
                        for b4 in range(4):
                            gb = tt * 4 + b4
                            psv = psm_r.get()
                            for kc in range(8):
                                MM(psv[:, 0:128], xT[:, kc, b4 * 128:(b4 + 1) * 128], Wd[:, kc, 256:384], kc == 0, kc == 7, [xT, Wd], [psv])
                            CP("act", VA[:, gb, 0:64], psv[:, 0:64], [psv], [VA])
                            CP("act", VA[:, gb, 65:129], psv[:, 64:128], [psv], [VA])
                if KEV == 1:
                    return
                acc_of = {}

                def stage_a(qt, kb):
                    dj = 4 * qt - kb + 3
                    near = dj <= 15
                    pss = pss_r.get()
                    MM(pss[:], KT[:, kb * 128:(kb + 1) * 128], QT[:, qt * 512:(qt + 1) * 512], True, not near, [KT, QT], [pss])
                    if near:
                        MM(pss[:], ident_b[:], BT[:, dj, :], False, True, [ident_b, BT], [pss])
                    pt = pt_r.get()
                    if near:
                        ACT(pt[:], pss[:], AF.Exp, [pss], [pt])
                    else:
                        ACT(pt[:], pss[:], AF.Exp, [pss, B31], [pt], bias=B31[:, 0:1])
                    return pt

                def stage_b(qt, kb, pt, m=m):
                    nkb = 4 * qt + 4
                    if kb == 0:
                        acc_of[qt] = (pso_r.get(), pso_r.get())
                    oa, ob = acc_of[qt]
                    MM(oa[0:65, :], VA[:, kb, 0:65], pt[:], kb == 0, kb == nkb - 1, [VA, pt], [oa])
                    MM(ob[0:64, :], VA[:, kb, 65:129], pt[:], kb == 0, kb == nkb - 1, [VA, pt], [ob])
                    if kb < nkb - 1:
                        return
                    osa = osb_r.get()
                    osb = osb_r.get()
                    CP("dve", osa[0:65, :], oa[0:65, :], [oa], [osa])
                    CP("act", osb[0:64, :], ob[0:64, :], [ob], [osb])
                    ptra = psm_r.get()
                    ptrb = psm_r.get()
                    for b4 in range(4):
                        TR(ptra[:, b4 * 128:(b4 + 1) * 128], osa[:, b4 * 128:(b4 + 1) * 128], ident_f[:], [osa, ident_f], [ptra])
                        TR(ptrb[:, b4 * 128:(b4 + 1) * 128], osb[:, b4 * 128:(b4 + 1) * 128], ident_f[:], [osb, ident_f], [ptrb])
                    for b4 in range(4):
                        gb = qt * 4 + b4
                        sm = sm_r.get()
                        RECIP(sm[:, 0:1], ptra[:, b4 * 128 + 64:b4 * 128 + 65], [ptra], [sm])
                        om = om_r.get()
                        TS("dve", om[:, 0:64], ptra[:, b4 * 128:b4 * 128 + 64], sm[:, 0:1], ALU.mult, [ptra, sm], [om])
                        TS("dve", om[:, 64:128], ptrb[:, b4 * 128:b4 * 128 + 64], sm[:, 0:1], ALU.mult, [ptrb, sm], [om])
                        if m == 0:
                            S.dma("sp", O0S[gb * 128:(gb + 1) * 128, :], om[:], O0S, om)
                        else:
                            o0 = o0_r.get()
                            S.dma("sp", o0[:], O0S[gb * 128:(gb + 1) * 128, :], o0, O0S)
                            av = a_r.get()
                            STT(av[:], om[:], lw[:, 5:6], o0[:], ALU.mult, ALU.add, [om, lw, o0], [av])
                            ACT(junk[:], av[:], AF.Square, [av], [junk, sm], accum=sm[:, 1:2])
                            TS("dve", sm[:, 2:3], sm[:, 1:2], 1.0 / 128, ALU.mult, [sm], [sm], s2=RMS_EPS, op1=ALU.add)
                            ACT(sm[:, 3:4], sm[:, 2:3], AF.Ln, [sm], [sm])
                            ACT(sm[:, 4:5], sm[:, 3:4], AF.Exp, [sm], [sm], scale=-0.5)
                            ao = ao_r.get()
                            STT(ao[:], av[:], sm[:, 4:5], SUBW[:], ALU.mult, ALU.mult, [av, sm, SUBW], [ao])
                            S.dma("sp", OTOK[gb * 128:(gb + 1) * 128, 0:128], ao[:], OTOK, ao)

                pend = []
                for qt in range(NQT):
                    for kb in range(4 * qt + 4):
                        pend.append((qt, kb, stage_a(qt, kb)))
                        if len(pend) > 2:
                            stage_b(*pend.pop(0))
                while pend:
                    stage_b(*pend.pop(0))
        if KEV <= 2:
            with S.phase():
                zb = S.sbuf("zb", [128, 128], BF16)
                MEMSET("dve", zb[:], 0.0, [zb])
                for gb in range(NBF):
                    S.dma("sp", OTOK[gb * 128:(gb + 1) * 128, 128:256], zb[:], OTOK, zb)
            o_allgather()
            return
        gdn_phase(L)
        o_allgather()

    def gdn_phase(L):
        i = L // 2
        SC = 128 ** -0.5
        with S.phase():
            Wg = S.sbuf("Wg", [128, 8, 514], BF16)
            for q4 in range(4):
                S.dma("pool", Wg[:, 2 * q4:2 * q4 + 2, :],
                      we_in.ap()[i, q4 * 256:(q4 + 1) * 256, 384:898].rearrange("(k p) n -> p k n", p=128), Wg, we_in)
            CW = S.sbuf("CW", [128, 12], F32)
            S.dma("sp", CW[:], convw.ap()[i], CW, convw)
            GNW = S.sbuf("GNW", [128, 128], F32)
            S.dma("sp", GNW[:], gnw.ap()[i, :].partition_broadcast(128), GNW, gnw)
            AD = S.sbuf("AD", [128, 4], F32)
            S.dma("sp", AD[:, 0:1], alog.ap()[i, :].partition_broadcast(128), AD, alog)
            S.dma("sp", AD[:, 1:2], dtb.ap()[i, :].partition_broadcast(128), AD, dtb)
            ACT(AD[:, 2:3], AD[:, 0:1], AF.Exp, [AD], [AD])
            TS("dve", AD[:, 2:3], AD[:, 2:3], -1.0, ALU.mult, [AD], [AD])
            MUs = S.sbuf("MUs", [128, 128], F32)
            MUi = S.sbuf("MUi", [128, 128], F32)
            MLs = S.sbuf("MLs", [128, 128], F32)
            for (t_, base, cm, pat) in ((MUs, -1, -1, 1), (MUi, 0, -1, 1), (MLs, -1, 1, -1)):
                MEMSET("pool", t_[:], 1.0, [t_])
                ASEL(t_[:], t_[:], [[pat, 128]], ALU.is_ge, 0.0, base, cm, [t_], [t_])
            St = [S.sbuf("St%d" % k, [128, 128], F32) for k in range(2)]
            MEMSET("dve", St[0][:], 0.0, [St[0]])
            cb = [S.sbuf("cb%d" % g, [128, 515], F32) for g in range(3)]
            for g in range(3):
                MEMSET("dve", cb[g][:], 0.0, [cb[g]])
            hal = S.sbuf("hal", [128, 3, 3], F32)
            xT_r = S.ring("xT", [128, 8, 512], BF16, 2)
            y_r = S.ring("y", [128, 512], F32, 2)
            fT = [S.ring("fT%d" % g, [128, 512], F32, 2) for g in range(3)]
            sq_r = S.ring("sqg", [128, 512], F32, 2)
            zt_r = S.ring("ztk", [128, 4, 128], F32, 2)
            gb_r = S.ring("gbt", [128, 12], F32, 2)
            w_r = S.ring("wk", [128, 128], F32, 44)
            w2_r = S.ring("wk2", [128, 256], F32, 13)
            c_r = S.ring("colg", [128, 8], F32, 9)
            bo_r = S.ring("bo", [128, 128], BF16, 2)
            ps_r = S.ring("psg", [128, 512], F32, 7, psum=True)
            pso_r = S.ring("psgo", [128, 512], F32, 1, psum=True)
            cur = 0
            for tt in range(NQT):
                xT = xT_r.get()
                load_xT(xT, tt)
                fts = []
                for g in range(3):
                    psf = ps_r.get()
                    for kc in range(8):
                        MM(psf[:], Wg[:, kc, g * 128:(g + 1) * 128], xT[:, kc, :], kc == 0, kc == 7, [Wg, xT], [psf])
                    CP("dve", hal[:, g, :], cb[g][:, 512:515], [cb[g]], [hal])
                    CP("act", cb[g][:, 3:515], psf[:], [psf], [cb[g]])
                    CP("dve", cb[g][:, 0:3], hal[:, g, :], [hal], [cb[g]])
                    y = y_r.get()
                    TS("dve", y[:], cb[g][:, 0:512], CW[:, g * 4:g * 4 + 1], ALU.mult, [cb[g], CW], [y])
                    for j in range(1, 4):
                        STT(y[:], cb[g][:, j:j + 512], CW[:, g * 4 + j:g * 4 + j + 1], y[:], ALU.mult, ALU.add, [cb[g], CW, y], [y])
                    ft = fT[g].get()
                    ACT(ft[:], y[:], AF.Silu, [y], [ft])
                    if g < 2:
                        sq = sq_r.get()
                        TT("pool", sq[:], ft[:], ft[:], ALU.mult, [ft], [sq])
                        pss = ps_r.get()
                        MM(pss[:], ones_f[:], sq[:], True, True, [ones_f, sq], [pss])
                        TS("dve", sq[:], pss[:], RMS_EPS, ALU.add, [pss], [sq])
                        ACT(sq[:], sq[:], AF.Ln, [sq], [sq])
                        ACT(sq[:], sq[:], AF.Exp, [sq], [sq], scale=-0.5)
                        TT("dve", ft[:], ft[:], sq[:], ALU.mult, [ft, sq], [ft])
                    fts.append(ft)
                qTt, kTt, vTt = fts
                zt = zt_r.get()
                gbt = gb_r.get()
                for b4 in range(4):
                    pz = ps_r.get()
                    for kc in range(8):
                        MM(pz[:, 0:130], xT[:, kc, b4 * 128:(b4 + 1) * 128], Wg[:, kc, 384:514], kc == 0, kc == 7, [xT, Wg], [pz])
                    ACT(zt[:, b4, :], pz[:, 0:128], AF.Silu, [pz], [zt])
                    ACT(gbt[:, 4 + b4:5 + b4], pz[:, 128:129], AF.Sigmoid, [pz], [gbt])
                    cg = c_r.get()
                    ACT(cg[:, 0:1], pz[:, 129:130], AF.Exp, [pz, AD], [cg], bias=AD[:, 1:2])
                    ACT(cg[:, 1:2], cg[:, 0:1], AF.Ln, [cg], [cg], bias=1.0)
                    TS("dve", gbt[:, b4:b4 + 1], cg[:, 1:2], AD[:, 2:3], ALU.mult, [cg, AD], [gbt])
                pgc = ps_r.get()
                MM(pgc[:, 0:4], triU_f[:], gbt[:, 0:4], True, True, [triU_f, gbt], [pgc])
                CP("dve", gbt[:, 8:12], pgc[:, 0:4], [pgc], [gbt])
                for b4 in range(4):
                    gbk = tt * 4 + b4
                    blk = slice(b4 * 128, (b4 + 1) * 128)
                    gc = gbt[:, 8 + b4:9 + b4]
                    beta = gbt[:, 4 + b4:5 + b4]
                    pk = ps_r.get()
                    TR(pk[:, 0:128], kTt[:, blk], ident_f[:], [kTt, ident_f], [pk])
                    Kc = w_r.get()
                    CP("act", Kc[:], pk[:, 0:128], [pk], [Kc])
                    pv = ps_r.get()
                    TR(pv[:, 0:128], vTt[:, blk], ident_f[:], [vTt, ident_f], [pv])
                    Vc = w_r.get()
                    CP("act", Vc[:], pv[:, 0:128], [pv], [Vc])
                    DG = w2_r.get()
                    TS("dve", DG[:, 0:128], ident_f[:], gc, ALU.mult, [ident_f, gbt], [DG])
                    TS("dve", DG[:, 128:256], ident_f[:], beta, ALU.mult, [ident_f, gbt], [DG])
                    pb = ps_r.get()
                    MM(pb[:, 0:256], ones_f[:], DG[:], True, True, [ones_f, DG], [pb])
                    GB = w2_r.get()
                    CP("act", GB[:], pb[:, 0:256], [pb], [GB])
                    dlt = w_r.get()
                    TS("dve", dlt[:], GB[:, 0:128], gc, ALU.subtract, [GB, gbt], [dlt])
                    ET = w_r.get()
                    TS("dve", ET[:], dlt[:], 0.0, ALU.min, [dlt], [ET])
                    ACT(ET[:], ET[:], AF.Exp, [ET], [ET])
                    E2 = w_r.get()
                    TS("dve", E2[:], dlt[:], -1.0, ALU.mult, [dlt], [E2], s2=0.0, op1=ALU.min)
                    ACT(E2[:], E2[:], AF.Exp, [E2], [E2])
                    EG = w_r.get()
                    ACT(EG[:], GB[:, 0:128], AF.Exp, [GB], [EG])
                    pkk = ps_r.get()
                    MM(pkk[:, 0:128], kTt[:, blk], kTt[:, blk], True, True, [kTt], [pkk])
                    Nj = w_r.get()
                    TT("dve", Nj[:], pkk[:, 0:128], GB[:, 128:256], ALU.mult, [pkk, GB], [Nj])
                    TT("dve", Nj[:], Nj[:], ET[:], ALU.mult, [Nj, ET], [Nj])
                    STT(Nj[:], Nj[:], -1.0, MUs[:], ALU.mult, ALU.mult, [Nj, MUs], [Nj])
                    Pj = w_r.get()
                    STT(Pj[:], pkk[:, 0:128], beta, E2[:], ALU.mult, ALU.mult, [pkk, gbt, E2], [Pj])
                    STT(Pj[:], Pj[:], -1.0, MLs[:], ALU.mult, ALU.mult, [Pj, MLs], [Pj])
                    cg = c_r.get()
                    ACT(cg[:, 0:1], gc, AF.Exp, [gbt], [cg])
                    TT("dve", cg[:, 1:2], cg[:, 0:1], beta, ALU.mult, [cg, gbt], [cg])
                    Rm = w2_r.get()
                    TS("dve", Rm[:, 0:128], Vc[:], beta, ALU.mult, [Vc, gbt], [Rm])
                    TS("dve", Rm[:, 128:256], Kc[:], cg[:, 1:2], ALU.mult, [Kc, cg], [Rm])
                    for j in range(7):
                        pr_ = ps_r.get()
                        MM(pr_[:, 0:256], Nj[:], Rm[:], True, True, [Nj, Rm], [pr_])
                        Rn = w2_r.get()
                        TT("dve", Rn[:], Rm[:], pr_[:, 0:256], ALU.add, [Rm, pr_], [Rn])
                        Rm = Rn
                        if j < 6:
                            pn = ps_r.get()
                            MM(pn[:, 0:128], Pj[:], Nj[:], True, True, [Pj, Nj], [pn])
                            pp = ps_r.get()
                            MM(pp[:, 0:128], Nj[:], Pj[:], True, True, [Nj, Pj], [pp])
                            Nn = w_r.get()
                            CP("act", Nn[:], pn[:, 0:128], [pn], [Nn])
                            Pn = w_r.get()
                            CP("act", Pn[:], pp[:, 0:128], [pp], [Pn])
                            Nj, Pj = Nn, Pn
                    pw = ps_r.get()
                    TR(pw[:, 0:128], Rm[:, 128:256], ident_f[:], [Rm, ident_f], [pw])
                    WT = w_r.get()
                    CP("act", WT[:], pw[:, 0:128], [pw], [WT])
                    Sc, Sn = St[cur], St[1 - cur]
                    pws = ps_r.get()
                    MM(pws[:, 0:128], WT[:], Sc[:], True, True, [WT, Sc], [pws])
                    Vn = w_r.get()
                    TT("dve", Vn[:], Rm[:, 0:128], pws[:, 0:128], ALU.subtract, [Rm, pws], [Vn])
                    pqk = ps_r.get()
                    MM(pqk[:, 0:128], kTt[:, blk], qTt[:, blk], True, True, [kTt, qTt], [pqk])
                    qki = w_r.get()
                    TT("dve", qki[:], pqk[:, 0:128], ET[:], ALU.mult, [pqk, ET], [qki])
                    STT(qki[:], qki[:], SC, MUi[:], ALU.mult, ALU.mult, [qki, MUi], [qki])
                    QdT = w_r.get()
                    STT(QdT[:], qTt[:, blk], SC, EG[:], ALU.mult, ALU.mult, [qTt, EG], [QdT])
                    po = pso_r.get()
                    MM(po[:, 0:128], QdT[:], Sc[:], True, False, [QdT, Sc], [po])
                    MM(po[:, 0:128], qki[:], Vn[:], False, True, [qki, Vn], [po])
                    TT("dve", cg[:, 2:3], GB[:, 127:128], gc, ALU.subtract, [GB, gbt], [cg])
                    ACT(cg[:, 3:4], cg[:, 2:3], AF.Exp, [cg], [cg])
                    Kd = w_r.get()
                    TS("dve", Kd[:], Kc[:], cg[:, 3:4], ALU.mult, [Kc, cg], [Kd])
                    psn = ps_r.get()
                    MM(psn[:, 0:128], Kd[:], Vn[:], True, True, [Kd, Vn], [psn])
                    STT(Sn[:], Sc[:], EG[:, 127:128], psn[:, 0:128], ALU.mult, ALU.add, [Sc, EG, psn], [Sn])
                    cur = 1 - cur
                    jk = w_r.get()
                    ACT(jk[:], po[:, 0:128], AF.Square, [po], [jk, cg], accum=cg[:, 4:5])
                    TS("dve", cg[:, 5:6], cg[:, 4:5], 1.0 / 128, ALU.mult, [cg], [cg], s2=RMS_EPS, op1=ALU.add)
                    ACT(cg[:, 6:7], cg[:, 5:6], AF.Ln, [cg], [cg])
                    ACT(cg[:, 7:8], cg[:, 6:7], AF.Exp, [cg], [cg], scale=-0.5)
                    STT(jk[:], po[:, 0:128], cg[:, 7:8], GNW[:], ALU.mult, ALU.mult, [po, cg, GNW], [jk])
                    bo = bo_r.get()
                    TT("dve", bo[:], jk[:], zt[:, b4, :], ALU.mult, [jk, zt], [bo])
                    S.dma("sp", OTOK[gbk * 128:(gbk + 1) * 128, 128:256], bo[:], OTOK, bo)


    phase_x0()
    xres = x_in
    if S.w_pending and not (cfg.mixer and cfg.LAYERS[0] % 2 == 0):
        S.w_pending = False
        with S.phase():
            w_collectives()
    for li, L in enumerate(cfg.LAYERS):
        is_last = li == len(cfg.LAYERS) - 1
        if cfg.mixer:
            if L % 2 == 0:
                even_mixer(L)
            else:
                odd_mixer(L)
        xnext = XR[li % 2]
        phase_ln_moe(L, xres, xnext, is_last)
        xres = xnext


def _even_mixer(S, cfg, L):
    raise NotImplementedError


def _odd_mixer(S, cfg, L):
    raise NotImplementedError


def t5_onehot():
    n = np.arange(2560) - 511
    nn = np.maximum(n, 0)
    nf = np.maximum(nn, 1).astype(np.float32)
    large = 16 + (np.log(nf / 16) / math.log(2048 / 16) * 16).astype(np.int32)
    large = np.minimum(large, 31)
    bucket = np.where(nn < 16, nn, large)
    oh = np.zeros((33, 2560), np.float32)
    valid = n >= 0
    oh[bucket[valid], np.nonzero(valid)[0]] = 1.0
    oh[32, ~valid] = NEG
    return oh


def prep_inputs(inp, cfg):
    SEQ, TOK, NEXP, EPC, NLW, WL0 = cfg.SEQ, cfg.TOK, cfg.NEXP, cfg.EPC, cfg.NLW, cfg.WL0
    f = lambda a: np.ascontiguousarray(np.asarray(a, dtype=np.float32))
    x = f(inp["x"])
    maps = []
    lng = f(np.concatenate([inp["ln_mix_g"], inp["ln_ffn_g"]], 0))
    lnb = f(np.concatenate([inp["ln_mix_b"], inp["ln_ffn_b"]], 0))
    if cfg.moe:
        bg = f(inp["moe_b_gate_up"]).reshape(4, NEXP, 16, 128)
        bgu = np.ascontiguousarray(bg.transpose(3, 0, 1, 2).reshape(128, 4 * NEXP * 16))
        bdn = f(inp["moe_b_down"]).reshape(4 * NEXP, D)
        wgu = np.asarray(inp["moe_w_gate_up"])
        wdn = np.asarray(inp["moe_w_down"])
    if cfg.mixer:
        ewi, ewo = f(inp["even_w_in"]), f(inp["even_w_out"])
        owi, owo = f(inp["odd_w_in"]), f(inp["odd_w_out"])
        cw = f(inp["gdn_conv_w"])
        oh = t5_onehot()
    for c in range(8):
        b, r = c // 4, c % 4
        m = {"x_in": np.ascontiguousarray(x[b, r * TOK:(r + 1) * TOK, :]), "lng": lng, "lnb": lnb}
        if cfg.moe:
            m["router_w"] = f(inp["router_w"])
            m["router_b"] = f(inp["router_b"])
            m["bgu"] = bgu
            m["bdn"] = bdn
            m["wgu_sh"] = np.ascontiguousarray(wgu[WL0:WL0 + NLW, c * EPC:(c + 1) * EPC], dtype=np.float32).reshape(NLW * EPC * D, 2 * D)
            m["wdn_sh"] = np.ascontiguousarray(wdn[WL0:WL0 + NLW, c * EPC:(c + 1) * EPC], dtype=np.float32).reshape(NLW * EPC * D, D)
        if cfg.mixer:
            h = r
            A = 512
            cols = np.concatenate([np.arange(h * 128, h * 128 + 128), A + np.arange(h * 128, h * 128 + 128),
                                   2 * A + np.arange(h * 128, h * 128 + 128),
                                   1536 + np.arange(h * 128, h * 128 + 128), 2048 + np.arange(h * 128, h * 128 + 128),
                                   2560 + np.arange(h * 128, h * 128 + 128), 3072 + np.arange(h * 128, h * 128 + 128),
                                   [3584 + h], [3588 + h]])
            m["we_in"] = np.ascontiguousarray(ewi[:, :, cols])
            perm = np.concatenate([np.concatenate([np.arange(s_ * 128, s_ * 128 + 128), 512 + np.arange(s_ * 128, s_ * 128 + 128)]) for s_ in range(4)])
            m["we_out"] = np.ascontiguousarray(ewo[:, perm, :])
            TC = min(2048, SEQ)
            gtok = r * TOK + np.arange(TOK)
            jj, tt_ = gtok // TC, gtok % TC
            oi = np.stack([(jj * 4 + s_) * TC + tt_ for s_ in range(4)], 1)
            m["oidx"] = np.ascontiguousarray(oi.reshape(TOK // 128, 128, 4).transpose(1, 0, 2).reshape(128, TOK // 32).astype(np.int32))
            ccols = np.concatenate([np.arange(h * 128, h * 128 + 128), 512 + np.arange(h * 128, h * 128 + 128), 1024 + np.arange(h * 128, h * 128 + 128)])
            m["convw"] = np.ascontiguousarray(cw[:, :, ccols].reshape(2, 4, 3, 128).transpose(0, 3, 2, 1).reshape(2, 128, 12))
            m["lamp"] = f(inp["diff_lambda"]).reshape(2, 256)
            m["subw"] = f(inp["diff_subln_w"])
            m["gnw"] = f(inp["gdn_norm_w"])
            m["alog"] = np.ascontiguousarray(f(inp["gdn_a_log"])[:, h:h + 1])
            m["dtb"] = np.ascontiguousarray(f(inp["gdn_dt_bias"])[:, h:h + 1])
            m["t5col"] = np.ascontiguousarray(f(inp["t5_bias"])[:, h:h + 1])
            m["t5oh"] = oh
            oc = []
            for hh in range(4 * r, 4 * r + 4):
                oc += [np.arange(hh * 64, hh * 64 + 64), 1024 + np.arange(hh * 64, hh * 64 + 64),
                       2048 + np.arange(hh * 64, hh * 64 + 64), [4096 + hh], 3072 + np.arange(hh * 64, hh * 64 + 64)]
            oc = np.concatenate(oc)
            m["wo_in"] = np.ascontiguousarray(owi[:, :, oc])
            m["wo_out"] = owo
            m["qkw"] = np.ascontiguousarray(f(inp["fox_qk_norm_w"]).transpose(0, 2, 1))
            m["fb"] = np.ascontiguousarray(f(inp["fox_forget_b"])[:, 4 * r:4 * r + 4])
        maps.append(m)
    return maps


_CACHE = {}


def run_cfg(inp, cfg):
    key = (cfg.SEQ, cfg.NEXP, cfg.CAP, cfg.LAYERS, cfg.NLW, cfg.WL0, cfg.mixer, cfg.moe)
    if key not in _CACHE:
        _CACHE[key] = build(cfg)
    nc = _CACHE[key]
    maps = prep_inputs(inp, cfg)
    has_even = cfg.mixer and any(L % 2 == 0 for L in cfg.LAYERS)
    has_odd = cfg.mixer and any(L % 2 == 1 for L in cfg.LAYERS)
    ev = ("we_in", "we_out", "convw", "lamp", "subw", "gnw", "alog", "dtb", "t5col", "t5oh")
    od = ("wo_in", "wo_out", "qkw", "fb")
    for m in maps:
        for k in list(m):
            if (k in ev and not has_even) or (k in od and not has_odd):
                del m[k]
    res = run_bass_kernel_spmd(nc, maps, core_ids=list(range(8)))
    outs = [res.results[c]["out"] for c in range(8)]
    TOK = cfg.TOK
    full = np.zeros((2, cfg.SEQ, D), np.float32)
    for c in range(8):
        full[c // 4, (c % 4) * TOK:(c % 4 + 1) * TOK, :] = outs[c]
    return full


def kernel(**inputs):
    return run_cfg(inputs, Cfg())
```

```python
import math
from contextlib import ExitStack
import numpy as np
import concourse.bass as bass
import concourse.mybir as mybir
from concourse.bass_utils import run_bass_kernel_spmd

F32 = mybir.dt.float32
BF16 = mybir.dt.bfloat16
I32 = mybir.dt.int32
U32 = mybir.dt.uint32
AF = mybir.ActivationFunctionType
ALU = mybir.AluOpType
AX = mybir.AxisListType

COMPUTE = ("pe", "act", "dve", "pool")


class Buf:
    def __init__(self, name, handle, is_dram=False):
        self.name = name
        self.h = handle
        self.is_dram = is_dram
        self.w = {}
        self.r = {}
        self.dsem = None
        self.dcnt = 0

    def __getitem__(self, idx):
        return self.h.ap()[idx] if self.is_dram else self.h[idx]

    def ap(self):
        return self.h.ap() if self.is_dram else self.h[:]


class Sched:
    def __init__(self, nc, es):
        self.nc = nc
        self.es = es
        self.streams = {k: [] for k in ("pe", "act", "dve", "pool", "sp")}
        self.sems = {}
        self.latest = {}
        self.cnt = {k: 0 for k in COMPUTE}
        self.waited = {k: {} for k in self.streams}
        self.nbuf = 0
        self.es_local = es
        self.dq = {}
        self.dq_i = {}
        self.all_bufs = []
        import os
        self.sw_thresh = int(os.environ.get("KSW", "30000"))
        for k in COMPUTE:
            self._sem("E_" + k)

    def _sem(self, key):
        if key not in self.sems:
            self.sems[key] = self.es.enter_context(self.nc.semaphore("s%d_%s" % (len(self.sems), key[:12])))
            self.latest[key] = 0
        return self.sems[key]

    def sbuf(self, name, shape, dt):
        self.nbuf += 1
        h = self.es_local.enter_context(self.nc.sbuf_tensor("%s_%d" % (name, self.nbuf), list(shape), dt))
        b = Buf(name, h)
        b.local = self.es_local is not self.es
        self.all_bufs.append(b)
        return b

    def psum(self, name, shape, dt=F32):
        self.nbuf += 1
        h = self.es_local.enter_context(self.nc.psum_tensor("%s_%d" % (name, self.nbuf), list(shape), dt))
        b = Buf(name, h)
        b.local = self.es_local is not self.es
        self.all_bufs.append(b)
        return b

    def ring(self, name, shape, dt, n, psum=False):
        return Ring([(self.psum if psum else self.sbuf)("%s%d" % (name, i), shape, dt) for i in range(n)])

    def _dq(self, kind):
        if kind not in self.dq:
            n = {"hw": 12, "sw": 8, "cc": 1}[kind]
            self.dq[kind] = ["D_%s_%d" % (kind, i) for i in range(n)]
            for k in self.dq[kind]:
                self._sem(k)
            self.dq_i[kind] = 0
        key = self.dq[kind][self.dq_i[kind] % len(self.dq[kind])]
        self.dq_i[kind] += 1
        return key

    def phase(self):
        return _Phase(self)

    def dram(self, name, shape, dt, kind=None):
        if kind is None:
            h = self.nc.dram_tensor(name, list(shape), dt)
        else:
            h = self.nc.dram_tensor(name, list(shape), dt, kind=kind)
        b = Buf(name, h, is_dram=True)
        self.all_bufs.append(b)
        return b

    def _need(self, eng, reads, writes, mykey, prev=None):
        need = {}

        def add(k, v):
            if k == mykey == "E_pe":
                return
            if need.get(k, 0) < v:
                need[k] = v
        if prev is not None and prev[1] > 0:
            add(*prev)
        for b in reads:
            for k, v in b.w.items():
                add(k, v)
        for b in writes:
            for k, v in b.w.items():
                add(k, v)
            for k, v in b.r.items():
                add(k, v)
        out = []
        wd = self.waited[eng]
        for k, v in need.items():
            if wd.get(k, 0) < v:
                wd[k] = v
                out.append((self.sems[k], v))
        return out

    def op(self, eng, fn, reads=(), writes=()):
        key = "E_" + eng
        waits = self._need(eng, reads, writes, key)
        self.cnt[eng] += 1
        val = self.cnt[eng]
        self.latest[key] = val
        sem = self.sems[key]
        st = self.streams[eng]
        for s, v in waits:
            st.append(lambda e, s=s, v=v: e.wait_ge(s, v))
        st.append(lambda e, fn=fn, sem=sem: fn(e).then_inc(sem, 1))
        for b in reads:
            if b.r.get(key, 0) < val:
                b.r[key] = val
        for b in writes:
            b.w = {key: val}
            b.r = {}

    def dma(self, q, out_ap, in_ap, dst, src, fn=None, extra=(), **kw):
        key = self._dq("sw" if q == "pool" else "hw")
        wr = [dst] if (dst.r or not all(k.startswith("D_") for k in dst.w)) else []
        waits = self._need(q, ([src] if src is not None else []) + list(extra), wr, key, prev=(key, self.latest[key]))
        val = self.latest[key] + 16
        self.latest[key] = val
        sem = self.sems[key]
        st = self.streams[q]
        for s, v in waits:
            st.append(lambda e, s=s, v=v: e.wait_ge(s, v))
        if fn is None:
            st.append(lambda e: e.dma_start(out=out_ap, in_=in_ap, **kw).then_inc(sem, 16))
        else:
            st.append(lambda e: fn(e).then_inc(sem, 16))
        for b in ([src] if src is not None else []) + list(extra):
            if b.r.get(key, 0) < val:
                b.r[key] = val
        if wr:
            dst.w = {key: val}
        else:
            dst.w[key] = val
        dst.r = {}

    def cc(self, kind, op, groups, src, dst, src_ap=None, dst_ap=None):
        sap = src.h.ap().opt() if src_ap is None else src_ap.opt()
        dap = dst.h.ap().opt() if dst_ap is None else dst_ap.opt()
        key = self._dq("cc")
        waits = self._need("pool", [src], [dst], key)
        val = self.latest[key] + 1
        self.latest[key] = val
        sem = self.sems[key]
        st = self.streams["pool"]
        for s, v in waits:
            st.append(lambda e, s=s, v=v: e.wait_ge(s, v))
        st.append(lambda e: e.collective_compute(kind, op, replica_groups=groups,
                                                 ins=[sap], outs=[dap]).then_inc(sem, 1))
        st.append(lambda e: e.wait_ge(sem, val))
        self.waited["pool"][key] = val
        src.r[key] = val
        dst.w = {key: val}
        dst.r = {}

    def barrier(self):
        for eng, st in self.streams.items():
            wd = self.waited[eng]
            for k, v in self.latest.items():
                if v > 0 and wd.get(k, 0) < v:
                    wd[k] = v
                    st.append(lambda e, s=self.sems[k], v=v: e.wait_ge(s, v))

    def emit(self):
        self.barrier()
        for eng in COMPUTE:
            key = "E_" + eng
            if self.cnt[eng] > self.sw_thresh:
                self.nsw = getattr(self, "nsw", 0) + 1
                self.sems[key] = self.es.enter_context(self.nc.semaphore("sw%d_%s" % (self.nsw, eng)))
                self.cnt[eng] = 0
                self.latest[key] = 0
                for wd in self.waited.values():
                    wd.pop(key, None)
                for b in self.all_bufs:
                    b.w.pop(key, None)
                    b.r.pop(key, None)
        streams = self.streams
        self.streams = {k: [] for k in streams}
        self.ninst = getattr(self, "ninst", 0) + sum(len(v) for v in streams.values())
        import os
        if os.environ.get("KCOUNT"):
            return
        self._emit(streams)

    def _emit(self, streams):
        class _S:
            pass
        self_ = _S()
        self_.streams = streams
        nc = self.nc
        self = self_
        with nc.Block() as block:
            @block.tensor
            def _(e):
                for f in self.streams["pe"]:
                    f(e)

            @block.scalar
            def _(e):
                for f in self.streams["act"]:
                    f(e)

            @block.vector
            def _(e):
                for f in self.streams["dve"]:
                    f(e)

            @block.gpsimd
            def _(e):
                for f in self.streams["pool"]:
                    f(e)

            @block.sync
            def _(e):
                for f in self.streams["sp"]:
                    f(e)


class Ring:
    def __init__(self, bufs):
        self.bufs = bufs
        self.i = 0

    def get(self):
        b = self.bufs[self.i % len(self.bufs)]
        self.i += 1
        return b


class _Phase:
    def __init__(self, S):
        self.S = S

    def __enter__(self):
        self.old = self.S.es_local
        self.stack = ExitStack()
        self.stack.__enter__()
        self.S.es_local = self.stack
        return self.S

    def __exit__(self, *a):
        S = self.S
        stop = False
        if a[0] is None:
            S.emit()

            S.nphase = getattr(S, "nphase", 0) + 1
            import os
            stop = S.nphase == int(os.environ.get("KSTOP", "0"))
        S.es_local = self.old
        self.stack.__exit__(*a)
        if stop:
            raise _StopBuild()
        return False


class _StopBuild(Exception):
    pass


D = 1024
ALPHA = (2 * 4) ** 0.25
LN_EPS = 1e-5
RMS_EPS = 1e-6
G4 = [[0, 1, 2, 3], [4, 5, 6, 7]]
G2 = [[0, 4], [1, 5], [2, 6], [3, 7]]
NEG = -30000.0


class Cfg:
    def __init__(self, SEQ=16384, NEXP=32, CAP=640, LAYERS=(0, 1, 2, 3), NLW=4, WL0=0, mixer=True, moe=True):
        self.SEQ, self.NEXP, self.CAP, self.LAYERS, self.NLW = SEQ, NEXP, CAP, tuple(LAYERS), NLW
        self.mixer, self.moe, self.WL0 = mixer, moe, WL0
        self.TOK = SEQ // 4
        self.EPC = NEXP // 8


def build(cfg):
    nc = bass.Bass("TRN2", target_bir_lowering=False)
    es = ExitStack()
    with es:
        S = Sched(nc, es)
        try:
            _build(S, cfg)
        except _StopBuild:
            pass
        print("kernel build: instructions incl. waits =", getattr(S, "ninst", 0), flush=True)
    return nc


def _build(S, cfg):
    SEQ, TOK, NEXP, C, EPC, NLW = cfg.SEQ, cfg.TOK, cfg.NEXP, cfg.CAP, cfg.EPC, cfg.NLW
    NBL = TOK // 128
    NTL = TOK // 512
    NBF = SEQ // 128
    NQT = SEQ // 512
    CB = C // 128
    TRASH = NEXP * C

    def MM(out, lhsT, rhs, start, stop, R, W):
        S.op("pe", lambda e: e.matmul(out, lhsT, rhs, start=start, stop=stop), reads=R, writes=W)

    def TR(out, in_, ident, R, W):
        S.op("pe", lambda e: e.transpose(out, in_, ident), reads=R, writes=W)

    def ACT(out, in_, func, R, W, bias=None, scale=None, accum=None):
        kw = {}
        if bias is not None:
            kw["bias"] = bias
        if scale is not None:
            kw["scale"] = scale
        if accum is not None:
            kw["accum_out"] = accum
        S.op("act", lambda e: e.activation(out=out, in_=in_, func=func, **kw), reads=R, writes=W)

    def TS(eng, out, in0, s1, op0, R, W, s2=None, op1=None, accum=None):
        kw = {}
        if op1 is not None:
            kw["op1"] = op1
        if accum is not None:
            kw["accum_out"] = accum
        S.op(eng, lambda e: e.tensor_scalar(out=out, in0=in0, scalar1=s1, scalar2=s2, op0=op0, **kw), reads=R, writes=W)

    def TT(eng, out, in0, in1, op, R, W):
        S.op(eng, lambda e: e.tensor_tensor(out=out, in0=in0, in1=in1, op=op), reads=R, writes=W)

    def STT(out, in0, scalar, in1, op0, op1, R, W, accum=None):
        kw = {}
        if accum is not None:
            kw["accum_out"] = accum
        S.op("dve", lambda e: e.scalar_tensor_tensor(out=out, in0=in0, scalar=scalar, in1=in1, op0=op0, op1=op1, **kw),
             reads=R, writes=W)

    def CP(eng, out, in_, R, W):
        if eng == "act":
            S.op("act", lambda e: e.copy(out=out, in_=in_), reads=R, writes=W)
        else:
            S.op(eng, lambda e: e.tensor_copy(out=out, in_=in_), reads=R, writes=W)

    def MEMSET(eng, ap, val, W):
        S.op(eng, lambda e: e.memset(ap, val), writes=W)

    def RECIP(out, in_, R, W):
        S.op("dve", lambda e: e.reciprocal(out=out, in_=in_), reads=R, writes=W)

    def ASEL(out, in_, pattern, cmp, fill, base, cm, R, W):
        S.op("pool", lambda e: e.affine_select(out=out, in_=in_, pattern=pattern, compare_op=cmp, fill=fill,
                                                base=base, channel_multiplier=cm), reads=R, writes=W)

    def din(name, shape, dt=F32):
        return S.dram(name, shape, dt, kind="ExternalInput")

    x_in = din("x_in", [TOK, D])
    lng = din("lng", [8, D])
    lnb = din("lnb", [8, D])
    out = S.dram("out", [TOK, D], F32, kind="ExternalOutput")
    if cfg.moe:
        router_w = din("router_w", [4, D, NEXP])
        router_b = din("router_b", [4, NEXP])
        bgu = din("bgu", [128, 4 * NEXP * 16])
        bdn = din("bdn", [4 * NEXP, D])
        wgu_sh = din("wgu_sh", [NLW * EPC * D, 2 * D])
        wdn_sh = din("wdn_sh", [NLW * EPC * D, D])
    has_even = cfg.mixer and any(L % 2 == 0 for L in cfg.LAYERS)
    has_odd = cfg.mixer and any(L % 2 == 1 for L in cfg.LAYERS)
    if has_even:
        we_in = din("we_in", [2, D, 898])
        we_out = din("we_out", [2, D, D])
        convw = din("convw", [2, 128, 12])
        lamp = din("lamp", [2, 256])
        subw = din("subw", [2, 128])
        gnw = din("gnw", [2, 128])
        alog = din("alog", [2, 1])
        dtb = din("dtb", [2, 1])
        t5col = din("t5col", [32, 1])
        t5oh = din("t5oh", [33, 2560])
    if has_odd:
        wo_in = din("wo_in", [2, D, 1028])
        wo_out = din("wo_out", [2, D, D])
        qkw = din("qkw", [2, 64, 2])
        fb = din("fb", [2, 4])

    XR = [S.dram("xres0", [TOK, D], F32), S.dram("xres1", [TOK, D], F32)]
    XR1 = S.dram("xr1", [TOK, D], F32)
    XTB = S.dram("xtb", [D, TOK], BF16)
    XTALL = S.dram("xtall", [4 * D, TOK], BF16)
    TC = min(2048, SEQ)
    if cfg.mixer:
        OTOK = S.dram("otok", [SEQ, 256], BF16)
        OALL = S.dram("oall", [4 * SEQ, 256], BF16)
        oidx = din("oidx", [128, NBL * 4], I32)
        CQ = S.dram("cq", [3, SEQ], BF16)
        O0S = S.dram("o0s", [SEQ, 128], F32)
        BV = S.dram("bv", [2560], F32)
        SKW = S.dram("skw", [16, 128 * 640], F32)
    if cfg.moe:
        XDISP = S.dram("xdisp", [NEXP * C + 128, D], BF16)
        YDISP = S.dram("ydisp", [NEXP * C + 128, D], F32)
        R1 = NLW * EPC * D
        WB1G = S.dram("wb1g", [R1, 2 * D], BF16)
        WB1D = S.dram("wb1d", [R1, D], BF16)
        RGG, RGD = 256, 512
        NCG, NCD = R1 // RGG, R1 // RGD
        WALLG = [S.dram("wallg%d" % i, [16 * 8 * RGG, 2 * D], BF16) for i in range((NCG + 15) // 16)]
        WALLD = [S.dram("walld%d" % i, [16 * 8 * RGD, D], BF16) for i in range((NCD + 15) // 16)]
        S1G = [S.dram("s1g%d" % i, [2 * RGG, 2 * D], BF16) for i in range(2)]
        S1D = [S.dram("s1d%d" % i, [2 * RGD, D], BF16) for i in range(2)]

    ident_f = S.sbuf("ident_f", [128, 128], F32)
    ident_b = S.sbuf("ident_b", [128, 128], BF16)
    ones_f = S.sbuf("ones_f", [128, 128], F32)
    ones_b = S.sbuf("ones_b", [128, 128], BF16)
    triU_f = S.sbuf("triU_f", [128, 128], F32)
    Ls_b = S.sbuf("Ls_b", [128, 128], BF16)
    DESTI = S.sbuf("DESTI", [128, NBL * 4], I32)
    GATE = S.sbuf("GATE", [128, NBL * 4], F32)
    CNT = S.sbuf("CNT", [128, NEXP], F32)
    EOFF = S.sbuf("EOFF", [128, NEXP], F32)
    with S.phase():
        tmpf = S.sbuf("tmpf", [128, 128], F32)
        tmpi = S.sbuf("tmpi", [128, NEXP], I32)
        MEMSET("pool", ident_f[:], 0.0, [ident_f])
        ASEL(ident_f[:], ident_f[:], [[-1, 128]], ALU.not_equal, 1.0, 0, 1, [ident_f], [ident_f])
        CP("dve", ident_b[:], ident_f[:], [ident_f], [ident_b])
        MEMSET("pool", ones_f[:], 1.0, [ones_f])
        MEMSET("pool", ones_b[:], 1.0, [ones_b])
        MEMSET("pool", triU_f[:], 1.0, [triU_f])
        ASEL(triU_f[:], triU_f[:], [[1, 128]], ALU.is_ge, 0.0, 0, -1, [triU_f], [triU_f])
        MEMSET("pool", tmpf[:], 1.0, [tmpf])
        ASEL(tmpf[:], tmpf[:], [[1, 128]], ALU.is_ge, 0.0, -1, -1, [tmpf], [tmpf])
        CP("dve", Ls_b[:], tmpf[:], [tmpf], [Ls_b])
        S.op("pool", lambda e: e.iota(tmpi[:], pattern=[[C, NEXP]], base=0, channel_multiplier=0), writes=[tmpi])
        CP("dve", EOFF[:], tmpi[:], [tmpi], [EOFF])

    if cfg.moe:
        with S.phase():
            st_r = S.ring("wst", [128, 2 * D], F32, 3)
            sb_r = S.ring("wsb", [128, 2 * D], BF16, 3)
            n = 0
            for (src, b1, ncol) in ((wgu_sh, WB1G, 2 * D), (wdn_sh, WB1D, D)):
                rows = 128 * (2 * D // ncol)
                for r0 in range(0, R1, rows):
                    st = st_r.get()
                    sb = sb_r.get()
                    k = rows // 128
                    S.dma("sp", st[:].rearrange("p (k n) -> p k n", k=k), src[r0:r0 + rows, :].rearrange("(k p) n -> p k n", p=128), st, src)
                    CP("act" if n % 2 == 0 else "dve", sb[:], st[:], [st], [sb])
                    S.dma("sp", b1[r0:r0 + rows, :].rearrange("(k p) n -> p k n", p=128), sb[:].rearrange("p (k n) -> p k n", k=k), b1, sb)
                    n += 1
            zt = S.sbuf("zt", [128, D], F32)
            ztb = S.sbuf("ztb", [128, D], BF16)
            MEMSET("dve", zt[:], 0.0, [zt])
            MEMSET("dve", ztb[:], 0.0, [ztb])
            for r0 in range(0, NEXP * C + 128, 128):
                S.dma("sp", XDISP[r0:r0 + 128, :], ztb[:], XDISP, ztb)
                S.dma("sp", YDISP[r0:r0 + 128, :], zt[:], YDISP, zt)

    def w_collectives(which=(0, 1)):
        for wi_, (b1, s1s, walls, RG, NC_) in enumerate(((WB1G, S1G, WALLG, RGG, NCG), (WB1D, S1D, WALLD, RGD, NCD))):
            if wi_ not in which:
                continue
            for i in range(NC_):
                s1 = s1s[i % 2]
                S.cc("AllGather", ALU.bypass, G2, b1, s1, src_ap=b1[i * RG:(i + 1) * RG, :])
                wt = walls[i // 16]
                for g in range(2):
                    base = (((i % 16) * 2 + g) * 4) * RG
                    S.cc("AllGather", ALU.bypass, G4, s1, wt, src_ap=s1[g * RG:(g + 1) * RG, :],
                         dst_ap=wt[base:base + 4 * RG, :])

    S.w_pending = cfg.moe

    def wloc(L, e, RG, chunk):
        c, le = e // EPC, e % EPC
        g, r4 = c // 4, c % 4
        rr = ((L - cfg.WL0) * EPC + le) * D + chunk * RG
        i = rr // RG
        return i // 16, ((((i % 16) * 2 + g) * 4 + r4) * RG)

    XTBv = XTB.ap().rearrange("(k p) t -> p k t", p=128)

    def emit_xT_block(xt_f32, xT_tile, col, ps_ring, cp_eng):
        ps = ps_ring.get()
        for kc in range(8):
            TR(ps[:, kc * 128:(kc + 1) * 128], xt_f32[:, kc * 128:(kc + 1) * 128], ident_f[:], [xt_f32, ident_f], [ps])
        CP(cp_eng, xT_tile[:, :, col:col + 128], ps[:].rearrange("p (k t) -> p k t", k=8), [ps], [xT_tile])

    def xt_allgather():
        for kc in range(8):
            S.cc("AllGather", ALU.bypass, G4, XTB, XTALL, src_ap=XTB[kc * 128:(kc + 1) * 128, :],
                 dst_ap=XTALL[kc * 512:(kc + 1) * 512, :])

    def phase_x0():
        with S.phase():
            xin_r = S.ring("xin", [128, D], F32, 3)
            xT_r = S.ring("xT", [128, 8, 512], BF16, 2)
            ps_r = S.ring("psx", [128, D], F32, 2, psum=True)
            for tl in range(NTL):
                xT = xT_r.get()
                for b4 in range(4):
                    b = tl * 4 + b4
                    xt = xin_r.get()
                    S.dma("sp", xt[:], x_in[b * 128:(b + 1) * 128, :], xt, x_in)
                    emit_xT_block(xt, xT, b4 * 128, ps_r, "act" if b4 % 2 == 0 else "dve")
                S.dma("sp", XTBv[:, :, tl * 512:(tl + 1) * 512], xT[:], XTB, xT)
            xt_allgather()

    def layer_norm(z, outt, gt, bt, st_r, junk):
        st = st_r.get()
        ACT(junk[:], z[:], AF.Identity, [z], [junk, st], accum=st[:, 0:1])
        ACT(junk[:], z[:], AF.Square, [z], [junk, st], accum=st[:, 1:2])
        TS("dve", st[:, 2:3], st[:, 0:1], 1.0 / D, ALU.mult, [st], [st])
        TT("dve", st[:, 3:4], st[:, 2:3], st[:, 2:3], ALU.mult, [st], [st])
        STT(st[:, 4:5], st[:, 1:2], 1.0 / D, st[:, 3:4], ALU.mult, ALU.subtract, [st], [st])
        TS("dve", st[:, 4:5], st[:, 4:5], LN_EPS, ALU.add, [st], [st])
        ACT(st[:, 5:6], st[:, 4:5], AF.Sqrt, [st], [st])
        RECIP(st[:, 6:7], st[:, 5:6], [st], [st])
        TS("dve", outt[:], z[:], st[:, 2:3], ALU.subtract, [z, st], [outt], s2=st[:, 6:7], op1=ALU.mult)
        TT("dve", outt[:], outt[:], gt[:], ALU.mult, [outt, gt], [outt])
        TT("dve", outt[:], outt[:], bt[:], ALU.add, [outt, bt], [outt])

    def phase_ln_moe(L, xres, xnext, is_last):
        with S.phase():
            gt = S.sbuf("gt", [128, D], F32)
            bt = S.sbuf("bt", [128, D], F32)
            S.dma("sp", gt[:], lng.ap()[L, :].partition_broadcast(128), gt, lng)
            S.dma("sp", bt[:], lnb.ap()[L, :].partition_broadcast(128), bt, lnb)
            xr_r = S.ring("xr", [128, D], F32, 2)
            if cfg.mixer:
                wsrc = we_out if L % 2 == 0 else wo_out
                WO = S.sbuf("WO", [128, 8, D], BF16)
                for q4 in range(4):
                    S.dma("pool", WO[:, 2 * q4:2 * q4 + 2, :],
                          wsrc.ap()[L // 2, q4 * 256:(q4 + 1) * 256, :].rearrange("(k p) n -> p k n", p=128), WO, wsrc)
                OIDX = S.sbuf("OIDX", [128, NBL * 4], I32)
                S.dma("sp", OIDX[:], oidx[:, :], OIDX, oidx)
                og_r = S.ring("og", [128, 4, 256], BF16, 2)
                oT_r = S.ring("oT", [128, 8, 128], BF16, 2)
                pso_r = S.ring("pso", [128, D], BF16, 1, psum=True)
                psh_r = S.ring("psh", [128, 512], F32, 2, psum=True)
            z_r = S.ring("z", [128, D], F32, 2)
            x1_r = S.ring("x1", [128, D], F32, 2)
            x1b_r = S.ring("x1b", [128, D], BF16, 2)
            junk = S.sbuf("junk", [128, D], F32)
            st_r = S.ring("st", [128, 8], F32, 2)
            if cfg.moe:
                RW = S.sbuf("RW", [128, 8, NEXP], F32)
                RB = S.sbuf("RB", [128, NEXP], F32)
                S.dma("sp", RW[:], router_w.ap()[L].rearrange("(k p) e -> p k e", p=128), RW, router_w)
                S.dma("sp", RB[:], router_b.ap()[L, :].partition_broadcast(128), RB, router_b)
                x1T_r = S.ring("x1T", [128, 8, 128], F32, 2)
                psx_r = S.ring("psx", [128, D], F32, 1, psum=True)
                psl_r = S.ring("psl", [128, 512], F32, 1, psum=True)
                psp_r = S.ring("psp", [128, 512], F32, 2, psum=True)
                sm_r = S.ring("sm", [128, 12, NEXP], F32, 2)
                mb_r = S.ring("mb", [128, NEXP], BF16, 2)
                t8_r = S.ring("t8", [128, 8], F32, 2)
                sc_r = S.ring("sc", [128, 16], F32, 2)
                MEMSET("dve", CNT[:], 0.0, [CNT])
            for b in range(NBL):
                xr = xr_r.get()
                S.dma("sp", xr[:], xres[b * 128:(b + 1) * 128, :], xr, xres)
                z = z_r.get()
                if cfg.mixer:
                    og = og_r.get()
                    for s4 in range(4):
                        col = b * 4 + s4
                        S.dma("pool", None, None, og, OALL, extra=[OIDX],
                              fn=lambda e, col=col, og=og, s4=s4: e.indirect_dma_start(
                                  out=og[:, s4, :], out_offset=None, in_=OALL.ap()[:, :],
                                  in_offset=bass.IndirectOffsetOnAxis(ap=OIDX[:, col:col + 1], axis=0)))
                    pso = pso_r.get()
                    for kc in range(8):
                        TR(pso[:, kc * 128:(kc + 1) * 128], og[:, kc // 2, (kc % 2) * 128:(kc % 2 + 1) * 128], ident_b[:], [og, ident_b], [pso])
                    oT = oT_r.get()
                    CP("act", oT[:], pso[:].rearrange("p (k t) -> p k t", k=8), [pso], [oT])
                    for half in range(2):
                        psh = psh_r.get()
                        for kc in range(8):
                            MM(psh[:], oT[:, kc, :], WO[:, kc, half * 512:(half + 1) * 512], kc == 0, kc == 7, [oT, WO], [psh])
                        STT(z[:, half * 512:(half + 1) * 512], xr[:, half * 512:(half + 1) * 512], ALPHA, psh[:], ALU.mult, ALU.add, [xr, psh], [z])
                else:
                    TS("dve", z[:], xr[:], ALPHA, ALU.mult, [xr], [z])
                x1 = x1_r.get()
                layer_norm(z, x1, gt, bt, st_r, junk)
                S.dma("sp", XR1[b * 128:(b + 1) * 128, :], x1[:], XR1, x1)
                if not cfg.moe:
                    continue
                x1b = x1b_r.get()
                CP("act", x1b[:], x1[:], [x1], [x1b])
                x1T = x1T_r.get()
                ps = psx_r.get()
                for kc in range(8):
                    TR(ps[:, kc * 128:(kc + 1) * 128], x1[:, kc * 128:(kc + 1) * 128], ident_f[:], [x1, ident_f], [ps])
                CP("act", x1T[:], ps[:].rearrange("p (k t) -> p k t", k=8), [ps], [x1T])
                psl = psl_r.get()
                for kc in range(8):
                    MM(psl[:, 0:NEXP], x1T[:, kc, :], RW[:, kc, :], kc == 0, kc == 7, [x1T, RW], [psl])
                sm = sm_r.get()
                lg, ex, mk, gd, gates, pos, dfull, oh, jk = (sm[:, i, :] for i in range(9))
                t8 = t8_r.get()
                sc = sc_r.get()
                TT("dve", lg, psl[:, 0:NEXP], RB[:], ALU.add, [psl, RB], [sm])
                S.op("dve", lambda e, t8=t8, lg=lg: e.max(out=t8[:], in_=lg), reads=[sm], writes=[t8])
                TS("dve", sc[:, 0:1], t8[:, 0:1], -1.0, ALU.mult, [t8], [sc])
                ACT(ex, lg, AF.Exp, [sm, sc], [sm], bias=sc[:, 0:1])
                TS("dve", mk, lg, t8[:, 3:4], ALU.is_ge, [sm, t8], [sm])
                TT("dve", gd, ex, mk, ALU.mult, [sm], [sm])
                S.op("dve", lambda e, sc=sc, gd=gd: e.tensor_reduce(out=sc[:, 1:2], in_=gd, axis=AX.X, op=ALU.add),
                     reads=[sm], writes=[sc])
                RECIP(sc[:, 2:3], sc[:, 1:2], [sc], [sc])
                TS("dve", gates, gd, sc[:, 2:3], ALU.mult, [sm, sc], [sm])
                mb = mb_r.get()
                CP("dve", mb[:], mk, [sm], [mb])
                psp = psp_r.get()
                MM(psp[:, 0:NEXP], Ls_b[:], mb[:], True, True, [Ls_b, mb], [psp])
                TT("dve", pos, psp[:, 0:NEXP], CNT[:], ALU.add, [psp, CNT], [sm])
                psc = psp_r.get()
                MM(psc[:, 0:NEXP], ones_b[:], mb[:], True, True, [ones_b, mb], [psc])
                TT("dve", CNT[:], CNT[:], psc[:, 0:NEXP], ALU.add, [CNT, psc], [CNT])
                TT("dve", dfull, pos, EOFF[:], ALU.add, [sm, EOFF], [sm])
                for k in range(4):
                    TS("dve", oh, lg, t8[:, k:k + 1], ALU.is_equal, [sm, t8], [sm])
                    TT("dve", jk, oh, pos, ALU.mult, [sm], [sm])
                    S.op("dve", lambda e, sc=sc, jk=jk: e.tensor_reduce(out=sc[:, 4:5], in_=jk, axis=AX.X, op=ALU.add),
                         reads=[sm], writes=[sc])
                    TT("dve", jk, oh, dfull, ALU.mult, [sm], [sm])
                    S.op("dve", lambda e, sc=sc, jk=jk: e.tensor_reduce(out=sc[:, 5:6], in_=jk, axis=AX.X, op=ALU.add),
                         reads=[sm], writes=[sc])
                    TT("dve", jk, oh, gates, ALU.mult, [sm], [sm])
                    S.op("dve", lambda e, sc=sc, jk=jk: e.tensor_reduce(out=sc[:, 6:7], in_=jk, axis=AX.X, op=ALU.add),
                         reads=[sm], writes=[sc])
                    TS("dve", sc[:, 7:8], sc[:, 4:5], float(C), ALU.is_lt, [sc], [sc])
                    TS("dve", sc[:, 8:9], sc[:, 5:6], float(TRASH), ALU.subtract, [sc], [sc])
                    TT("dve", sc[:, 8:9], sc[:, 8:9], sc[:, 7:8], ALU.mult, [sc], [sc])
                    TS("dve", sc[:, 8:9], sc[:, 8:9], float(TRASH), ALU.add, [sc], [sc])
                    col = b * 4 + k
                    CP("dve", DESTI[:, col:col + 1], sc[:, 8:9], [sc], [DESTI])
                    TT("dve", GATE[:, col:col + 1], sc[:, 6:7], sc[:, 7:8], ALU.mult, [sc], [GATE])
                    S.dma("pool", None, None, XDISP, x1b, extra=[DESTI],
                          fn=lambda e, col=col, x1b=x1b: e.indirect_dma_start(
                              out=XDISP.ap()[:, :], out_offset=bass.IndirectOffsetOnAxis(ap=DESTI[:, col:col + 1], axis=0),
                              in_=x1b[:, :], in_offset=None))
        if cfg.moe:
            with S.phase():
                BG = S.sbuf("BG", [128, NEXP * 16], F32)
                S.dma("sp", BG[:], bgu[:, L * NEXP * 16:(L + 1) * NEXP * 16], BG, bgu)
                wgu_r = S.ring("wgu", [128, 8, 2 * D], BF16, 2)
                wdn_r = S.ring("wdn", [128, 8, D], BF16, 2)
                bd_r = S.ring("bd", [128, D], F32, 2)
                xs_r = S.ring("xs", [128, CB, D], BF16, 2)
                xsT_r = S.ring("xsT", [128, 8, C], BF16, 1)
                aT_r = S.ring("aT", [128, 8, C], BF16, 1)
                tmp_r = S.ring("tmp", [128, 5, 512], F32, 2)
                ys_r = S.ring("ys", [128, D], F32, 2)
                pst_r = S.ring("pst", [128, D], BF16, 2, psum=True)
                psg_r = S.ring("psg", [128, 512], F32, 2, psum=True)
                psu_r = S.ring("psu", [128, 512], F32, 2, psum=True)
                psd_r = S.ring("psd", [128, 512], F32, 2, psum=True)
                chunks = [(c0, min(512, C - c0)) for c0 in range(0, C, 512)]
                def load_expert(e_):
                    wg = wgu_r.get()
                    wd = wdn_r.get()
                    for q4 in range(4):
                        ti, row = wloc(L, e_, RGG, q4)
                        S.dma("sp", wg[:, 2 * q4:2 * q4 + 2, :],
                              WALLG[ti].ap()[row:row + 256, :].rearrange("(k p) n -> p k n", p=128), wg, WALLG[ti])
                    for q4 in range(2):
                        ti, row = wloc(L, e_, RGD, q4)
                        S.dma("sp", wd[:, 4 * q4:4 * q4 + 4, :],
                              WALLD[ti].ap()[row:row + 512, :].rearrange("(k p) n -> p k n", p=128), wd, WALLD[ti])
                    bd = bd_r.get()
                    S.dma("sp", bd[:], bdn.ap()[L * NEXP + e_, :].partition_broadcast(128), bd, bdn)
                    xs = xs_r.get()
                    S.dma("sp", xs[:], XDISP.ap()[e_ * C:(e_ + 1) * C, :].rearrange("(c p) d -> p c d", p=128), xs, XDISP)
                    return wg, wd, bd, xs

                nxt = load_expert(0)
                for e_ in range(NEXP):
                    wg, wd, bd, xs = nxt
                    if e_ + 1 < NEXP:
                        nxt = load_expert(e_ + 1)
                    xsT = xsT_r.get()
                    for cb in range(CB):
                        pst = pst_r.get()
                        for kc in range(8):
                            TR(pst[:, kc * 128:(kc + 1) * 128], xs[:, cb, kc * 128:(kc + 1) * 128], ident_b[:], [xs, ident_b], [pst])
                        CP("act" if cb % 2 == 0 else "dve", xsT[:, :, cb * 128:(cb + 1) * 128],
                           pst[:].rearrange("p (k t) -> p k t", k=8), [pst], [xsT])
                    aT = aT_r.get()
                    for j in range(8):
                        for (c0, w) in chunks:
                            psg = psg_r.get()
                            psu = psu_r.get()
                            for kc in range(8):
                                MM(psg[:, 0:w], wg[:, kc, j * 128:(j + 1) * 128], xsT[:, kc, c0:c0 + w], kc == 0, kc == 7, [wg, xsT], [psg])
                            for kc in range(8):
                                MM(psu[:, 0:w], wg[:, kc, D + j * 128:D + (j + 1) * 128], xsT[:, kc, c0:c0 + w], kc == 0, kc == 7, [wg, xsT], [psu])
                            tmp = tmp_r.get()
                            g1, sg, u1, u2, tt_ = (tmp[:, i, 0:w] for i in range(5))
                            bgc = (e_ * 16 + j)
                            TS("dve", g1, psg[:, 0:w], BG[:, bgc:bgc + 1], ALU.add, [psg, BG], [tmp], s2=7.0, op1=ALU.min)
                            ACT(sg, g1, AF.Sigmoid, [tmp], [tmp], scale=1.702)
                            TS("dve", u1, psu[:, 0:w], BG[:, bgc + 8:bgc + 9], ALU.add, [psu, BG], [tmp], s2=7.0, op1=ALU.min)
                            TS("dve", u2, u1, -7.0, ALU.max, [tmp], [tmp], s2=1.0, op1=ALU.add)
                            TT("pool", tt_, g1, sg, ALU.mult, [tmp], [tmp])
                            TT("pool", aT[:, j, c0:c0 + w], tt_, u2, ALU.mult, [tmp], [aT])
                    for cb in range(CB):
                        ys = ys_r.get()
                        for half in range(2):
                            psd = psd_r.get()
                            for j in range(8):
                                MM(psd[:], aT[:, j, cb * 128:(cb + 1) * 128], wd[:, j, half * 512:(half + 1) * 512], j == 0, j == 7, [aT, wd], [psd])
                            TT("dve", ys[:, half * 512:(half + 1) * 512], psd[:], bd[:, half * 512:(half + 1) * 512], ALU.add, [psd, bd], [ys])
                        S.dma("sp", YDISP[e_ * C + cb * 128:e_ * C + (cb + 1) * 128, :], ys[:], YDISP, ys)
        with S.phase():
            gt = S.sbuf("gt", [128, D], F32)
            bt = S.sbuf("bt", [128, D], F32)
            S.dma("sp", gt[:], lng.ap()[4 + L, :].partition_broadcast(128), gt, lng)
            S.dma("sp", bt[:], lnb.ap()[4 + L, :].partition_broadcast(128), bt, lnb)
            x1_r = S.ring("x1", [128, D], F32, 2)
            acc_r = S.ring("acc", [128, D], F32, 2)
            yk_r = S.ring("yk", [128, D], F32, 4)
            x2_r = S.ring("x2", [128, D], F32, 2)
            junk = S.sbuf("junk", [128, D], F32)
            st_r = S.ring("st", [128, 8], F32, 2)
            xT_r = S.ring("xT", [128, 8, 512], BF16, 2)
            ps_r = S.ring("psx", [128, D], F32, 2, psum=True)
            dst = out if is_last else xnext
            xT = None
            for b in range(NBL):
                x1 = x1_r.get()
                S.dma("sp", x1[:], XR1[b * 128:(b + 1) * 128, :], x1, XR1)
                acc = acc_r.get()
                TS("dve", acc[:], x1[:], ALPHA, ALU.mult, [x1], [acc])
                if cfg.moe:
                    for k in range(4):
                        col = b * 4 + k
                        yk = yk_r.get()
                        S.dma("pool", None, None, yk, YDISP, extra=[DESTI],
                              fn=lambda e, col=col, yk=yk: e.indirect_dma_start(
                                  out=yk[:, :], out_offset=None, in_=YDISP.ap()[:, :],
                                  in_offset=bass.IndirectOffsetOnAxis(ap=DESTI[:, col:col + 1], axis=0)))
                        STT(acc[:], yk[:], GATE[:, col:col + 1], acc[:], ALU.mult, ALU.add, [yk, GATE, acc], [acc])
                x2 = x2_r.get()
                layer_norm(acc, x2, gt, bt, st_r, junk)
                S.dma("sp", dst[b * 128:(b + 1) * 128, :], x2[:], dst, x2)
                if not is_last:
                    if b % 4 == 0:
                        xT = xT_r.get()
                    emit_xT_block(x2, xT, (b % 4) * 128, ps_r, "act")
                    if b % 4 == 3:
                        tl = b // 4
                        S.dma("sp", XTBv[:, :, tl * 512:(tl + 1) * 512], xT[:], XTB, xT)
            if not is_last:
                xt_allgather()


    XTALLv = XTALL.ap().rearrange("(k r p) t -> r p k t", k=8, r=4) if True else None

    def load_xT(xT, tt):
        rank, lt = tt // NTL, tt % NTL
        S.dma("sp", xT[:], XTALLv[rank][:, :, lt * 512:(lt + 1) * 512], xT, XTALL)

    def o_allgather():
        for j in range(SEQ // TC):
            S.cc("AllGather", ALU.bypass, G4, OTOK, OALL, src_ap=OTOK[j * TC:(j + 1) * TC, :],
                 dst_ap=OALL[j * 4 * TC:(j + 1) * 4 * TC, :])

    def odd_mixer(L):
        i = L // 2
        with S.phase():
            Wb = S.sbuf("Wb", [128, 8, 1028], BF16)
            for q4 in range(4):
                S.dma("pool", Wb[:, 2 * q4:2 * q4 + 2, :],
                      wo_in.ap()[i, q4 * 256:(q4 + 1) * 256, :].rearrange("(k p) n -> p k n", p=128), Wb, wo_in)
            WQK = S.sbuf("WQK", [64, 2], F32)
            S.dma("sp", WQK[:], qkw.ap()[i], WQK, qkw)
            TS("dve", WQK[:, 0:1], WQK[:, 0:1], 0.125, ALU.mult, [WQK], [WQK])
            NFB = S.sbuf("NFB", [128, 4], F32)
            S.dma("sp", NFB[:], fb.ap()[i, :].partition_broadcast(128), NFB, fb)
            TS("dve", NFB[:], NFB[:], -1.0, ALU.mult, [NFB], [NFB])
            MKf = S.sbuf("MKf", [128, 4, 512], F32)
            MK = S.sbuf("MK", [128, 4, 512], BF16)
            MEMSET("pool", MKf[:], 0.0, [MKf])
            for j in range(4):
                ASEL(MKf[:, j, :], MKf[:, j, :], [[1, 512]], ALU.is_ge, NEG, -128 * j, -1, [MKf], [MKf])
            CP("dve", MK[:], MKf[:], [MKf], [MK])
            QT = S.sbuf("QT", [67, SEQ], BF16)
            KT = S.sbuf("KT", [67, SEQ], BF16)
            GTK = S.sbuf("GTK", [128, NBF, 64], BF16)
            VA = S.sbuf("VA", [128, NBF, 65], BF16)
            LF = S.sbuf("LF", [128, NBF], F32)
            CUM = S.sbuf("CUM", [128, NBF], F32)
            NEGCUM = S.sbuf("NEGCUM", [128, NBF], F32)
            scan = [S.sbuf("scan%d" % k, [128, NBF], F32) for k in range(2)]
            tot = S.sbuf("tot", [128, NBF], F32)
            ct = S.sbuf("ct", [128, 6, 128], F32)
            ctb = S.sbuf("ctb", [128, 3, 128], BF16)
            MEMSET("dve", VA[:, :, 64:65], 1.0, [VA])
            MEMSET("dve", KT[64:67, :], 1.0, [KT])
            xT_r = S.ring("xT", [128, 8, 512], BF16, 2)
            qf_r = S.ring("qf", [64, 512], F32, 2)
            sq_r = S.ring("sq", [64, 512], F32, 2)
            rs_r = S.ring("rs", [64, 512], F32, 2)
            g1_r = S.ring("g1", [128, 64], F32, 3)
            sm_r = S.ring("smo", [128, 8], F32, 4)
            pt_r = S.ring("pt", [128, 512], BF16, 4)
            ot_r = S.ring("ot", [128, 4, 64], BF16, 2)
            osb_r = S.ring("osb", [128, 512], F32, 2)
            for b_ in osb_r.bufs:
                MEMSET("dve", b_[:], 0.0, [b_])
            psm_r = S.ring("psm", [128, 512], F32, 3, psum=True)
            pss_r = S.ring("pss", [128, 512], F32, 3, psum=True)
            pso_r = S.ring("pso", [128, 512], F32, 2, psum=True)
            import os
            KODD = int(os.environ.get("KODD", "9"))
            if KODD == 0:
                return
            for h in range(4):
                w0 = h * 257
                for tt in range(NQT):
                    xT = xT_r.get()
                    load_xT(xT, tt)
                    for (coff, dst, wi) in ((0, QT, 0), (64, KT, 1)):
                        psq = psm_r.get()
                        for kc in range(8):
                            MM(psq[0:64, :], Wb[:, kc, w0 + coff:w0 + coff + 64], xT[:, kc, :], kc == 0, kc == 7, [Wb, xT], [psq])
                        qf = qf_r.get()
                        CP("act", qf[:], psq[0:64, :], [psq], [qf])
                        sq = sq_r.get()
                        TT("pool", sq[:], qf[:], qf[:], ALU.mult, [qf], [sq])
                        pssum = psm_r.get()
                        MM(pssum[0:64, :], ones_f[0:64, 0:64], sq[:], True, True, [ones_f, sq], [pssum])
                        rs = rs_r.get()
                        TS("dve", rs[:], pssum[0:64, :], 1.0 / 64, ALU.mult, [pssum], [rs], s2=RMS_EPS, op1=ALU.add)
                        ACT(rs[:], rs[:], AF.Ln, [rs], [rs])
                        ACT(rs[:], rs[:], AF.Exp, [rs], [rs], scale=-0.5)
                        STT(dst[0:64, tt * 512:(tt + 1) * 512], qf[:], WQK[:, wi:wi + 1], rs[:], ALU.mult, ALU.mult, [qf, WQK, rs], [dst])
                    for b4 in range(4):
                        gb = tt * 4 + b4
                        ps = psm_r.get()
                        for kc in range(8):
                            MM(ps[:, 0:129], xT[:, kc, b4 * 128:(b4 + 1) * 128], Wb[:, kc, w0 + 128:w0 + 257], kc == 0, kc == 7, [xT, Wb], [ps])
                        sm = sm_r.get()
                        CP("dve", VA[:, gb, 0:64], ps[:, 0:64], [ps], [VA])
                        g1 = g1_r.get()
                        ACT(g1[:], ps[:, 65:129], AF.Exp, [ps], [g1], scale=-1.0)
                        TS("pool", g1[:], g1[:], 1.0, ALU.add, [g1], [g1])
                        RECIP(g1[:], g1[:], [g1], [g1])
                        CP("pool", GTK[:, gb, :], g1[:], [g1], [GTK])
                        ACT(sm[:, 0:1], ps[:, 64:65], AF.Exp, [ps, NFB], [sm], bias=NFB[:, h:h + 1], scale=-1.0)
                        ACT(sm[:, 1:2], sm[:, 0:1], AF.Ln, [sm], [sm], bias=1.0)
                        TS("dve", LF[:, gb:gb + 1], sm[:, 1:2], -1.0, ALU.mult, [sm], [LF])
                if KODD == 1:
                    return
                psw = psm_r.get()
                MM(psw[:, 0:NBF], triU_f[:], LF[:], True, True, [triU_f, LF], [psw])
                pstot = psm_r.get()
                MM(pstot[:, 0:NBF], ones_f[:], LF[:], True, True, [ones_f, LF], [pstot])
                CP("dve", tot[:], pstot[:, 0:NBF], [pstot], [tot])
                CP("dve", scan[0][:], tot[:], [tot], [scan[0]])
                a, bq = scan[0], scan[1]
                sft = 1
                while sft < NBF:
                    TT("dve", bq[:, sft:NBF], a[:, sft:NBF], a[:, 0:NBF - sft], ALU.add, [a], [bq])
                    CP("dve", bq[:, 0:sft], a[:, 0:sft], [a], [bq])
                    a, bq = bq, a
                    sft *= 2
                TT("dve", CUM[:], psw[:, 0:NBF], a[:], ALU.add, [psw, a], [CUM])
                TT("dve", CUM[:], CUM[:], tot[:], ALU.subtract, [CUM, tot], [CUM])
                TS("dve", NEGCUM[:], CUM[:], -1.0, ALU.mult, [CUM], [NEGCUM])
                pct = psm_r.get()
                TR(pct[0:NBF, 0:128], CUM[:, 0:NBF], ident_f[:], [CUM, ident_f], [pct])
                CP("dve", ct[0:NBF, 0, :], pct[0:NBF, 0:128], [pct], [ct])
                CP("dve", ctb[0:NBF, 0, :], ct[0:NBF, 0, :], [ct], [ctb])
                CP("dve", ct[0:NBF, 1, :], ctb[0:NBF, 0, :], [ctb], [ct])
                TT("dve", ct[0:NBF, 2, :], ct[0:NBF, 0, :], ct[0:NBF, 1, :], ALU.subtract, [ct], [ct])
                CP("dve", ctb[0:NBF, 1, :], ct[0:NBF, 2, :], [ct], [ctb])
                CP("dve", ct[0:NBF, 3, :], ctb[0:NBF, 1, :], [ctb], [ct])
                TT("dve", ct[0:NBF, 4, :], ct[0:NBF, 2, :], ct[0:NBF, 3, :], ALU.subtract, [ct], [ct])
                CP("dve", ctb[0:NBF, 2, :], ct[0:NBF, 4, :], [ct], [ctb])
                for j in range(3):
                    S.dma("sp", CQ.ap()[j, :].rearrange("(j t) -> j t", t=128), ctb[0:NBF, j, :], CQ, ctb)
                for j in range(3):
                    S.dma("sp", QT[64 + j:65 + j, :], CQ[j:j + 1, :], QT, CQ)
                if KODD == 2:
                    return
                acc_of = {}

                def stage_a(qt, kb):
                    pss = pss_r.get()
                    diag = kb >= 4 * qt
                    MM(pss[:], KT[0:67, kb * 128:(kb + 1) * 128], QT[0:67, qt * 512:(qt + 1) * 512], True, not diag, [KT, QT], [pss])
                    if diag:
                        MM(pss[:], ident_b[:], MK[:, kb - 4 * qt, :], False, True, [ident_b, MK], [pss])
                    pt = pt_r.get()
                    ACT(pt[:], pss[:], AF.Exp, [pss, NEGCUM], [pt], bias=NEGCUM[:, kb:kb + 1])
                    return pt

                def stage_b(qt, kb, pt):
                    nkb = 4 * qt + 4
                    if kb == 0:
                        acc_of[qt] = pso_r.get()
                    oacc = acc_of[qt]
                    MM(oacc[0:65, :], VA[:, kb, :], pt[:], kb == 0, kb == nkb - 1, [VA, pt], [oacc])
                    if kb < nkb - 1:
                        return
                    osb = osb_r.get()
                    CP("dve", osb[0:65, :], oacc[0:65, :], [oacc], [osb])
                    ptr = psm_r.get()
                    for b4 in range(4):
                        TR(ptr[:, b4 * 128:(b4 + 1) * 128], osb[:, b4 * 128:(b4 + 1) * 128], ident_f[:], [osb, ident_f], [ptr])
                    ot = ot_r.get()
                    for b4 in range(4):
                        gb = qt * 4 + b4
                        sm = sm_r.get()
                        RECIP(sm[:, 0:1], ptr[:, b4 * 128 + 64:b4 * 128 + 65], [ptr], [sm])
                        STT(ot[:, b4, :], ptr[:, b4 * 128:b4 * 128 + 64], sm[:, 0:1], GTK[:, gb, :], ALU.mult, ALU.mult, [ptr, sm, GTK], [ot])
                    S.dma("sp", OTOK.ap()[qt * 512:(qt + 1) * 512, h * 64:(h + 1) * 64].rearrange("(b p) d -> p b d", p=128),
                          ot[:], OTOK, ot)

                pend = []
                for qt in range(NQT):
                    for kb in range(4 * qt + 4):
                        pend.append((qt, kb, stage_a(qt, kb)))
                        if len(pend) > 2:
                            stage_b(*pend.pop(0))
                while pend:
                    stage_b(*pend.pop(0))
                if KODD == 3:
                    return
            o_allgather()

    def even_mixer(L):
        i = L // 2
        lam_init = 0.8 - 0.6 * math.exp(-0.3 * L)
        import os
        KEV = int(os.environ.get("KEV", "9"))
        with S.phase():
            Wd = S.sbuf("Wd", [128, 8, 384], BF16)
            for q4 in range(4):
                S.dma("pool", Wd[:, 2 * q4:2 * q4 + 2, :],
                      we_in.ap()[i, q4 * 256:(q4 + 1) * 256, 0:384].rearrange("(k p) n -> p k n", p=128), Wd, we_in)
            LP = S.sbuf("LP", [128, 256], F32)
            S.dma("sp", LP[:], lamp.ap()[i, :].partition_broadcast(128), LP, lamp)
            lw = S.sbuf("lw", [128, 8], F32)
            pr = S.sbuf("pr", [128, 128], F32)
            TT("dve", pr[:, 0:64], LP[:, 0:64], LP[:, 64:128], ALU.mult, [LP], [pr])
            TT("dve", pr[:, 64:128], LP[:, 128:192], LP[:, 192:256], ALU.mult, [LP], [pr])
            S.op("dve", lambda e: e.tensor_reduce(out=lw[:, 0:2], in_=pr[:].rearrange("p (g d) -> p g d", g=2), axis=AX.X, op=ALU.add),
                 reads=[pr], writes=[lw])
            ACT(lw[:, 2:4], lw[:, 0:2], AF.Exp, [lw], [lw])
            TT("dve", lw[:, 4:5], lw[:, 2:3], lw[:, 3:4], ALU.subtract, [lw], [lw])
            TS("dve", lw[:, 4:5], lw[:, 4:5], lam_init, ALU.add, [lw], [lw])
            TS("dve", lw[:, 5:6], lw[:, 4:5], -1.0, ALU.mult, [lw], [lw])
            SUBW = S.sbuf("SUBW", [128, 128], F32)
            S.dma("sp", SUBW[:], subw.ap()[i, :].partition_broadcast(128), SUBW, subw)
            TS("dve", SUBW[:], SUBW[:], 1.0 - lam_init, ALU.mult, [SUBW], [SUBW])
            T5C = S.sbuf("T5C", [33, 1], F32)
            S.dma("sp", T5C[0:32, :], t5col[:, :], T5C, t5col)
            MEMSET("dve", T5C[32:33, :], 1.0, [T5C])
            OH = S.sbuf("OH", [33, 2560], F32)
            S.dma("sp", OH[:], t5oh[:, :], OH, t5oh)
            bvs = S.sbuf("bvs", [1, 2560], F32)
            psm_r = S.ring("psm", [128, 512], F32, 3, psum=True)
            pss_r = S.ring("pss", [128, 512], F32, 3, psum=True)
            pso_r = S.ring("pso", [128, 512], F32, 2, psum=True)
            for c5 in range(5):
                ps = psm_r.get()
                MM(ps[0:1, :], T5C[:, 0:1], OH[:, c5 * 512:(c5 + 1) * 512], True, True, [T5C, OH], [ps])
                CP("dve", bvs[0:1, c5 * 512:(c5 + 1) * 512], ps[0:1, :], [ps], [bvs])
            S.dma("sp", BV.ap().rearrange("(o n) -> o n", o=1), bvs[:], BV, bvs)
            BT = S.sbuf("BT", [128, 16, 512], BF16)
            skb_r = S.ring("skb", [128, 640], F32, 2)
            for j in range(16):
                skb = skb_r.get()
                S.dma("sp", skb[:], BV.ap()[128 * j:128 * j + 640].partition_broadcast(128), skb, BV)
                S.dma("sp", SKW.ap()[j, :].rearrange("(p n) -> p n", n=640), skb[:], SKW, skb)
                S.dma("pool", BT[:, j, :], bass.AP(tensor=SKW.h, offset=j * 128 * 640 + 127, ap=[[639, 128], [1, 512]]), BT, SKW)
            B31 = S.sbuf("B31", [128, 1], F32)
            S.dma("sp", B31[:], BV.ap()[2559:2560].partition_broadcast(128), B31, BV)
            if S.w_pending:
                S.w_pending = False
                S.w2_pending = True
                w_collectives((0,))
            QT = S.sbuf("QT", [64, SEQ], BF16)
            KT = S.sbuf("KT", [64, SEQ], BF16)
            VA = S.sbuf("VA", [128, NBF, 129], BF16)
            MEMSET("dve", VA[:, :, 64:65], 1.0, [VA])
            xT_r = S.ring("xT", [128, 8, 512], BF16, 2)
            pt_r = S.ring("pt", [128, 512], BF16, 4)
            osb_r = S.ring("osbd", [128, 512], F32, 4)
            for b_ in osb_r.bufs:
                MEMSET("dve", b_[:], 0.0, [b_])
            om_r = S.ring("om", [128, 128], F32, 3)
            o0_r = S.ring("o0", [128, 128], F32, 2)
            a_r = S.ring("a", [128, 128], F32, 2)
            ao_r = S.ring("ao", [128, 128], BF16, 2)
            sm_r = S.ring("smd", [128, 8], F32, 4)
            junk = S.sbuf("junkd", [128, 128], F32)
            for m in range(2):
                for tt in range(NQT):
                    xT = xT_r.get()
                    load_xT(xT, tt)
                    psq = psm_r.get()
                    for kc in range(8):
                        MM(psq[0:64, :], Wd[:, kc, m * 64:(m + 1) * 64], xT[:, kc, :], kc == 0, kc == 7, [Wd, xT], [psq])
                    ACT(QT[:, tt * 512:(tt + 1) * 512], psq[0:64, :], AF.Identity, [psq], [QT], scale=0.125)
                    psk = psm_r.get()
                    for kc in range(8):
                        MM(psk[0:64, :], Wd[:, kc, 128 + m * 64:128 + (m + 1) * 64], xT[:, kc, :], kc == 0, kc == 7, [Wd, xT], [psk])
                    CP("dve", KT[:, tt * 512:(tt + 1) * 512], psk[0:64, :], [psk], [KT])
                    if m == 0:
                        for b4 in range(4):
                            gb = tt * 4 + b4
                            psv = psm_r.get()
                            for kc in range(8):
                                MM(psv[:, 0:128], xT[:, kc, b4 * 128:(b4 + 1) * 128], Wd[:, kc, 256:384], kc == 0, kc == 7, [xT, Wd], [psv])
                            CP("act", VA[:, gb, 0:64], psv[:, 0:64], [psv], [VA])
                            CP("act", VA[:, gb, 65:129], psv[:, 64:128], [psv], [VA])
                if KEV == 1:
                    return
                acc_of = {}

                def stage_a(qt, kb):
                    dj = 4 * qt - kb + 3
                    near = dj <= 15
                    pss = pss_r.get()
                    MM(pss[:], KT[:, kb * 128:(kb + 1) * 128], QT[:, qt * 512:(qt + 1) * 512], True, not near, [KT, QT], [pss])
                    if near:
                        MM(pss[:], ident_b[:], BT[:, dj, :], False, True, [ident_b, BT], [pss])
                    pt = pt_r.get()
                    if near:
                        ACT(pt[:], pss[:], AF.Exp, [pss], [pt])
                    else:
                        ACT(pt[:], pss[:], AF.Exp, [pss, B31], [pt], bias=B31[:, 0:1])
                    return pt

                def stage_b(qt, kb, pt, m=m):
                    nkb = 4 * qt + 4
                    if kb == 0:
                        acc_of[qt] = (pso_r.get(), pso_r.get())
                    oa, ob = acc_of[qt]
                    MM(oa[0:65, :], VA[:, kb, 0:65], pt[:], kb == 0, kb == nkb - 1, [VA, pt], [oa])
                    MM(ob[0:64, :], VA[:, kb, 65:129], pt[:], kb == 0, kb == nkb - 1, [VA, pt], [ob])
                    if kb < nkb - 1:
                        return
                    osa = osb_r.get()
                    osb = osb_r.get()
                    CP("dve", osa[0:65, :], oa[0:65, :], [oa], [osa])
                    CP("act", osb[0:64, :], ob[0:64, :], [ob], [osb])
                    ptra = psm_r.get()
                    ptrb = psm_r.get()
                    for b4 in range(4):
                        TR(ptra[:, b4 * 128:(b4 + 1) * 128], osa[:, b4 * 128:(b4 + 1) * 128], ident_f[:], [osa, ident_f], [ptra])
                        TR(ptrb[:, b4 * 128:(b4 + 1) * 128], osb[:, b4 * 128:(b4 + 1) * 128], ident_f[:], [osb, ident_f], [ptrb])
                    for b4 in range(4):
                        gb = qt * 4 + b4
                        sm = sm_r.get()
                        RECIP(sm[:, 0:1], ptra[:, b4 * 128 + 64:b4 * 128 + 65], [ptra], [sm])
                        om = om_r.get()
                        TS("dve", om[:, 0:64], ptra[:, b4 * 128:b4 * 128 + 64], sm[:, 0:1], ALU.mult, [ptra, sm], [om])
                        TS("dve", om[:, 64:128], ptrb[:, b4 * 128:b4 * 128 + 64], sm[:, 0:1], ALU.mult, [ptrb, sm], [om])
                        if m == 0:
                            S.dma("sp", O0S[gb * 128:(gb + 1) * 128, :], om[:], O0S, om)
                        else:
                            o0 = o0_r.get()
                            S.dma("sp", o0[:], O0S[gb * 128:(gb + 1) * 128, :], o0, O0S)
                            av = a_r.get()
                            STT(av[:], om[:], lw[:, 5:6], o0[:], ALU.mult, ALU.add, [om, lw, o0], [av])
                            ACT(junk[:], av[:], AF.Square, [av], [junk, sm], accum=sm[:, 1:2])
                            TS("dve", sm[:, 2:3], sm[:, 1:2], 1.0 / 128, ALU.mult, [sm], [sm], s2=RMS_EPS, op1=ALU.add)
                            ACT(sm[:, 3:4], sm[:, 2:3], AF.Ln, [sm], [sm])
                            ACT(sm[:, 4:5], sm[:, 3:4], AF.Exp, [sm], [sm], scale=-0.5)
                            ao = ao_r.get()
                            STT(ao[:], av[:], sm[:, 4:5], SUBW[:], ALU.mult, ALU.mult, [av, sm, SUBW], [ao])
                            S.dma("sp", OTOK[gb * 128:(gb + 1) * 128, 0:128], ao[:], OTOK, ao)

                pend = []
                for qt in range(NQT):
                    for kb in range(4 * qt + 4):
                        pend.append((qt, kb, stage_a(qt, kb)))
                        if len(pend) > 2:
                            stage_b(*pend.pop(0))
                while pend:
                    stage_b(*pend.pop(0))
        if KEV <= 2:
            with S.phase():
                if getattr(S, "w2_pending", False):
                    S.w2_pending = False
                    w_collectives((1,))
                zb = S.sbuf("zb", [128, 128], BF16)
                MEMSET("dve", zb[:], 0.0, [zb])
                for gb in range(NBF):
                    S.dma("sp", OTOK[gb * 128:(gb + 1) * 128, 128:256], zb[:], OTOK, zb)
            o_allgather()
            return
        gdn_phase(L)
        o_allgather()

    def gdn_phase(L):
        i = L // 2
        SC = 128 ** -0.5
        with S.phase():
            Wg = S.sbuf("Wg", [128, 8, 514], BF16)
            for q4 in range(4):
                S.dma("pool", Wg[:, 2 * q4:2 * q4 + 2, :],
                      we_in.ap()[i, q4 * 256:(q4 + 1) * 256, 384:898].rearrange("(k p) n -> p k n", p=128), Wg, we_in)
            CW = S.sbuf("CW", [128, 12], F32)
            S.dma("sp", CW[:], convw.ap()[i], CW, convw)
            GNW = S.sbuf("GNW", [128, 128], F32)
            S.dma("sp", GNW[:], gnw.ap()[i, :].partition_broadcast(128), GNW, gnw)
            AD = S.sbuf("AD", [128, 4], F32)
            S.dma("sp", AD[:, 0:1], alog.ap()[i, :].partition_broadcast(128), AD, alog)
            S.dma("sp", AD[:, 1:2], dtb.ap()[i, :].partition_broadcast(128), AD, dtb)
            ACT(AD[:, 2:3], AD[:, 0:1], AF.Exp, [AD], [AD])
            TS("dve", AD[:, 2:3], AD[:, 2:3], -1.0, ALU.mult, [AD], [AD])
            MUs = S.sbuf("MUs", [128, 128], F32)
            MUi = S.sbuf("MUi", [128, 128], F32)
            MLs = S.sbuf("MLs", [128, 128], F32)
            for (t_, base, cm, pat) in ((MUs, -1, -1, 1), (MUi, 0, -1, 1), (MLs, -1, 1, -1)):
                MEMSET("pool", t_[:], 1.0, [t_])
                ASEL(t_[:], t_[:], [[pat, 128]], ALU.is_ge, 0.0, base, cm, [t_], [t_])
            if getattr(S, "w2_pending", False):
                S.w2_pending = False
                w_collectives((1,))
            St = [S.sbuf("St%d" % k, [128, 128], F32) for k in range(2)]
            MEMSET("dve", St[0][:], 0.0, [St[0]])
            cb = [S.sbuf("cb%d" % g, [128, 515], F32) for g in range(3)]
            for g in range(3):
                MEMSET("dve", cb[g][:], 0.0, [cb[g]])
            hal = S.sbuf("hal", [128, 3, 3], F32)
            xT_r = S.ring("xT", [128, 8, 512], BF16, 2)
            y_r = S.ring("y", [128, 512], F32, 2)
            fT = [S.ring("fT%d" % g, [128, 512], F32, 2) for g in range(3)]
            sq_r = S.ring("sqg", [128, 512], F32, 2)
            zt_r = S.ring("ztk", [128, 4, 128], F32, 2)
            gb_r = S.ring("gbt", [128, 12], F32, 2)
            w_r = S.ring("wk", [128, 128], F32, 44)
            w2_r = S.ring("wk2", [128, 256], F32, 13)
            c_r = S.ring("colg", [128, 8], F32, 9)
            bo_r = S.ring("bo", [128, 128], BF16, 2)
            ps_r = S.ring("psg", [128, 512], F32, 7, psum=True)
            pso_r = S.ring("psgo", [128, 512], F32, 1, psum=True)
            cur = 0
            for tt in range(NQT):
                xT = xT_r.get()
                load_xT(xT, tt)
                fts = []
                for g in range(3):
                    psf = ps_r.get()
                    for kc in range(8):
                        MM(psf[:], Wg[:, kc, g * 128:(g + 1) * 128], xT[:, kc, :], kc == 0, kc == 7, [Wg, xT], [psf])
                    CP("dve", hal[:, g, :], cb[g][:, 512:515], [cb[g]], [hal])
                    CP("act", cb[g][:, 3:515], psf[:], [psf], [cb[g]])
                    CP("dve", cb[g][:, 0:3], hal[:, g, :], [hal], [cb[g]])
                    y = y_r.get()
                    TS("dve", y[:], cb[g][:, 0:512], CW[:, g * 4:g * 4 + 1], ALU.mult, [cb[g], CW], [y])
                    for j in range(1, 4):
                        STT(y[:], cb[g][:, j:j + 512], CW[:, g * 4 + j:g * 4 + j + 1], y[:], ALU.mult, ALU.add, [cb[g], CW, y], [y])
                    ft = fT[g].get()
                    ACT(ft[:], y[:], AF.Silu, [y], [ft])
                    if g < 2:
                        sq = sq_r.get()
                        TT("dve", sq[:], ft[:], ft[:], ALU.mult, [ft], [sq])
                        pss = ps_r.get()
                        MM(pss[:], ones_f[:], sq[:], True, True, [ones_f, sq], [pss])
                        TS("dve", sq[:], pss[:], RMS_EPS, ALU.add, [pss], [sq])
                        ACT(sq[:], sq[:], AF.Ln, [sq], [sq])
                        ACT(sq[:], sq[:], AF.Exp, [sq], [sq], scale=-0.5)
                        TT("dve", ft[:], ft[:], sq[:], ALU.mult, [ft, sq], [ft])
                    fts.append(ft)
                qTt, kTt, vTt = fts
                zt = zt_r.get()
                gbt = gb_r.get()
                for b4 in range(4):
                    pz = ps_r.get()
                    for kc in range(8):
                        MM(pz[:, 0:130], xT[:, kc, b4 * 128:(b4 + 1) * 128], Wg[:, kc, 384:514], kc == 0, kc == 7, [xT, Wg], [pz])
                    ACT(zt[:, b4, :], pz[:, 0:128], AF.Silu, [pz], [zt])
                    ACT(gbt[:, 4 + b4:5 + b4], pz[:, 128:129], AF.Sigmoid, [pz], [gbt])
                    cg = c_r.get()
                    ACT(cg[:, 0:1], pz[:, 129:130], AF.Exp, [pz, AD], [cg], bias=AD[:, 1:2])
                    ACT(cg[:, 1:2], cg[:, 0:1], AF.Ln, [cg], [cg], bias=1.0)
                    TS("dve", gbt[:, b4:b4 + 1], cg[:, 1:2], AD[:, 2:3], ALU.mult, [cg, AD], [gbt])
                pgc = ps_r.get()
                MM(pgc[:, 0:4], triU_f[:], gbt[:, 0:4], True, True, [triU_f, gbt], [pgc])
                CP("dve", gbt[:, 8:12], pgc[:, 0:4], [pgc], [gbt])
                for b4 in range(4):
                    gbk = tt * 4 + b4
                    blk = slice(b4 * 128, (b4 + 1) * 128)
                    gc = gbt[:, 8 + b4:9 + b4]
                    beta = gbt[:, 4 + b4:5 + b4]
                    pk = ps_r.get()
                    TR(pk[:, 0:128], kTt[:, blk], ident_f[:], [kTt, ident_f], [pk])
                    Kc = w_r.get()
                    CP("act", Kc[:], pk[:, 0:128], [pk], [Kc])
                    pv = ps_r.get()
                    TR(pv[:, 0:128], vTt[:, blk], ident_f[:], [vTt, ident_f], [pv])
                    Vc = w_r.get()
                    CP("act", Vc[:], pv[:, 0:128], [pv], [Vc])
                    DG = w2_r.get()
                    TS("dve", DG[:, 0:128], ident_f[:], gc, ALU.mult, [ident_f, gbt], [DG])
                    TS("dve", DG[:, 128:256], ident_f[:], beta, ALU.mult, [ident_f, gbt], [DG])
                    pb = ps_r.get()
                    MM(pb[:, 0:256], ones_f[:], DG[:], True, True, [ones_f, DG], [pb])
                    GB = w2_r.get()
                    CP("act", GB[:], pb[:, 0:256], [pb], [GB])
                    dlt = w_r.get()
                    TS("dve", dlt[:], GB[:, 0:128], gc, ALU.subtract, [GB, gbt], [dlt])
                    ET = w_r.get()
                    TS("dve", ET[:], dlt[:], 0.0, ALU.min, [dlt], [ET])
                    ACT(ET[:], ET[:], AF.Exp, [ET], [ET])
                    E2 = w_r.get()
                    TS("dve", E2[:], dlt[:], -1.0, ALU.mult, [dlt], [E2], s2=0.0, op1=ALU.min)
                    ACT(E2[:], E2[:], AF.Exp, [E2], [E2])
                    EG = w_r.get()
                    ACT(EG[:], GB[:, 0:128], AF.Exp, [GB], [EG])
                    pkk = ps_r.get()
                    MM(pkk[:, 0:128], kTt[:, blk], kTt[:, blk], True, True, [kTt], [pkk])
                    Nj = w_r.get()
                    TT("dve", Nj[:], pkk[:, 0:128], GB[:, 128:256], ALU.mult, [pkk, GB], [Nj])
                    TT("dve", Nj[:], Nj[:], ET[:], ALU.mult, [Nj, ET], [Nj])
                    STT(Nj[:], Nj[:], -1.0, MUs[:], ALU.mult, ALU.mult, [Nj, MUs], [Nj])
                    Pj = w_r.get()
                    STT(Pj[:], pkk[:, 0:128], beta, E2[:], ALU.mult, ALU.mult, [pkk, gbt, E2], [Pj])
                    STT(Pj[:], Pj[:], -1.0, MLs[:], ALU.mult, ALU.mult, [Pj, MLs], [Pj])
                    cg = c_r.get()
                    ACT(cg[:, 0:1], gc, AF.Exp, [gbt], [cg])
                    TT("dve", cg[:, 1:2], cg[:, 0:1], beta, ALU.mult, [cg, gbt], [cg])
                    Rm = w2_r.get()
                    TS("dve", Rm[:, 0:128], Vc[:], beta, ALU.mult, [Vc, gbt], [Rm])
                    TS("dve", Rm[:, 128:256], Kc[:], cg[:, 1:2], ALU.mult, [Kc, cg], [Rm])
                    for j in range(7):
                        pr_ = ps_r.get()
                        MM(pr_[:, 0:256], Nj[:], Rm[:], True, True, [Nj, Rm], [pr_])
                        Rn = w2_r.get()
                        TT("dve", Rn[:], Rm[:], pr_[:, 0:256], ALU.add, [Rm, pr_], [Rn])
                        Rm = Rn
                        if j < 6:
                            pn = ps_r.get()
                            MM(pn[:, 0:128], Pj[:], Nj[:], True, True, [Pj, Nj], [pn])
                            pp = ps_r.get()
                            MM(pp[:, 0:128], Nj[:], Pj[:], True, True, [Nj, Pj], [pp])
                            Nn = w_r.get()
                            CP("act", Nn[:], pn[:, 0:128], [pn], [Nn])
                            Pn = w_r.get()
                            CP("act", Pn[:], pp[:, 0:128], [pp], [Pn])
                            Nj, Pj = Nn, Pn
                    pw = ps_r.get()
                    TR(pw[:, 0:128], Rm[:, 128:256], ident_f[:], [Rm, ident_f], [pw])
                    WT = w_r.get()
                    CP("act", WT[:], pw[:, 0:128], [pw], [WT])
                    Sc, Sn = St[cur], St[1 - cur]
                    pws = ps_r.get()
                    MM(pws[:, 0:128], WT[:], Sc[:], True, True, [WT, Sc], [pws])
                    Vn = w_r.get()
                    TT("dve", Vn[:], Rm[:, 0:128], pws[:, 0:128], ALU.subtract, [Rm, pws], [Vn])
                    pqk = ps_r.get()
                    MM(pqk[:, 0:128], kTt[:, blk], qTt[:, blk], True, True, [kTt, qTt], [pqk])
                    qki = w_r.get()
                    TT("dve", qki[:], pqk[:, 0:128], ET[:], ALU.mult, [pqk, ET], [qki])
                    STT(qki[:], qki[:], SC, MUi[:], ALU.mult, ALU.mult, [qki, MUi], [qki])
                    QdT = w_r.get()
                    STT(QdT[:], qTt[:, blk], SC, EG[:], ALU.mult, ALU.mult, [qTt, EG], [QdT])
                    po = pso_r.get()
                    MM(po[:, 0:128], QdT[:], Sc[:], True, False, [QdT, Sc], [po])
                    MM(po[:, 0:128], qki[:], Vn[:], False, True, [qki, Vn], [po])
                    TT("dve", cg[:, 2:3], GB[:, 127:128], gc, ALU.subtract, [GB, gbt], [cg])
                    ACT(cg[:, 3:4], cg[:, 2:3], AF.Exp, [cg], [cg])
                    Kd = w_r.get()
                    TS("dve", Kd[:], Kc[:], cg[:, 3:4], ALU.mult, [Kc, cg], [Kd])
                    psn = ps_r.get()
                    MM(psn[:, 0:128], Kd[:], Vn[:], True, True, [Kd, Vn], [psn])
                    STT(Sn[:], Sc[:], EG[:, 127:128], psn[:, 0:128], ALU.mult, ALU.add, [Sc, EG, psn], [Sn])
                    cur = 1 - cur
                    jk = w_r.get()
                    ACT(jk[:], po[:, 0:128], AF.Square, [po], [jk, cg], accum=cg[:, 4:5])
                    TS("dve", cg[:, 5:6], cg[:, 4:5], 1.0 / 128, ALU.mult, [cg], [cg], s2=RMS_EPS, op1=ALU.add)
                    ACT(cg[:, 6:7], cg[:, 5:6], AF.Ln, [cg], [cg])
                    ACT(cg[:, 7:8], cg[:, 6:7], AF.Exp, [cg], [cg], scale=-0.5)
                    STT(jk[:], po[:, 0:128], cg[:, 7:8], GNW[:], ALU.mult, ALU.mult, [po, cg, GNW], [jk])
                    bo = bo_r.get()
                    TT("dve", bo[:], jk[:], zt[:, b4, :], ALU.mult, [jk, zt], [bo])
                    S.dma("sp", OTOK[gbk * 128:(gbk + 1) * 128, 128:256], bo[:], OTOK, bo)


    phase_x0()
    xres = x_in
    if S.w_pending and not (cfg.mixer and cfg.LAYERS[0] % 2 == 0):
        S.w_pending = False
        with S.phase():
            w_collectives()
    for li, L in enumerate(cfg.LAYERS):
        is_last = li == len(cfg.LAYERS) - 1
        if cfg.mixer:
            if L % 2 == 0:
                even_mixer(L)
            else:
                odd_mixer(L)
        xnext = XR[li % 2]
        phase_ln_moe(L, xres, xnext, is_last)
        xres = xnext


def _even_mixer(S, cfg, L):
    raise NotImplementedError


def _odd_mixer(S, cfg, L):
    raise NotImplementedError


def t5_onehot():
    n = np.arange(2560) - 511
    nn = np.maximum(n, 0)
    nf = np.maximum(nn, 1).astype(np.float32)
    large = 16 + (np.log(nf / 16) / math.log(2048 / 16) * 16).astype(np.int32)
    large = np.minimum(large, 31)
    bucket = np.where(nn < 16, nn, large)
    oh = np.zeros((33, 2560), np.float32)
    valid = n >= 0
    oh[bucket[valid], np.nonzero(valid)[0]] = 1.0
    oh[32, ~valid] = NEG
    return oh


def prep_inputs(inp, cfg):
    SEQ, TOK, NEXP, EPC, NLW, WL0 = cfg.SEQ, cfg.TOK, cfg.NEXP, cfg.EPC, cfg.NLW, cfg.WL0
    f = lambda a: np.ascontiguousarray(np.asarray(a, dtype=np.float32))
    x = f(inp["x"])
    maps = []
    lng = f(np.concatenate([inp["ln_mix_g"], inp["ln_ffn_g"]], 0))
    lnb = f(np.concatenate([inp["ln_mix_b"], inp["ln_ffn_b"]], 0))
    if cfg.moe:
        bg = f(inp["moe_b_gate_up"]).reshape(4, NEXP, 16, 128)
        bgu = np.ascontiguousarray(bg.transpose(3, 0, 1, 2).reshape(128, 4 * NEXP * 16))
        bdn = f(inp["moe_b_down"]).reshape(4 * NEXP, D)
        wgu = np.asarray(inp["moe_w_gate_up"])
        wdn = np.asarray(inp["moe_w_down"])
    if cfg.mixer:
        ewi, ewo = f(inp["even_w_in"]), f(inp["even_w_out"])
        owi, owo = f(inp["odd_w_in"]), f(inp["odd_w_out"])
        cw = f(inp["gdn_conv_w"])
        oh = t5_onehot()
    for c in range(8):
        b, r = c // 4, c % 4
        m = {"x_in": np.ascontiguousarray(x[b, r * TOK:(r + 1) * TOK, :]), "lng": lng, "lnb": lnb}
        if cfg.moe:
            m["router_w"] = f(inp["router_w"])
            m["router_b"] = f(inp["router_b"])
            m["bgu"] = bgu
            m["bdn"] = bdn
            m["wgu_sh"] = np.ascontiguousarray(wgu[WL0:WL0 + NLW, c * EPC:(c + 1) * EPC], dtype=np.float32).reshape(NLW * EPC * D, 2 * D)
            m["wdn_sh"] = np.ascontiguousarray(wdn[WL0:WL0 + NLW, c * EPC:(c + 1) * EPC], dtype=np.float32).reshape(NLW * EPC * D, D)
        if cfg.mixer:
            h = r
            A = 512
            cols = np.concatenate([np.arange(h * 128, h * 128 + 128), A + np.arange(h * 128, h * 128 + 128),
                                   2 * A + np.arange(h * 128, h * 128 + 128),
                                   1536 + np.arange(h * 128, h * 128 + 128), 2048 + np.arange(h * 128, h * 128 + 128),
                                   2560 + np.arange(h * 128, h * 128 + 128), 3072 + np.arange(h * 128, h * 128 + 128),
                                   [3584 + h], [3588 + h]])
            m["we_in"] = np.ascontiguousarray(ewi[:, :, cols])
            perm = np.concatenate([np.concatenate([np.arange(s_ * 128, s_ * 128 + 128), 512 + np.arange(s_ * 128, s_ * 128 + 128)]) for s_ in range(4)])
            m["we_out"] = np.ascontiguousarray(ewo[:, perm, :])
            TC = min(2048, SEQ)
            gtok = r * TOK + np.arange(TOK)
            jj, tt_ = gtok // TC, gtok % TC
            oi = np.stack([(jj * 4 + s_) * TC + tt_ for s_ in range(4)], 1)
            m["oidx"] = np.ascontiguousarray(oi.reshape(TOK // 128, 128, 4).transpose(1, 0, 2).reshape(128, TOK // 32).astype(np.int32))
            ccols = np.concatenate([np.arange(h * 128, h * 128 + 128), 512 + np.arange(h * 128, h * 128 + 128), 1024 + np.arange(h * 128, h * 128 + 128)])
            m["convw"] = np.ascontiguousarray(cw[:, :, ccols].reshape(2, 4, 3, 128).transpose(0, 3, 2, 1).reshape(2, 128, 12))
            m["lamp"] = f(inp["diff_lambda"]).reshape(2, 256)
            m["subw"] = f(inp["diff_subln_w"])
            m["gnw"] = f(inp["gdn_norm_w"])
            m["alog"] = np.ascontiguousarray(f(inp["gdn_a_log"])[:, h:h + 1])
            m["dtb"] = np.ascontiguousarray(f(inp["gdn_dt_bias"])[:, h:h + 1])
            m["t5col"] = np.ascontiguousarray(f(inp["t5_bias"])[:, h:h + 1])
            m["t5oh"] = oh
            oc = []
            for hh in range(4 * r, 4 * r + 4):
                oc += [np.arange(hh * 64, hh * 64 + 64), 1024 + np.arange(hh * 64, hh * 64 + 64),
                       2048 + np.arange(hh * 64, hh * 64 + 64), [4096 + hh], 3072 + np.arange(hh * 64, hh * 64 + 64)]
            oc = np.concatenate(oc)
            m["wo_in"] = np.ascontiguousarray(owi[:, :, oc])
            m["wo_out"] = owo
            m["qkw"] = np.ascontiguousarray(f(inp["fox_qk_norm_w"]).transpose(0, 2, 1))
            m["fb"] = np.ascontiguousarray(f(inp["fox_forget_b"])[:, 4 * r:4 * r + 4])
        maps.append(m)
    return maps


_CACHE = {}


def run_cfg(inp, cfg):
    key = (cfg.SEQ, cfg.NEXP, cfg.CAP, cfg.LAYERS, cfg.NLW, cfg.WL0, cfg.mixer, cfg.moe)
    if key not in _CACHE:
        _CACHE[key] = build(cfg)
    nc = _CACHE[key]
    maps = prep_inputs(inp, cfg)
    has_even = cfg.mixer and any(L % 2 == 0 for L in cfg.LAYERS)
    has_odd = cfg.mixer and any(L % 2 == 1 for L in cfg.LAYERS)
    ev = ("we_in", "we_out", "convw", "lamp", "subw", "gnw", "alog", "dtb", "t5col", "t5oh")
    od = ("wo_in", "wo_out", "qkw", "fb")
    for m in maps:
        for k in list(m):
            if (k in ev and not has_even) or (k in od and not has_odd):
                del m[k]
    res = run_bass_kernel_spmd(nc, maps, core_ids=list(range(8)))
    outs = [res.results[c]["out"] for c in range(8)]
    TOK = cfg.TOK
    full = np.zeros((2, cfg.SEQ, D), np.float32)
    for c in range(8):
        full[c // 4, (c % 4) * TOK:(c % 4 + 1) * TOK, :] = outs[c]
    return full


def kernel(**inputs):
    return run_cfg(inputs, Cfg())
```
